# Optimizing a Trainium2 kernel written in Bass

```python
import numpy as np
import jax
import jax.numpy as jnp
from jax import lax

D_MODEL = 2048
BATCH = 4
SEQ = 4096
DEPTH = 2

HEAD_DIM = 128
MIX_WIDTH = D_MODEL
NSA_WIDTH = MIX_WIDTH // 2
GLA_WIDTH = MIX_WIDTH - NSA_WIDTH
NSA_HEADS = NSA_WIDTH // HEAD_DIM
NSA_KV_HEADS = NSA_HEADS // 4
NSA_GROUP = NSA_HEADS // NSA_KV_HEADS
CMP_BLOCK = 32
CMP_STRIDE = 16
CMP_HIDDEN = 256
SLC_BLOCK = 64
SLC_TOPN = 16
WINDOW = 512
NSA_QBLOCK = 64
FORCE_SCORE = 1e9
GLA_DV = 256
GLA_HEADS = GLA_WIDTH // GLA_DV
GLA_DK = GLA_DV // 2
GLA_GATE_RANK = 16
GLA_GATE_NORM = 16.0
GLA_CHUNK = 64
ROPE_THETA = 500000.0
ROPE_DIMS = HEAD_DIM // 4
D_FF = 256 * ((8 * D_MODEL // 3 + 255) // 256)
N_EXPERTS = 8
TOP_K = 2
D_FF_EXPERT = 7 * D_MODEL // 2
MOE_BLOCK = 512
N_DENSE = (DEPTH + 1) // 2
N_MOE = DEPTH // 2
LN_EPS = 1e-5
NORM_EPS = 1e-6
DEEPNORM_ALPHA = (2 * DEPTH) ** 0.25
DEEPNORM_BETA = (8 * DEPTH) ** -0.25
ADA_INIT_SCALE = 0.1
SPLIT_SIZES = (NSA_HEADS * HEAD_DIM,) + (NSA_KV_HEADS * HEAD_DIM,) * 6 + (NSA_HEADS * 3, GLA_HEADS * GLA_DK, GLA_HEADS * GLA_DK, GLA_HEADS * GLA_DV, GLA_GATE_RANK, GLA_HEADS * GLA_DV)
IN_WIDTH = sum(SPLIT_SIZES)

kernel_name = 'hybrid_nsa_gla_moe_deepnorm_adaln'


def layer_norm(x, g, b):
    xf = x.astype(jnp.float32)
    mu = jnp.mean(xf, axis=-1, keepdims=True)
    var = jnp.mean(jnp.square(xf - mu), axis=-1, keepdims=True)
    return ((xf - mu) * lax.rsqrt(var + LN_EPS) * g + b).astype(x.dtype)


def rotary_partial(x, pos):
    half = ROPE_DIMS // 2
    inv = ROPE_THETA ** (-jnp.arange(half, dtype=jnp.float32) * 2.0 / ROPE_DIMS)
    ang = pos.astype(jnp.float32)[:, None] * inv[None, :]
    cos = jnp.cos(ang)[:, None, :]
    sin = jnp.sin(ang)[:, None, :]
    x1 = x[..., :half].astype(jnp.float32)
    x2 = x[..., half:ROPE_DIMS].astype(jnp.float32)
    r1 = (x1 * cos - x2 * sin).astype(x.dtype)
    r2 = (x2 * cos + x1 * sin).astype(x.dtype)
    return jnp.concatenate([r1, r2, x[..., ROPE_DIMS:]], axis=-1)


def masked_softmax(s, mask):
    s = jnp.where(mask, s.astype(jnp.float32), -1e30)
    p = jax.nn.softmax(s, axis=-1)
    return jnp.where(mask, p, 0.0)


def nsa_attention(q, k_cmp, v_cmp, k_slc, v_slc, k_win, v_win, gates,
                  cmp_pos_k, cmp_w1_k, cmp_w2_k, cmp_pos_v, cmp_w1_v, cmp_w2_v):
    B, T = q.shape[0], q.shape[1]
    dt = q.dtype
    Hkv, G, Dh, QB = NSA_KV_HEADS, NSA_GROUP, HEAD_DIM, NSA_QBLOCK
    pos = jnp.arange(T)
    scale = HEAD_DIM ** -0.5

    n_cmp = (T - CMP_BLOCK) // CMP_STRIDE + 1
    cmp_idx = jnp.arange(n_cmp)[:, None] * CMP_STRIDE + jnp.arange(CMP_BLOCK)[None, :]

    def compress(a, pe, w1, w2):
        blocks = a[:, cmp_idx] + pe[None, None, :, None, :]
        blocks = blocks.transpose(0, 1, 3, 2, 4).reshape(B, n_cmp, Hkv, CMP_BLOCK * Dh)
        return jax.nn.gelu(blocks @ w1) @ w2

    kc = compress(k_cmp, cmp_pos_k, cmp_w1_k, cmp_w2_k)
    vc = compress(v_cmp, cmp_pos_v, cmp_w1_v, cmp_w2_v)
    cmp_start = jnp.arange(n_cmp) * CMP_STRIDE
    cmp_end = cmp_start + CMP_BLOCK - 1

    n_slc = T // SLC_BLOCK
    n_sel = min(SLC_TOPN, n_slc)
    blk_start = jnp.arange(n_slc) * SLC_BLOCK
    overlap = ((cmp_start[:, None] < blk_start[None, :] + SLC_BLOCK)
               & (cmp_start[:, None] + CMP_BLOCK > blk_start[None, :])).astype(jnp.float32)

    q_rot = rotary_partial(q, pos) * scale
    q_nope = q * scale
    k_blocks = rotary_partial(k_slc, pos).reshape(B, n_slc, SLC_BLOCK, Hkv, Dh).transpose(0, 3, 1, 2, 4)
    v_blocks = v_slc.reshape(B, n_slc, SLC_BLOCK, Hkv, Dh).transpose(0, 3, 1, 2, 4)
    pad = ((0, 0), (WINDOW, 0), (0, 0), (0, 0))
    kw = jnp.pad(rotary_partial(k_win, pos), pad)
    vw = jnp.pad(v_win, pad)
    gate = jax.nn.sigmoid(gates.astype(jnp.float32))

    n_qb = T // QB
    bi = jnp.arange(B)[:, None, None, None]
    hi = jnp.arange(Hkv)[None, :, None, None]
    j_blk = jnp.arange(n_slc)

    def to_blocks(a):
        return a.reshape((B, n_qb, QB) + a.shape[2:]).swapaxes(0, 1)

    def block_fn(args):
        i, qr, qn, g = args
        t = i * QB + jnp.arange(QB)
        qr = qr.reshape(B, QB, Hkv, G, Dh)
        qn = qn.reshape(B, QB, Hkv, G, Dh)
        s_c = jnp.einsum('bqhgd,bchd->bhgqc', qn, kc)
        p_c = masked_softmax(s_c, cmp_end[None, :] <= t[:, None])
        o_c = jnp.einsum('bhgqc,bchd->bqhgd', p_c.astype(dt), vc)
        imp = jnp.einsum('bhgqc,cn->bhqn', p_c, overlap)
        cur = t // SLC_BLOCK
        forced = (j_blk[None, :] == 0) | (j_blk[None, :] == cur[:, None]) | (j_blk[None, :] == cur[:, None] - 1)
        future = blk_start[None, :] > t[:, None]
        imp = jnp.where(future, -jnp.inf, jnp.where(forced, FORCE_SCORE, imp))
        _, sel = lax.top_k(imp, n_sel)
        k_sel = k_blocks[bi, hi, sel]
        v_sel = v_blocks[bi, hi, sel]
        s_s = jnp.einsum('bqhgd,bhqnkd->bhgqnk', qr, k_sel)
        key_pos = sel[..., None] * SLC_BLOCK + jnp.arange(SLC_BLOCK)
        mask_s = (key_pos <= t[None, None, :, None, None]).reshape(B, Hkv, 1, QB, n_sel * SLC_BLOCK)
        p_s = masked_softmax(s_s.reshape(B, Hkv, G, QB, n_sel * SLC_BLOCK), mask_s).reshape(s_s.shape)
        o_s = jnp.einsum('bhgqnk,bhqnkd->bqhgd', p_s.astype(dt), v_sel)
        start = i * QB
        k_band = lax.dynamic_slice_in_dim(kw, start, WINDOW + QB, axis=1)
        v_band = lax.dynamic_slice_in_dim(vw, start, WINDOW + QB, axis=1)
        wpos = start - WINDOW + jnp.arange(WINDOW + QB)
        diff = t[:, None] - wpos[None, :]
        mask_w = (wpos[None, :] >= 0) & (diff >= 0) & (diff < WINDOW)
        s_w = jnp.einsum('bqhgd,bkhd->bhgqk', qr, k_band)
        p_w = masked_softmax(s_w, mask_w)
        o_w = jnp.einsum('bhgqk,bkhd->bqhgd', p_w.astype(dt), v_band)
        g = g.reshape(B, QB, Hkv, G, 3)
        o = g[..., 0:1] * o_c + g[..., 1:2] * o_s + g[..., 2:3] * o_w
        return o.reshape(B, QB, NSA_HEADS * Dh).astype(dt)

    out = lax.map(block_fn, (jnp.arange(n_qb), to_blocks(q_rot), to_blocks(q_nope), to_blocks(gate)))
    return out.swapaxes(0, 1).reshape(B, T, NSA_HEADS * Dh)


def gla_chunked(q, k, v, log_a):
    B, T, H, Dk = q.shape
    Dv = v.shape[-1]
    C = GLA_CHUNK
    n = T // C

    def chunks(a):
        return a.astype(jnp.float32).reshape(B, n, C, H, a.shape[-1])

    qf = chunks(q) * (Dk ** -0.5)
    kf = chunks(k)
    vf = chunks(v)
    b = jnp.cumsum(chunks(log_a), axis=2)
    b_last = b[:, :, -1]
    q_dec = qf * jnp.exp(b)
    k_intra = kf * jnp.exp(-b)
    k_state = kf * jnp.exp(b_last[:, :, None] - b)
    causal = jnp.tril(jnp.ones((C, C), dtype=bool))
    A = jnp.where(causal, jnp.einsum('bnihd,bnjhd->bnhij', q_dec, k_intra), 0.0)
    o_intra = jnp.einsum('bnhij,bnjhe->bnihe', A, vf)

    def step(S, inp):
        qd, ksd, vv, bl = inp
        o = jnp.einsum('bihd,bhde->bihe', qd, S)
        S = S * jnp.exp(bl)[..., None] + jnp.einsum('bjhd,bjhe->bhde', ksd, vv)
        return S, o

    S0 = jnp.zeros((B, H, Dk, Dv), jnp.float32)
    _, o_inter = lax.scan(step, S0, (q_dec.swapaxes(0, 1), k_state.swapaxes(0, 1), vf.swapaxes(0, 1), b_last.swapaxes(0, 1)))
    o = o_intra + o_inter.swapaxes(0, 1)
    return o.reshape(B, T, H, Dv)


def hybrid_mixer(h, w_in, cmp_pos_k, cmp_w1_k, cmp_w2_k, cmp_pos_v, cmp_w1_v, cmp_w2_v,
                 gla_w_a2, gla_b_a, gla_norm_w, w_out):
    B, T, _ = h.shape
    idx = np.cumsum(SPLIT_SIZES)[:-1].tolist()
    (nq, kc, vc, ks, vs, kw, vw, ng, gq, gk, gv, ga, gg) = jnp.split(h @ w_in, idx, axis=-1)

    def heads(a, n_h):
        return a.reshape(B, T, n_h, a.shape[-1] // n_h)

    nsa_out = nsa_attention(heads(nq, NSA_HEADS), heads(kc, NSA_KV_HEADS), heads(vc, NSA_KV_HEADS),
                            heads(ks, NSA_KV_HEADS), heads(vs, NSA_KV_HEADS), heads(kw, NSA_KV_HEADS),
                            heads(vw, NSA_KV_HEADS), heads(ng, NSA_HEADS),
                            cmp_pos_k, cmp_w1_k, cmp_w2_k, cmp_pos_v, cmp_w1_v, cmp_w2_v)
    log_a = jax.nn.log_sigmoid((ga @ gla_w_a2 + gla_b_a).astype(jnp.float32)) / GLA_GATE_NORM
    o = gla_chunked(heads(gq, GLA_HEADS), heads(gk, GLA_HEADS), heads(gv, GLA_HEADS), heads(log_a, GLA_HEADS))
    o = o * lax.rsqrt(jnp.mean(jnp.square(o), axis=-1, keepdims=True) + NORM_EPS) * gla_norm_w
    gla_out = (o * jax.nn.silu(heads(gg, GLA_HEADS).astype(jnp.float32))).reshape(B, T, GLA_WIDTH).astype(h.dtype)
    return jnp.concatenate([nsa_out, gla_out], axis=-1) @ w_out


def swiglu(h, w_gate, w_up, w_down):
    return (jax.nn.silu(h @ w_gate) * (h @ w_up)) @ w_down


def moe_swiglu(h, w_router, w_gate, w_up, w_down):
    B, T, D = h.shape
    xt = h.reshape(-1, D)
    N = xt.shape[0]
    A = N * TOP_K
    logits = (xt @ w_router).astype(jnp.float32)
    top_val, top_idx = lax.top_k(logits, TOP_K)
    comb = jax.nn.softmax(top_val, axis=-1)
    flat_e = top_idx.reshape(-1).astype(jnp.int32)
    flat_tok = jnp.repeat(jnp.arange(N, dtype=jnp.int32), TOP_K)
    order = jnp.argsort(flat_e)
    e_sorted = flat_e[order]
    tok_sorted = flat_tok[order]
    w_sorted = comb.reshape(-1)[order]
    counts = jnp.bincount(flat_e, length=N_EXPERTS)
    padded = (counts + MOE_BLOCK - 1) // MOE_BLOCK * MOE_BLOCK
    pad_end = jnp.cumsum(padded)
    pad_start = pad_end - padded
    seg_start = jnp.cumsum(counts) - counts
    dest = pad_start[e_sorted] + jnp.arange(A, dtype=jnp.int32) - seg_start[e_sorted]
    n_blocks = -(-A // MOE_BLOCK) + N_EXPERTS
    slot_tok = jnp.full((n_blocks * MOE_BLOCK,), N, jnp.int32).at[dest].set(tok_sorted)
    block_e = jnp.minimum(jnp.searchsorted(pad_end, jnp.arange(n_blocks) * MOE_BLOCK, side='right'), N_EXPERTS - 1)
    x_pad = jnp.concatenate([xt, jnp.zeros((1, D), xt.dtype)], axis=0)

    def expert_block(args):
        tok, e = args
        xb = x_pad[tok]
        return (jax.nn.silu(xb @ w_gate[e]) * (xb @ w_up[e])) @ w_down[e]

    y = lax.map(expert_block, (slot_tok.reshape(n_blocks, MOE_BLOCK), block_e)).reshape(-1, D)
    y_assign = y[dest] * w_sorted[:, None].astype(y.dtype)
    out = jax.ops.segment_sum(y_assign, tok_sorted, num_segments=N)
    return out.reshape(B, T, D)


def setup_inputs(seed: int = 0) -> dict:
    key = jax.random.key(seed)
    ks = jax.random.split(key, 26)
    L, D = DEPTH, D_MODEL

    def nrm(k, shape, s):
        return jax.random.normal(k, shape, jnp.float32) * s

    return {
        'x': nrm(ks[0], (BATCH, SEQ, D), 1.0),
        'c': nrm(ks[1], (BATCH, D), 1.0),
        'w_ada': nrm(ks[2], (L, D, 6 * D), ADA_INIT_SCALE * D ** -0.5),
        'b_ada': nrm(ks[3], (L, 6 * D), 0.01),
        'w_in': nrm(ks[4], (L, D, IN_WIDTH), D ** -0.5),
        'cmp_pos_k': nrm(ks[5], (L, CMP_BLOCK, HEAD_DIM), 0.02),
        'cmp_w1_k': nrm(ks[6], (L, CMP_BLOCK * HEAD_DIM, CMP_HIDDEN), (CMP_BLOCK * HEAD_DIM) ** -0.5),
        'cmp_w2_k': nrm(ks[7], (L, CMP_HIDDEN, HEAD_DIM), CMP_HIDDEN ** -0.5),
        'cmp_pos_v': nrm(ks[8], (L, CMP_BLOCK, HEAD_DIM), 0.02),
        'cmp_w1_v': nrm(ks[9], (L, CMP_BLOCK * HEAD_DIM, CMP_HIDDEN), (CMP_BLOCK * HEAD_DIM) ** -0.5),
        'cmp_w2_v': nrm(ks[10], (L, CMP_HIDDEN, HEAD_DIM), CMP_HIDDEN ** -0.5),
        'gla_w_a2': nrm(ks[11], (L, GLA_GATE_RANK, GLA_HEADS * GLA_DK), GLA_GATE_RANK ** -0.5),
        'gla_b_a': nrm(ks[12], (L, GLA_HEADS * GLA_DK), 0.01),
        'gla_norm_w': 1.0 + nrm(ks[13], (L, GLA_DV), 0.02),
        'w_out': nrm(ks[14], (L, MIX_WIDTH, D), DEEPNORM_BETA * MIX_WIDTH ** -0.5),
        'ln_mix_g': 1.0 + nrm(ks[15], (L, D), 0.02),
        'ln_mix_b': nrm(ks[16], (L, D), 0.02),
        'ln_ffn_g': 1.0 + nrm(ks[17], (L, D), 0.02),
        'ln_ffn_b': nrm(ks[18], (L, D), 0.02),
        'ffn_w_gate': nrm(ks[19], (N_DENSE, D, D_FF), D ** -0.5),
        'ffn_w_up': nrm(ks[20], (N_DENSE, D, D_FF), D ** -0.5),
        'ffn_w_down': nrm(ks[21], (N_DENSE, D_FF, D), DEEPNORM_BETA * D_FF ** -0.5),
        'moe_router': nrm(ks[22], (N_MOE, D, N_EXPERTS), D ** -0.5),
        'moe_w_gate': nrm(ks[23], (N_MOE, N_EXPERTS, D, D_FF_EXPERT), D ** -0.5),
        'moe_w_up': nrm(ks[24], (N_MOE, N_EXPERTS, D, D_FF_EXPERT), D ** -0.5),
        'moe_w_down': nrm(ks[25], (N_MOE, N_EXPERTS, D_FF_EXPERT, D), DEEPNORM_BETA * D_FF_EXPERT ** -0.5),
    }


def reference(x, c, w_ada, b_ada, w_in, cmp_pos_k, cmp_w1_k, cmp_w2_k, cmp_pos_v, cmp_w1_v, cmp_w2_v,
              gla_w_a2, gla_b_a, gla_norm_w, w_out, ln_mix_g, ln_mix_b, ln_ffn_g, ln_ffn_b,
              ffn_w_gate, ffn_w_up, ffn_w_down, moe_router, moe_w_gate, moe_w_up, moe_w_down):
    cond = jax.nn.silu(c)
    for layer in range(DEPTH):
        mod = cond @ w_ada[layer] + b_ada[layer]
        sh_a, sc_a, g_a, sh_f, sc_f, g_f = jnp.split(mod, 6, axis=-1)
        h = x * (1.0 + sc_a[:, None, :]) + sh_a[:, None, :]
        y = hybrid_mixer(h, w_in[layer], cmp_pos_k[layer], cmp_w1_k[layer], cmp_w2_k[layer],
                         cmp_pos_v[layer], cmp_w1_v[layer], cmp_w2_v[layer],
                         gla_w_a2[layer], gla_b_a[layer], gla_norm_w[layer], w_out[layer])
        x = layer_norm(DEEPNORM_ALPHA * x + (1.0 + g_a[:, None, :]) * y, ln_mix_g[layer], ln_mix_b[layer])
        h = x * (1.0 + sc_f[:, None, :]) + sh_f[:, None, :]
        if layer % 2 == 0:
            i = layer // 2
            y = swiglu(h, ffn_w_gate[i], ffn_w_up[i], ffn_w_down[i])
        else:
            i = layer // 2
            y = moe_swiglu(h, moe_router[i], moe_w_gate[i], moe_w_up[i], moe_w_down[i])
        x = layer_norm(DEEPNORM_ALPHA * x + (1.0 + g_f[:, None, :]) * y, ln_ffn_g[layer], ln_ffn_b[layer])
    return x
```

```python
import numpy as np
import concourse.bass as bass
import concourse.mybir as mybir
from concourse.bass_utils import run_bass_kernel_spmd

F32 = mybir.dt.float32
BF16 = mybir.dt.bfloat16
I32 = mybir.dt.int32
U32 = mybir.dt.uint32
AF = mybir.ActivationFunctionType
ALU = mybir.AluOpType
AX = mybir.AxisListType

ENGS = ("tensor", "vector", "scalar", "gpsimd", "sync")

D = 2048
T = 4096
DEPTH = 2
KC = D // 128
HD = 128
SPLIT = (1024, 256, 256, 256, 256, 256, 256, 24, 512, 512, 1024, 16, 1024)
SEG = ("nq", "kc", "vc", "ks", "vs", "kw", "vw", "ng", "gq", "gk", "gv", "ga", "gg")
OFF = {}
_o = 0
for _n, _s in zip(SEG, SPLIT):
    OFF[_n] = (_o, _s)
    _o += _s
IN_W = _o
D_FF = 5632
NE = 8
D_FFE = 7168
ALPHA = (2 * DEPTH) ** 0.25
LN_EPS = 1e-5
NORM_EPS = 1e-6
NEG = -30000.0
SCALE = HD ** -0.5


class Sem:
    def __init__(self, h, kind, uid):
        self.h = h
        self.kind = kind
        self.count = 0
        self.key = "s%d" % uid


class Buf:
    def __init__(self, t, name):
        self.t = t
        self.name = name
        self.last_w = None
        self.readers = {}
        self.sem = {"sw": None, "hw": None}
        self.dlast = {"sw": 0, "hw": 0}
        self.dma_w = {"sw": 0, "hw": 0}
        self.psum = False
        self.fdeps = []

    def __getitem__(self, idx):
        return self.t[idx]

    def reset(self):
        self.last_w = None
        self.readers = {}
        self.fdeps = []
        self.dlast["hw"] = 0
        self.dma_w["hw"] = 0


class _Rec:
    def __init__(self):
        self.calls = []

    def __getattr__(self, name):
        def f(*a, **kw):
            self.calls.append((name, a, kw))
            return self
        return f


class Prog:
    def __init__(self, nc, same_engine_raw=True):
        self.nc = nc
        self.same_engine_raw = same_engine_raw
        self.psem = {}
        self.cnt = {e: 0 for e in ENGS}
        self.lists = {e: [] for e in ENGS}
        self.seen = {e: {} for e in ENGS}
        self.bufs = []
        self._cms = []
        self._semcms = []
        self.pool = {"sw": [], "hw": []}
        self.allsems = []
        for e in ENGS:
            cm = nc.semaphore("p_" + e)
            self.psem[e] = cm.__enter__()
            self._semcms.append(cm)
        self.n_inst = 0
        self.uid = 0
        self._bcreg = {}
        self._bcset = set()

    def mark(self):
        return len(self._cms)

    def release(self, mark):
        while len(self._cms) > mark:
            cm, b = self._cms.pop()
            cm.__exit__(None, None, None)
            if b is not None:
                self._drop(b)

    def _drop(self, b):
        for kind in ("sw", "hw"):
            if b.sem[kind] is not None:
                self.pool[kind].append(b.sem[kind])
                b.sem[kind] = None
        if b in self.bufs:
            self.bufs.remove(b)
        for v in getattr(b, "views", []):
            self._drop(v)

    def sb(self, name, shape, dt):
        self.uid += 1
        cm = self.nc.sbuf_tensor("%s_%d" % (name, self.uid), list(shape), dt)
        t = cm.__enter__()
        b = Buf(t, "%s_%d" % (name, self.uid))
        b.views = []
        self._cms.append((cm, b))
        self.bufs.append(b)
        return b

    def ps(self, name, shape, dt=F32):
        self.uid += 1
        cm = self.nc.psum_tensor("%s_%d" % (name, self.uid), list(shape), dt)
        t = cm.__enter__()
        b = Buf(t, "%s_%d" % (name, self.uid))
        b.psum = True
        b.views = []
        self._cms.append((cm, b))
        self.bufs.append(b)
        return b

    def view(self, buf, name=None):
        self.uid += 1
        b = Buf(buf.t, "%s_v%d" % (buf.name, self.uid))
        buf.views.append(b)
        self.bufs.append(b)
        return b

    def _getsem(self, b, kind):
        if b.sem[kind] is None:
            if self.pool[kind]:
                b.sem[kind] = self.pool[kind].pop()
            else:
                self.uid += 1
                cm = self.nc.semaphore("d%s_%d" % (kind, self.uid))
                h = cm.__enter__()
                self._semcms.append(cm)
                sm = Sem(h, kind, self.uid)
                self.allsems.append(sm)
                b.sem[kind] = sm
        return b.sem[kind]

    def _waits(self, e, reads, writes):
        w = {}

        def need(sem, val, key):
            if val <= 0:
                return
            if self.seen[e].get(key, 0) >= val:
                return
            if key not in w or w[key][1] < val:
                w[key] = (sem, val)

        for r in reads:
            if r.last_w is not None:
                we, n = r.last_w
                if we != e or self.same_engine_raw:
                    need(self.psem[we], n, "p_" + we)
            for kind in ("sw", "hw"):
                if r.dma_w[kind] > 0:
                    need(r.sem[kind].h, 16 * r.dma_w[kind], r.sem[kind].key)
            if r.psum:
                for re_, n in r.readers.items():
                    if re_ != e:
                        need(self.psem[re_], n, "p_" + re_)
        for b in writes:
            if b.last_w is not None:
                we, n = b.last_w
                if we != e:
                    need(self.psem[we], n, "p_" + we)
            for re_, n in b.readers.items():
                if re_ != e:
                    need(self.psem[re_], n, "p_" + re_)
            for kind in ("sw", "hw"):
                if b.dlast[kind] > 0:
                    need(b.sem[kind].h, 16 * b.dlast[kind], b.sem[kind].key)
            for (fs, fv, fk) in b.fdeps:
                need(fs, fv, fk)
        for key, (sem, val) in w.items():
            self.seen[e][key] = val
        return list(w.values())

    def op(self, e, fn, reads=(), writes=(), signal=True):
        waits = self._waits(e, reads, writes)
        n = self.cnt[e] + 1
        if signal:
            self.cnt[e] = n
        psem = self.psem[e]
        rec = _Rec()
        fn(rec)
        assert len(rec.calls) == 1, rec.calls
        name, a, kw = rec.calls[0]

        def thunk(engine, waits=waits, name=name, a=a, kw=kw, signal=signal, psem=psem):
            for sem, val in waits:
                engine.wait_ge(sem, val)
            ins = getattr(engine, name)(*a, **kw)
            if signal:
                ins.then_inc(psem, 1)

        self.lists[e].append(thunk)
        self.n_inst += 1
        for r in reads:
            r.readers[e] = n
        for b in writes:
            b.last_w = (e, n)
            b.readers = {}
            b.dma_w = {"sw": 0, "hw": 0}
            b.fdeps = []

    def _dma_book(self, q, reads, writes):
        kind = "sw" if q == "gpsimd" else "hw"
        waits = self._waits(q, reads, writes)
        allb = list(writes) + list(reads)
        prim = allb[0]
        sm = self._getsem(prim, kind)
        sm.count += 1
        prim.dlast[kind] = sm.count
        for b in allb[1:]:
            assert b not in writes
            b.fdeps.append((sm.h, 16 * sm.count, sm.key))
        for b in writes:
            b.dma_w[kind] = sm.count
            b.last_w = None
            b.readers = {}
            b.fdeps = []
        return waits, sm.h

    def dma(self, q, out_ap, in_ap, reads=(), writes=(), **kw):
        waits, semh = self._dma_book(q, reads, writes)

        def thunk(engine, waits=waits, semh=semh, out_ap=out_ap, in_ap=in_ap, kw=kw):
            for sem, val in waits:
                engine.wait_ge(sem, val)
            engine.dma_start(out=out_ap, in_=in_ap, **kw).then_inc(semh, 16)

        self.lists[q].append(thunk)
        self.n_inst += 1

    def gather(self, out_ap, in_ap, idx_ap, reads=(), writes=(), scatter=False, **kw):
        q = "gpsimd"
        waits, semh = self._dma_book(q, reads, writes)
        bc = kw.pop("bounds_check", None)

        def thunk(engine, waits=waits, semh=semh, kw=kw, bc=bc):
            for sem, val in waits:
                engine.wait_ge(sem, val)
            if bc is not None:
                if bc not in self._bcreg:
                    self._bcreg[bc] = engine.alloc_register("bcreg_%d" % int(bc))
                if bc not in self._bcset:
                    engine.reg_mov(self._bcreg[bc], bc)
                    self._bcset.add(bc)
                kw = dict(kw)
                kw["bounds_check"] = self._bcreg[bc]
            if scatter:
                ins = engine.indirect_dma_start(out=out_ap, out_offset=bass.IndirectOffsetOnAxis(ap=idx_ap, axis=0),
                                                in_=in_ap, in_offset=None, **kw)
            else:
                ins = engine.indirect_dma_start(out=out_ap, out_offset=None, in_=in_ap,
                                                in_offset=bass.IndirectOffsetOnAxis(ap=idx_ap, axis=0), **kw)
            ins.then_inc(semh, 16)

        self.lists[q].append(thunk)
        self.n_inst += 1

    def flush(self, final=False):
        nc = self.nc
        drain = []
        for sm in self.allsems:
            if sm.count > 0:
                drain.append((sm.h, 16 * sm.count))
        for e in ENGS:
            if e != "sync" and self.cnt[e] > 0:
                drain.append((self.psem[e], self.cnt[e]))

        def dthunk(engine, drain=drain):
            for sem, val in drain:
                engine.wait_ge(sem, val)

        self.lists["sync"].append(dthunk)
        lists = self.lists
        with nc.Block() as block:
            @block.tensor
            def _(eng):
                for th in lists["tensor"]:
                    th(eng)

            @block.vector
            def _(eng):
                for th in lists["vector"]:
                    th(eng)

            @block.scalar
            def _(eng):
                for th in lists["scalar"]:
                    th(eng)

            @block.gpsimd
            def _(eng):
                for th in lists["gpsimd"]:
                    th(eng)

            @block.sync
            def _(eng):
                for th in lists["sync"]:
                    th(eng)
        if not final:
            nc.all_engine_barrier()
            sems = [self.psem[e] for e in ENGS] + [sm.h for sm in self.allsems if sm.kind == "hw" and sm.count > 0]
            with nc.Block() as block:
                @block.sync
                def _(eng):
                    for s_ in sems:
                        eng.sem_clear(s_)
            nc.all_engine_barrier()
        for sm in self.allsems:
            if sm.kind == "hw":
                sm.count = 0
        self.lists = {e: [] for e in ENGS}
        self.cnt = {e: 0 for e in ENGS}
        self.seen = {e: {} for e in ENGS}
        self._bcset = set()
        for b in self.bufs:
            b.reset()

    def close(self):
        self.release(0)
        for cm in reversed(self._semcms):
            cm.__exit__(None, None, None)
        self._semcms = []


def host_consts():
    c = {}
    c["ident"] = np.eye(128, dtype=np.float32)
    half = 16
    inv = (500000.0 ** (-np.arange(half, dtype=np.float32) * 2.0 / 32)).astype(np.float32)
    ang = np.arange(T, dtype=np.float32)[None, :] * inv[:, None]
    cos = np.cos(ang).astype(np.float32)
    sin = np.sin(ang).astype(np.float32)
    c["cos"] = np.concatenate([cos, cos], 0)
    c["sin"] = np.concatenate([sin, sin], 0)
    pm = np.zeros((128, 128), np.float32)
    for i in range(16):
        pm[i + 16, i] = -1.0
        pm[i, i + 16] = 1.0
    c["pm"] = pm
    kk = np.arange(128)[:, None]
    tt = np.arange(128)[None, :]
    c["cb"] = np.where(kk > tt, NEG, 0.0).astype(np.float32)
    c["wbm"] = np.where(kk <= tt, NEG, 0.0).astype(np.float32)
    es = np.zeros((64, 32, 128), np.float32)
    for kt in range(32):
        for m in range(128):
            es[2 * kt + m // 64, kt, m] = 1.0
    c["esel"] = es.reshape(64, 32 * 128)
    cst = np.arange(256) * 16
    bst = np.arange(64) * 64
    ov = ((cst[:, None] < bst[None, :] + 64) & (cst[:, None] + 32 > bst[None, :])).astype(np.float32)
    ov[255] = 0.0
    c["ovl"] = np.ascontiguousarray(ov.reshape(2, 128, 64).transpose(1, 0, 2)).reshape(128, 128)
    c["mb"] = (16.0 * kk - tt).astype(np.float32)
    keep = np.zeros((128, 32, 64), np.float32)
    add = np.zeros((128, 32, 64), np.float32)
    n = np.arange(64)[None, :]
    for qt in range(32):
        t = qt * 128 + np.arange(128)[:, None]
        cur = t // 64
        forced = (n == 0) | (n == cur) | (n == cur - 1)
        future = (n * 64) > t
        keep[:, qt, :] = np.where(forced | future, 0.0, 1.0)
        add[:, qt, :] = np.where(future, -1e30, np.where(forced, 1e9, 0.0))
    c["keep"] = keep.reshape(128, 32 * 64)
    c["addc"] = add.reshape(128, 32 * 64)
    sel = np.zeros((12, 12, 128), np.float32)
    for r in range(12):
        sel[r, r, :] = 1.0
    c["sel"] = sel.reshape(12, 12 * 128)
    same = (kk // 64) == (tt // 64)
    c["slmat"] = (kk < tt).astype(np.float32)
    c["eoff"] = np.tile((np.arange(NE) * 1280.0)[None, :], (128, 1)).astype(np.float32)
    c["tri2"] = (same & (kk <= tt)).astype(np.float32)
    c["sumat"] = (same & (kk > tt)).astype(np.float32)
    return c


class K:
    pass


def build(debug=(), phases=None, layers=(0, 1), l1_from_x=False):
    nc = bass.Bass("TRN2", target_bir_lowering=False)
    k = K()
    k.nc = nc
    dbg = set(debug)

    def din(name, shape, dt=F32):
        return nc.dram_tensor(name, list(shape), dt, kind="ExternalInput").ap()

    def dscr(name, shape, dt):
        kind = "ExternalOutput" if name in dbg else "Internal"
        return nc.dram_tensor(name, list(shape), dt, kind=kind).ap()

    k.x = din("x", [T, D])
    k.cT = din("cT", [128, KC])
    k.w_ada = din("w_ada", [DEPTH, D, 6 * D])
    k.b_ada = din("b_ada", [DEPTH, 6 * D])
    k.w_in = din("w_in", [DEPTH, D, IN_W])
    k.ident = din("ident", [128, 128])
    k.cos = din("cos", [32, T])
    k.sin = din("sin", [32, T])
    k.pm = din("pm", [128, 128])
    k.cb = din("cb", [128, 128])
    k.wbm = din("wbm", [128, 128])
    k.esel = din("esel", [64, 32 * 128])
    k.ovl = din("ovl", [128, 128])
    k.mb = din("mb", [128, 128])
    k.keep = din("keep", [128, 32 * 64])
    k.addc = din("addc", [128, 32 * 64])
    k.sel = din("sel", [12, 12 * 128])
    k.w_out = din("w_out", [DEPTH, D, D])
    k.ln_mix_g = din("ln_mix_g", [DEPTH, D])
    k.ln_mix_b = din("ln_mix_b", [DEPTH, D])
    k.ln_ffn_g = din("ln_ffn_g", [DEPTH, D])
    k.ln_ffn_b = din("ln_ffn_b", [DEPTH, D])
    k.ffn_w_gate = din("ffn_w_gate", [1, D, D_FF])
    k.ffn_w_up = din("ffn_w_up", [1, D, D_FF])
    k.ffn_w_down = din("ffn_w_down", [1, D_FF, D])
    k.moe_router = din("moe_router", [1, D, NE])
    k.moe_w_gate = din("moe_w_gate", [1, NE, D, D_FFE])
    k.moe_w_up = din("moe_w_up", [1, NE, D, D_FFE])
    k.moe_w_down = din("moe_w_down", [1, NE, D_FFE, D])
    k.slmat = din("slmat", [128, 128])
    k.eoff = din("eoff", [128, NE])
    k.tri2 = din("tri2", [128, 128])
    k.sumat = din("sumat", [128, 128])
    k.gla_w_a2 = din("gla_w_a2", [DEPTH, 16, 512])
    k.gla_b_a = din("gla_b_a", [DEPTH, 512])
    k.gla_nwT = din("gla_nwT", [DEPTH, 128, 2])
    k.cmp_w1 = {"k": din("cmp_w1_k", [DEPTH, 4096, 256]), "v": din("cmp_w1_v", [DEPTH, 4096, 256])}
    k.cmp_w2 = {"k": din("cmp_w2_k", [DEPTH, 256, 128]), "v": din("cmp_w2_v", [DEPTH, 256, 128])}
    k.cmp_peT = {"k": din("cmp_peT_k", [DEPTH, 128, 32]), "v": din("cmp_peT_v", [DEPTH, 128, 32])}
    k.out = nc.dram_tensor("out", [T, D], F32, kind="ExternalOutput").ap()

    k.modv = dscr("modv", [DEPTH, 6 * D], F32)
    k.qn = dscr("qn", [1024, T], BF16)
    k.qr = dscr("qr", [1024, T], BF16)
    k.kcT = dscr("kcT", [256, T], BF16)
    k.vcT = dscr("vcT", [256, T], BF16)
    k.ksT = dscr("ksT", [256, T], BF16)
    k.kwT = dscr("kwT", [256, T], BF16)
    k.ngT = dscr("ngT", [24, T], BF16)
    k.gqT = dscr("gqT", [512, T], BF16)
    k.gkT = dscr("gkT", [512, T], BF16)
    k.gaT = dscr("gaT", [16, T], BF16)
    k.ggT = dscr("ggT", [1024, T], BF16)
    k.vs = dscr("vs", [T, 256], BF16)
    k.vw = dscr("vw", [T, 256], BF16)
    k.gk = dscr("gk", [T, 512], BF16)
    k.gv = dscr("gv", [T, 1024], BF16)
    k.kcmpT = dscr("kcmpT", [2, 128, 256], BF16)
    k.vcmp = dscr("vcmp", [2, 256, 128], BF16)
    k.mixT = dscr("mixT", [D, T], BF16)
    k.x1 = dscr("x1", [T, D], F32)
    k.xa = dscr("xa", [T, D], F32)
    k.yffn = dscr("yffn", [T, D], F32)
    k.AT = dscr("AT", [D_FFE, T], BF16)
    k.Xs = dscr("Xs", [NE * 1280, D], BF16)
    k.Ys = dscr("Ys", [NE * 1280, D], F32)
    k.midx = dscr("midx", [T, 2], I32)
    k.mwts = dscr("mwts", [T, 2], F32)

    P = Prog(nc)
    k.P = P
    ph = phases

    if ph is None or "mod" in ph:
        phase_mod(k)
    for l in layers:
        xsrc = k.x if (l == 0 or l1_from_x) else k.xa
        if ph is None or "proj" in ph:
            phase_proj(k, l, xsrc)
        if ph is None or "cmp" in ph:
            phase_cmp(k, l)
        if ph is None or "nsa" in ph:
            phase_nsa(k, l)
        if ph is None or "gla" in ph:
            phase_gla(k, l)
        if ph is None or "wout" in ph:
            phase_wout(k, l, xsrc, k.x1)
        xdst = k.out if l == DEPTH - 1 else k.xa
        if l % 2 == 0:
            if ph is None or "ffn" in ph:
                ffn_gateup(k, k.x1, T, k.ffn_w_gate[l // 2], k.ffn_w_up[l // 2], D_FF, k.AT[0:D_FF, :], mod=l)
                ffn_down(k, k.AT[0:D_FF, :], T, k.ffn_w_down[l // 2], D_FF, k.yffn)
            if ph is None or "ln2" in ph:
                phase_ln2(k, l, k.x1, k.yffn, xdst)
        else:
            if ph is None or "route" in ph:
                phase_moe_route(k, l)
            if ph is None or "experts" in ph:
                phase_moe_experts(k, l)
            if ph is None or "ln2" in ph:
                phase_moe_ln2(k, l, k.x1, xdst)

    P.flush(final=True)
    P.close()
    return nc


def phase_mod(k):
    P = k.P
    m0 = P.mark()
    cc = P.sb("cc", [128, KC], F32)
    cs = P.sb("cs", [128, KC], F32)
    condB = P.sb("condB", [128, KC, 128], F32)
    wb = [P.sb("wada%d" % i, [128, KC, 512], F32) for i in range(2)]
    bb = [P.sb("bada%d" % i, [128, 512], F32) for i in range(2)]
    mo = [P.sb("mo%d" % i, [128, 512], F32) for i in range(2)]
    pm = [P.ps("pmod%d" % i, [128, 512]) for i in range(2)]
    P.dma("sync", cc[:], k.cT, writes=[cc])
    P.op("scalar", lambda e: e.activation(out=cs[:], in_=cc[:], func=AF.Silu), reads=[cc], writes=[cs])
    P.op("vector", lambda e: e.tensor_copy(out=condB[:], in_=cs[:].unsqueeze(2).to_broadcast([128, KC, 128])),
         reads=[cs], writes=[condB])
    it = 0
    import os
    NBL = int(os.environ.get("NBL", "24"))
    VAR = os.environ.get("VAR", "")
    for l in range(DEPTH):
        wv = k.w_ada[l].rearrange("(c p) n -> p c n", p=128)
        for nb in range(NBL):
            i = it % 2
            it += 1
            n0 = nb * 512
            P.dma("sync", wb[i][:], wv[:, :, n0:n0 + 512], writes=[wb[i]])
            if VAR == "nobb":
                P.op("vector", lambda e, i=i: e.memset(bb[i][:], 0.0), writes=[bb[i]])
            else:
                P.dma("gpsimd", bb[i][:], k.b_ada[l, n0:n0 + 512].partition_broadcast(128), writes=[bb[i]])
            for c in range(KC):
                P.op("tensor", lambda e, i=i, c=c: e.matmul(pm[i][:], lhsT=condB[:, c, :], rhs=wb[i][:, c, :],
                                                             start=(c == 0), stop=(c == KC - 1)),
                     reads=[condB, wb[i]], writes=[pm[i]], signal=(c == KC - 1))
            seg = nb // 4
            add1 = 1.0 if seg in (1, 2, 4, 5) else 0.0
            P.op("vector", lambda e, i=i, add1=add1: e.scalar_tensor_tensor(
                out=mo[i][:], in0=pm[i][:], scalar=add1, in1=bb[i][:], op0=ALU.add, op1=ALU.add),
                reads=[pm[i], bb[i]], writes=[mo[i]])
            P.dma("gpsimd", k.modv[l:l + 1, n0:n0 + 512], mo[i][0:1, :], reads=[mo[i]])
    P.flush()
    P.release(m0)


def build_hT(k, xsrc, t0, ntiles, screp, shrep, identf, hT, hviews, xt, ptr):
    P = k.P
    for j in range(ntiles):
        xb = xt[j % 2]
        P.dma("sync", xb[:], xsrc[t0 + j * 128:t0 + (j + 1) * 128, :], writes=[xb])
        P.op("vector", lambda e, xb=xb: e.tensor_tensor(out=xb[:], in0=xb[:], in1=screp[:], op=ALU.mult),
             reads=[xb, screp], writes=[xb])
        P.op("gpsimd", lambda e, xb=xb: e.tensor_tensor(out=xb[:], in0=xb[:], in1=shrep[:], op=ALU.add),
             reads=[xb, shrep], writes=[xb])
        for g in range(KC // 4):
            pt = ptr[(j * 4 + g) % 2]
            for q in range(4):
                c = g * 4 + q
                P.op("tensor", lambda e, xb=xb, pt=pt, q=q, c=c: e.transpose(
                    out=pt[:, q, :], in_=xb[:, c * 128:(c + 1) * 128], identity=identf[:]),
                    reads=[xb, identf], writes=[pt], signal=(q == 3))
            eng = "scalar" if (g % 2 == 0) else "vector"
            if eng == "scalar":
                P.op("scalar", lambda e, pt=pt, g=g, j=j: e.copy(out=hT[:, g * 4:(g + 1) * 4, j * 128:(j + 1) * 128], in_=pt[:]),
                     reads=[pt], writes=[hviews[j][0]])
            else:
                P.op("vector", lambda e, pt=pt, g=g, j=j: e.tensor_copy(out=hT[:, g * 4:(g + 1) * 4, j * 128:(j + 1) * 128], in_=pt[:]),
                     reads=[pt], writes=[hviews[j][1]])


def phase_proj(k, l, xsrc):
    P = k.P
    TH = 2048
    for th in range(T // TH):
        t0 = th * TH
        m0 = P.mark()
        screp = P.sb("screp", [128, D], F32)
        shrep = P.sb("shrep", [128, D], F32)
        identf = P.sb("identf", [128, 128], F32)
        hT = P.sb("hT", [128, KC, TH], BF16)
        hviews = [(P.view(hT), P.view(hT)) for _ in range(TH // 128)]
        xt = [P.sb("xt%d" % i, [128, D], F32) for i in range(2)]
        ptr = [P.ps("ptr%d" % i, [128, 4, 128]) for i in range(2)]
        cosb = P.sb("cosb", [32, TH], F32)
        sinb = P.sb("sinb", [32, TH], F32)
        pmb = P.sb("pmb", [128, 128], BF16)
        wbk = [P.sb("wblk%d" % i, [128, KC, 512], BF16) for i in range(2)]
        pacc = [P.ps("pacc%d" % i, [128, 512]) for i in range(3)]
        prot = [P.ps("prot%d" % i, [128, 512]) for i in range(2)]
        ost = [P.sb("ost%d" % i, [128, 512], BF16) for i in range(4)]
        ost2 = [P.sb("ost2%d" % i, [128, 512], BF16) for i in range(2)]
        t1 = [P.sb("t1%d" % i, [32, 512], F32) for i in range(2)]
        t2 = [P.sb("t2%d" % i, [32, 512], F32) for i in range(2)]

        P.dma("sync", screp[:], k.modv[l, D:2 * D].partition_broadcast(128), writes=[screp])
        P.dma("sync", shrep[:], k.modv[l, 0:D].partition_broadcast(128), writes=[shrep])
        P.dma("sync", identf[:], k.ident, writes=[identf])
        P.dma("sync", cosb[:], k.cos[:, t0:t0 + TH], writes=[cosb])
        P.dma("sync", sinb[:], k.sin[:, t0:t0 + TH], writes=[sinb])
        P.dma("gpsimd", pmb[:], k.pm, writes=[pmb])
        build_hT(k, xsrc, t0, TH // 128, screp, shrep, identf, hT, hviews, xt, ptr)

        wv = k.w_in[l].rearrange("(c p) n -> p c n", p=128)
        cnt = {"w": 0, "acc": 0, "ost": 0, "ost2": 0, "rot": 0}

        def hreads(tb):
            r = []
            for j in range(tb * 4, tb * 4 + 4):
                r += [hviews[j][0], hviews[j][1]]
            return r

        def load_w(c0, ncols):
            wb = wbk[cnt["w"] % 2]
            cnt["w"] += 1
            P.dma("gpsimd", wb[:, :, 0:ncols], wv[:, :, c0:c0 + ncols], writes=[wb])
            return wb

        def ftype(seg, dst, mode):
            c0, n = OFF[seg]
            for b0 in range(0, n, 512):
                nb = min(512, n - b0)
                wb = load_w(c0 + b0, nb)
                for ct in range(0, nb, 128):
                    m = min(128, nb - ct)
                    row0 = b0 + ct
                    for tb in range(TH // 512):
                        pa = pacc[cnt["acc"] % 3]
                        cnt["acc"] += 1
                        for c in range(KC):
                            P.op("tensor", lambda e, pa=pa, wb=wb, ct=ct, m=m, c=c, tb=tb: e.matmul(
                                pa[0:m, :], lhsT=wb[:, c, ct:ct + m], rhs=hT[:, c, tb * 512:(tb + 1) * 512],
                                start=(c == 0), stop=(c == KC - 1)),
                                reads=[wb] + hreads(tb), writes=[pa], signal=(c == KC - 1))
                        ob = ost[cnt["ost"] % 4]
                        cnt["ost"] += 1
                        tok = slice(t0 + tb * 512, t0 + (tb + 1) * 512)
                        if mode == "plain":
                            P.op("scalar", lambda e, ob=ob, pa=pa, m=m: e.copy(out=ob[0:m, :], in_=pa[0:m, :]),
                                 reads=[pa], writes=[ob])
                            P.dma("sync", dst[row0:row0 + m, tok], ob[0:m, :], reads=[ob])
                        elif mode == "sigmoid":
                            P.op("scalar", lambda e, ob=ob, pa=pa, m=m: e.activation(out=ob[0:m, :], in_=pa[0:m, :], func=AF.Sigmoid),
                                 reads=[pa], writes=[ob])
                            P.dma("sync", dst[row0:row0 + m, tok], ob[0:m, :], reads=[ob])
                        elif mode == "silu":
                            P.op("scalar", lambda e, ob=ob, pa=pa, m=m: e.activation(out=ob[0:m, :], in_=pa[0:m, :], func=AF.Silu),
                                 reads=[pa], writes=[ob])
                            P.dma("sync", dst[row0:row0 + m, tok], ob[0:m, :], reads=[ob])
                        else:
                            dn, dr = mode[1], mode[2]
                            P.op("scalar", lambda e, ob=ob, pa=pa: e.copy(out=ob[:], in_=pa[:]), reads=[pa], writes=[ob])
                            pr = prot[cnt["rot"] % 2]
                            a1 = t1[cnt["rot"] % 2]
                            a2 = t2[cnt["rot"] % 2]
                            cnt["rot"] += 1
                            P.op("tensor", lambda e, pr=pr, ob=ob: e.matmul(pr[:], lhsT=pmb[:], rhs=ob[:], start=True, stop=True),
                                 reads=[pmb, ob], writes=[pr])
                            ltok = slice(tb * 512, (tb + 1) * 512)
                            P.op("vector", lambda e, a1=a1, pa=pa, ltok=ltok: e.tensor_tensor(out=a1[:], in0=pa[0:32, :], in1=cosb[:, ltok], op=ALU.mult),
                                 reads=[pa, cosb], writes=[a1])
                            P.op("vector", lambda e, a2=a2, pr=pr, ltok=ltok: e.tensor_tensor(out=a2[:], in0=pr[0:32, :], in1=sinb[:, ltok], op=ALU.mult),
                                 reads=[pr, sinb], writes=[a2])
                            if dn is not None:
                                P.dma("sync", dn[row0:row0 + 128, tok], ob[:], reads=[ob])
                                o2 = ost2[cnt["ost2"] % 2]
                                cnt["ost2"] += 1
                                P.op("scalar", lambda e, o2=o2, pa=pa: e.copy(out=o2[:], in_=pa[:]), reads=[pa], writes=[o2])
                                P.op("vector", lambda e, o2=o2, a1=a1, a2=a2: e.tensor_tensor(out=o2[0:32, :], in0=a1[:], in1=a2[:], op=ALU.add),
                                     reads=[a1, a2], writes=[o2])
                                P.dma("sync", dr[row0:row0 + 128, tok], o2[:], reads=[o2])
                            else:
                                P.op("vector", lambda e, ob=ob, a1=a1, a2=a2: e.tensor_tensor(out=ob[0:32, :], in0=a1[:], in1=a2[:], op=ALU.add),
                                     reads=[a1, a2, ob], writes=[ob])
                                P.dma("sync", dr[row0:row0 + 128, tok], ob[:], reads=[ob])

        def ttype(seg, dst):
            c0, n = OFF[seg]
            for b0 in range(0, n, 512):
                nb = min(512, n - b0)
                wb = load_w(c0 + b0, nb)
                for j in range(TH // 128):
                    pa = pacc[cnt["acc"] % 3]
                    cnt["acc"] += 1
                    for c in range(KC):
                        P.op("tensor", lambda e, pa=pa, wb=wb, nb=nb, c=c, j=j: e.matmul(
                            pa[:, 0:nb], lhsT=hT[:, c, j * 128:(j + 1) * 128], rhs=wb[:, c, 0:nb],
                            start=(c == 0), stop=(c == KC - 1)),
                            reads=[wb, hviews[j][0], hviews[j][1]], writes=[pa], signal=(c == KC - 1))
                    ob = ost[cnt["ost"] % 4]
                    cnt["ost"] += 1
                    P.op("scalar", lambda e, ob=ob, pa=pa, nb=nb: e.copy(out=ob[:, 0:nb], in_=pa[:, 0:nb]), reads=[pa], writes=[ob])
                    P.dma("sync", dst[t0 + j * 128:t0 + (j + 1) * 128, b0:b0 + nb], ob[:, 0:nb], reads=[ob])

        import os
        SEGS = os.environ.get("SEGS", "")
        plan = [("nq", "f", None, ("rope", k.qn, k.qr)), ("kc", "f", k.kcT, "plain"), ("vc", "f", k.vcT, "plain"),
                ("ks", "f", None, ("rope", None, k.ksT)), ("vs", "t", k.vs, None), ("kw", "f", None, ("rope", None, k.kwT)),
                ("vw", "t", k.vw, None), ("ng", "f", k.ngT, "sigmoid"), ("gq", "f", k.gqT, "plain"), ("gk", "f", k.gkT, "plain"),
                ("gk", "t", k.gk, None), ("gv", "t", k.gv, None), ("ga", "f", k.gaT, "plain"), ("gg", "f", k.ggT, "silu")]
        for (sg, ty, dst, mode) in plan:
            if SEGS and (sg + ty) not in SEGS.split(","):
                continue
            if ty == "f":
                ftype(sg, dst, mode)
            else:
                ttype(sg, dst)
        P.flush()
        P.release(m0)


def phase_cmp(k, l):
    P = k.P
    m0 = P.mark()
    aT = [P.sb("aT%d" % i, [128, T], BF16) for i in range(2)]
    w1b = [P.sb("w1b%d" % i, [128, 32, 256], BF16) for i in range(2)]
    w2b = [P.sb("w2b%d" % i, [128, 2, 128], BF16) for i in range(2)]
    peT = [P.sb("peT%d" % i, [128, 32], BF16) for i in range(2)]
    ph = [P.ps("ph%d" % i, [128, 512]) for i in range(2)]
    pc = P.ps("pc", [128, 512])
    po = P.ps("pcmpo", [128, 512])
    cst = P.sb("cst", [128, 2], F32)
    xh = [P.sb("xh%d" % i, [128, 256], F32) for i in range(2)]
    uu = [P.sb("uu%d" % i, [128, 256], F32) for i in range(2)]
    sg = [P.sb("sgm%d" % i, [128, 256], F32) for i in range(2)]
    gb = [P.sb("gb%d" % i, [128, 256], BF16) for i in range(2)]
    ocp = [P.sb("ocp%d" % i, [128, 256], BF16) for i in range(2)]
    for i in range(2):
        P.op("vector", lambda e, i=i: e.memset(gb[i][:], 0.0), writes=[gb[i]])
        P.op("vector", lambda e, i=i: e.memset(ocp[i][:], 0.0), writes=[ocp[i]])
    it = 0
    for si, src in enumerate(("k", "v")):
        w1 = k.cmp_w1[src][l].rearrange("(l d) m -> d l m", d=128)
        for q4 in range(4):
            P.dma("gpsimd", w1b[si][:, q4 * 8:(q4 + 1) * 8, :], w1[:, q4 * 8:(q4 + 1) * 8, :], writes=[w1b[si]])
        P.dma("gpsimd", w2b[si][:], k.cmp_w2[src][l].rearrange("(c p) n -> p c n", p=128), writes=[w2b[si]])
        P.dma("gpsimd", peT[si][:], k.cmp_peT[src][l], writes=[peT[si]])
        srcT = k.kcT if src == "k" else k.vcT
        for hk in range(2):
            a = aT[it % 2]
            oc = ocp[it % 2]
            it += 1
            P.dma("sync", a[:], srcT[hk * 128:(hk + 1) * 128, :], writes=[a])
            for mc in range(2):
                for ll in range(32):
                    P.op("tensor", lambda e, a=a, mc=mc, ll=ll, si=si: e.matmul(
                        ph[mc][:, 0:255], lhsT=w1b[si][:, ll, mc * 128:(mc + 1) * 128], rhs=a[:, ll:ll + 4065:16],
                        start=(ll == 0), stop=(ll == 31)), reads=[w1b[si], a], writes=[ph[mc]], signal=(ll == 31))
                for ll in range(32):
                    P.op("tensor", lambda e, mc=mc, ll=ll, si=si: e.matmul(
                        pc[:, mc:mc + 1], lhsT=w1b[si][:, ll, mc * 128:(mc + 1) * 128], rhs=peT[si][:, ll:ll + 1],
                        start=(ll == 0), stop=(ll == 31)), reads=[w1b[si], peT[si]], writes=[pc], signal=(ll == 31))
            P.op("vector", lambda e: e.tensor_copy(out=cst[:], in_=pc[:, 0:2]), reads=[pc], writes=[cst])
            for mc in range(2):
                x_, u_, s_, g_ = xh[mc], uu[mc], sg[mc], gb[mc]
                P.op("vector", lambda e, x_=x_, mc=mc: e.tensor_scalar(out=x_[:, 0:255], in0=ph[mc][:, 0:255], scalar1=cst[:, mc:mc + 1],
                                                                      scalar2=None, op0=ALU.add), reads=[ph[mc], cst], writes=[x_])
                P.op("vector", lambda e, x_=x_, u_=u_: e.tensor_tensor(out=u_[:, 0:255], in0=x_[:, 0:255], in1=x_[:, 0:255], op=ALU.mult),
                     reads=[x_], writes=[u_])
                P.op("vector", lambda e, u_=u_: e.tensor_scalar(out=u_[:, 0:255], in0=u_[:, 0:255], scalar1=0.044715, scalar2=1.0,
                                                               op0=ALU.mult, op1=ALU.add), reads=[u_], writes=[u_])
                P.op("vector", lambda e, x_=x_, u_=u_: e.tensor_tensor(out=u_[:, 0:255], in0=u_[:, 0:255], in1=x_[:, 0:255], op=ALU.mult),
                     reads=[x_, u_], writes=[u_])
                P.op("scalar", lambda e, u_=u_, s_=s_: e.activation(out=s_[:, 0:255], in_=u_[:, 0:255], func=AF.Sigmoid, scale=1.5957691216057308),
                     reads=[u_], writes=[s_])
                P.op("vector", lambda e, x_=x_, s_=s_, g_=g_: e.tensor_tensor(out=g_[:, 0:255], in0=x_[:, 0:255], in1=s_[:, 0:255], op=ALU.mult),
                     reads=[x_, s_], writes=[g_])
            if src == "k":
                for mc in range(2):
                    P.op("tensor", lambda e, mc=mc, si=si: e.matmul(po[:, 0:255], lhsT=w2b[si][:, mc, :], rhs=gb[mc][:, 0:255],
                                                                   start=(mc == 0), stop=(mc == 1)),
                         reads=[w2b[si], gb[mc]], writes=[po], signal=(mc == 1))
                P.op("scalar", lambda e, oc=oc: e.copy(out=oc[:, 0:255], in_=po[:, 0:255]), reads=[po], writes=[oc])
                P.dma("sync", k.kcmpT[hk], oc[:], reads=[oc])
            else:
                for ct in range(2):
                    for mc in range(2):
                        P.op("tensor", lambda e, mc=mc, ct=ct, si=si: e.matmul(
                            po[:, ct * 128:(ct + 1) * 128], lhsT=gb[mc][:, ct * 128:(ct + 1) * 128], rhs=w2b[si][:, mc, :],
                            start=(mc == 0), stop=(mc == 1)), reads=[w2b[si], gb[mc]], writes=[po], signal=(mc == 1))
                P.op("scalar", lambda e, oc=oc: e.copy(out=oc[:], in_=po[:, 0:256]), reads=[po], writes=[oc])
                P.dma("sync", k.vcmp[hk].rearrange("(c p) d -> p c d", p=128), oc[:].rearrange("p (c d) -> p c d", c=2), reads=[oc])
    P.flush()
    P.release(m0)


def phase_nsa(k, l, hks=(0, 1)):
    P = k.P
    m0 = P.mark()
    cst_f = {}
    for nm, shp in (("cb", [128, 128]), ("wbm", [128, 128]), ("esel", [64, 32 * 128]), ("ovl", [128, 128]), ("sel", [12, 12 * 128]),
                    ("ident", [128, 128])):
        b = P.sb("c_" + nm, shp, BF16)
        P.dma("gpsimd", b[:], getattr(k, nm), writes=[b])
        cst_f[nm] = b
    cb, wbm, esel, ovl, sel, identb = (cst_f[n] for n in ("cb", "wbm", "esel", "ovl", "sel", "ident"))
    identf = P.sb("identf", [128, 128], F32)
    P.dma("sync", identf[:], k.ident, writes=[identf])
    mb = P.sb("mb", [128, 128], F32)
    P.dma("sync", mb[:], k.mb, writes=[mb])
    keep = P.sb("keep", [128, 32 * 64], F32)
    addc = P.sb("addc", [128, 32 * 64], F32)
    P.dma("sync", keep[:], k.keep, writes=[keep])
    P.dma("sync", addc[:], k.addc, writes=[addc])
    onesb = P.sb("onesb", [128, 128], BF16)
    P.op("vector", lambda e: e.memset(onesb[:], 1.0), writes=[onesb])

    ksT = P.sb("ksT", [128, T], BF16)
    kwT = P.sb("kwT", [128, T], BF16)
    vs = P.sb("vs", [128, 32, 128], BF16)
    vw = P.sb("vw", [128, 32, 128], BF16)
    kcm = P.sb("kcm", [128, 256], BF16)
    vcm = P.sb("vcm", [128, 2, 128], BF16)
    sgt = P.sb("sgt", [12, T], BF16)
    qnb = [P.sb("qnb%d" % i, [128, 4, 512], BF16) for i in range(2)]
    qrb = [P.sb("qrb%d" % i, [128, 4, 512], BF16) for i in range(2)]
    S = [P.ps("S%d" % i, [128, 512]) for i in range(2)]
    BD = [P.ps("BD%d" % i, [128, 512]) for i in range(2)]
    BO = [P.ps("BO%d" % i, [128, 512]) for i in range(2)]
    BG = P.ps("BG", [128, 512])
    BM = P.ps("BM", [128, 512])
    pT = [P.sb("pT%d" % i, [128, 512], BF16) for i in range(3)]
    pTc = [P.sb("pTc%d" % i, [128, 512], BF16) for i in range(2)]
    pn = [P.sb("pn%d" % i, [128, 512], BF16) for i in range(2)]
    bc = [P.sb("bc%d" % i, [128, 128], BF16) for i in range(2)]
    W = [P.sb("W%d" % i, [128, 512], F32) for i in range(2)]
    Wg = [P.sb("Wg%d" % i, [128, 512], F32) for i in range(2)]
    tmp = [P.sb("tmp%d" % i, [128, 512], F32) for i in range(2)]
    acc = [P.sb("acc%d" % i, [128, 512], F32) for i in range(2)]
    accb = [P.sb("accb%d" % i, [128, 512], BF16) for i in range(2)]
    impT = P.sb("impT", [64, 128], F32)
    imp = P.sb("imp", [128, 64], F32)
    imp2 = P.sb("imp2", [128, 64], F32)
    m8a = P.sb("m8a", [128, 8], F32)
    m8b = P.sb("m8b", [128, 8], F32)
    bias = P.sb("bias", [128, 64], BF16)
    biasT = P.sb("biasT", [64, 128], BF16)
    cnt = {"s": 0, "pt": 0, "bc": 0, "set": 0, "w": 0}

    def rhs4(buf, qi):
        return buf[:, :, qi * 128:(qi + 1) * 128]

    def as4(ap):
        return ap.rearrange("p (g t) -> p g t", g=4)

    def bcast4(ap, np_):
        return ap.unsqueeze(1).to_broadcast([np_, 4, 128])

    for hk in hks:
        P.dma("sync", ksT[:], k.ksT[hk * 128:(hk + 1) * 128, :], writes=[ksT])
        P.dma("sync", kwT[:], k.kwT[hk * 128:(hk + 1) * 128, :], writes=[kwT])
        for q4 in range(4):
            P.dma("sync", vs[:, q4 * 8:(q4 + 1) * 8, :],
                  k.vs[q4 * 1024:(q4 + 1) * 1024, hk * 128:(hk + 1) * 128].rearrange("(t p) d -> p t d", p=128), writes=[vs])
            P.dma("sync", vw[:, q4 * 8:(q4 + 1) * 8, :],
                  k.vw[q4 * 1024:(q4 + 1) * 1024, hk * 128:(hk + 1) * 128].rearrange("(t p) d -> p t d", p=128), writes=[vw])
        P.dma("sync", kcm[:], k.kcmpT[hk], writes=[kcm])
        P.dma("sync", vcm[:], k.vcmp[hk].rearrange("(c p) d -> p c d", p=128), writes=[vcm])
        P.dma("sync", sgt[:], k.ngT[hk * 12:(hk + 1) * 12, :], writes=[sgt])
        for qt in range(T // 128):
            qb, qi = qt // 4, qt % 4
            qn_, qr_ = qnb[qb % 2], qrb[qb % 2]
            if qi == 0:
                tok = slice(qb * 512, (qb + 1) * 512)
                P.dma("sync", qn_[:], k.qn[hk * 512:(hk + 1) * 512, tok].rearrange("(g d) t -> d g t", d=128), writes=[qn_])
                P.dma("sync", qr_[:], k.qr[hk * 512:(hk + 1) * 512, tok].rearrange("(g d) t -> d g t", d=128), writes=[qr_])
            tsl = slice(qt * 128, (qt + 1) * 128)

            def gates(j):
                for g in range(4):
                    r = 3 * g + j
                    P.op("tensor", lambda e, g=g, r=r: e.matmul(BG[:, g * 128:(g + 1) * 128], lhsT=sel[:, r * 128:(r + 1) * 128],
                                                                rhs=sgt[:, tsl], start=True, stop=True),
                         reads=[sel, sgt], writes=[BG], signal=(g == 3))

            def combine(st, first, w_):
                wg = Wg[cnt["w"] % 2]
                tp = tmp[cnt["w"] % 2]
                cnt["w"] += 1
                ac = acc[qt % 2]
                P.op("vector", lambda e, wg=wg, w_=w_: e.tensor_tensor(out=wg[:], in0=BG[:], in1=w_[:], op=ALU.mult),
                     reads=[BG, w_], writes=[wg])
                if first:
                    P.op("vector", lambda e, wg=wg, ac=ac, st=st: e.tensor_tensor(out=ac[:], in0=BO[st][:], in1=wg[:], op=ALU.mult),
                         reads=[BO[st], wg], writes=[ac])
                else:
                    P.op("vector", lambda e, wg=wg, tp=tp, st=st: e.tensor_tensor(out=tp[:], in0=BO[st][:], in1=wg[:], op=ALU.mult),
                         reads=[BO[st], wg], writes=[tp])
                    P.op("gpsimd", lambda e, tp=tp, ac=ac: e.tensor_tensor(out=ac[:], in0=ac[:], in1=tp[:], op=ALU.add),
                         reads=[ac, tp], writes=[ac])

            st = cnt["set"] % 2
            cnt["set"] += 1
            nct = 1 if qt <= 15 else 2
            for ct in range(nct):
                s_ = S[cnt["s"] % 2]
                cnt["s"] += 1
                need_mask = not (ct == 0 and qt >= 17)
                P.op("tensor", lambda e, s_=s_, ct=ct, qn_=qn_: e.matmul(as4(s_[:]), lhsT=kcm[:, ct * 128:(ct + 1) * 128], rhs=rhs4(qn_, qi),
                                                                        start=True, stop=not need_mask),
                     reads=[kcm, qn_], writes=[s_], signal=not need_mask)
                if need_mask:
                    b_ = bc[cnt["bc"] % 2]
                    cnt["bc"] += 1
                    thr = float(128 * qt - 2048 * ct - 31)
                    P.op("gpsimd", lambda e, b_=b_, thr=thr: e.tensor_scalar(out=b_[:], in0=mb[:], scalar1=thr, scalar2=NEG, op0=ALU.is_gt, op1=ALU.mult),
                         reads=[mb], writes=[b_])
                    P.op("tensor", lambda e, s_=s_, b_=b_: e.matmul(as4(s_[:]), lhsT=identb[:], rhs=bcast4(b_[:], 128), start=False, stop=True),
                         reads=[identb, b_], writes=[s_])
                p_ = pTc[ct]
                P.op("scalar", lambda e, s_=s_, p_=p_: e.activation(out=p_[:], in_=s_[:], func=AF.Exp, scale=SCALE), reads=[s_], writes=[p_])
                P.op("tensor", lambda e, p_=p_, st=st, ct=ct: e.matmul(BD[st][:], lhsT=onesb[:], rhs=p_[:], start=(ct == 0), stop=(ct == nct - 1)),
                     reads=[onesb, p_], writes=[BD[st]], signal=(ct == nct - 1))
                P.op("tensor", lambda e, p_=p_, st=st, ct=ct: e.matmul(BO[st][:], lhsT=vcm[:, ct, :], rhs=p_[:], start=(ct == 0), stop=(ct == nct - 1)),
                     reads=[vcm, p_], writes=[BO[st]], signal=(ct == nct - 1))
            w_ = W[cnt["w"] % 2]
            P.op("vector", lambda e, w_=w_, st=st: e.tensor_scalar(out=w_[:], in0=BD[st][:], scalar1=1e-30, scalar2=None, op0=ALU.add),
                 reads=[BD[st]], writes=[w_])
            P.op("vector", lambda e, w_=w_: e.reciprocal(out=w_[:], in_=w_[:]), reads=[w_], writes=[w_])
            if qt >= 8:
                for ct in range(nct):
                    P.op("gpsimd", lambda e, ct=ct, w_=w_: e.tensor_tensor(out=pn[ct][:], in0=pTc[ct][:], in1=w_[:], op=ALU.mult),
                         reads=[pTc[ct], w_], writes=[pn[ct]])
                n_mm = nct * 4
                i_mm = 0
                for ct in range(nct):
                    for g in range(4):
                        P.op("tensor", lambda e, ct=ct, g=g, i_mm=i_mm: e.matmul(BM[0:64, 0:128], lhsT=ovl[:, ct * 64:(ct + 1) * 64],
                                                                                rhs=pn[ct][:, g * 128:(g + 1) * 128],
                                                                                start=(i_mm == 0), stop=(i_mm == n_mm - 1)),
                             reads=[ovl, pn[ct]], writes=[BM], signal=(i_mm == n_mm - 1))
                        i_mm += 1
                P.op("vector", lambda e: e.tensor_copy(out=impT[:], in_=BM[0:64, 0:128]), reads=[BM], writes=[impT])
                P.op("tensor", lambda e: e.transpose(out=BM[:, 128:192], in_=impT[:], identity=identf[0:64, 0:64]),
                     reads=[impT, identf], writes=[BM])
                P.op("vector", lambda e: e.tensor_tensor(out=imp[:], in0=BM[:, 128:192], in1=keep[:, qt * 64:(qt + 1) * 64], op=ALU.mult),
                     reads=[BM, keep], writes=[imp])
                P.op("vector", lambda e: e.tensor_tensor(out=imp[:], in0=imp[:], in1=addc[:, qt * 64:(qt + 1) * 64], op=ALU.add),
                     reads=[imp, addc], writes=[imp])
                P.op("vector", lambda e: e.max(out=m8a[:], in_=imp[:]), reads=[imp], writes=[m8a])
                P.op("vector", lambda e: e.match_replace(out=imp2[:], in_to_replace=m8a[:], in_values=imp[:], imm_value=-3.0e38),
                     reads=[imp, m8a], writes=[imp2])
                P.op("vector", lambda e: e.max(out=m8b[:], in_=imp2[:]), reads=[imp2], writes=[m8b])
                P.op("vector", lambda e: e.tensor_scalar(out=bias[:], in0=imp[:], scalar1=m8b[:, 7:8], scalar2=NEG, op0=ALU.is_lt, op1=ALU.mult),
                     reads=[imp, m8b], writes=[bias])
                P.op("tensor", lambda e: e.transpose(out=BM[0:64, 256:320].bitcast(BF16), in_=bias[:], identity=identb[:]),
                     reads=[bias, identb], writes=[BM])
                P.op("vector", lambda e: e.tensor_copy(out=biasT[:], in_=BM[0:64, 256:320].bitcast(BF16)), reads=[BM], writes=[biasT])
            gates(0)
            combine(st, True, w_)

            def branch(kT_, v_, kts, kind):
                st = cnt["set"] % 2
                cnt["set"] += 1
                nk = len(kts)
                for ii, kt in enumerate(kts):
                    s_ = S[cnt["s"] % 2]
                    cnt["s"] += 1
                    extra = []
                    if kind == "slc":
                        if qt >= 8:
                            extra.append(("sel", kt))
                        if kt == qt:
                            extra.append(("cb", None))
                    else:
                        if kt == qt:
                            extra.append(("cb", None))
                        if kt == qt - 4:
                            extra.append(("wb", None))
                    P.op("tensor", lambda e, s_=s_, kt=kt, kT_=kT_: e.matmul(as4(s_[:]), lhsT=kT_[:, kt * 128:(kt + 1) * 128], rhs=rhs4(qr_, qi),
                                                                            start=True, stop=(len(extra) == 0)),
                         reads=[kT_, qr_], writes=[s_], signal=(len(extra) == 0))
                    for xi, (xk, xa) in enumerate(extra):
                        last = (xi == len(extra) - 1)
                        if xk == "sel":
                            P.op("tensor", lambda e, s_=s_, xa=xa, last=last: e.matmul(as4(s_[:]), lhsT=esel[:, xa * 128:(xa + 1) * 128],
                                                                                      rhs=bcast4(biasT[:], 64), start=False, stop=last),
                                 reads=[esel, biasT], writes=[s_], signal=last)
                        else:
                            mk = cb if xk == "cb" else wbm
                            P.op("tensor", lambda e, s_=s_, mk=mk, last=last: e.matmul(as4(s_[:]), lhsT=identb[:], rhs=bcast4(mk[:], 128),
                                                                                      start=False, stop=last),
                                 reads=[identb, mk], writes=[s_], signal=last)
                    p_ = pT[cnt["pt"] % 3]
                    cnt["pt"] += 1
                    P.op("scalar", lambda e, s_=s_, p_=p_: e.activation(out=p_[:], in_=s_[:], func=AF.Exp, scale=SCALE), reads=[s_], writes=[p_])
                    P.op("tensor", lambda e, p_=p_, st=st, ii=ii: e.matmul(BD[st][:], lhsT=onesb[:], rhs=p_[:], start=(ii == 0), stop=(ii == nk - 1)),
                         reads=[onesb, p_], writes=[BD[st]], signal=(ii == nk - 1))
                    P.op("tensor", lambda e, p_=p_, st=st, ii=ii, kt=kt, v_=v_: e.matmul(BO[st][:], lhsT=v_[:, kt, :], rhs=p_[:], start=(ii == 0), stop=(ii == nk - 1)),
                         reads=[v_, p_], writes=[BO[st]], signal=(ii == nk - 1))
                w2 = W[cnt["w"] % 2]
                P.op("vector", lambda e, w2=w2, st=st: e.reciprocal(out=w2[:], in_=BD[st][:]), reads=[BD[st]], writes=[w2])
                return st, w2

            st, w2 = branch(ksT, vs, list(range(0, qt + 1)), "slc")
            gates(1)
            combine(st, False, w2)
            st, w2 = branch(kwT, vw, list(range(max(0, qt - 4), qt + 1)), "win")
            gates(2)
            combine(st, False, w2)
            ac = acc[qt % 2]
            ab = accb[qt % 2]
            P.op("gpsimd", lambda e, ac=ac, ab=ab: e.tensor_copy(out=ab[:], in_=ac[:]), reads=[ac], writes=[ab])
            P.dma("sync", k.mixT[hk * 512:(hk + 1) * 512, tsl].rearrange("(g d) t -> d g t", d=128), as4(ab[:]), reads=[ab])
    P.flush()
    P.release(m0)


def phase_gla(k, l):
    P = k.P
    m0 = P.mark()
    GS = 128 ** -0.5
    wa2 = P.sb("wa2", [16, 512], BF16)
    brow = P.sb("brow", [1, 512], BF16)
    ones1 = P.sb("ones1", [1, 128], BF16)
    tri2f = P.sb("tri2f", [128, 128], F32)
    suf = P.sb("suf", [128, 128], F32)
    onesb = P.sb("onesb", [128, 128], BF16)
    nw = P.sb("nw", [128, 2], F32)
    P.dma("gpsimd", wa2[:], k.gla_w_a2[l], writes=[wa2])
    P.dma("gpsimd", brow[:], k.gla_b_a[l:l + 1, :], writes=[brow])
    P.dma("sync", tri2f[:], k.tri2, writes=[tri2f])
    P.dma("sync", suf[:], k.sumat, writes=[suf])
    P.dma("sync", nw[:], k.gla_nwT[l], writes=[nw])
    P.op("vector", lambda e: e.memset(ones1[:], 1.0), writes=[ones1])
    P.op("vector", lambda e: e.memset(onesb[:], 1.0), writes=[onesb])
    Sf = P.sb("Sf", [128, 4, 256], F32)
    Sb = P.sb("Sb", [128, 4, 256], BF16)
    Sfv = [P.view(Sf) for _ in range(4)]
    Sbv = [P.view(Sb) for _ in range(4)]
    for h in range(4):
        P.op("vector", lambda e, h=h: e.memset(Sf[:, h, :], 0.0), writes=[Sfv[h]])
        P.op("vector", lambda e, h=h: e.memset(Sb[:, h, :], 0.0), writes=[Sbv[h]])
    gqTb = [P.sb("gqTb%d" % i, [128, 4, 512], BF16) for i in range(2)]
    gkTb = [P.sb("gkTb%d" % i, [128, 4, 512], BF16) for i in range(2)]
    ggb = [P.sb("ggb%d" % i, [128, 8, 512], BF16) for i in range(2)]
    gaTb = [P.sb("gaTb%d" % i, [16, 512], BF16) for i in range(2)]
    gkt = [P.sb("gkt%d" % i, [128, 512], BF16) for i in range(2)]
    gvt = [P.sb("gvt%d" % i, [128, 1024], BF16) for i in range(2)]
    Lt = [P.sb("Lt%d" % i, [128, 512], F32) for i in range(2)]
    E1 = [P.sb("E1%d" % i, [128, 512], F32) for i in range(2)]
    kst = [P.sb("kst%d" % i, [128, 512], BF16) for i in range(2)]
    EbT = [P.sb("EbT%d" % i, [128, 128], F32) for i in range(2)]
    EnbT = [P.sb("EnbT%d" % i, [128, 128], F32) for i in range(2)]
    qdT = [P.sb("qdT%d" % i, [128, 128], BF16) for i in range(2)]
    kiT = [P.sb("kiT%d" % i, [128, 128], BF16) for i in range(2)]
    ATm = [P.sb("ATm%d" % i, [128, 128], BF16) for i in range(2)]
    o1 = [P.sb("o1%d" % i, [128, 256], F32) for i in range(2)]
    sq = [P.sb("sq%d" % i, [128, 256], BF16) for i in range(2)]
    lnr = [P.sb("lnr%d" % i, [128, 128], F32) for i in range(2)]
    rstd = [P.sb("rstd%d" % i, [128, 128], F32) for i in range(2)]
    tmpo = [P.sb("tmpo%d" % i, [128, 256], F32) for i in range(2)]
    outb = [P.sb("outb%d" % i, [128, 2, 128], BF16) for i in range(2)]
    pz = P.ps("pz", [128, 512])
    pcs = P.ps("pcs", [128, 512])
    psu = P.ps("psu", [128, 512])
    pcsT = P.ps("pcsT", [128, 512])
    pAT = P.ps("pAT", [128, 512])
    po = P.ps("po", [128, 512])
    pS = P.ps("pS", [128, 512])
    pss = P.ps("pss", [128, 512])
    hc = 0
    for tt in range(T // 128):
        tb, ti = tt // 4, tt % 4
        gq_, gk_, gg_, ga_ = gqTb[tb % 2], gkTb[tb % 2], ggb[tb % 2], gaTb[tb % 2]
        if ti == 0:
            tok = slice(tb * 512, (tb + 1) * 512)
            P.dma("sync", gq_[:], k.gqT[:, tok].rearrange("(h d) t -> d h t", d=128), writes=[gq_])
            P.dma("sync", gk_[:], k.gkT[:, tok].rearrange("(h d) t -> d h t", d=128), writes=[gk_])
            P.dma("sync", gg_[:], k.ggT[:, tok].rearrange("(c d) t -> d c t", d=128), writes=[gg_])
            P.dma("sync", ga_[:], k.gaT[:, tok], writes=[ga_])
        lsl = slice(ti * 128, (ti + 1) * 128)
        tsl = slice(tt * 128, (tt + 1) * 128)
        gkt_, gvt_ = gkt[tt % 2], gvt[tt % 2]
        P.dma("sync", gkt_[:], k.gk[tsl, :], writes=[gkt_])
        P.dma("sync", gvt_[:], k.gv[tsl, :], writes=[gvt_])
        L_, E1_, kst_ = Lt[tt % 2], E1[tt % 2], kst[tt % 2]
        P.op("tensor", lambda e: e.matmul(pz[:], lhsT=ga_[:, lsl], rhs=wa2[:], start=True, stop=False), reads=[ga_, wa2], writes=[pz], signal=False)
        P.op("tensor", lambda e: e.matmul(pz[:], lhsT=ones1[:], rhs=brow[:], start=False, stop=True), reads=[ones1, brow], writes=[pz])
        P.op("scalar", lambda e: e.activation(out=L_[:], in_=pz[:], func=AF.Exp, scale=-1.0), reads=[pz], writes=[L_])
        P.op("scalar", lambda e: e.activation(out=L_[:], in_=L_[:], func=AF.Ln, bias=1.0), reads=[L_], writes=[L_])
        P.op("tensor", lambda e: e.matmul(pcs[:], lhsT=tri2f[:], rhs=L_[:], start=True, stop=True), reads=[tri2f, L_], writes=[pcs])
        P.op("tensor", lambda e: e.matmul(psu[:], lhsT=suf[:], rhs=L_[:], start=True, stop=True), reads=[suf, L_], writes=[psu])
        P.op("scalar", lambda e: e.activation(out=E1_[:], in_=psu[:], func=AF.Exp, scale=-1.0 / 16.0), reads=[psu], writes=[E1_])
        P.op("vector", lambda e: e.tensor_tensor(out=kst_[:], in0=gkt_[:], in1=E1_[:], op=ALU.mult), reads=[gkt_, E1_], writes=[kst_])
        for h in range(4):
            i2 = hc % 2
            hc += 1
            Eb_, Enb_, qd_, ki_, AT_ = EbT[i2], EnbT[i2], qdT[i2], kiT[i2], ATm[i2]
            o1_, sq_, lnr_, rs_, tp_, ob_ = o1[i2], sq[i2], lnr[i2], rstd[i2], tmpo[i2], outb[i2]
            hs = slice(h * 128, (h + 1) * 128)
            P.op("tensor", lambda e: e.matmul(pcsT[:, 0:128], lhsT=L_[:, hs], rhs=tri2f[:], start=True, stop=True),
                 reads=[L_, tri2f], writes=[pcsT])
            P.op("scalar", lambda e: e.activation(out=Eb_[:], in_=pcsT[:, 0:128], func=AF.Exp, scale=-1.0 / 16.0), reads=[pcsT], writes=[Eb_])
            P.op("scalar", lambda e: e.activation(out=Enb_[:], in_=pcsT[:, 0:128], func=AF.Exp, scale=1.0 / 16.0), reads=[pcsT], writes=[Enb_])
            P.op("vector", lambda e: e.scalar_tensor_tensor(out=qd_[:], in0=gq_[:, h, lsl], scalar=GS, in1=Eb_[:], op0=ALU.mult, op1=ALU.mult),
                 reads=[gq_, Eb_], writes=[qd_])
            P.op("gpsimd", lambda e: e.tensor_tensor(out=ki_[:], in0=gk_[:, h, lsl], in1=Enb_[:], op=ALU.mult), reads=[gk_, Enb_], writes=[ki_])
            P.op("tensor", lambda e: e.matmul(pAT[:, 0:128], lhsT=ki_[:], rhs=qd_[:], start=True, stop=True), reads=[ki_, qd_], writes=[pAT])
            P.op("vector", lambda e: e.tensor_tensor(out=AT_[:], in0=pAT[:, 0:128], in1=tri2f[:], op=ALU.mult), reads=[pAT, tri2f], writes=[AT_])
            for hf in range(2):
                cs_ = slice(hf * 64, (hf + 1) * 64)
                for dvc in range(2):
                    oc = slice(dvc * 128 + hf * 64, dvc * 128 + hf * 64 + 64)
                    P.op("tensor", lambda e: e.matmul(po[:, oc], lhsT=gvt_[:, h * 256 + dvc * 128:h * 256 + dvc * 128 + 128], rhs=AT_[:, cs_],
                                                      start=True, stop=False), reads=[gvt_, AT_], writes=[po], signal=False)
                    P.op("tensor", lambda e: e.matmul(po[:, oc], lhsT=Sb[:, h, dvc * 128:(dvc + 1) * 128], rhs=qd_[:, cs_],
                                                      start=False, stop=True), reads=[Sbv[h], qd_], writes=[po],
                         signal=(hf == 1 and dvc == 1))
                P.op("tensor", lambda e: e.matmul(pS[:, 0:256], lhsT=kst_[cs_, hs], rhs=gvt_[cs_, h * 256:(h + 1) * 256], start=True, stop=True),
                     reads=[kst_, gvt_], writes=[pS])
                col = hf * 64 + 63
                P.op("vector", lambda e: e.scalar_tensor_tensor(out=Sf[:, h, :], in0=Sf[:, h, :], scalar=Eb_[:, col:col + 1], in1=pS[:, 0:256],
                                                                op0=ALU.mult, op1=ALU.add), reads=[Sfv[h], Eb_, pS], writes=[Sfv[h]])
                P.op("gpsimd", lambda e: e.tensor_copy(out=Sb[:, h, :], in_=Sf[:, h, :]), reads=[Sfv[h]], writes=[Sbv[h]])
            P.op("scalar", lambda e: e.copy(out=o1_[:], in_=po[:, 0:256]), reads=[po], writes=[o1_])
            P.op("gpsimd", lambda e: e.tensor_tensor(out=sq_[:], in0=o1_[:], in1=o1_[:], op=ALU.mult), reads=[o1_], writes=[sq_])
            P.op("tensor", lambda e: e.matmul(pss[:, 0:128], lhsT=onesb[:], rhs=sq_[:, 0:128], start=True, stop=False), reads=[onesb, sq_], writes=[pss], signal=False)
            P.op("tensor", lambda e: e.matmul(pss[:, 0:128], lhsT=onesb[:], rhs=sq_[:, 128:256], start=False, stop=True), reads=[onesb, sq_], writes=[pss])
            P.op("scalar", lambda e: e.activation(out=lnr_[:], in_=pss[:, 0:128], func=AF.Ln, scale=1.0 / 256.0, bias=NORM_EPS), reads=[pss], writes=[lnr_])
            P.op("scalar", lambda e: e.activation(out=rs_[:], in_=lnr_[:], func=AF.Exp, scale=-0.5), reads=[lnr_], writes=[rs_])
            for dvc in range(2):
                ds_ = slice(dvc * 128, (dvc + 1) * 128)
                P.op("vector", lambda e: e.tensor_tensor(out=tp_[:, ds_], in0=o1_[:, ds_], in1=rs_[:], op=ALU.mult), reads=[o1_, rs_], writes=[tp_])
                P.op("vector", lambda e: e.scalar_tensor_tensor(out=ob_[:, dvc, :], in0=gg_[:, h * 2 + dvc, lsl], scalar=nw[:, dvc:dvc + 1], in1=tp_[:, ds_],
                                                                op0=ALU.mult, op1=ALU.mult), reads=[gg_, nw, tp_], writes=[ob_])
            P.dma("sync", k.mixT[1024 + h * 256:1024 + (h + 1) * 256, tsl].rearrange("(c d) t -> d c t", d=128), ob_[:], reads=[ob_])
    P.flush()
    P.release(m0)


def ln_tile(P, r, lng, lnb, stats, mv, rstd):
    for c in range(4):
        P.op("vector", lambda e, c=c: e.bn_stats(out=stats[:, c, :], in_=r[:, c * 512:(c + 1) * 512]), reads=[r], writes=[stats])
    P.op("vector", lambda e: e.bn_aggr(out=mv[:], in_=stats[:]), reads=[stats], writes=[mv])
    P.op("scalar", lambda e: e.activation(out=rstd[:], in_=mv[:, 1:2], func=AF.Sqrt, bias=LN_EPS), reads=[mv], writes=[rstd])
    P.op("vector", lambda e: e.reciprocal(out=rstd[:], in_=rstd[:]), reads=[rstd], writes=[rstd])
    P.op("vector", lambda e: e.tensor_scalar(out=r[:], in0=r[:], scalar1=mv[:, 0:1], scalar2=rstd[:, 0:1], op0=ALU.subtract, op1=ALU.mult),
         reads=[r, mv, rstd], writes=[r])
    P.op("gpsimd", lambda e: e.tensor_tensor(out=r[:], in0=r[:], in1=lng[:], op=ALU.mult), reads=[r, lng], writes=[r])
    P.op("gpsimd", lambda e: e.tensor_tensor(out=r[:], in0=r[:], in1=lnb[:], op=ALU.add), reads=[r, lnb], writes=[r])


def phase_wout(k, l, xsrc, xdst):
    P = k.P
    m0 = P.mark()
    wo = P.sb("wo", [128, KC, D], BF16)
    wv = k.w_out[l].rearrange("(c p) n -> p c n", p=128)
    for q in range(4):
        P.dma("gpsimd", wo[:, :, q * 512:(q + 1) * 512], wv[:, :, q * 512:(q + 1) * 512], writes=[wo])
    garep = P.sb("garep", [128, D], F32)
    lng = P.sb("lng", [128, D], F32)
    lnb = P.sb("lnb", [128, D], F32)
    P.dma("sync", garep[:], k.modv[l, 2 * D:3 * D].partition_broadcast(128), writes=[garep])
    P.dma("sync", lng[:], k.ln_mix_g[l].partition_broadcast(128), writes=[lng])
    P.dma("sync", lnb[:], k.ln_mix_b[l].partition_broadcast(128), writes=[lnb])
    mixb = [P.sb("mixb%d" % i, [128, KC, 512], BF16) for i in range(2)]
    xt = [P.sb("xt%d" % i, [128, D], F32) for i in range(2)]
    rt = [P.sb("rt%d" % i, [128, D], F32) for i in range(2)]
    stats = P.sb("stats", [128, 4, 6], F32)
    mv = P.sb("mv", [128, 2], F32)
    rstd = P.sb("rstd", [128, 1], F32)
    py = [P.ps("py%d" % i, [128, 512]) for i in range(8)]
    mixv = k.mixT.rearrange("(c p) t -> p c t", p=128)
    for tt in range(T // 128):
        tb, ti = tt // 4, tt % 4
        mb_ = mixb[tb % 2]
        if ti == 0:
            for q in range(4):
                P.dma("sync", mb_[:, q * 4:(q + 1) * 4, :], mixv[:, q * 4:(q + 1) * 4, tb * 512:(tb + 1) * 512], writes=[mb_])
        x_ = xt[tt % 2]
        r_ = rt[tt % 2]
        tsl = slice(tt * 128, (tt + 1) * 128)
        P.dma("sync", x_[:], xsrc[tsl, :], writes=[x_])
        for db in range(4):
            p_ = py[(tt % 2) * 4 + db]
            for c in range(KC):
                P.op("tensor", lambda e: e.matmul(p_[:], lhsT=mb_[:, c, ti * 128:(ti + 1) * 128], rhs=wo[:, c, db * 512:(db + 1) * 512],
                                                  start=(c == 0), stop=(c == KC - 1)), reads=[mb_, wo], writes=[p_], signal=(c == KC - 1))
            P.op("vector", lambda e: e.tensor_tensor(out=r_[:, db * 512:(db + 1) * 512], in0=p_[:], in1=garep[:, db * 512:(db + 1) * 512], op=ALU.mult),
                 reads=[p_, garep], writes=[r_])
        P.op("vector", lambda e: e.scalar_tensor_tensor(out=r_[:], in0=x_[:], scalar=ALPHA, in1=r_[:], op0=ALU.mult, op1=ALU.add),
             reads=[x_, r_], writes=[r_])
        ln_tile(P, r_, lng, lnb, stats, mv, rstd)
        P.dma("sync", xdst[tsl, :], r_[:], reads=[r_])
    P.flush()
    P.release(m0)


def ffn_gateup(k, src, nrows, wg, wu, dff, AT, mod=None, src_bf16=False):
    P = k.P
    RH = min(nrows, 2048)
    for r0 in range(0, nrows, RH):
        nr = min(RH, nrows - r0)
        m0 = P.mark()
        identf = P.sb("identf", [128, 128], F32)
        P.dma("sync", identf[:], k.ident, writes=[identf])
        hT = P.sb("hT", [128, KC, RH], BF16)
        hviews = [(P.view(hT), P.view(hT)) for _ in range(nr // 128)]
        ptr = [P.ps("ptr%d" % i, [128, 4, 128]) for i in range(2)]
        if mod is not None:
            l = mod
            screp = P.sb("screp", [128, D], F32)
            shrep = P.sb("shrep", [128, D], F32)
            xt = [P.sb("xt%d" % i, [128, D], F32) for i in range(2)]
            P.dma("sync", screp[:], k.modv[l, 4 * D:5 * D].partition_broadcast(128), writes=[screp])
            P.dma("sync", shrep[:], k.modv[l, 3 * D:4 * D].partition_broadcast(128), writes=[shrep])
            build_hT(k, src, r0, nr // 128, screp, shrep, identf, hT, hviews, xt, ptr)
        else:
            identb = P.sb("identb", [128, 128], BF16)
            P.dma("gpsimd", identb[:], k.ident, writes=[identb])
            xt = [P.sb("xtb%d" % i, [128, D], BF16) for i in range(2)]
            for j in range(nr // 128):
                xb = xt[j % 2]
                P.dma("sync", xb[:], src[r0 + j * 128:r0 + (j + 1) * 128, :], writes=[xb])
                for g in range(KC // 4):
                    pt = ptr[(j * 4 + g) % 2]
                    ptb = pt[:].rearrange("p q t -> p (q t)")[:, 0:256].bitcast(BF16).rearrange("p (q t) -> p q t", q=4)
                    for q in range(4):
                        c = g * 4 + q
                        P.op("tensor", lambda e: e.transpose(out=ptb[:, q, :], in_=xb[:, c * 128:(c + 1) * 128], identity=identb[:]),
                             reads=[xb, identb], writes=[pt], signal=(q == 3))
                    if g % 2 == 0:
                        P.op("scalar", lambda e: e.copy(out=hT[:, g * 4:(g + 1) * 4, j * 128:(j + 1) * 128], in_=ptb), reads=[pt], writes=[hviews[j][0]])
                    else:
                        P.op("vector", lambda e: e.tensor_copy(out=hT[:, g * 4:(g + 1) * 4, j * 128:(j + 1) * 128], in_=ptb), reads=[pt], writes=[hviews[j][1]])
        wgb = [P.sb("wgb%d" % i, [128, KC, 256], BF16) for i in range(2)]
        wub = [P.sb("wub%d" % i, [128, KC, 256], BF16) for i in range(2)]
        pg = [P.ps("pg%d" % i, [128, 512]) for i in range(2)]
        pu = [P.ps("pu%d" % i, [128, 512]) for i in range(2)]
        sgb = [P.sb("sgb%d" % i, [128, 512], BF16) for i in range(2)]
        ab = [P.sb("ab%d" % i, [128, 512], BF16) for i in range(3)]
        wgv = wg.rearrange("(c p) n -> p c n", p=128)
        wuv = wu.rearrange("(c p) n -> p c n", p=128)
        blocks = [(b0, min(512, nr - b0)) for b0 in range(0, nr, 512)]
        it = 0
        for fb in range(dff // 256):
            wg_, wu_ = wgb[fb % 2], wub[fb % 2]
            P.dma("gpsimd", wg_[:], wgv[:, :, fb * 256:(fb + 1) * 256], writes=[wg_])
            P.dma("gpsimd", wu_[:], wuv[:, :, fb * 256:(fb + 1) * 256], writes=[wu_])
            for ft in range(2):
                f0 = fb * 256 + ft * 128
                for (b0, bn) in blocks:
                    hr = []
                    for j in range(b0 // 128, (b0 + bn) // 128):
                        hr += [hviews[j][0], hviews[j][1]]
                    pg_, pu_ = pg[it % 2], pu[it % 2]
                    sg_, a_ = sgb[it % 2], ab[it % 3]
                    it += 1
                    for c in range(KC):
                        P.op("tensor", lambda e: e.matmul(pg_[:, 0:bn], lhsT=wg_[:, c, ft * 128:(ft + 1) * 128], rhs=hT[:, c, b0:b0 + bn],
                                                          start=(c == 0), stop=(c == KC - 1)), reads=[wg_] + hr, writes=[pg_], signal=(c == KC - 1))
                    for c in range(KC):
                        P.op("tensor", lambda e: e.matmul(pu_[:, 0:bn], lhsT=wu_[:, c, ft * 128:(ft + 1) * 128], rhs=hT[:, c, b0:b0 + bn],
                                                          start=(c == 0), stop=(c == KC - 1)), reads=[wu_] + hr, writes=[pu_], signal=(c == KC - 1))
                    P.op("scalar", lambda e: e.activation(out=sg_[:, 0:bn], in_=pg_[:, 0:bn], func=AF.Silu), reads=[pg_], writes=[sg_])
                    P.op("vector", lambda e: e.tensor_tensor(out=a_[:, 0:bn], in0=pu_[:, 0:bn], in1=sg_[:, 0:bn], op=ALU.mult), reads=[pu_, sg_], writes=[a_])
                    P.dma("sync", AT[f0:f0 + 128, r0 + b0:r0 + b0 + bn], a_[:, 0:bn], reads=[a_])
        P.flush()
        P.release(m0)


class _PV:
    def __init__(self, b):
        self.b = b

    def __getitem__(self, idx):
        return self.b.t[:].rearrange("p (q t) -> p q t", q=4)[idx]


def ffn_down(k, AT, nrows, wd, dff, Y, row_off=0):
    P = k.P
    m0 = P.mark()
    FC = dff // 128
    G = 4
    assert FC % G == 0
    wdb = [P.sb("wdb%d" % i, [128, FC, 512], BF16) for i in range(1)]
    atb = [P.sb("atb%d" % i, [128, FC, 256], BF16) for i in range(2)]
    yb = [P.sb("yb%d" % i, [128, 512], F32) for i in range(3)]
    py = [P.ps("pyd%d" % i, [128, 512]) for i in range(3)]
    wdv = wd.rearrange("(c p) n -> p c n", p=128)
    atv = AT.rearrange("(c p) t -> p c t", p=128)
    it = 0
    ib = 0
    for db in range(4):
        w_ = wdb[0]
        for q in range(G):
            cs = slice(q * (FC // G), (q + 1) * (FC // G))
            P.dma("gpsimd", w_[:, cs, :], wdv[:, cs, db * 512:(db + 1) * 512], writes=[w_])
        for b0 in range(0, nrows, 256):
            bn = min(256, nrows - b0)
            a_ = atb[ib % 2]
            ib += 1
            for q in range(G):
                cs = slice(q * (FC // G), (q + 1) * (FC // G))
                P.dma("sync", a_[:, cs, 0:bn], atv[:, cs, b0:b0 + bn], writes=[a_])
            for j in range(bn // 128):
                p_ = py[it % 3]
                y_ = yb[it % 3]
                it += 1
                for c in range(FC):
                    P.op("tensor", lambda e: e.matmul(p_[:], lhsT=a_[:, c, j * 128:(j + 1) * 128], rhs=w_[:, c, :], start=(c == 0), stop=(c == FC - 1)),
                         reads=[a_, w_], writes=[p_], signal=(c == FC - 1))
                P.op("scalar", lambda e: e.copy(out=y_[:], in_=p_[:]), reads=[p_], writes=[y_])
                rs = slice(row_off + b0 + j * 128, row_off + b0 + (j + 1) * 128)
                P.dma("sync", Y[rs, db * 512:(db + 1) * 512], y_[:], reads=[y_])
    P.flush()
    P.release(m0)


def phase_ln2(k, l, xsrc, ysrc, xdst):
    P = k.P
    m0 = P.mark()
    gfrep = P.sb("gfrep", [128, D], F32)
    lng = P.sb("lng", [128, D], F32)
    lnb = P.sb("lnb", [128, D], F32)
    P.dma("sync", gfrep[:], k.modv[l, 5 * D:6 * D].partition_broadcast(128), writes=[gfrep])
    P.dma("sync", lng[:], k.ln_ffn_g[l].partition_broadcast(128), writes=[lng])
    P.dma("sync", lnb[:], k.ln_ffn_b[l].partition_broadcast(128), writes=[lnb])
    xt = [P.sb("xt%d" % i, [128, D], F32) for i in range(2)]
    rt = [P.sb("rt%d" % i, [128, D], F32) for i in range(2)]
    stats = P.sb("stats", [128, 4, 6], F32)
    mv = P.sb("mv", [128, 2], F32)
    rstd = P.sb("rstd", [128, 1], F32)
    for tt in range(T // 128):
        x_, r_ = xt[tt % 2], rt[tt % 2]
        tsl = slice(tt * 128, (tt + 1) * 128)
        P.dma("sync", x_[:], xsrc[tsl, :], writes=[x_])
        P.dma("gpsimd", r_[:], ysrc[tsl, :], writes=[r_])
        P.op("vector", lambda e: e.tensor_tensor(out=r_[:], in0=r_[:], in1=gfrep[:], op=ALU.mult), reads=[r_, gfrep], writes=[r_])
        P.op("vector", lambda e: e.scalar_tensor_tensor(out=r_[:], in0=x_[:], scalar=ALPHA, in1=r_[:], op0=ALU.mult, op1=ALU.add),
             reads=[x_, r_], writes=[r_])
        ln_tile(P, r_, lng, lnb, stats, mv, rstd)
        P.dma("sync", xdst[tsl, :], r_[:], reads=[r_])
    P.flush()
    P.release(m0)


CAP = 1280
NSLOT = NE * CAP
BIGIDX = 1.0e6


def phase_moe_route(k, l):
    P = k.P
    m0 = P.mark()
    zt = P.sb("zt", [128, D], BF16)
    P.op("vector", lambda e: e.memset(zt[:], 0.0), writes=[zt])
    for s0 in range(0, NSLOT, 128):
        P.dma("sync" if (s0 // 128) % 2 == 0 else "gpsimd", k.Xs[s0:s0 + 128, :], zt[:], reads=[zt])
    P.flush()
    P.release(m0)

    m0 = P.mark()
    screp = P.sb("screp", [128, D], F32)
    shrep = P.sb("shrep", [128, D], F32)
    identf = P.sb("identf", [128, 128], F32)
    wr = P.sb("wr", [128, KC, NE], F32)
    SLb = P.sb("SLb", [128, 128], BF16)
    onesb = P.sb("onesb", [128, 128], BF16)
    eoff = P.sb("eoff", [128, NE], F32)
    base = P.sb("base", [128, NE], F32)
    P.dma("sync", screp[:], k.modv[l, 4 * D:5 * D].partition_broadcast(128), writes=[screp])
    P.dma("sync", shrep[:], k.modv[l, 3 * D:4 * D].partition_broadcast(128), writes=[shrep])
    P.dma("sync", identf[:], k.ident, writes=[identf])
    P.dma("sync", wr[:], k.moe_router[l // 2].rearrange("(c p) e -> p c e", p=128), writes=[wr])
    P.dma("gpsimd", SLb[:], k.slmat, writes=[SLb])
    P.dma("sync", eoff[:], k.eoff, writes=[eoff])
    P.op("vector", lambda e: e.memset(onesb[:], 1.0), writes=[onesb])
    P.op("vector", lambda e: e.memset(base[:], 0.0), writes=[base])
    xt = [P.sb("xt%d" % i, [128, D], F32) for i in range(2)]
    hb = [P.sb("hb%d" % i, [128, D], BF16) for i in range(2)]
    hTf = [P.sb("hTf%d" % i, [128, KC, 128], F32) for i in range(2)]
    ptr = [P.ps("ptr%d" % i, [128, 4, 128]) for i in range(2)]
    plog = P.ps("plog", [128, 512])
    pcum = P.ps("pcum", [128, 512])
    sm = {}
    for nm in ("lg", "m8", "sel", "sel1", "sel2", "ex", "exs", "comb", "tmp8", "pos", "dest", "valid", "selb"):
        sm[nm] = [P.sb(nm + "%d" % i, [128, NE], BF16 if nm == "selb" else F32) for i in range(2)]
    c1 = {}
    for nm in ("nm1", "den", "rden"):
        c1[nm] = [P.sb(nm + "%d" % i, [128, 1], F32) for i in range(2)]
    wts = [P.sb("wts%d" % i, [128, 2], F32) for i in range(2)]
    dd = [P.sb("dd%d" % i, [128, 2], F32) for i in range(2)]
    idx = [P.sb("idx%d" % i, [128, 2], I32) for i in range(2)]
    for tt in range(T // 128):
        i2 = tt % 2
        x_, hb_, hT_ = xt[i2], hb[i2], hTf[i2]
        tsl = slice(tt * 128, (tt + 1) * 128)
        P.dma("sync", x_[:], k.x1[tsl, :], writes=[x_])
        P.op("vector", lambda e: e.tensor_tensor(out=x_[:], in0=x_[:], in1=screp[:], op=ALU.mult), reads=[x_, screp], writes=[x_])
        P.op("gpsimd", lambda e: e.tensor_tensor(out=x_[:], in0=x_[:], in1=shrep[:], op=ALU.add), reads=[x_, shrep], writes=[x_])
        P.op("scalar", lambda e: e.copy(out=hb_[:], in_=x_[:]), reads=[x_], writes=[hb_])
        for g in range(KC // 4):
            pt = ptr[g % 2]
            for q in range(4):
                c = g * 4 + q
                P.op("tensor", lambda e: e.transpose(out=pt[:, q, :], in_=x_[:, c * 128:(c + 1) * 128], identity=identf[:]),
                     reads=[x_, identf], writes=[pt], signal=(q == 3))
            if g % 2 == 0:
                P.op("scalar", lambda e: e.copy(out=hT_[:, g * 4:(g + 1) * 4, :], in_=pt[:]), reads=[pt], writes=[hT_])
            else:
                P.op("vector", lambda e: e.tensor_copy(out=hT_[:, g * 4:(g + 1) * 4, :], in_=pt[:]), reads=[pt], writes=[hT_])
        for c in range(KC):
            P.op("tensor", lambda e: e.matmul(plog[:, 0:NE], lhsT=hT_[:, c, :], rhs=wr[:, c, :], start=(c == 0), stop=(c == KC - 1)),
                 reads=[hT_, wr], writes=[plog], signal=(c == KC - 1))
        S = {n: v[i2] for n, v in sm.items()}
        C1 = {n: v[i2] for n, v in c1.items()}
        w_, d_, ix_ = wts[i2], dd[i2], idx[i2]
        V = "vector"
        P.op(V, lambda e: e.tensor_copy(out=S["lg"][:], in_=plog[:, 0:NE]), reads=[plog], writes=[S["lg"]])
        P.op(V, lambda e: e.max(out=S["m8"][:], in_=S["lg"][:]), reads=[S["lg"]], writes=[S["m8"]])
        P.op(V, lambda e: e.tensor_scalar(out=S["sel"][:], in0=S["lg"][:], scalar1=S["m8"][:, 1:2], scalar2=None, op0=ALU.is_ge),
             reads=[S["lg"], S["m8"]], writes=[S["sel"]])
        P.op(V, lambda e: e.tensor_scalar(out=S["sel1"][:], in0=S["lg"][:], scalar1=S["m8"][:, 0:1], scalar2=None, op0=ALU.is_ge),
             reads=[S["lg"], S["m8"]], writes=[S["sel1"]])
        P.op(V, lambda e: e.tensor_tensor(out=S["sel2"][:], in0=S["sel"][:], in1=S["sel1"][:], op=ALU.subtract),
             reads=[S["sel"], S["sel1"]], writes=[S["sel2"]])
        P.op(V, lambda e: e.tensor_scalar(out=C1["nm1"][:], in0=S["m8"][:, 0:1], scalar1=-1.0, scalar2=None, op0=ALU.mult),
             reads=[S["m8"]], writes=[C1["nm1"]])
        P.op("scalar", lambda e: e.activation(out=S["ex"][:], in_=S["lg"][:], func=AF.Exp, bias=C1["nm1"][:, 0:1]),
             reads=[S["lg"], C1["nm1"]], writes=[S["ex"]])
        P.op(V, lambda e: e.tensor_tensor(out=S["exs"][:], in0=S["ex"][:], in1=S["sel"][:], op=ALU.mult), reads=[S["ex"], S["sel"]], writes=[S["exs"]])
        P.op(V, lambda e: e.reduce_sum(out=C1["den"][:], in_=S["exs"][:], axis=AX.X), reads=[S["exs"]], writes=[C1["den"]])
        P.op(V, lambda e: e.reciprocal(out=C1["rden"][:], in_=C1["den"][:]), reads=[C1["den"]], writes=[C1["rden"]])
        P.op(V, lambda e: e.tensor_scalar(out=S["comb"][:], in0=S["exs"][:], scalar1=C1["rden"][:, 0:1], scalar2=None, op0=ALU.mult),
             reads=[S["exs"], C1["rden"]], writes=[S["comb"]])
        for j, sn in enumerate(("sel1", "sel2")):
            P.op(V, lambda e: e.tensor_tensor(out=S["tmp8"][:], in0=S["comb"][:], in1=S[sn][:], op=ALU.mult), reads=[S["comb"], S[sn]], writes=[S["tmp8"]])
            P.op(V, lambda e: e.reduce_sum(out=w_[:, j:j + 1], in_=S["tmp8"][:], axis=AX.X), reads=[S["tmp8"]], writes=[w_])
        P.op(V, lambda e: e.tensor_copy(out=S["selb"][:], in_=S["sel"][:]), reads=[S["sel"]], writes=[S["selb"]])
        P.op("tensor", lambda e: e.matmul(pcum[:, 0:NE], lhsT=SLb[:], rhs=S["selb"][:], start=True, stop=True), reads=[SLb, S["selb"]], writes=[pcum], signal=False)
        P.op("tensor", lambda e: e.matmul(pcum[:, NE:2 * NE], lhsT=onesb[:], rhs=S["selb"][:], start=True, stop=True), reads=[onesb, S["selb"]], writes=[pcum])
        P.op(V, lambda e: e.tensor_tensor(out=S["pos"][:], in0=pcum[:, 0:NE], in1=base[:], op=ALU.add), reads=[pcum, base], writes=[S["pos"]])
        P.op(V, lambda e: e.tensor_tensor(out=base[:], in0=pcum[:, NE:2 * NE], in1=base[:], op=ALU.add), reads=[pcum, base], writes=[base])
        P.op(V, lambda e: e.tensor_scalar(out=S["valid"][:], in0=S["pos"][:], scalar1=float(CAP), scalar2=None, op0=ALU.is_lt),
             reads=[S["pos"]], writes=[S["valid"]])
        P.op(V, lambda e: e.tensor_tensor(out=S["dest"][:], in0=S["pos"][:], in1=eoff[:], op=ALU.add), reads=[S["pos"], eoff], writes=[S["dest"]])
        P.op(V, lambda e: e.scalar_tensor_tensor(out=S["dest"][:], in0=S["dest"][:], scalar=-BIGIDX, in1=S["valid"][:], op0=ALU.add, op1=ALU.mult),
             reads=[S["dest"], S["valid"]], writes=[S["dest"]])
        P.op(V, lambda e: e.tensor_scalar(out=S["dest"][:], in0=S["dest"][:], scalar1=BIGIDX, scalar2=None, op0=ALU.add),
             reads=[S["dest"]], writes=[S["dest"]])
        for j, sn in enumerate(("sel1", "sel2")):
            P.op(V, lambda e: e.tensor_tensor(out=S["tmp8"][:], in0=S["dest"][:], in1=S[sn][:], op=ALU.mult), reads=[S["dest"], S[sn]], writes=[S["tmp8"]])
            P.op(V, lambda e: e.reduce_sum(out=d_[:, j:j + 1], in_=S["tmp8"][:], axis=AX.X), reads=[S["tmp8"]], writes=[d_])
        P.op(V, lambda e: e.tensor_copy(out=ix_[:], in_=d_[:]), reads=[d_], writes=[ix_])
        P.dma("sync", k.midx[tsl, :], ix_[:], reads=[ix_])
        P.dma("sync", k.mwts[tsl, :], w_[:], reads=[w_])
        for j in range(2):
            P.gather(k.Xs, hb_[:], ix_[:, j:j + 1], reads=[hb_, ix_], scatter=True, bounds_check=NSLOT - 1, oob_is_err=False)
    P.flush()
    P.release(m0)


def phase_moe_experts(k, l):
    i = l // 2
    for e_ in range(NE):
        ffn_gateup(k, k.Xs[e_ * CAP:(e_ + 1) * CAP, :], CAP, k.moe_w_gate[i, e_], k.moe_w_up[i, e_], D_FFE, k.AT[:, 0:CAP], mod=None)
        ffn_down(k, k.AT[:, 0:CAP], CAP, k.moe_w_down[i, e_], D_FFE, k.Ys, row_off=e_ * CAP)


def phase_moe_ln2(k, l, xsrc, xdst):
    P = k.P
    m0 = P.mark()
    gfrep = P.sb("gfrep", [128, D], F32)
    lng = P.sb("lng", [128, D], F32)
    lnb = P.sb("lnb", [128, D], F32)
    P.dma("sync", gfrep[:], k.modv[l, 5 * D:6 * D].partition_broadcast(128), writes=[gfrep])
    P.dma("sync", lng[:], k.ln_ffn_g[l].partition_broadcast(128), writes=[lng])
    P.dma("sync", lnb[:], k.ln_ffn_b[l].partition_broadcast(128), writes=[lnb])
    xt = [P.sb("xt%d" % i, [128, D], F32) for i in range(2)]
    y1 = [P.sb("y1%d" % i, [128, D], F32) for i in range(2)]
    y2 = [P.sb("y2%d" % i, [128, D], F32) for i in range(2)]
    rt = [P.sb("rt%d" % i, [128, D], F32) for i in range(2)]
    idx = [P.sb("idx%d" % i, [128, 2], I32) for i in range(2)]
    wts = [P.sb("wts%d" % i, [128, 2], F32) for i in range(2)]
    stats = P.sb("stats", [128, 4, 6], F32)
    mv = P.sb("mv", [128, 2], F32)
    rstd = P.sb("rstd", [128, 1], F32)
    for tt in range(T // 128):
        i2 = tt % 2
        x_, r_, a_, b_, ix_, w_ = xt[i2], rt[i2], y1[i2], y2[i2], idx[i2], wts[i2]
        tsl = slice(tt * 128, (tt + 1) * 128)
        P.dma("sync", x_[:], xsrc[tsl, :], writes=[x_])
        P.dma("sync", ix_[:], k.midx[tsl, :], writes=[ix_])
        P.dma("sync", w_[:], k.mwts[tsl, :], writes=[w_])
        P.op("gpsimd", lambda e: e.memset(a_[:], 0.0), writes=[a_])
        P.op("gpsimd", lambda e: e.memset(b_[:], 0.0), writes=[b_])
        P.gather(a_[:], k.Ys, ix_[:, 0:1], reads=[ix_], writes=[a_], bounds_check=NSLOT - 1, oob_is_err=False)
        P.gather(b_[:], k.Ys, ix_[:, 1:2], reads=[ix_], writes=[b_], bounds_check=NSLOT - 1, oob_is_err=False)
        P.op("vector", lambda e: e.tensor_scalar(out=r_[:], in0=a_[:], scalar1=w_[:, 0:1], scalar2=None, op0=ALU.mult), reads=[a_, w_], writes=[r_])
        P.op("vector", lambda e: e.scalar_tensor_tensor(out=r_[:], in0=b_[:], scalar=w_[:, 1:2], in1=r_[:], op0=ALU.mult, op1=ALU.add),
             reads=[b_, w_, r_], writes=[r_])
        P.op("gpsimd", lambda e: e.tensor_tensor(out=r_[:], in0=r_[:], in1=gfrep[:], op=ALU.mult), reads=[r_, gfrep], writes=[r_])
        P.op("vector", lambda e: e.scalar_tensor_tensor(out=r_[:], in0=x_[:], scalar=ALPHA, in1=r_[:], op0=ALU.mult, op1=ALU.add),
             reads=[x_, r_], writes=[r_])
        ln_tile(P, r_, lng, lnb, stats, mv, rstd)
        P.dma("sync", xdst[tsl, :], r_[:], reads=[r_])
    P.flush()
    P.release(m0)


_CACHE = {}


def make_in_maps(inputs, n_cores=8):
    hc = host_consts()
    maps = []
    for core in range(n_cores):
        b = core % 4
        m = {
            "x": np.ascontiguousarray(inputs["x"][b]),
            "cT": np.ascontiguousarray(inputs["c"][b].reshape(KC, 128).T),
            "w_ada": inputs["w_ada"],
            "b_ada": inputs["b_ada"],
            "w_in": inputs["w_in"],
            "cmp_w1_k": inputs["cmp_w1_k"], "cmp_w1_v": inputs["cmp_w1_v"],
            "cmp_w2_k": inputs["cmp_w2_k"], "cmp_w2_v": inputs["cmp_w2_v"],
            "w_out": inputs["w_out"], "ln_mix_g": inputs["ln_mix_g"], "ln_mix_b": inputs["ln_mix_b"],
            "ln_ffn_g": inputs["ln_ffn_g"], "ln_ffn_b": inputs["ln_ffn_b"],
            "moe_router": inputs["moe_router"], "moe_w_gate": inputs["moe_w_gate"], "moe_w_up": inputs["moe_w_up"],
            "moe_w_down": inputs["moe_w_down"],
            "ffn_w_gate": inputs["ffn_w_gate"], "ffn_w_up": inputs["ffn_w_up"], "ffn_w_down": inputs["ffn_w_down"],
            "gla_w_a2": inputs["gla_w_a2"], "gla_b_a": inputs["gla_b_a"],
            "gla_nwT": np.ascontiguousarray(inputs["gla_norm_w"].reshape(DEPTH, 2, 128).transpose(0, 2, 1)),
            "cmp_peT_k": np.ascontiguousarray(inputs["cmp_pos_k"].transpose(0, 2, 1)),
            "cmp_peT_v": np.ascontiguousarray(inputs["cmp_pos_v"].transpose(0, 2, 1)),
        }
        m.update(hc)
        maps.append(m)
    return maps


N_CORES = 4


def kernel(**inputs):
    inputs = {k_: np.asarray(v) for k_, v in inputs.items()}
    nc = build()
    maps = make_in_maps(inputs, n_cores=N_CORES)
    res = run_bass_kernel_spmd(nc, maps, core_ids=list(range(N_CORES)))
    out = np.stack([np.asarray(res.results[b]["out"]) for b in range(4)], 0)
    return out.astype(np.float32)
```

```python
import numpy as np
import concourse.bass as bass
import concourse.mybir as mybir
from concourse.bass_utils import run_bass_kernel_spmd

F32 = mybir.dt.float32
BF16 = mybir.dt.bfloat16
I32 = mybir.dt.int32
U32 = mybir.dt.uint32
AF = mybir.ActivationFunctionType
ALU = mybir.AluOpType
AX = mybir.AxisListType

ENGS = ("tensor", "vector", "scalar", "gpsimd", "sync")

D = 2048
T = 4096
DEPTH = 2
KC = D // 128
HD = 128
SPLIT = (1024, 256, 256, 256, 256, 256, 256, 24, 512, 512, 1024, 16, 1024)
SEG = ("nq", "kc", "vc", "ks", "vs", "kw", "vw", "ng", "gq", "gk", "gv", "ga", "gg")
OFF = {}
_o = 0
for _n, _s in zip(SEG, SPLIT):
    OFF[_n] = (_o, _s)
    _o += _s
IN_W = _o
D_FF = 5632
NE = 8
D_FFE = 7168
ALPHA = (2 * DEPTH) ** 0.25
LN_EPS = 1e-5
NORM_EPS = 1e-6
NEG = -30000.0
SCALE = HD ** -0.5


class Sem:
    def __init__(self, h, kind, uid):
        self.h = h
        self.kind = kind
        self.count = 0
        self.key = "s%d" % uid


class Buf:
    def __init__(self, t, name):
        self.t = t
        self.name = name
        self.last_w = None
        self.readers = {}
        self.sem = {"sw": None, "hw": None}
        self.dlast = {"sw": 0, "hw": 0}
        self.dma_w = {"sw": 0, "hw": 0}
        self.psum = False
        self.fdeps = []

    def __getitem__(self, idx):
        return self.t[idx]

    def reset(self):
        self.last_w = None
        self.readers = {}
        self.fdeps = []
        self.dlast["hw"] = 0
        self.dma_w["hw"] = 0


class _Rec:
    def __init__(self):
        self.calls = []

    def __getattr__(self, name):
        def f(*a, **kw):
            self.calls.append((name, a, kw))
            return self
        return f


class Prog:
    def __init__(self, nc, same_engine_raw=True):
        self.nc = nc
        self.same_engine_raw = same_engine_raw
        self.psem = {}
        self.cnt = {e: 0 for e in ENGS}
        self.lists = {e: [] for e in ENGS}
        self.seen = {e: {} for e in ENGS}
        self.bufs = []
        self._cms = []
        self._semcms = []
        self.pool = {"sw": [], "hw": []}
        self.allsems = []
        for e in ENGS:
            cm = nc.semaphore("p_" + e)
            self.psem[e] = cm.__enter__()
            self._semcms.append(cm)
        self.n_inst = 0
        self.uid = 0
        self._bcreg = {}
        self._bcset = set()

    def mark(self):
        return len(self._cms)

    def release(self, mark):
        while len(self._cms) > mark:
            cm, b = self._cms.pop()
            cm.__exit__(None, None, None)
            if b is not None:
                self._drop(b)

    def _drop(self, b):
        for kind in ("sw", "hw"):
            if b.sem[kind] is not None:
                self.pool[kind].append(b.sem[kind])
                b.sem[kind] = None
        if b in self.bufs:
            self.bufs.remove(b)
        for v in getattr(b, "views", []):
            self._drop(v)

    def sb(self, name, shape, dt):
        self.uid += 1
        cm = self.nc.sbuf_tensor("%s_%d" % (name, self.uid), list(shape), dt)
        t = cm.__enter__()
        b = Buf(t, "%s_%d" % (name, self.uid))
        b.views = []
        self._cms.append((cm, b))
        self.bufs.append(b)
        return b

    def ps(self, name, shape, dt=F32):
        self.uid += 1
        cm = self.nc.psum_tensor("%s_%d" % (name, self.uid), list(shape), dt)
        t = cm.__enter__()
        b = Buf(t, "%s_%d" % (name, self.uid))
        b.psum = True
        b.views = []
        self._cms.append((cm, b))
        self.bufs.append(b)
        return b

    def view(self, buf, name=None):
        self.uid += 1
        b = Buf(buf.t, "%s_v%d" % (buf.name, self.uid))
        buf.views.append(b)
        self.bufs.append(b)
        return b

    def _getsem(self, b, kind):
        if b.sem[kind] is None:
            if self.pool[kind]:
                b.sem[kind] = self.pool[kind].pop()
            else:
                self.uid += 1
                cm = self.nc.semaphore("d%s_%d" % (kind, self.uid))
                h = cm.__enter__()
                self._semcms.append(cm)
                sm = Sem(h, kind, self.uid)
                self.allsems.append(sm)
                b.sem[kind] = sm
        return b.sem[kind]

    def _waits(self, e, reads, writes):
        w = {}

        def need(sem, val, key):
            if val <= 0:
                return
            if self.seen[e].get(key, 0) >= val:
                return
            if key not in w or w[key][1] < val:
                w[key] = (sem, val)

        for r in reads:
            if r.last_w is not None:
                we, n = r.last_w
                if we != e or self.same_engine_raw:
                    need(self.psem[we], n, "p_" + we)
            for kind in ("sw", "hw"):
                if r.dma_w[kind] > 0:
                    need(r.sem[kind].h, 16 * r.dma_w[kind], r.sem[kind].key)
            if r.psum:
                for re_, n in r.readers.items():
                    if re_ != e:
                        need(self.psem[re_], n, "p_" + re_)
        for b in writes:
            if b.last_w is not None:
                we, n = b.last_w
                if we != e:
                    need(self.psem[we], n, "p_" + we)
            for re_, n in b.readers.items():
                if re_ != e:
                    need(self.psem[re_], n, "p_" + re_)
            for kind in ("sw", "hw"):
                if b.dlast[kind] > 0:
                    need(b.sem[kind].h, 16 * b.dlast[kind], b.sem[kind].key)
            for (fs, fv, fk) in b.fdeps:
                need(fs, fv, fk)
        for key, (sem, val) in w.items():
            self.seen[e][key] = val
        return list(w.values())

    def op(self, e, fn, reads=(), writes=(), signal=True):
        waits = self._waits(e, reads, writes)
        n = self.cnt[e] + 1
        if signal:
            self.cnt[e] = n
        psem = self.psem[e]
        rec = _Rec()
        fn(rec)
        assert len(rec.calls) == 1, rec.calls
        name, a, kw = rec.calls[0]

        def thunk(engine, waits=waits, name=name, a=a, kw=kw, signal=signal, psem=psem):
            for sem, val in waits:
                engine.wait_ge(sem, val)
            ins = getattr(engine, name)(*a, **kw)
            if signal:
                ins.then_inc(psem, 1)

        self.lists[e].append(thunk)
        self.n_inst += 1
        for r in reads:
            r.readers[e] = n
        for b in writes:
            b.last_w = (e, n)
            b.readers = {}
            b.dma_w = {"sw": 0, "hw": 0}
            b.fdeps = []

    def _dma_book(self, q, reads, writes):
        kind = "sw" if q == "gpsimd" else "hw"
        waits = self._waits(q, reads, writes)
        allb = list(writes) + list(reads)
        prim = allb[0]
        sm = self._getsem(prim, kind)
        sm.count += 1
        prim.dlast[kind] = sm.count
        for b in allb[1:]:
            assert b not in writes
            b.fdeps.append((sm.h, 16 * sm.count, sm.key))
        for b in writes:
            b.dma_w[kind] = sm.count
            b.last_w = None
            b.readers = {}
            b.fdeps = []
        return waits, sm.h

    def dma(self, q, out_ap, in_ap, reads=(), writes=(), **kw):
        waits, semh = self._dma_book(q, reads, writes)

        def thunk(engine, waits=waits, semh=semh, out_ap=out_ap, in_ap=in_ap, kw=kw):
            for sem, val in waits:
                engine.wait_ge(sem, val)
            engine.dma_start(out=out_ap, in_=in_ap, **kw).then_inc(semh, 16)

        self.lists[q].append(thunk)
        self.n_inst += 1

    def gather(self, out_ap, in_ap, idx_ap, reads=(), writes=(), scatter=False, **kw):
        q = "gpsimd"
        waits, semh = self._dma_book(q, reads, writes)
        bc = kw.pop("bounds_check", None)

        def thunk(engine, waits=waits, semh=semh, kw=kw, bc=bc):
            for sem, val in waits:
                engine.wait_ge(sem, val)
            if bc is not None:
                if bc not in self._bcreg:
                    self._bcreg[bc] = engine.alloc_register("bcreg_%d" % int(bc))
                if bc not in self._bcset:
                    engine.reg_mov(self._bcreg[bc], bc)
                    self._bcset.add(bc)
                kw = dict(kw)
                kw["bounds_check"] = self._bcreg[bc]
            if scatter:
                ins = engine.indirect_dma_start(out=out_ap, out_offset=bass.IndirectOffsetOnAxis(ap=idx_ap, axis=0),
                                                in_=in_ap, in_offset=None, **kw)
            else:
                ins = engine.indirect_dma_start(out=out_ap, out_offset=None, in_=in_ap,
                                                in_offset=bass.IndirectOffsetOnAxis(ap=idx_ap, axis=0), **kw)
            ins.then_inc(semh, 16)

        self.lists[q].append(thunk)
        self.n_inst += 1

    def flush(self, final=False):
        nc = self.nc
        drain = []
        for sm in self.allsems:
            if sm.count > 0:
                drain.append((sm.h, 16 * sm.count))
        for e in ENGS:
            if e != "sync" and self.cnt[e] > 0:
                drain.append((self.psem[e], self.cnt[e]))

        def dthunk(engine, drain=drain):
            for sem, val in drain:
                engine.wait_ge(sem, val)

        self.lists["sync"].append(dthunk)
        lists = self.lists
        with nc.Block() as block:
            @block.tensor
            def _(eng):
                for th in lists["tensor"]:
                    th(eng)

            @block.vector
            def _(eng):
                for th in lists["vector"]:
                    th(eng)

            @block.scalar
            def _(eng):
                for th in lists["scalar"]:
                    th(eng)

            @block.gpsimd
            def _(eng):
                for th in lists["gpsimd"]:
                    th(eng)

            @block.sync
            def _(eng):
                for th in lists["sync"]:
                    th(eng)
        if not final:
            nc.all_engine_barrier()
            sems = [self.psem[e] for e in ENGS] + [sm.h for sm in self.allsems if sm.kind == "hw" and sm.count > 0]
            with nc.Block() as block:
                @block.sync
                def _(eng):
                    for s_ in sems:
                        eng.sem_clear(s_)
            nc.all_engine_barrier()
        for sm in self.allsems:
            if sm.kind == "hw":
                sm.count = 0
        self.lists = {e: [] for e in ENGS}
        self.cnt = {e: 0 for e in ENGS}
        self.seen = {e: {} for e in ENGS}
        self._bcset = set()
        for b in self.bufs:
            b.reset()

    def close(self):
        self.release(0)
        for cm in reversed(self._semcms):
            cm.__exit__(None, None, None)
        self._semcms = []


def host_consts():
    c = {}
    c["ident"] = np.eye(128, dtype=np.float32)
    half = 16
    inv = (500000.0 ** (-np.arange(half, dtype=np.float32) * 2.0 / 32)).astype(np.float32)
    ang = np.arange(T, dtype=np.float32)[None, :] * inv[:, None]
    cos = np.cos(ang).astype(np.float32)
    sin = np.sin(ang).astype(np.float32)
    c["cos"] = np.concatenate([cos, cos], 0)
    c["sin"] = np.concatenate([sin, sin], 0)
    pm = np.zeros((128, 128), np.float32)
    for i in range(16):
        pm[i + 16, i] = -1.0
        pm[i, i + 16] = 1.0
    c["pm"] = pm
    kk = np.arange(128)[:, None]
    tt = np.arange(128)[None, :]
    c["cb"] = np.where(kk > tt, NEG, 0.0).astype(np.float32)
    c["wbm"] = np.where(kk <= tt, NEG, 0.0).astype(np.float32)
    es = np.zeros((64, 32, 128), np.float32)
    for kt in range(32):
        for m in range(128):
            es[2 * kt + m // 64, kt, m] = 1.0
    c["esel"] = es.reshape(64, 32 * 128)
    cst = np.arange(256) * 16
    bst = np.arange(64) * 64
    ov = ((cst[:, None] < bst[None, :] + 64) & (cst[:, None] + 32 > bst[None, :])).astype(np.float32)
    ov[255] = 0.0
    c["ovl"] = np.ascontiguousarray(ov.reshape(2, 128, 64).transpose(1, 0, 2)).reshape(128, 128)
    c["mb"] = (16.0 * kk - tt).astype(np.float32)
    keep = np.zeros((128, 32, 64), np.float32)
    add = np.zeros((128, 32, 64), np.float32)
    n = np.arange(64)[None, :]
    for qt in range(32):
        t = qt * 128 + np.arange(128)[:, None]
        cur = t // 64
        forced = (n == 0) | (n == cur) | (n == cur - 1)
        future = (n * 64) > t
        keep[:, qt, :] = np.where(forced | future, 0.0, 1.0)
        add[:, qt, :] = np.where(future, -1e30, np.where(forced, 1e9, 0.0))
    c["keep"] = keep.reshape(128, 32 * 64)
    c["addc"] = add.reshape(128, 32 * 64)
    sel = np.zeros((12, 12, 128), np.float32)
    for r in range(12):
        sel[r, r, :] = 1.0
    c["sel"] = sel.reshape(12, 12 * 128)
    same = (kk // 64) == (tt // 64)
    c["slmat"] = (kk < tt).astype(np.float32)
    c["eoff"] = np.tile((np.arange(NE) * 768.0)[None, :], (128, 1)).astype(np.float32)
    c["tri2"] = (same & (kk <= tt)).astype(np.float32)
    c["sumat"] = (same & (kk > tt)).astype(np.float32)
    return c


class K:
    pass


def build(debug=(), phases=None, layers=(0, 1), l1_from_x=False):
    nc = bass.Bass("TRN2", target_bir_lowering=False)
    k = K()
    k.nc = nc
    dbg = set(debug)

    def din(name, shape, dt=F32):
        return nc.dram_tensor(name, list(shape), dt, kind="ExternalInput").ap()

    def dscr(name, shape, dt):
        kind = "ExternalOutput" if name in dbg else "Internal"
        return nc.dram_tensor(name, list(shape), dt, kind=kind).ap()

    k.x = din("x", [T, D])
    k.cT = din("cT", [128, KC])
    k.w_ada = din("w_ada", [DEPTH, D, 6 * D])
    k.b_ada = din("b_ada", [DEPTH, 6 * D])
    k.w_in = din("w_in", [DEPTH, D, IN_W])
    k.ident = din("ident", [128, 128])
    k.cos = din("cos", [32, T])
    k.sin = din("sin", [32, T])
    k.pm = din("pm", [128, 128])
    k.cb = din("cb", [128, 128])
    k.wbm = din("wbm", [128, 128])
    k.esel = din("esel", [64, 32 * 128])
    k.ovl = din("ovl", [128, 128])
    k.mb = din("mb", [128, 128])
    k.keep = din("keep", [128, 32 * 64])
    k.addc = din("addc", [128, 32 * 64])
    k.sel = din("sel", [12, 12 * 128])
    k.w_out = din("w_out", [DEPTH, D, D])
    k.ln_mix_g = din("ln_mix_g", [DEPTH, D])
    k.ln_mix_b = din("ln_mix_b", [DEPTH, D])
    k.ln_ffn_g = din("ln_ffn_g", [DEPTH, D])
    k.ln_ffn_b = din("ln_ffn_b", [DEPTH, D])
    k.ffn_w_gate = din("ffn_w_gate", [1, D, D_FF])
    k.ffn_w_up = din("ffn_w_up", [1, D, D_FF])
    k.ffn_w_down = din("ffn_w_down", [1, D_FF, D])
    k.moe_router = din("moe_router", [1, D, NE])
    k.moe_w_gate = din("moe_w_gate", [1, NE, D, D_FFE])
    k.moe_w_up = din("moe_w_up", [1, NE, D, D_FFE])
    k.moe_w_down = din("moe_w_down", [1, NE, D_FFE, D])
    k.slmat = din("slmat", [128, 128])
    k.eoff = din("eoff", [128, NE])
    k.tri2 = din("tri2", [128, 128])
    k.sumat = din("sumat", [128, 128])
    k.gla_w_a2 = din("gla_w_a2", [DEPTH, 16, 512])
    k.gla_b_a = din("gla_b_a", [DEPTH, 512])
    k.gla_nwT = din("gla_nwT", [DEPTH, 128, 2])
    k.cmp_w1 = {"k": din("cmp_w1_k", [DEPTH, 4096, 256]), "v": din("cmp_w1_v", [DEPTH, 4096, 256])}
    k.cmp_w2 = {"k": din("cmp_w2_k", [DEPTH, 256, 128]), "v": din("cmp_w2_v", [DEPTH, 256, 128])}
    k.cmp_peT = {"k": din("cmp_peT_k", [DEPTH, 128, 32]), "v": din("cmp_peT_v", [DEPTH, 128, 32])}
    k.tokidx = din("tokidx", [2048, 1], I32)
    k.out = nc.dram_tensor("out", [2048, D], F32, kind="ExternalOutput").ap()

    k.modv = dscr("modv", [DEPTH, 6 * D], F32)
    k.qn = dscr("qn", [1024, T], BF16)
    k.qr = dscr("qr", [1024, T], BF16)
    k.kcT = dscr("kcT", [256, T], BF16)
    k.vcT = dscr("vcT", [256, T], BF16)
    k.ksT = dscr("ksT", [256, T], BF16)
    k.kwT = dscr("kwT", [256, T], BF16)
    k.ngT = dscr("ngT", [24, T], BF16)
    k.gqT = dscr("gqT", [512, T], BF16)
    k.gkT = dscr("gkT", [512, T], BF16)
    k.gaT = dscr("gaT", [16, T], BF16)
    k.ggT = dscr("ggT", [1024, T], BF16)
    k.vs = dscr("vs", [T, 256], BF16)
    k.vw = dscr("vw", [T, 256], BF16)
    k.gk = dscr("gk", [T, 512], BF16)
    k.gv = dscr("gv", [T, 1024], BF16)
    k.kcmpT = dscr("kcmpT", [2, 128, 256], BF16)
    k.vcmp = dscr("vcmp", [2, 256, 128], BF16)
    k.mixT = dscr("mixT", [D, T], BF16)
    k.x1 = dscr("x1", [T, D], F32)
    k.xa = dscr("xa", [T, D], F32)
    k.yffn = dscr("yffn", [T, D], F32)
    k.AT = dscr("AT", [D_FFE, T], BF16)
    k.Xs = dscr("Xs", [NE * 768, D], BF16)
    k.Ys = dscr("Ys", [NE * 768, D], F32)
    k.midx = dscr("midx", [2048, 2], I32)
    k.mwts = dscr("mwts", [2048, 2], F32)

    P = Prog(nc)
    k.P = P
    ph = phases

    if ph is None or "mod" in ph:
        phase_mod(k)
    for l in layers:
        xsrc = k.x if (l == 0 or l1_from_x) else k.xa
        if ph is None or "proj" in ph:
            phase_proj(k, l, xsrc)
        if ph is None or "cmp" in ph:
            phase_cmp(k, l)
        if ph is None or "nsa" in ph:
            phase_nsa(k, l)
        if ph is None or "gla" in ph:
            phase_gla(k, l)
        if ph is None or "wout" in ph:
            phase_wout(k, l, xsrc, k.x1)
        xdst = k.out if l == DEPTH - 1 else k.xa
        if l % 2 == 0:
            if ph is None or "ffn" in ph:
                ffn_gateup(k, k.x1, T, k.ffn_w_gate[l // 2], k.ffn_w_up[l // 2], D_FF, k.AT[0:D_FF, :], mod=l)
                ffn_down(k, k.AT[0:D_FF, :], T, k.ffn_w_down[l // 2], D_FF, k.yffn)
            if ph is None or "ln2" in ph:
                phase_ln2(k, l, k.x1, k.yffn, xdst)
        else:
            if ph is None or "route" in ph:
                phase_moe_route(k, l)
            if ph is None or "experts" in ph:
                phase_moe_experts(k, l)
            if ph is None or "ln2" in ph:
                phase_moe_ln2(k, l, k.x1, xdst)

    P.flush(final=True)
    P.close()
    return nc


def phase_mod(k):
    P = k.P
    m0 = P.mark()
    cc = P.sb("cc", [128, KC], F32)
    cs = P.sb("cs", [128, KC], F32)
    condB = P.sb("condB", [128, KC, 128], F32)
    wb = [P.sb("wada%d" % i, [128, KC, 512], F32) for i in range(2)]
    bb = [P.sb("bada%d" % i, [128, 512], F32) for i in range(2)]
    mo = [P.sb("mo%d" % i, [128, 512], F32) for i in range(2)]
    pm = [P.ps("pmod%d" % i, [128, 512]) for i in range(2)]
    P.dma("sync", cc[:], k.cT, writes=[cc])
    P.op("scalar", lambda e: e.activation(out=cs[:], in_=cc[:], func=AF.Silu), reads=[cc], writes=[cs])
    P.op("vector", lambda e: e.tensor_copy(out=condB[:], in_=cs[:].unsqueeze(2).to_broadcast([128, KC, 128])),
         reads=[cs], writes=[condB])
    it = 0
    import os
    NBL = int(os.environ.get("NBL", "24"))
    VAR = os.environ.get("VAR", "")
    for l in range(DEPTH):
        wv = k.w_ada[l].rearrange("(c p) n -> p c n", p=128)
        for nb in range(NBL):
            i = it % 2
            it += 1
            n0 = nb * 512
            P.dma("sync", wb[i][:], wv[:, :, n0:n0 + 512], writes=[wb[i]])
            if VAR == "nobb":
                P.op("vector", lambda e, i=i: e.memset(bb[i][:], 0.0), writes=[bb[i]])
            else:
                P.dma("gpsimd", bb[i][:], k.b_ada[l, n0:n0 + 512].partition_broadcast(128), writes=[bb[i]])
            for c in range(KC):
                P.op("tensor", lambda e, i=i, c=c: e.matmul(pm[i][:], lhsT=condB[:, c, :], rhs=wb[i][:, c, :],
                                                             start=(c == 0), stop=(c == KC - 1)),
                     reads=[condB, wb[i]], writes=[pm[i]], signal=(c == KC - 1))
            seg = nb // 4
            add1 = 1.0 if seg in (1, 2, 4, 5) else 0.0
            P.op("vector", lambda e, i=i, add1=add1: e.scalar_tensor_tensor(
                out=mo[i][:], in0=pm[i][:], scalar=add1, in1=bb[i][:], op0=ALU.add, op1=ALU.add),
                reads=[pm[i], bb[i]], writes=[mo[i]])
            P.dma("gpsimd", k.modv[l:l + 1, n0:n0 + 512], mo[i][0:1, :], reads=[mo[i]])
    P.flush()
    P.release(m0)


def build_hT(k, xsrc, t0, ntiles, screp, shrep, identf, hT, hviews, xt, ptr):
    P = k.P
    for j in range(ntiles):
        xb = xt[j % 2]
        P.dma("sync", xb[:], xsrc[t0 + j * 128:t0 + (j + 1) * 128, :], writes=[xb])
        P.op("vector", lambda e, xb=xb: e.tensor_tensor(out=xb[:], in0=xb[:], in1=screp[:], op=ALU.mult),
             reads=[xb, screp], writes=[xb])
        P.op("gpsimd", lambda e, xb=xb: e.tensor_tensor(out=xb[:], in0=xb[:], in1=shrep[:], op=ALU.add),
             reads=[xb, shrep], writes=[xb])
        for g in range(KC // 4):
            pt = ptr[(j * 4 + g) % 2]
            for q in range(4):
                c = g * 4 + q
                P.op("tensor", lambda e, xb=xb, pt=pt, q=q, c=c: e.transpose(
                    out=pt[:, q, :], in_=xb[:, c * 128:(c + 1) * 128], identity=identf[:]),
                    reads=[xb, identf], writes=[pt], signal=(q == 3))
            eng = "scalar" if (g % 2 == 0) else "vector"
            if eng == "scalar":
                P.op("scalar", lambda e, pt=pt, g=g, j=j: e.copy(out=hT[:, g * 4:(g + 1) * 4, j * 128:(j + 1) * 128], in_=pt[:]),
                     reads=[pt], writes=[hviews[j][0]])
            else:
                P.op("vector", lambda e, pt=pt, g=g, j=j: e.tensor_copy(out=hT[:, g * 4:(g + 1) * 4, j * 128:(j + 1) * 128], in_=pt[:]),
                     reads=[pt], writes=[hviews[j][1]])


def phase_proj(k, l, xsrc):
    P = k.P
    TH = 2048
    for th in range(T // TH):
        t0 = th * TH
        m0 = P.mark()
        screp = P.sb("screp", [128, D], F32)
        shrep = P.sb("shrep", [128, D], F32)
        identf = P.sb("identf", [128, 128], F32)
        hT = P.sb("hT", [128, KC, TH], BF16)
        hviews = [(P.view(hT), P.view(hT)) for _ in range(TH // 128)]
        xt = [P.sb("xt%d" % i, [128, D], F32) for i in range(2)]
        ptr = [P.ps("ptr%d" % i, [128, 4, 128]) for i in range(2)]
        cosb = P.sb("cosb", [32, TH], F32)
        sinb = P.sb("sinb", [32, TH], F32)
        pmb = P.sb("pmb", [128, 128], BF16)
        wbk = [P.sb("wblk%d" % i, [128, KC, 512], BF16) for i in range(2)]
        pacc = [P.ps("pacc%d" % i, [128, 512]) for i in range(3)]
        prot = [P.ps("prot%d" % i, [128, 512]) for i in range(2)]
        ost = [P.sb("ost%d" % i, [128, 512], BF16) for i in range(4)]
        ost2 = [P.sb("ost2%d" % i, [128, 512], BF16) for i in range(2)]
        t1 = [P.sb("t1%d" % i, [32, 512], F32) for i in range(2)]
        t2 = [P.sb("t2%d" % i, [32, 512], F32) for i in range(2)]

        P.dma("sync", screp[:], k.modv[l, D:2 * D].partition_broadcast(128), writes=[screp])
        P.dma("sync", shrep[:], k.modv[l, 0:D].partition_broadcast(128), writes=[shrep])
        P.dma("sync", identf[:], k.ident, writes=[identf])
        P.dma("sync", cosb[:], k.cos[:, t0:t0 + TH], writes=[cosb])
        P.dma("sync", sinb[:], k.sin[:, t0:t0 + TH], writes=[sinb])
        P.dma("gpsimd", pmb[:], k.pm, writes=[pmb])
        build_hT(k, xsrc, t0, TH // 128, screp, shrep, identf, hT, hviews, xt, ptr)

        wv = k.w_in[l].rearrange("(c p) n -> p c n", p=128)
        cnt = {"w": 0, "acc": 0, "ost": 0, "ost2": 0, "rot": 0}

        def hreads(tb):
            r = []
            for j in range(tb * 4, tb * 4 + 4):
                r += [hviews[j][0], hviews[j][1]]
            return r

        def load_w(c0, ncols):
            wb = wbk[cnt["w"] % 2]
            cnt["w"] += 1
            P.dma("gpsimd", wb[:, :, 0:ncols], wv[:, :, c0:c0 + ncols], writes=[wb])
            return wb

        def ftype(seg, dst, mode):
            c0, n = OFF[seg]
            for b0 in range(0, n, 512):
                nb = min(512, n - b0)
                wb = load_w(c0 + b0, nb)
                for ct in range(0, nb, 128):
                    m = min(128, nb - ct)
                    row0 = b0 + ct
                    for tb in range(TH // 512):
                        pa = pacc[cnt["acc"] % 3]
                        cnt["acc"] += 1
                        for c in range(KC):
                            P.op("tensor", lambda e, pa=pa, wb=wb, ct=ct, m=m, c=c, tb=tb: e.matmul(
                                pa[0:m, :], lhsT=wb[:, c, ct:ct + m], rhs=hT[:, c, tb * 512:(tb + 1) * 512],
                                start=(c == 0), stop=(c == KC - 1)),
                                reads=[wb] + hreads(tb), writes=[pa], signal=(c == KC - 1))
                        ob = ost[cnt["ost"] % 4]
                        cnt["ost"] += 1
                        tok = slice(t0 + tb * 512, t0 + (tb + 1) * 512)
                        if mode == "plain":
                            P.op("scalar", lambda e, ob=ob, pa=pa, m=m: e.copy(out=ob[0:m, :], in_=pa[0:m, :]),
                                 reads=[pa], writes=[ob])
                            P.dma("sync", dst[row0:row0 + m, tok], ob[0:m, :], reads=[ob])
                        elif mode == "sigmoid":
                            P.op("scalar", lambda e, ob=ob, pa=pa, m=m: e.activation(out=ob[0:m, :], in_=pa[0:m, :], func=AF.Sigmoid),
                                 reads=[pa], writes=[ob])
                            P.dma("sync", dst[row0:row0 + m, tok], ob[0:m, :], reads=[ob])
                        elif mode == "silu":
                            P.op("scalar", lambda e, ob=ob, pa=pa, m=m: e.activation(out=ob[0:m, :], in_=pa[0:m, :], func=AF.Silu),
                                 reads=[pa], writes=[ob])
                            P.dma("sync", dst[row0:row0 + m, tok], ob[0:m, :], reads=[ob])
                        else:
                            dn, dr = mode[1], mode[2]
                            P.op("scalar", lambda e, ob=ob, pa=pa: e.copy(out=ob[:], in_=pa[:]), reads=[pa], writes=[ob])
                            pr = prot[cnt["rot"] % 2]
                            a1 = t1[cnt["rot"] % 2]
                            a2 = t2[cnt["rot"] % 2]
                            cnt["rot"] += 1
                            P.op("tensor", lambda e, pr=pr, ob=ob: e.matmul(pr[:], lhsT=pmb[:], rhs=ob[:], start=True, stop=True),
                                 reads=[pmb, ob], writes=[pr])
                            ltok = slice(tb * 512, (tb + 1) * 512)
                            P.op("vector", lambda e, a1=a1, pa=pa, ltok=ltok: e.tensor_tensor(out=a1[:], in0=pa[0:32, :], in1=cosb[:, ltok], op=ALU.mult),
                                 reads=[pa, cosb], writes=[a1])
                            P.op("vector", lambda e, a2=a2, pr=pr, ltok=ltok: e.tensor_tensor(out=a2[:], in0=pr[0:32, :], in1=sinb[:, ltok], op=ALU.mult),
                                 reads=[pr, sinb], writes=[a2])
                            if dn is not None:
                                P.dma("sync", dn[row0:row0 + 128, tok], ob[:], reads=[ob])
                                o2 = ost2[cnt["ost2"] % 2]
                                cnt["ost2"] += 1
                                P.op("scalar", lambda e, o2=o2, pa=pa: e.copy(out=o2[:], in_=pa[:]), reads=[pa], writes=[o2])
                                P.op("vector", lambda e, o2=o2, a1=a1, a2=a2: e.tensor_tensor(out=o2[0:32, :], in0=a1[:], in1=a2[:], op=ALU.add),
                                     reads=[a1, a2], writes=[o2])
                                P.dma("sync", dr[row0:row0 + 128, tok], o2[:], reads=[o2])
                            else:
                                P.op("vector", lambda e, ob=ob, a1=a1, a2=a2: e.tensor_tensor(out=ob[0:32, :], in0=a1[:], in1=a2[:], op=ALU.add),
                                     reads=[a1, a2, ob], writes=[ob])
                                P.dma("sync", dr[row0:row0 + 128, tok], ob[:], reads=[ob])

        def ttype(seg, dst):
            c0, n = OFF[seg]
            for b0 in range(0, n, 512):
                nb = min(512, n - b0)
                wb = load_w(c0 + b0, nb)
                for j in range(TH // 128):
                    pa = pacc[cnt["acc"] % 3]
                    cnt["acc"] += 1
                    for c in range(KC):
                        P.op("tensor", lambda e, pa=pa, wb=wb, nb=nb, c=c, j=j: e.matmul(
                            pa[:, 0:nb], lhsT=hT[:, c, j * 128:(j + 1) * 128], rhs=wb[:, c, 0:nb],
                            start=(c == 0), stop=(c == KC - 1)),
                            reads=[wb, hviews[j][0], hviews[j][1]], writes=[pa], signal=(c == KC - 1))
                    ob = ost[cnt["ost"] % 4]
                    cnt["ost"] += 1
                    P.op("scalar", lambda e, ob=ob, pa=pa, nb=nb: e.copy(out=ob[:, 0:nb], in_=pa[:, 0:nb]), reads=[pa], writes=[ob])
                    P.dma("sync", dst[t0 + j * 128:t0 + (j + 1) * 128, b0:b0 + nb], ob[:, 0:nb], reads=[ob])

        import os
        SEGS = os.environ.get("SEGS", "")
        plan = [("nq", "f", None, ("rope", k.qn, k.qr)), ("kc", "f", k.kcT, "plain"), ("vc", "f", k.vcT, "plain"),
                ("ks", "f", None, ("rope", None, k.ksT)), ("vs", "t", k.vs, None), ("kw", "f", None, ("rope", None, k.kwT)),
                ("vw", "t", k.vw, None), ("ng", "f", k.ngT, "sigmoid"), ("gq", "f", k.gqT, "plain"), ("gk", "f", k.gkT, "plain"),
                ("gk", "t", k.gk, None), ("gv", "t", k.gv, None), ("ga", "f", k.gaT, "plain"), ("gg", "f", k.ggT, "silu")]
        for (sg, ty, dst, mode) in plan:
            if SEGS and (sg + ty) not in SEGS.split(","):
                continue
            if ty == "f":
                ftype(sg, dst, mode)
            else:
                ttype(sg, dst)
        P.flush()
        P.release(m0)


def phase_cmp(k, l):
    P = k.P
    m0 = P.mark()
    aT = [P.sb("aT%d" % i, [128, T], BF16) for i in range(2)]
    w1b = [P.sb("w1b%d" % i, [128, 32, 256], BF16) for i in range(2)]
    w2b = [P.sb("w2b%d" % i, [128, 2, 128], BF16) for i in range(2)]
    peT = [P.sb("peT%d" % i, [128, 32], BF16) for i in range(2)]
    ph = [P.ps("ph%d" % i, [128, 512]) for i in range(2)]
    pc = P.ps("pc", [128, 512])
    po = P.ps("pcmpo", [128, 512])
    cst = P.sb("cst", [128, 2], F32)
    xh = [P.sb("xh%d" % i, [128, 256], F32) for i in range(2)]
    uu = [P.sb("uu%d" % i, [128, 256], F32) for i in range(2)]
    sg = [P.sb("sgm%d" % i, [128, 256], F32) for i in range(2)]
    gb = [P.sb("gb%d" % i, [128, 256], BF16) for i in range(2)]
    ocp = [P.sb("ocp%d" % i, [128, 256], BF16) for i in range(2)]
    for i in range(2):
        P.op("vector", lambda e, i=i: e.memset(gb[i][:], 0.0), writes=[gb[i]])
        P.op("vector", lambda e, i=i: e.memset(ocp[i][:], 0.0), writes=[ocp[i]])
    it = 0
    for si, src in enumerate(("k", "v")):
        w1 = k.cmp_w1[src][l].rearrange("(l d) m -> d l m", d=128)
        for q4 in range(4):
            P.dma("gpsimd", w1b[si][:, q4 * 8:(q4 + 1) * 8, :], w1[:, q4 * 8:(q4 + 1) * 8, :], writes=[w1b[si]])
        P.dma("gpsimd", w2b[si][:], k.cmp_w2[src][l].rearrange("(c p) n -> p c n", p=128), writes=[w2b[si]])
        P.dma("gpsimd", peT[si][:], k.cmp_peT[src][l], writes=[peT[si]])
        srcT = k.kcT if src == "k" else k.vcT
        for hk in range(2):
            a = aT[it % 2]
            oc = ocp[it % 2]
            it += 1
            P.dma("sync", a[:], srcT[hk * 128:(hk + 1) * 128, :], writes=[a])
            for mc in range(2):
                for ll in range(32):
                    P.op("tensor", lambda e, a=a, mc=mc, ll=ll, si=si: e.matmul(
                        ph[mc][:, 0:255], lhsT=w1b[si][:, ll, mc * 128:(mc + 1) * 128], rhs=a[:, ll:ll + 4065:16],
                        start=(ll == 0), stop=(ll == 31)), reads=[w1b[si], a], writes=[ph[mc]], signal=(ll == 31))
                for ll in range(32):
                    P.op("tensor", lambda e, mc=mc, ll=ll, si=si: e.matmul(
                        pc[:, mc:mc + 1], lhsT=w1b[si][:, ll, mc * 128:(mc + 1) * 128], rhs=peT[si][:, ll:ll + 1],
                        start=(ll == 0), stop=(ll == 31)), reads=[w1b[si], peT[si]], writes=[pc], signal=(ll == 31))
            P.op("vector", lambda e: e.tensor_copy(out=cst[:], in_=pc[:, 0:2]), reads=[pc], writes=[cst])
            for mc in range(2):
                x_, u_, s_, g_ = xh[mc], uu[mc], sg[mc], gb[mc]
                P.op("vector", lambda e, x_=x_, mc=mc: e.tensor_scalar(out=x_[:, 0:255], in0=ph[mc][:, 0:255], scalar1=cst[:, mc:mc + 1],
                                                                      scalar2=None, op0=ALU.add), reads=[ph[mc], cst], writes=[x_])
                P.op("vector", lambda e, x_=x_, u_=u_: e.tensor_tensor(out=u_[:, 0:255], in0=x_[:, 0:255], in1=x_[:, 0:255], op=ALU.mult),
                     reads=[x_], writes=[u_])
                P.op("vector", lambda e, u_=u_: e.tensor_scalar(out=u_[:, 0:255], in0=u_[:, 0:255], scalar1=0.044715, scalar2=1.0,
                                                               op0=ALU.mult, op1=ALU.add), reads=[u_], writes=[u_])
                P.op("vector", lambda e, x_=x_, u_=u_: e.tensor_tensor(out=u_[:, 0:255], in0=u_[:, 0:255], in1=x_[:, 0:255], op=ALU.mult),
                     reads=[x_, u_], writes=[u_])
                P.op("scalar", lambda e, u_=u_, s_=s_: e.activation(out=s_[:, 0:255], in_=u_[:, 0:255], func=AF.Sigmoid, scale=1.5957691216057308),
                     reads=[u_], writes=[s_])
                P.op("vector", lambda e, x_=x_, s_=s_, g_=g_: e.tensor_tensor(out=g_[:, 0:255], in0=x_[:, 0:255], in1=s_[:, 0:255], op=ALU.mult),
                     reads=[x_, s_], writes=[g_])
            if src == "k":
                for mc in range(2):
                    P.op("tensor", lambda e, mc=mc, si=si: e.matmul(po[:, 0:255], lhsT=w2b[si][:, mc, :], rhs=gb[mc][:, 0:255],
                                                                   start=(mc == 0), stop=(mc == 1)),
                         reads=[w2b[si], gb[mc]], writes=[po], signal=(mc == 1))
                P.op("scalar", lambda e, oc=oc: e.copy(out=oc[:, 0:255], in_=po[:, 0:255]), reads=[po], writes=[oc])
                P.dma("sync", k.kcmpT[hk], oc[:], reads=[oc])
            else:
                for ct in range(2):
                    for mc in range(2):
                        P.op("tensor", lambda e, mc=mc, ct=ct, si=si: e.matmul(
                            po[:, ct * 128:(ct + 1) * 128], lhsT=gb[mc][:, ct * 128:(ct + 1) * 128], rhs=w2b[si][:, mc, :],
                            start=(mc == 0), stop=(mc == 1)), reads=[w2b[si], gb[mc]], writes=[po], signal=(mc == 1))
                P.op("scalar", lambda e, oc=oc: e.copy(out=oc[:], in_=po[:, 0:256]), reads=[po], writes=[oc])
                P.dma("sync", k.vcmp[hk].rearrange("(c p) d -> p c d", p=128), oc[:].rearrange("p (c d) -> p c d", c=2), reads=[oc])
    P.flush()
    P.release(m0)


def phase_nsa(k, l, hks=(0, 1)):
    P = k.P
    m0 = P.mark()
    cst_f = {}
    for nm, shp in (("cb", [128, 128]), ("wbm", [128, 128]), ("esel", [64, 32 * 128]), ("ovl", [128, 128]), ("sel", [12, 12 * 128]),
                    ("ident", [128, 128])):
        b = P.sb("c_" + nm, shp, BF16)
        P.dma("gpsimd", b[:], getattr(k, nm), writes=[b])
        cst_f[nm] = b
    cb, wbm, esel, ovl, sel, identb = (cst_f[n] for n in ("cb", "wbm", "esel", "ovl", "sel", "ident"))
    identf = P.sb("identf", [128, 128], F32)
    P.dma("sync", identf[:], k.ident, writes=[identf])
    mb = P.sb("mb", [128, 128], F32)
    P.dma("sync", mb[:], k.mb, writes=[mb])
    keep = P.sb("keep", [128, 32 * 64], F32)
    addc = P.sb("addc", [128, 32 * 64], F32)
    P.dma("sync", keep[:], k.keep, writes=[keep])
    P.dma("sync", addc[:], k.addc, writes=[addc])
    onesb = P.sb("onesb", [128, 128], BF16)
    P.op("vector", lambda e: e.memset(onesb[:], 1.0), writes=[onesb])

    ksT = P.sb("ksT", [128, T], BF16)
    kwT = P.sb("kwT", [128, T], BF16)
    vs = P.sb("vs", [128, 32, 128], BF16)
    vw = P.sb("vw", [128, 32, 128], BF16)
    kcm = P.sb("kcm", [128, 256], BF16)
    vcm = P.sb("vcm", [128, 2, 128], BF16)
    sgt = P.sb("sgt", [12, T], BF16)
    qnb = [P.sb("qnb%d" % i, [128, 4, 512], BF16) for i in range(2)]
    qrb = [P.sb("qrb%d" % i, [128, 4, 512], BF16) for i in range(2)]
    S = [P.ps("S%d" % i, [128, 512]) for i in range(2)]
    BD = [P.ps("BD%d" % i, [128, 512]) for i in range(2)]
    BO = [P.ps("BO%d" % i, [128, 512]) for i in range(2)]
    BG = P.ps("BG", [128, 512])
    BM = P.ps("BM", [128, 512])
    pT = [P.sb("pT%d" % i, [128, 512], BF16) for i in range(3)]
    pTc = [P.sb("pTc%d" % i, [128, 512], BF16) for i in range(2)]
    pn = [P.sb("pn%d" % i, [128, 512], BF16) for i in range(2)]
    bc = [P.sb("bc%d" % i, [128, 128], BF16) for i in range(2)]
    W = [P.sb("W%d" % i, [128, 512], F32) for i in range(2)]
    Wg = [P.sb("Wg%d" % i, [128, 512], F32) for i in range(2)]
    tmp = [P.sb("tmp%d" % i, [128, 512], F32) for i in range(2)]
    acc = [P.sb("acc%d" % i, [128, 512], F32) for i in range(2)]
    accb = [P.sb("accb%d" % i, [128, 512], BF16) for i in range(2)]
    impT = P.sb("impT", [64, 128], F32)
    imp = P.sb("imp", [128, 64], F32)
    imp2 = P.sb("imp2", [128, 64], F32)
    m8a = P.sb("m8a", [128, 8], F32)
    m8b = P.sb("m8b", [128, 8], F32)
    bias = P.sb("bias", [128, 64], BF16)
    biasT = P.sb("biasT", [64, 128], BF16)
    cnt = {"s": 0, "pt": 0, "bc": 0, "set": 0, "w": 0}

    def rhs4(buf, qi):
        return buf[:, :, qi * 128:(qi + 1) * 128]

    def as4(ap):
        return ap.rearrange("p (g t) -> p g t", g=4)

    def bcast4(ap, np_):
        return ap.unsqueeze(1).to_broadcast([np_, 4, 128])

    for hk in hks:
        P.dma("sync", ksT[:], k.ksT[hk * 128:(hk + 1) * 128, :], writes=[ksT])
        P.dma("sync", kwT[:], k.kwT[hk * 128:(hk + 1) * 128, :], writes=[kwT])
        for q4 in range(4):
            P.dma("sync", vs[:, q4 * 8:(q4 + 1) * 8, :],
                  k.vs[q4 * 1024:(q4 + 1) * 1024, hk * 128:(hk + 1) * 128].rearrange("(t p) d -> p t d", p=128), writes=[vs])
            P.dma("sync", vw[:, q4 * 8:(q4 + 1) * 8, :],
                  k.vw[q4 * 1024:(q4 + 1) * 1024, hk * 128:(hk + 1) * 128].rearrange("(t p) d -> p t d", p=128), writes=[vw])
        P.dma("sync", kcm[:], k.kcmpT[hk], writes=[kcm])
        P.dma("sync", vcm[:], k.vcmp[hk].rearrange("(c p) d -> p c d", p=128), writes=[vcm])
        P.dma("sync", sgt[:], k.ngT[hk * 12:(hk + 1) * 12, :], writes=[sgt])
        for qt in range(T // 128):
            qb, qi = qt // 4, qt % 4
            qn_, qr_ = qnb[qb % 2], qrb[qb % 2]
            if qi == 0:
                tok = slice(qb * 512, (qb + 1) * 512)
                P.dma("sync", qn_[:], k.qn[hk * 512:(hk + 1) * 512, tok].rearrange("(g d) t -> d g t", d=128), writes=[qn_])
                P.dma("sync", qr_[:], k.qr[hk * 512:(hk + 1) * 512, tok].rearrange("(g d) t -> d g t", d=128), writes=[qr_])
            tsl = slice(qt * 128, (qt + 1) * 128)

            def gates(j):
                for g in range(4):
                    r = 3 * g + j
                    P.op("tensor", lambda e, g=g, r=r: e.matmul(BG[:, g * 128:(g + 1) * 128], lhsT=sel[:, r * 128:(r + 1) * 128],
                                                                rhs=sgt[:, tsl], start=True, stop=True),
                         reads=[sel, sgt], writes=[BG], signal=(g == 3))

            def combine(st, first, w_):
                wg = Wg[cnt["w"] % 2]
                tp = tmp[cnt["w"] % 2]
                cnt["w"] += 1
                ac = acc[qt % 2]
                P.op("vector", lambda e, wg=wg, w_=w_: e.tensor_tensor(out=wg[:], in0=BG[:], in1=w_[:], op=ALU.mult),
                     reads=[BG, w_], writes=[wg])
                if first:
                    P.op("vector", lambda e, wg=wg, ac=ac, st=st: e.tensor_tensor(out=ac[:], in0=BO[st][:], in1=wg[:], op=ALU.mult),
                         reads=[BO[st], wg], writes=[ac])
                else:
                    P.op("vector", lambda e, wg=wg, tp=tp, st=st: e.tensor_tensor(out=tp[:], in0=BO[st][:], in1=wg[:], op=ALU.mult),
                         reads=[BO[st], wg], writes=[tp])
                    P.op("gpsimd", lambda e, tp=tp, ac=ac: e.tensor_tensor(out=ac[:], in0=ac[:], in1=tp[:], op=ALU.add),
                         reads=[ac, tp], writes=[ac])

            st = cnt["set"] % 2
            cnt["set"] += 1
            nct = 1 if qt <= 15 else 2
            for ct in range(nct):
                s_ = S[cnt["s"] % 2]
                cnt["s"] += 1
                need_mask = not (ct == 0 and qt >= 17)
                P.op("tensor", lambda e, s_=s_, ct=ct, qn_=qn_: e.matmul(as4(s_[:]), lhsT=kcm[:, ct * 128:(ct + 1) * 128], rhs=rhs4(qn_, qi),
                                                                        start=True, stop=not need_mask),
                     reads=[kcm, qn_], writes=[s_], signal=not need_mask)
                if need_mask:
                    b_ = bc[cnt["bc"] % 2]
                    cnt["bc"] += 1
                    thr = float(128 * qt - 2048 * ct - 31)
                    P.op("gpsimd", lambda e, b_=b_, thr=thr: e.tensor_scalar(out=b_[:], in0=mb[:], scalar1=thr, scalar2=NEG, op0=ALU.is_gt, op1=ALU.mult),
                         reads=[mb], writes=[b_])
                    P.op("tensor", lambda e, s_=s_, b_=b_: e.matmul(as4(s_[:]), lhsT=identb[:], rhs=bcast4(b_[:], 128), start=False, stop=True),
                         reads=[identb, b_], writes=[s_])
                p_ = pTc[ct]
                P.op("scalar", lambda e, s_=s_, p_=p_: e.activation(out=p_[:], in_=s_[:], func=AF.Exp, scale=SCALE), reads=[s_], writes=[p_])
                P.op("tensor", lambda e, p_=p_, st=st, ct=ct: e.matmul(BD[st][:], lhsT=onesb[:], rhs=p_[:], start=(ct == 0), stop=(ct == nct - 1)),
                     reads=[onesb, p_], writes=[BD[st]], signal=(ct == nct - 1))
                P.op("tensor", lambda e, p_=p_, st=st, ct=ct: e.matmul(BO[st][:], lhsT=vcm[:, ct, :], rhs=p_[:], start=(ct == 0), stop=(ct == nct - 1)),
                     reads=[vcm, p_], writes=[BO[st]], signal=(ct == nct - 1))
            w_ = W[cnt["w"] % 2]
            P.op("vector", lambda e, w_=w_, st=st: e.tensor_scalar(out=w_[:], in0=BD[st][:], scalar1=1e-30, scalar2=None, op0=ALU.add),
                 reads=[BD[st]], writes=[w_])
            P.op("vector", lambda e, w_=w_: e.reciprocal(out=w_[:], in_=w_[:]), reads=[w_], writes=[w_])
            if qt >= 8:
                for ct in range(nct):
                    P.op("gpsimd", lambda e, ct=ct, w_=w_: e.tensor_tensor(out=pn[ct][:], in0=pTc[ct][:], in1=w_[:], op=ALU.mult),
                         reads=[pTc[ct], w_], writes=[pn[ct]])
                n_mm = nct * 4
                i_mm = 0
                for ct in range(nct):
                    for g in range(4):
                        P.op("tensor", lambda e, ct=ct, g=g, i_mm=i_mm: e.matmul(BM[0:64, 0:128], lhsT=ovl[:, ct * 64:(ct + 1) * 64],
                                                                                rhs=pn[ct][:, g * 128:(g + 1) * 128],
                                                                                start=(i_mm == 0), stop=(i_mm == n_mm - 1)),
                             reads=[ovl, pn[ct]], writes=[BM], signal=(i_mm == n_mm - 1))
                        i_mm += 1
                P.op("vector", lambda e: e.tensor_copy(out=impT[:], in_=BM[0:64, 0:128]), reads=[BM], writes=[impT])
                P.op("tensor", lambda e: e.transpose(out=BM[:, 128:192], in_=impT[:], identity=identf[0:64, 0:64]),
                     reads=[impT, identf], writes=[BM])
                P.op("vector", lambda e: e.tensor_tensor(out=imp[:], in0=BM[:, 128:192], in1=keep[:, qt * 64:(qt + 1) * 64], op=ALU.mult),
                     reads=[BM, keep], writes=[imp])
                P.op("vector", lambda e: e.tensor_tensor(out=imp[:], in0=imp[:], in1=addc[:, qt * 64:(qt + 1) * 64], op=ALU.add),
                     reads=[imp, addc], writes=[imp])
                P.op("vector", lambda e: e.max(out=m8a[:], in_=imp[:]), reads=[imp], writes=[m8a])
                P.op("vector", lambda e: e.match_replace(out=imp2[:], in_to_replace=m8a[:], in_values=imp[:], imm_value=-3.0e38),
                     reads=[imp, m8a], writes=[imp2])
                P.op("vector", lambda e: e.max(out=m8b[:], in_=imp2[:]), reads=[imp2], writes=[m8b])
                P.op("vector", lambda e: e.tensor_scalar(out=bias[:], in0=imp[:], scalar1=m8b[:, 7:8], scalar2=NEG, op0=ALU.is_lt, op1=ALU.mult),
                     reads=[imp, m8b], writes=[bias])
                P.op("tensor", lambda e: e.transpose(out=BM[0:64, 256:320].bitcast(BF16), in_=bias[:], identity=identb[:]),
                     reads=[bias, identb], writes=[BM])
                P.op("vector", lambda e: e.tensor_copy(out=biasT[:], in_=BM[0:64, 256:320].bitcast(BF16)), reads=[BM], writes=[biasT])
            gates(0)
            combine(st, True, w_)

            def branch(kT_, v_, kts, kind):
                st = cnt["set"] % 2
                cnt["set"] += 1
                nk = len(kts)
                for ii, kt in enumerate(kts):
                    s_ = S[cnt["s"] % 2]
                    cnt["s"] += 1
                    extra = []
                    if kind == "slc":
                        if qt >= 8:
                            extra.append(("sel", kt))
                        if kt == qt:
                            extra.append(("cb", None))
                    else:
                        if kt == qt:
                            extra.append(("cb", None))
                        if kt == qt - 4:
                            extra.append(("wb", None))
                    P.op("tensor", lambda e, s_=s_, kt=kt, kT_=kT_: e.matmul(as4(s_[:]), lhsT=kT_[:, kt * 128:(kt + 1) * 128], rhs=rhs4(qr_, qi),
                                                                            start=True, stop=(len(extra) == 0)),
                         reads=[kT_, qr_], writes=[s_], signal=(len(extra) == 0))
                    for xi, (xk, xa) in enumerate(extra):
                        last = (xi == len(extra) - 1)
                        if xk == "sel":
                            P.op("tensor", lambda e, s_=s_, xa=xa, last=last: e.matmul(as4(s_[:]), lhsT=esel[:, xa * 128:(xa + 1) * 128],
                                                                                      rhs=bcast4(biasT[:], 64), start=False, stop=last),
                                 reads=[esel, biasT], writes=[s_], signal=last)
                        else:
                            mk = cb if xk == "cb" else wbm
                            P.op("tensor", lambda e, s_=s_, mk=mk, last=last: e.matmul(as4(s_[:]), lhsT=identb[:], rhs=bcast4(mk[:], 128),
                                                                                      start=False, stop=last),
                                 reads=[identb, mk], writes=[s_], signal=last)
                    p_ = pT[cnt["pt"] % 3]
                    cnt["pt"] += 1
                    P.op("scalar", lambda e, s_=s_, p_=p_: e.activation(out=p_[:], in_=s_[:], func=AF.Exp, scale=SCALE), reads=[s_], writes=[p_])
                    P.op("tensor", lambda e, p_=p_, st=st, ii=ii: e.matmul(BD[st][:], lhsT=onesb[:], rhs=p_[:], start=(ii == 0), stop=(ii == nk - 1)),
                         reads=[onesb, p_], writes=[BD[st]], signal=(ii == nk - 1))
                    P.op("tensor", lambda e, p_=p_, st=st, ii=ii, kt=kt, v_=v_: e.matmul(BO[st][:], lhsT=v_[:, kt, :], rhs=p_[:], start=(ii == 0), stop=(ii == nk - 1)),
                         reads=[v_, p_], writes=[BO[st]], signal=(ii == nk - 1))
                w2 = W[cnt["w"] % 2]
                P.op("vector", lambda e, w2=w2, st=st: e.reciprocal(out=w2[:], in_=BD[st][:]), reads=[BD[st]], writes=[w2])
                return st, w2

            st, w2 = branch(ksT, vs, list(range(0, qt + 1)), "slc")
            gates(1)
            combine(st, False, w2)
            st, w2 = branch(kwT, vw, list(range(max(0, qt - 4), qt + 1)), "win")
            gates(2)
            combine(st, False, w2)
            ac = acc[qt % 2]
            ab = accb[qt % 2]
            P.op("gpsimd", lambda e, ac=ac, ab=ab: e.tensor_copy(out=ab[:], in_=ac[:]), reads=[ac], writes=[ab])
            P.dma("sync", k.mixT[hk * 512:(hk + 1) * 512, tsl].rearrange("(g d) t -> d g t", d=128), as4(ab[:]), reads=[ab])
    P.flush()
    P.release(m0)


def phase_gla(k, l):
    P = k.P
    m0 = P.mark()
    GS = 128 ** -0.5
    wa2 = P.sb("wa2", [16, 512], BF16)
    brow = P.sb("brow", [1, 512], BF16)
    ones1 = P.sb("ones1", [1, 128], BF16)
    tri2f = P.sb("tri2f", [128, 128], F32)
    suf = P.sb("suf", [128, 128], F32)
    onesb = P.sb("onesb", [128, 128], BF16)
    nw = P.sb("nw", [128, 2], F32)
    P.dma("gpsimd", wa2[:], k.gla_w_a2[l], writes=[wa2])
    P.dma("gpsimd", brow[:], k.gla_b_a[l:l + 1, :], writes=[brow])
    P.dma("sync", tri2f[:], k.tri2, writes=[tri2f])
    P.dma("sync", suf[:], k.sumat, writes=[suf])
    P.dma("sync", nw[:], k.gla_nwT[l], writes=[nw])
    P.op("vector", lambda e: e.memset(ones1[:], 1.0), writes=[ones1])
    P.op("vector", lambda e: e.memset(onesb[:], 1.0), writes=[onesb])
    Sf = P.sb("Sf", [128, 4, 256], F32)
    Sb = P.sb("Sb", [128, 4, 256], BF16)
    Sfv = [P.view(Sf) for _ in range(4)]
    Sbv = [P.view(Sb) for _ in range(4)]
    for h in range(4):
        P.op("vector", lambda e, h=h: e.memset(Sf[:, h, :], 0.0), writes=[Sfv[h]])
        P.op("vector", lambda e, h=h: e.memset(Sb[:, h, :], 0.0), writes=[Sbv[h]])
    gqTb = [P.sb("gqTb%d" % i, [128, 4, 512], BF16) for i in range(2)]
    gkTb = [P.sb("gkTb%d" % i, [128, 4, 512], BF16) for i in range(2)]
    ggb = [P.sb("ggb%d" % i, [128, 8, 512], BF16) for i in range(2)]
    gaTb = [P.sb("gaTb%d" % i, [16, 512], BF16) for i in range(2)]
    gkt = [P.sb("gkt%d" % i, [128, 512], BF16) for i in range(2)]
    gvt = [P.sb("gvt%d" % i, [128, 1024], BF16) for i in range(2)]
    Lt = [P.sb("Lt%d" % i, [128, 512], F32) for i in range(2)]
    E1 = [P.sb("E1%d" % i, [128, 512], F32) for i in range(2)]
    kst = [P.sb("kst%d" % i, [128, 512], BF16) for i in range(2)]
    EbT = [P.sb("EbT%d" % i, [128, 128], F32) for i in range(2)]
    EnbT = [P.sb("EnbT%d" % i, [128, 128], F32) for i in range(2)]
    qdT = [P.sb("qdT%d" % i, [128, 128], BF16) for i in range(2)]
    kiT = [P.sb("kiT%d" % i, [128, 128], BF16) for i in range(2)]
    ATm = [P.sb("ATm%d" % i, [128, 128], BF16) for i in range(2)]
    o1 = [P.sb("o1%d" % i, [128, 256], F32) for i in range(2)]
    sq = [P.sb("sq%d" % i, [128, 256], BF16) for i in range(2)]
    lnr = [P.sb("lnr%d" % i, [128, 128], F32) for i in range(2)]
    rstd = [P.sb("rstd%d" % i, [128, 128], F32) for i in range(2)]
    tmpo = [P.sb("tmpo%d" % i, [128, 256], F32) for i in range(2)]
    outb = [P.sb("outb%d" % i, [128, 2, 128], BF16) for i in range(2)]
    pz = P.ps("pz", [128, 512])
    pcs = P.ps("pcs", [128, 512])
    psu = P.ps("psu", [128, 512])
    pcsT = P.ps("pcsT", [128, 512])
    pAT = P.ps("pAT", [128, 512])
    po = P.ps("po", [128, 512])
    pS = P.ps("pS", [128, 512])
    pss = P.ps("pss", [128, 512])
    hc = 0
    for tt in range(T // 128):
        tb, ti = tt // 4, tt % 4
        gq_, gk_, gg_, ga_ = gqTb[tb % 2], gkTb[tb % 2], ggb[tb % 2], gaTb[tb % 2]
        if ti == 0:
            tok = slice(tb * 512, (tb + 1) * 512)
            P.dma("sync", gq_[:], k.gqT[:, tok].rearrange("(h d) t -> d h t", d=128), writes=[gq_])
            P.dma("sync", gk_[:], k.gkT[:, tok].rearrange("(h d) t -> d h t", d=128), writes=[gk_])
            P.dma("sync", gg_[:], k.ggT[:, tok].rearrange("(c d) t -> d c t", d=128), writes=[gg_])
            P.dma("sync", ga_[:], k.gaT[:, tok], writes=[ga_])
        lsl = slice(ti * 128, (ti + 1) * 128)
        tsl = slice(tt * 128, (tt + 1) * 128)
        gkt_, gvt_ = gkt[tt % 2], gvt[tt % 2]
        P.dma("sync", gkt_[:], k.gk[tsl, :], writes=[gkt_])
        P.dma("sync", gvt_[:], k.gv[tsl, :], writes=[gvt_])
        L_, E1_, kst_ = Lt[tt % 2], E1[tt % 2], kst[tt % 2]
        P.op("tensor", lambda e: e.matmul(pz[:], lhsT=ga_[:, lsl], rhs=wa2[:], start=True, stop=False), reads=[ga_, wa2], writes=[pz], signal=False)
        P.op("tensor", lambda e: e.matmul(pz[:], lhsT=ones1[:], rhs=brow[:], start=False, stop=True), reads=[ones1, brow], writes=[pz])
        P.op("scalar", lambda e: e.activation(out=L_[:], in_=pz[:], func=AF.Exp, scale=-1.0), reads=[pz], writes=[L_])
        P.op("scalar", lambda e: e.activation(out=L_[:], in_=L_[:], func=AF.Ln, bias=1.0), reads=[L_], writes=[L_])
        P.op("tensor", lambda e: e.matmul(pcs[:], lhsT=tri2f[:], rhs=L_[:], start=True, stop=True), reads=[tri2f, L_], writes=[pcs])
        P.op("tensor", lambda e: e.matmul(psu[:], lhsT=suf[:], rhs=L_[:], start=True, stop=True), reads=[suf, L_], writes=[psu])
        P.op("scalar", lambda e: e.activation(out=E1_[:], in_=psu[:], func=AF.Exp, scale=-1.0 / 16.0), reads=[psu], writes=[E1_])
        P.op("vector", lambda e: e.tensor_tensor(out=kst_[:], in0=gkt_[:], in1=E1_[:], op=ALU.mult), reads=[gkt_, E1_], writes=[kst_])
        for h in range(4):
            i2 = hc % 2
            hc += 1
            Eb_, Enb_, qd_, ki_, AT_ = EbT[i2], EnbT[i2], qdT[i2], kiT[i2], ATm[i2]
            o1_, sq_, lnr_, rs_, tp_, ob_ = o1[i2], sq[i2], lnr[i2], rstd[i2], tmpo[i2], outb[i2]
            hs = slice(h * 128, (h + 1) * 128)
            P.op("tensor", lambda e: e.matmul(pcsT[:, 0:128], lhsT=L_[:, hs], rhs=tri2f[:], start=True, stop=True),
                 reads=[L_, tri2f], writes=[pcsT])
            P.op("scalar", lambda e: e.activation(out=Eb_[:], in_=pcsT[:, 0:128], func=AF.Exp, scale=-1.0 / 16.0), reads=[pcsT], writes=[Eb_])
            P.op("scalar", lambda e: e.activation(out=Enb_[:], in_=pcsT[:, 0:128], func=AF.Exp, scale=1.0 / 16.0), reads=[pcsT], writes=[Enb_])
            P.op("vector", lambda e: e.scalar_tensor_tensor(out=qd_[:], in0=gq_[:, h, lsl], scalar=GS, in1=Eb_[:], op0=ALU.mult, op1=ALU.mult),
                 reads=[gq_, Eb_], writes=[qd_])
            P.op("gpsimd", lambda e: e.tensor_tensor(out=ki_[:], in0=gk_[:, h, lsl], in1=Enb_[:], op=ALU.mult), reads=[gk_, Enb_], writes=[ki_])
            P.op("tensor", lambda e: e.matmul(pAT[:, 0:128], lhsT=ki_[:], rhs=qd_[:], start=True, stop=True), reads=[ki_, qd_], writes=[pAT])
            P.op("vector", lambda e: e.tensor_tensor(out=AT_[:], in0=pAT[:, 0:128], in1=tri2f[:], op=ALU.mult), reads=[pAT, tri2f], writes=[AT_])
            for hf in range(2):
                cs_ = slice(hf * 64, (hf + 1) * 64)
                for dvc in range(2):
                    oc = slice(dvc * 128 + hf * 64, dvc * 128 + hf * 64 + 64)
                    P.op("tensor", lambda e: e.matmul(po[:, oc], lhsT=gvt_[:, h * 256 + dvc * 128:h * 256 + dvc * 128 + 128], rhs=AT_[:, cs_],
                                                      start=True, stop=False), reads=[gvt_, AT_], writes=[po], signal=False)
                    P.op("tensor", lambda e: e.matmul(po[:, oc], lhsT=Sb[:, h, dvc * 128:(dvc + 1) * 128], rhs=qd_[:, cs_],
                                                      start=False, stop=True), reads=[Sbv[h], qd_], writes=[po],
                         signal=(hf == 1 and dvc == 1))
                P.op("tensor", lambda e: e.matmul(pS[:, 0:256], lhsT=kst_[cs_, hs], rhs=gvt_[cs_, h * 256:(h + 1) * 256], start=True, stop=True),
                     reads=[kst_, gvt_], writes=[pS])
                col = hf * 64 + 63
                P.op("vector", lambda e: e.scalar_tensor_tensor(out=Sf[:, h, :], in0=Sf[:, h, :], scalar=Eb_[:, col:col + 1], in1=pS[:, 0:256],
                                                                op0=ALU.mult, op1=ALU.add), reads=[Sfv[h], Eb_, pS], writes=[Sfv[h]])
                P.op("gpsimd", lambda e: e.tensor_copy(out=Sb[:, h, :], in_=Sf[:, h, :]), reads=[Sfv[h]], writes=[Sbv[h]])
            P.op("scalar", lambda e: e.copy(out=o1_[:], in_=po[:, 0:256]), reads=[po], writes=[o1_])
            P.op("gpsimd", lambda e: e.tensor_tensor(out=sq_[:], in0=o1_[:], in1=o1_[:], op=ALU.mult), reads=[o1_], writes=[sq_])
            P.op("tensor", lambda e: e.matmul(pss[:, 0:128], lhsT=onesb[:], rhs=sq_[:, 0:128], start=True, stop=False), reads=[onesb, sq_], writes=[pss], signal=False)
            P.op("tensor", lambda e: e.matmul(pss[:, 0:128], lhsT=onesb[:], rhs=sq_[:, 128:256], start=False, stop=True), reads=[onesb, sq_], writes=[pss])
            P.op("scalar", lambda e: e.activation(out=lnr_[:], in_=pss[:, 0:128], func=AF.Ln, scale=1.0 / 256.0, bias=NORM_EPS), reads=[pss], writes=[lnr_])
            P.op("scalar", lambda e: e.activation(out=rs_[:], in_=lnr_[:], func=AF.Exp, scale=-0.5), reads=[lnr_], writes=[rs_])
            for dvc in range(2):
                ds_ = slice(dvc * 128, (dvc + 1) * 128)
                P.op("vector", lambda e: e.tensor_tensor(out=tp_[:, ds_], in0=o1_[:, ds_], in1=rs_[:], op=ALU.mult), reads=[o1_, rs_], writes=[tp_])
                P.op("vector", lambda e: e.scalar_tensor_tensor(out=ob_[:, dvc, :], in0=gg_[:, h * 2 + dvc, lsl], scalar=nw[:, dvc:dvc + 1], in1=tp_[:, ds_],
                                                                op0=ALU.mult, op1=ALU.mult), reads=[gg_, nw, tp_], writes=[ob_])
            P.dma("sync", k.mixT[1024 + h * 256:1024 + (h + 1) * 256, tsl].rearrange("(c d) t -> d c t", d=128), ob_[:], reads=[ob_])
    P.flush()
    P.release(m0)


def ln_tile(P, r, lng, lnb, stats, mv, rstd):
    for c in range(4):
        P.op("vector", lambda e, c=c: e.bn_stats(out=stats[:, c, :], in_=r[:, c * 512:(c + 1) * 512]), reads=[r], writes=[stats])
    P.op("vector", lambda e: e.bn_aggr(out=mv[:], in_=stats[:]), reads=[stats], writes=[mv])
    P.op("scalar", lambda e: e.activation(out=rstd[:], in_=mv[:, 1:2], func=AF.Sqrt, bias=LN_EPS), reads=[mv], writes=[rstd])
    P.op("vector", lambda e: e.reciprocal(out=rstd[:], in_=rstd[:]), reads=[rstd], writes=[rstd])
    P.op("vector", lambda e: e.tensor_scalar(out=r[:], in0=r[:], scalar1=mv[:, 0:1], scalar2=rstd[:, 0:1], op0=ALU.subtract, op1=ALU.mult),
         reads=[r, mv, rstd], writes=[r])
    P.op("gpsimd", lambda e: e.tensor_tensor(out=r[:], in0=r[:], in1=lng[:], op=ALU.mult), reads=[r, lng], writes=[r])
    P.op("gpsimd", lambda e: e.tensor_tensor(out=r[:], in0=r[:], in1=lnb[:], op=ALU.add), reads=[r, lnb], writes=[r])


def phase_wout(k, l, xsrc, xdst):
    P = k.P
    m0 = P.mark()
    wo = P.sb("wo", [128, KC, D], BF16)
    wv = k.w_out[l].rearrange("(c p) n -> p c n", p=128)
    for q in range(4):
        P.dma("gpsimd", wo[:, :, q * 512:(q + 1) * 512], wv[:, :, q * 512:(q + 1) * 512], writes=[wo])
    garep = P.sb("garep", [128, D], F32)
    lng = P.sb("lng", [128, D], F32)
    lnb = P.sb("lnb", [128, D], F32)
    P.dma("sync", garep[:], k.modv[l, 2 * D:3 * D].partition_broadcast(128), writes=[garep])
    P.dma("sync", lng[:], k.ln_mix_g[l].partition_broadcast(128), writes=[lng])
    P.dma("sync", lnb[:], k.ln_mix_b[l].partition_broadcast(128), writes=[lnb])
    mixb = [P.sb("mixb%d" % i, [128, KC, 512], BF16) for i in range(2)]
    xt = [P.sb("xt%d" % i, [128, D], F32) for i in range(2)]
    rt = [P.sb("rt%d" % i, [128, D], F32) for i in range(2)]
    stats = P.sb("stats", [128, 4, 6], F32)
    mv = P.sb("mv", [128, 2], F32)
    rstd = P.sb("rstd", [128, 1], F32)
    py = [P.ps("py%d" % i, [128, 512]) for i in range(8)]
    mixv = k.mixT.rearrange("(c p) t -> p c t", p=128)
    for tt in range(T // 128):
        tb, ti = tt // 4, tt % 4
        mb_ = mixb[tb % 2]
        if ti == 0:
            for q in range(4):
                P.dma("sync", mb_[:, q * 4:(q + 1) * 4, :], mixv[:, q * 4:(q + 1) * 4, tb * 512:(tb + 1) * 512], writes=[mb_])
        x_ = xt[tt % 2]
        r_ = rt[tt % 2]
        tsl = slice(tt * 128, (tt + 1) * 128)
        P.dma("sync", x_[:], xsrc[tsl, :], writes=[x_])
        for db in range(4):
            p_ = py[(tt % 2) * 4 + db]
            for c in range(KC):
                P.op("tensor", lambda e: e.matmul(p_[:], lhsT=mb_[:, c, ti * 128:(ti + 1) * 128], rhs=wo[:, c, db * 512:(db + 1) * 512],
                                                  start=(c == 0), stop=(c == KC - 1)), reads=[mb_, wo], writes=[p_], signal=(c == KC - 1))
            P.op("vector", lambda e: e.tensor_tensor(out=r_[:, db * 512:(db + 1) * 512], in0=p_[:], in1=garep[:, db * 512:(db + 1) * 512], op=ALU.mult),
                 reads=[p_, garep], writes=[r_])
        P.op("vector", lambda e: e.scalar_tensor_tensor(out=r_[:], in0=x_[:], scalar=ALPHA, in1=r_[:], op0=ALU.mult, op1=ALU.add),
             reads=[x_, r_], writes=[r_])
        ln_tile(P, r_, lng, lnb, stats, mv, rstd)
        P.dma("sync", xdst[tsl, :], r_[:], reads=[r_])
    P.flush()
    P.release(m0)


def ffn_gateup(k, src, nrows, wg, wu, dff, AT, mod=None, src_bf16=False):
    P = k.P
    RH = min(nrows, 2048)
    for r0 in range(0, nrows, RH):
        nr = min(RH, nrows - r0)
        m0 = P.mark()
        identf = P.sb("identf", [128, 128], F32)
        P.dma("sync", identf[:], k.ident, writes=[identf])
        hT = P.sb("hT", [128, KC, RH], BF16)
        hviews = [(P.view(hT), P.view(hT)) for _ in range(nr // 128)]
        ptr = [P.ps("ptr%d" % i, [128, 4, 128]) for i in range(2)]
        if mod is not None:
            l = mod
            screp = P.sb("screp", [128, D], F32)
            shrep = P.sb("shrep", [128, D], F32)
            xt = [P.sb("xt%d" % i, [128, D], F32) for i in range(2)]
            P.dma("sync", screp[:], k.modv[l, 4 * D:5 * D].partition_broadcast(128), writes=[screp])
            P.dma("sync", shrep[:], k.modv[l, 3 * D:4 * D].partition_broadcast(128), writes=[shrep])
            build_hT(k, src, r0, nr // 128, screp, shrep, identf, hT, hviews, xt, ptr)
        else:
            identb = P.sb("identb", [128, 128], BF16)
            P.dma("gpsimd", identb[:], k.ident, writes=[identb])
            xt = [P.sb("xtb%d" % i, [128, D], BF16) for i in range(2)]
            for j in range(nr // 128):
                xb = xt[j % 2]
                P.dma("sync", xb[:], src[r0 + j * 128:r0 + (j + 1) * 128, :], writes=[xb])
                for g in range(KC // 4):
                    pt = ptr[(j * 4 + g) % 2]
                    ptb = pt[:].rearrange("p q t -> p (q t)")[:, 0:256].bitcast(BF16).rearrange("p (q t) -> p q t", q=4)
                    for q in range(4):
                        c = g * 4 + q
                        P.op("tensor", lambda e: e.transpose(out=ptb[:, q, :], in_=xb[:, c * 128:(c + 1) * 128], identity=identb[:]),
                             reads=[xb, identb], writes=[pt], signal=(q == 3))
                    if g % 2 == 0:
                        P.op("scalar", lambda e: e.copy(out=hT[:, g * 4:(g + 1) * 4, j * 128:(j + 1) * 128], in_=ptb), reads=[pt], writes=[hviews[j][0]])
                    else:
                        P.op("vector", lambda e: e.tensor_copy(out=hT[:, g * 4:(g + 1) * 4, j * 128:(j + 1) * 128], in_=ptb), reads=[pt], writes=[hviews[j][1]])
        wgb = [P.sb("wgb%d" % i, [128, KC, 256], BF16) for i in range(2)]
        wub = [P.sb("wub%d" % i, [128, KC, 256], BF16) for i in range(2)]
        pg = [P.ps("pg%d" % i, [128, 512]) for i in range(2)]
        pu = [P.ps("pu%d" % i, [128, 512]) for i in range(2)]
        sgb = [P.sb("sgb%d" % i, [128, 512], BF16) for i in range(2)]
        ab = [P.sb("ab%d" % i, [128, 512], BF16) for i in range(3)]
        wgv = wg.rearrange("(c p) n -> p c n", p=128)
        wuv = wu.rearrange("(c p) n -> p c n", p=128)
        blocks = [(b0, min(512, nr - b0)) for b0 in range(0, nr, 512)]
        it = 0
        for fb in range(dff // 256):
            wg_, wu_ = wgb[fb % 2], wub[fb % 2]
            P.dma("gpsimd", wg_[:], wgv[:, :, fb * 256:(fb + 1) * 256], writes=[wg_])
            P.dma("gpsimd", wu_[:], wuv[:, :, fb * 256:(fb + 1) * 256], writes=[wu_])
            for ft in range(2):
                f0 = fb * 256 + ft * 128
                for (b0, bn) in blocks:
                    hr = []
                    for j in range(b0 // 128, (b0 + bn) // 128):
                        hr += [hviews[j][0], hviews[j][1]]
                    pg_, pu_ = pg[it % 2], pu[it % 2]
                    sg_, a_ = sgb[it % 2], ab[it % 3]
                    it += 1
                    for c in range(KC):
                        P.op("tensor", lambda e: e.matmul(pg_[:, 0:bn], lhsT=wg_[:, c, ft * 128:(ft + 1) * 128], rhs=hT[:, c, b0:b0 + bn],
                                                          start=(c == 0), stop=(c == KC - 1)), reads=[wg_] + hr, writes=[pg_], signal=(c == KC - 1))
                    for c in range(KC):
                        P.op("tensor", lambda e: e.matmul(pu_[:, 0:bn], lhsT=wu_[:, c, ft * 128:(ft + 1) * 128], rhs=hT[:, c, b0:b0 + bn],
                                                          start=(c == 0), stop=(c == KC - 1)), reads=[wu_] + hr, writes=[pu_], signal=(c == KC - 1))
                    P.op("scalar", lambda e: e.activation(out=sg_[:, 0:bn], in_=pg_[:, 0:bn], func=AF.Silu), reads=[pg_], writes=[sg_])
                    P.op("vector", lambda e: e.tensor_tensor(out=a_[:, 0:bn], in0=pu_[:, 0:bn], in1=sg_[:, 0:bn], op=ALU.mult), reads=[pu_, sg_], writes=[a_])
                    P.dma("sync", AT[f0:f0 + 128, r0 + b0:r0 + b0 + bn], a_[:, 0:bn], reads=[a_])
        P.flush()
        P.release(m0)


class _PV:
    def __init__(self, b):
        self.b = b

    def __getitem__(self, idx):
        return self.b.t[:].rearrange("p (q t) -> p q t", q=4)[idx]


def ffn_down(k, AT, nrows, wd, dff, Y, row_off=0):
    P = k.P
    m0 = P.mark()
    FC = dff // 128
    G = 4
    assert FC % G == 0
    wdb = [P.sb("wdb%d" % i, [128, FC, 512], BF16) for i in range(1)]
    atb = [P.sb("atb%d" % i, [128, FC, 256], BF16) for i in range(2)]
    yb = [P.sb("yb%d" % i, [128, 512], F32) for i in range(3)]
    py = [P.ps("pyd%d" % i, [128, 512]) for i in range(3)]
    wdv = wd.rearrange("(c p) n -> p c n", p=128)
    atv = AT.rearrange("(c p) t -> p c t", p=128)
    it = 0
    ib = 0
    for db in range(4):
        w_ = wdb[0]
        for q in range(G):
            cs = slice(q * (FC // G), (q + 1) * (FC // G))
            P.dma("gpsimd", w_[:, cs, :], wdv[:, cs, db * 512:(db + 1) * 512], writes=[w_])
        for b0 in range(0, nrows, 256):
            bn = min(256, nrows - b0)
            a_ = atb[ib % 2]
            ib += 1
            for q in range(G):
                cs = slice(q * (FC // G), (q + 1) * (FC // G))
                P.dma("sync", a_[:, cs, 0:bn], atv[:, cs, b0:b0 + bn], writes=[a_])
            for j in range(bn // 128):
                p_ = py[it % 3]
                y_ = yb[it % 3]
                it += 1
                for c in range(FC):
                    P.op("tensor", lambda e: e.matmul(p_[:], lhsT=a_[:, c, j * 128:(j + 1) * 128], rhs=w_[:, c, :], start=(c == 0), stop=(c == FC - 1)),
                         reads=[a_, w_], writes=[p_], signal=(c == FC - 1))
                P.op("scalar", lambda e: e.copy(out=y_[:], in_=p_[:]), reads=[p_], writes=[y_])
                rs = slice(row_off + b0 + j * 128, row_off + b0 + (j + 1) * 128)
                P.dma("sync", Y[rs, db * 512:(db + 1) * 512], y_[:], reads=[y_])
    P.flush()
    P.release(m0)


def phase_ln2(k, l, xsrc, ysrc, xdst):
    P = k.P
    m0 = P.mark()
    gfrep = P.sb("gfrep", [128, D], F32)
    lng = P.sb("lng", [128, D], F32)
    lnb = P.sb("lnb", [128, D], F32)
    P.dma("sync", gfrep[:], k.modv[l, 5 * D:6 * D].partition_broadcast(128), writes=[gfrep])
    P.dma("sync", lng[:], k.ln_ffn_g[l].partition_broadcast(128), writes=[lng])
    P.dma("sync", lnb[:], k.ln_ffn_b[l].partition_broadcast(128), writes=[lnb])
    xt = [P.sb("xt%d" % i, [128, D], F32) for i in range(2)]
    rt = [P.sb("rt%d" % i, [128, D], F32) for i in range(2)]
    stats = P.sb("stats", [128, 4, 6], F32)
    mv = P.sb("mv", [128, 2], F32)
    rstd = P.sb("rstd", [128, 1], F32)
    for tt in range(T // 128):
        x_, r_ = xt[tt % 2], rt[tt % 2]
        tsl = slice(tt * 128, (tt + 1) * 128)
        P.dma("sync", x_[:], xsrc[tsl, :], writes=[x_])
        P.dma("gpsimd", r_[:], ysrc[tsl, :], writes=[r_])
        P.op("vector", lambda e: e.tensor_tensor(out=r_[:], in0=r_[:], in1=gfrep[:], op=ALU.mult), reads=[r_, gfrep], writes=[r_])
        P.op("vector", lambda e: e.scalar_tensor_tensor(out=r_[:], in0=x_[:], scalar=ALPHA, in1=r_[:], op0=ALU.mult, op1=ALU.add),
             reads=[x_, r_], writes=[r_])
        ln_tile(P, r_, lng, lnb, stats, mv, rstd)
        P.dma("sync", xdst[tsl, :], r_[:], reads=[r_])
    P.flush()
    P.release(m0)


CAP = 768
TM = 2048
NSLOT = NE * CAP
BIGIDX = 1.0e6


def phase_moe_route(k, l):
    P = k.P
    m0 = P.mark()
    zt = P.sb("zt", [128, D], BF16)
    P.op("vector", lambda e: e.memset(zt[:], 0.0), writes=[zt])
    for s0 in range(0, NSLOT, 128):
        P.dma("sync" if (s0 // 128) % 2 == 0 else "gpsimd", k.Xs[s0:s0 + 128, :], zt[:], reads=[zt])
    P.flush()
    P.release(m0)

    m0 = P.mark()
    screp = P.sb("screp", [128, D], F32)
    shrep = P.sb("shrep", [128, D], F32)
    identf = P.sb("identf", [128, 128], F32)
    wr = P.sb("wr", [128, KC, NE], F32)
    SLb = P.sb("SLb", [128, 128], BF16)
    onesb = P.sb("onesb", [128, 128], BF16)
    eoff = P.sb("eoff", [128, NE], F32)
    base = P.sb("base", [128, NE], F32)
    P.dma("sync", screp[:], k.modv[l, 4 * D:5 * D].partition_broadcast(128), writes=[screp])
    P.dma("sync", shrep[:], k.modv[l, 3 * D:4 * D].partition_broadcast(128), writes=[shrep])
    P.dma("sync", identf[:], k.ident, writes=[identf])
    P.dma("sync", wr[:], k.moe_router[l // 2].rearrange("(c p) e -> p c e", p=128), writes=[wr])
    P.dma("gpsimd", SLb[:], k.slmat, writes=[SLb])
    P.dma("sync", eoff[:], k.eoff, writes=[eoff])
    P.op("vector", lambda e: e.memset(onesb[:], 1.0), writes=[onesb])
    P.op("vector", lambda e: e.memset(base[:], 0.0), writes=[base])
    xt = [P.sb("xt%d" % i, [128, D], F32) for i in range(2)]
    hb = [P.sb("hb%d" % i, [128, D], BF16) for i in range(2)]
    hTf = [P.sb("hTf%d" % i, [128, KC, 128], F32) for i in range(2)]
    ptr = [P.ps("ptr%d" % i, [128, 4, 128]) for i in range(2)]
    plog = P.ps("plog", [128, 512])
    pcum = P.ps("pcum", [128, 512])
    sm = {}
    for nm in ("lg", "m8", "sel", "sel1", "sel2", "ex", "exs", "comb", "tmp8", "pos", "dest", "valid", "selb"):
        sm[nm] = [P.sb(nm + "%d" % i, [128, NE], BF16 if nm == "selb" else F32) for i in range(2)]
    c1 = {}
    for nm in ("nm1", "den", "rden"):
        c1[nm] = [P.sb(nm + "%d" % i, [128, 1], F32) for i in range(2)]
    wts = [P.sb("wts%d" % i, [128, 2], F32) for i in range(2)]
    dd = [P.sb("dd%d" % i, [128, 2], F32) for i in range(2)]
    idx = [P.sb("idx%d" % i, [128, 2], I32) for i in range(2)]
    tix = [P.sb("tix%d" % i, [128, 1], I32) for i in range(2)]
    for tt in range(TM // 128):
        i2 = tt % 2
        x_, hb_, hT_ = xt[i2], hb[i2], hTf[i2]
        tsl = slice(tt * 128, (tt + 1) * 128)
        P.dma("sync", tix[i2][:], k.tokidx[tsl, :], writes=[tix[i2]])
        P.gather(x_[:], k.x1, tix[i2][:, 0:1], reads=[tix[i2]], writes=[x_], bounds_check=T - 1, oob_is_err=False)
        P.op("vector", lambda e: e.tensor_tensor(out=x_[:], in0=x_[:], in1=screp[:], op=ALU.mult), reads=[x_, screp], writes=[x_])
        P.op("gpsimd", lambda e: e.tensor_tensor(out=x_[:], in0=x_[:], in1=shrep[:], op=ALU.add), reads=[x_, shrep], writes=[x_])
        P.op("scalar", lambda e: e.copy(out=hb_[:], in_=x_[:]), reads=[x_], writes=[hb_])
        for g in range(KC // 4):
            pt = ptr[g % 2]
            for q in range(4):
                c = g * 4 + q
                P.op("tensor", lambda e: e.transpose(out=pt[:, q, :], in_=x_[:, c * 128:(c + 1) * 128], identity=identf[:]),
                     reads=[x_, identf], writes=[pt], signal=(q == 3))
            if g % 2 == 0:
                P.op("scalar", lambda e: e.copy(out=hT_[:, g * 4:(g + 1) * 4, :], in_=pt[:]), reads=[pt], writes=[hT_])
            else:
                P.op("vector", lambda e: e.tensor_copy(out=hT_[:, g * 4:(g + 1) * 4, :], in_=pt[:]), reads=[pt], writes=[hT_])
        for c in range(KC):
            P.op("tensor", lambda e: e.matmul(plog[:, 0:NE], lhsT=hT_[:, c, :], rhs=wr[:, c, :], start=(c == 0), stop=(c == KC - 1)),
                 reads=[hT_, wr], writes=[plog], signal=(c == KC - 1))
        S = {n: v[i2] for n, v in sm.items()}
        C1 = {n: v[i2] for n, v in c1.items()}
        w_, d_, ix_ = wts[i2], dd[i2], idx[i2]
        V = "vector"
        P.op(V, lambda e: e.tensor_copy(out=S["lg"][:], in_=plog[:, 0:NE]), reads=[plog], writes=[S["lg"]])
        P.op(V, lambda e: e.max(out=S["m8"][:], in_=S["lg"][:]), reads=[S["lg"]], writes=[S["m8"]])
        P.op(V, lambda e: e.tensor_scalar(out=S["sel"][:], in0=S["lg"][:], scalar1=S["m8"][:, 1:2], scalar2=None, op0=ALU.is_ge),
             reads=[S["lg"], S["m8"]], writes=[S["sel"]])
        P.op(V, lambda e: e.tensor_scalar(out=S["sel1"][:], in0=S["lg"][:], scalar1=S["m8"][:, 0:1], scalar2=None, op0=ALU.is_ge),
             reads=[S["lg"], S["m8"]], writes=[S["sel1"]])
        P.op(V, lambda e: e.tensor_tensor(out=S["sel2"][:], in0=S["sel"][:], in1=S["sel1"][:], op=ALU.subtract),
             reads=[S["sel"], S["sel1"]], writes=[S["sel2"]])
        P.op(V, lambda e: e.tensor_scalar(out=C1["nm1"][:], in0=S["m8"][:, 0:1], scalar1=-1.0, scalar2=None, op0=ALU.mult),
             reads=[S["m8"]], writes=[C1["nm1"]])
        P.op("scalar", lambda e: e.activation(out=S["ex"][:], in_=S["lg"][:], func=AF.Exp, bias=C1["nm1"][:, 0:1]),
             reads=[S["lg"], C1["nm1"]], writes=[S["ex"]])
        P.op(V, lambda e: e.tensor_tensor(out=S["exs"][:], in0=S["ex"][:], in1=S["sel"][:], op=ALU.mult), reads=[S["ex"], S["sel"]], writes=[S["exs"]])
        P.op(V, lambda e: e.reduce_sum(out=C1["den"][:], in_=S["exs"][:], axis=AX.X), reads=[S["exs"]], writes=[C1["den"]])
        P.op(V, lambda e: e.reciprocal(out=C1["rden"][:], in_=C1["den"][:]), reads=[C1["den"]], writes=[C1["rden"]])
        P.op(V, lambda e: e.tensor_scalar(out=S["comb"][:], in0=S["exs"][:], scalar1=C1["rden"][:, 0:1], scalar2=None, op0=ALU.mult),
             reads=[S["exs"], C1["rden"]], writes=[S["comb"]])
        for j, sn in enumerate(("sel1", "sel2")):
            P.op(V, lambda e: e.tensor_tensor(out=S["tmp8"][:], in0=S["comb"][:], in1=S[sn][:], op=ALU.mult), reads=[S["comb"], S[sn]], writes=[S["tmp8"]])
            P.op(V, lambda e: e.reduce_sum(out=w_[:, j:j + 1], in_=S["tmp8"][:], axis=AX.X), reads=[S["tmp8"]], writes=[w_])
        P.op(V, lambda e: e.tensor_copy(out=S["selb"][:], in_=S["sel"][:]), reads=[S["sel"]], writes=[S["selb"]])
        P.op("tensor", lambda e: e.matmul(pcum[:, 0:NE], lhsT=SLb[:], rhs=S["selb"][:], start=True, stop=True), reads=[SLb, S["selb"]], writes=[pcum], signal=False)
        P.op("tensor", lambda e: e.matmul(pcum[:, NE:2 * NE], lhsT=onesb[:], rhs=S["selb"][:], start=True, stop=True), reads=[onesb, S["selb"]], writes=[pcum])
        P.op(V, lambda e: e.tensor_tensor(out=S["pos"][:], in0=pcum[:, 0:NE], in1=base[:], op=ALU.add), reads=[pcum, base], writes=[S["pos"]])
        P.op(V, lambda e: e.tensor_tensor(out=base[:], in0=pcum[:, NE:2 * NE], in1=base[:], op=ALU.add), reads=[pcum, base], writes=[base])
        P.op(V, lambda e: e.tensor_scalar(out=S["valid"][:], in0=S["pos"][:], scalar1=float(CAP), scalar2=None, op0=ALU.is_lt),
             reads=[S["pos"]], writes=[S["valid"]])
        P.op(V, lambda e: e.tensor_tensor(out=S["dest"][:], in0=S["pos"][:], in1=eoff[:], op=ALU.add), reads=[S["pos"], eoff], writes=[S["dest"]])
        P.op(V, lambda e: e.scalar_tensor_tensor(out=S["dest"][:], in0=S["dest"][:], scalar=-BIGIDX, in1=S["valid"][:], op0=ALU.add, op1=ALU.mult),
             reads=[S["dest"], S["valid"]], writes=[S["dest"]])
        P.op(V, lambda e: e.tensor_scalar(out=S["dest"][:], in0=S["dest"][:], scalar1=BIGIDX, scalar2=None, op0=ALU.add),
             reads=[S["dest"]], writes=[S["dest"]])
        for j, sn in enumerate(("sel1", "sel2")):
            P.op(V, lambda e: e.tensor_tensor(out=S["tmp8"][:], in0=S["dest"][:], in1=S[sn][:], op=ALU.mult), reads=[S["dest"], S[sn]], writes=[S["tmp8"]])
            P.op(V, lambda e: e.reduce_sum(out=d_[:, j:j + 1], in_=S["tmp8"][:], axis=AX.X), reads=[S["tmp8"]], writes=[d_])
        P.op(V, lambda e: e.tensor_copy(out=ix_[:], in_=d_[:]), reads=[d_], writes=[ix_])
        P.dma("sync", k.midx[tsl, :], ix_[:], reads=[ix_])
        P.dma("sync", k.mwts[tsl, :], w_[:], reads=[w_])
        for j in range(2):
            P.gather(k.Xs, hb_[:], ix_[:, j:j + 1], reads=[hb_, ix_], scatter=True, bounds_check=NSLOT - 1, oob_is_err=False)
    P.flush()
    P.release(m0)


def phase_moe_experts(k, l):
    i = l // 2
    for e_ in range(NE):
        ffn_gateup(k, k.Xs[e_ * CAP:(e_ + 1) * CAP, :], CAP, k.moe_w_gate[i, e_], k.moe_w_up[i, e_], D_FFE, k.AT[:, 0:CAP], mod=None)
        ffn_down(k, k.AT[:, 0:CAP], CAP, k.moe_w_down[i, e_], D_FFE, k.Ys, row_off=e_ * CAP)


def phase_moe_ln2(k, l, xsrc, xdst):
    P = k.P
    m0 = P.mark()
    gfrep = P.sb("gfrep", [128, D], F32)
    lng = P.sb("lng", [128, D], F32)
    lnb = P.sb("lnb", [128, D], F32)
    P.dma("sync", gfrep[:], k.modv[l, 5 * D:6 * D].partition_broadcast(128), writes=[gfrep])
    P.dma("sync", lng[:], k.ln_ffn_g[l].partition_broadcast(128), writes=[lng])
    P.dma("sync", lnb[:], k.ln_ffn_b[l].partition_broadcast(128), writes=[lnb])
    xt = [P.sb("xt%d" % i, [128, D], F32) for i in range(2)]
    y1 = [P.sb("y1%d" % i, [128, D], F32) for i in range(2)]
    y2 = [P.sb("y2%d" % i, [128, D], F32) for i in range(2)]
    rt = [P.sb("rt%d" % i, [128, D], F32) for i in range(2)]
    idx = [P.sb("idx%d" % i, [128, 2], I32) for i in range(2)]
    wts = [P.sb("wts%d" % i, [128, 2], F32) for i in range(2)]
    stats = P.sb("stats", [128, 4, 6], F32)
    mv = P.sb("mv", [128, 2], F32)
    rstd = P.sb("rstd", [128, 1], F32)
    tix = [P.sb("tix%d" % i, [128, 1], I32) for i in range(2)]
    for tt in range(TM // 128):
        i2 = tt % 2
        x_, r_, a_, b_, ix_, w_ = xt[i2], rt[i2], y1[i2], y2[i2], idx[i2], wts[i2]
        tsl = slice(tt * 128, (tt + 1) * 128)
        P.dma("sync", tix[i2][:], k.tokidx[tsl, :], writes=[tix[i2]])
        P.gather(x_[:], xsrc, tix[i2][:, 0:1], reads=[tix[i2]], writes=[x_], bounds_check=T - 1, oob_is_err=False)
        P.dma("sync", ix_[:], k.midx[tsl, :], writes=[ix_])
        P.dma("sync", w_[:], k.mwts[tsl, :], writes=[w_])
        P.op("gpsimd", lambda e: e.memset(a_[:], 0.0), writes=[a_])
        P.op("gpsimd", lambda e: e.memset(b_[:], 0.0), writes=[b_])
        P.gather(a_[:], k.Ys, ix_[:, 0:1], reads=[ix_], writes=[a_], bounds_check=NSLOT - 1, oob_is_err=False)
        P.gather(b_[:], k.Ys, ix_[:, 1:2], reads=[ix_], writes=[b_], bounds_check=NSLOT - 1, oob_is_err=False)
        P.op("vector", lambda e: e.tensor_scalar(out=r_[:], in0=a_[:], scalar1=w_[:, 0:1], scalar2=None, op0=ALU.mult), reads=[a_, w_], writes=[r_])
        P.op("vector", lambda e: e.scalar_tensor_tensor(out=r_[:], in0=b_[:], scalar=w_[:, 1:2], in1=r_[:], op0=ALU.mult, op1=ALU.add),
             reads=[b_, w_, r_], writes=[r_])
        P.op("gpsimd", lambda e: e.tensor_tensor(out=r_[:], in0=r_[:], in1=gfrep[:], op=ALU.mult), reads=[r_, gfrep], writes=[r_])
        P.op("vector", lambda e: e.scalar_tensor_tensor(out=r_[:], in0=x_[:], scalar=ALPHA, in1=r_[:], op0=ALU.mult, op1=ALU.add),
             reads=[x_, r_], writes=[r_])
        ln_tile(P, r_, lng, lnb, stats, mv, rstd)
        P.dma("sync", xdst[tsl, :], r_[:], reads=[r_])
    P.flush()
    P.release(m0)


_CACHE = {}


def make_in_maps(inputs, n_cores=8):
    hc = host_consts()
    maps = []
    for core in range(n_cores):
        b = (core // 2) % 4
        r = core % 2
        m = {
            "x": np.ascontiguousarray(inputs["x"][b]),
            "cT": np.ascontiguousarray(inputs["c"][b].reshape(KC, 128).T),
            "tokidx": (r * TM + np.arange(TM, dtype=np.int32)).reshape(TM, 1),
            "w_ada": inputs["w_ada"],
            "b_ada": inputs["b_ada"],
            "w_in": inputs["w_in"],
            "w_out": inputs["w_out"], "ln_mix_g": inputs["ln_mix_g"], "ln_mix_b": inputs["ln_mix_b"],
            "ln_ffn_g": inputs["ln_ffn_g"], "ln_ffn_b": inputs["ln_ffn_b"],
            "moe_router": inputs["moe_router"], "moe_w_gate": inputs["moe_w_gate"], "moe_w_up": inputs["moe_w_up"],
            "moe_w_down": inputs["moe_w_down"],
            "ffn_w_gate": inputs["ffn_w_gate"], "ffn_w_up": inputs["ffn_w_up"], "ffn_w_down": inputs["ffn_w_down"],
            "gla_w_a2": inputs["gla_w_a2"], "gla_b_a": inputs["gla_b_a"],
            "gla_nwT": np.ascontiguousarray(inputs["gla_norm_w"].reshape(DEPTH, 2, 128).transpose(0, 2, 1)),
            "cmp_w1_k": inputs["cmp_w1_k"], "cmp_w1_v": inputs["cmp_w1_v"],
            "cmp_w2_k": inputs["cmp_w2_k"], "cmp_w2_v": inputs["cmp_w2_v"],
            "cmp_peT_k": np.ascontiguousarray(inputs["cmp_pos_k"].transpose(0, 2, 1)),
            "cmp_peT_v": np.ascontiguousarray(inputs["cmp_pos_v"].transpose(0, 2, 1)),
        }
        m.update(hc)
        maps.append(m)
    return maps


N_CORES = 8


def kernel(**inputs):
    inputs = {k_: np.asarray(v) for k_, v in inputs.items()}
    nc = build()
    maps = make_in_maps(inputs, n_cores=N_CORES)
    res = run_bass_kernel_spmd(nc, maps, core_ids=list(range(N_CORES)))
    out = np.empty((4, T, D), np.float32)
    for core in range(N_CORES):
        b, r = core // 2, core % 2
        out[b, r * TM:(r + 1) * TM] = np.asarray(res.results[core]["out"])
    return out
```

```python
import numpy as np
import concourse.bass as bass
import concourse.mybir as mybir
from concourse.bass_utils import run_bass_kernel_spmd

F32 = mybir.dt.float32
BF16 = mybir.dt.bfloat16
I32 = mybir.dt.int32
U32 = mybir.dt.uint32
AF = mybir.ActivationFunctionType
ALU = mybir.AluOpType
AX = mybir.AxisListType

ENGS = ("tensor", "vector", "scalar", "gpsimd", "sync")

D = 2048
T = 4096
DEPTH = 2
KC = D // 128
HD = 128
SPLIT = (1024, 256, 256, 256, 256, 256, 256, 24, 512, 512, 1024, 16, 1024)
SEG = ("nq", "kc", "vc", "ks", "vs", "kw", "vw", "ng", "gq", "gk", "gv", "ga", "gg")
OFF = {}
_o = 0
for _n, _s in zip(SEG, SPLIT):
    OFF[_n] = (_o, _s)
    _o += _s
IN_W = _o
D_FF = 5632
NE = 8
D_FFE = 7168
ALPHA = (2 * DEPTH) ** 0.25
LN_EPS = 1e-5
NORM_EPS = 1e-6
NEG = -30000.0
SCALE = HD ** -0.5


class Sem:
    def __init__(self, h, kind, uid):
        self.h = h
        self.kind = kind
        self.count = 0
        self.key = "s%d" % uid


class Buf:
    def __init__(self, t, name):
        self.t = t
        self.name = name
        self.last_w = None
        self.readers = {}
        self.sem = {"sw": None, "hw": None}
        self.dlast = {"sw": 0, "hw": 0}
        self.dma_w = {"sw": 0, "hw": 0}
        self.psum = False
        self.fdeps = []

    def __getitem__(self, idx):
        return self.t[idx]

    def reset(self):
        self.last_w = None
        self.readers = {}
        self.fdeps = []
        self.dlast["hw"] = 0
        self.dma_w["hw"] = 0


class _Rec:
    def __init__(self):
        self.calls = []

    def __getattr__(self, name):
        def f(*a, **kw):
            self.calls.append((name, a, kw))
            return self
        return f


class Prog:
    def __init__(self, nc, same_engine_raw=True):
        self.nc = nc
        self.same_engine_raw = same_engine_raw
        self.psem = {}
        self.cnt = {e: 0 for e in ENGS}
        self.lists = {e: [] for e in ENGS}
        self.seen = {e: {} for e in ENGS}
        self.bufs = []
        self._cms = []
        self._semcms = []
        self.pool = {"sw": [], "hw": []}
        self.allsems = []
        for e in ENGS:
            cm = nc.semaphore("p_" + e)
            self.psem[e] = cm.__enter__()
            self._semcms.append(cm)
        self.n_inst = 0
        self.uid = 0
        self._bcreg = {}
        self._bcset = set()

    def mark(self):
        return len(self._cms)

    def release(self, mark):
        while len(self._cms) > mark:
            cm, b = self._cms.pop()
            cm.__exit__(None, None, None)
            if b is not None:
                self._drop(b)

    def _drop(self, b):
        for kind in ("sw", "hw"):
            if b.sem[kind] is not None:
                self.pool[kind].append(b.sem[kind])
                b.sem[kind] = None
        if b in self.bufs:
            self.bufs.remove(b)
        for v in getattr(b, "views", []):
            self._drop(v)

    def sb(self, name, shape, dt):
        self.uid += 1
        cm = self.nc.sbuf_tensor("%s_%d" % (name, self.uid), list(shape), dt)
        t = cm.__enter__()
        b = Buf(t, "%s_%d" % (name, self.uid))
        b.views = []
        self._cms.append((cm, b))
        self.bufs.append(b)
        return b

    def ps(self, name, shape, dt=F32):
        self.uid += 1
        cm = self.nc.psum_tensor("%s_%d" % (name, self.uid), list(shape), dt)
        t = cm.__enter__()
        b = Buf(t, "%s_%d" % (name, self.uid))
        b.psum = True
        b.views = []
        self._cms.append((cm, b))
        self.bufs.append(b)
        return b

    def view(self, buf, name=None):
        self.uid += 1
        b = Buf(buf.t, "%s_v%d" % (buf.name, self.uid))
        buf.views.append(b)
        self.bufs.append(b)
        return b

    def _getsem(self, b, kind):
        if b.sem[kind] is None:
            if self.pool[kind]:
                b.sem[kind] = self.pool[kind].pop()
            else:
                self.uid += 1
                cm = self.nc.semaphore("d%s_%d" % (kind, self.uid))
                h = cm.__enter__()
                self._semcms.append(cm)
                sm = Sem(h, kind, self.uid)
                self.allsems.append(sm)
                b.sem[kind] = sm
        return b.sem[kind]

    def _waits(self, e, reads, writes):
        w = {}

        def need(sem, val, key):
            if val <= 0:
                return
            if self.seen[e].get(key, 0) >= val:
                return
            if key not in w or w[key][1] < val:
                w[key] = (sem, val)

        for r in reads:
            if r.last_w is not None:
                we, n = r.last_w
                if we != e or self.same_engine_raw:
                    need(self.psem[we], n, "p_" + we)
            for kind in ("sw", "hw"):
                if r.dma_w[kind] > 0:
                    need(r.sem[kind].h, 16 * r.dma_w[kind], r.sem[kind].key)
            if r.psum:
                for re_, n in r.readers.items():
                    if re_ != e:
                        need(self.psem[re_], n, "p_" + re_)
        for b in writes:
            if b.last_w is not None:
                we, n = b.last_w
                if we != e:
                    need(self.psem[we], n, "p_" + we)
            for re_, n in b.readers.items():
                if re_ != e:
                    need(self.psem[re_], n, "p_" + re_)
            for kind in ("sw", "hw"):
                if b.dlast[kind] > 0:
                    need(b.sem[kind].h, 16 * b.dlast[kind], b.sem[kind].key)
            for (fs, fv, fk) in b.fdeps:
                need(fs, fv, fk)
        for key, (sem, val) in w.items():
            self.seen[e][key] = val
        return list(w.values())

    def op(self, e, fn, reads=(), writes=(), signal=True):
        waits = self._waits(e, reads, writes)
        n = self.cnt[e] + 1
        if signal:
            self.cnt[e] = n
        psem = self.psem[e]
        rec = _Rec()
        fn(rec)
        assert len(rec.calls) == 1, rec.calls
        name, a, kw = rec.calls[0]

        def thunk(engine, waits=waits, name=name, a=a, kw=kw, signal=signal, psem=psem):
            for sem, val in waits:
                engine.wait_ge(sem, val)
            ins = getattr(engine, name)(*a, **kw)
            if signal:
                ins.then_inc(psem, 1)

        self.lists[e].append(thunk)
        self.n_inst += 1
        for r in reads:
            r.readers[e] = n
        for b in writes:
            b.last_w = (e, n)
            b.readers = {}
            b.dma_w = {"sw": 0, "hw": 0}
            b.fdeps = []

    def _dma_book(self, q, reads, writes):
        kind = "sw" if q == "gpsimd" else "hw"
        waits = self._waits(q, reads, writes)
        allb = list(writes) + list(reads)
        prim = allb[0]
        sm = self._getsem(prim, kind)
        sm.count += 1
        prim.dlast[kind] = sm.count
        for b in allb[1:]:
            assert b not in writes
            b.fdeps.append((sm.h, 16 * sm.count, sm.key))
        for b in writes:
            b.dma_w[kind] = sm.count
            b.last_w = None
            b.readers = {}
            b.fdeps = []
        return waits, sm.h

    def dma(self, q, out_ap, in_ap, reads=(), writes=(), **kw):
        waits, semh = self._dma_book(q, reads, writes)

        def thunk(engine, waits=waits, semh=semh, out_ap=out_ap, in_ap=in_ap, kw=kw):
            for sem, val in waits:
                engine.wait_ge(sem, val)
            engine.dma_start(out=out_ap, in_=in_ap, **kw).then_inc(semh, 16)

        self.lists[q].append(thunk)
        self.n_inst += 1

    def gather(self, out_ap, in_ap, idx_ap, reads=(), writes=(), scatter=False, **kw):
        q = "gpsimd"
        waits, semh = self._dma_book(q, reads, writes)
        bc = kw.pop("bounds_check", None)

        def thunk(engine, waits=waits, semh=semh, kw=kw, bc=bc):
            for sem, val in waits:
                engine.wait_ge(sem, val)
            if bc is not None:
                if bc not in self._bcreg:
                    self._bcreg[bc] = engine.alloc_register("bcreg_%d" % int(bc))
                if bc not in self._bcset:
                    engine.reg_mov(self._bcreg[bc], bc)
                    self._bcset.add(bc)
                kw = dict(kw)
                kw["bounds_check"] = self._bcreg[bc]
            if scatter:
                ins = engine.indirect_dma_start(out=out_ap, out_offset=bass.IndirectOffsetOnAxis(ap=idx_ap, axis=0),
                                                in_=in_ap, in_offset=None, **kw)
            else:
                ins = engine.indirect_dma_start(out=out_ap, out_offset=None, in_=in_ap,
                                                in_offset=bass.IndirectOffsetOnAxis(ap=idx_ap, axis=0), **kw)
            ins.then_inc(semh, 16)

        self.lists[q].append(thunk)
        self.n_inst += 1

    def flush(self, final=False):
        nc = self.nc
        drain = []
        for sm in self.allsems:
            if sm.count > 0:
                drain.append((sm.h, 16 * sm.count))
        for e in ENGS:
            if e != "sync" and self.cnt[e] > 0:
                drain.append((self.psem[e], self.cnt[e]))

        def dthunk(engine, drain=drain):
            for sem, val in drain:
                engine.wait_ge(sem, val)

        self.lists["sync"].append(dthunk)
        lists = self.lists
        with nc.Block() as block:
            @block.tensor
            def _(eng):
                for th in lists["tensor"]:
                    th(eng)

            @block.vector
            def _(eng):
                for th in lists["vector"]:
                    th(eng)

            @block.scalar
            def _(eng):
                for th in lists["scalar"]:
                    th(eng)

            @block.gpsimd
            def _(eng):
                for th in lists["gpsimd"]:
                    th(eng)

            @block.sync
            def _(eng):
                for th in lists["sync"]:
                    th(eng)
        if not final:
            nc.all_engine_barrier()
            sems = [self.psem[e] for e in ENGS] + [sm.h for sm in self.allsems if sm.kind == "hw" and sm.count > 0]
            with nc.Block() as block:
                @block.sync
                def _(eng):
                    for s_ in sems:
                        eng.sem_clear(s_)
            nc.all_engine_barrier()
        for sm in self.allsems:
            if sm.kind == "hw":
                sm.count = 0
        self.lists = {e: [] for e in ENGS}
        self.cnt = {e: 0 for e in ENGS}
        self.seen = {e: {} for e in ENGS}
        self._bcset = set()
        for b in self.bufs:
            b.reset()

    def close(self):
        self.release(0)
        for cm in reversed(self._semcms):
            cm.__exit__(None, None, None)
        self._semcms = []


def host_consts():
    c = {}
    c["ident"] = np.eye(128, dtype=np.float32)
    half = 16
    inv = (500000.0 ** (-np.arange(half, dtype=np.float32) * 2.0 / 32)).astype(np.float32)
    ang = np.arange(T, dtype=np.float32)[None, :] * inv[:, None]
    cos = np.cos(ang).astype(np.float32)
    sin = np.sin(ang).astype(np.float32)
    c["cos"] = np.concatenate([cos, cos], 0)
    c["sin"] = np.concatenate([sin, sin], 0)
    pm = np.zeros((128, 128), np.float32)
    for i in range(16):
        pm[i + 16, i] = -1.0
        pm[i, i + 16] = 1.0
    c["pm"] = pm
    kk = np.arange(128)[:, None]
    tt = np.arange(128)[None, :]
    c["cb"] = np.where(kk > tt, NEG, 0.0).astype(np.float32)
    c["wbm"] = np.where(kk <= tt, NEG, 0.0).astype(np.float32)
    es = np.zeros((64, 32, 128), np.float32)
    for kt in range(32):
        for m in range(128):
            es[2 * kt + m // 64, kt, m] = 1.0
    c["esel"] = es.reshape(64, 32 * 128)
    cst = np.arange(256) * 16
    bst = np.arange(64) * 64
    ov = ((cst[:, None] < bst[None, :] + 64) & (cst[:, None] + 32 > bst[None, :])).astype(np.float32)
    ov[255] = 0.0
    c["ovl"] = np.ascontiguousarray(ov.reshape(2, 128, 64).transpose(1, 0, 2)).reshape(128, 128)
    c["mb"] = (16.0 * kk - tt).astype(np.float32)
    keep = np.zeros((128, 32, 64), np.float32)
    add = np.zeros((128, 32, 64), np.float32)
    n = np.arange(64)[None, :]
    for qt in range(32):
        t = qt * 128 + np.arange(128)[:, None]
        cur = t // 64
        forced = (n == 0) | (n == cur) | (n == cur - 1)
        future = (n * 64) > t
        keep[:, qt, :] = np.where(forced | future, 0.0, 1.0)
        add[:, qt, :] = np.where(future, -1e30, np.where(forced, 1e9, 0.0))
    c["keep"] = keep.reshape(128, 32 * 64)
    c["addc"] = add.reshape(128, 32 * 64)
    sel = np.zeros((12, 12, 128), np.float32)
    for r in range(12):
        sel[r, r, :] = 1.0
    c["sel"] = sel.reshape(12, 12 * 128)
    same = (kk // 64) == (tt // 64)
    c["slmat"] = (kk < tt).astype(np.float32)
    c["eoff"] = np.tile((np.arange(NE) * 768.0)[None, :], (128, 1)).astype(np.float32)
    c["tri2"] = (same & (kk <= tt)).astype(np.float32)
    c["sumat"] = (same & (kk > tt)).astype(np.float32)
    return c


class K:
    pass


def build(debug=(), phases=None, layers=(0, 1), l1_from_x=False):
    nc = bass.Bass("TRN2", target_bir_lowering=False)
    k = K()
    k.nc = nc
    dbg = set(debug)

    def din(name, shape, dt=F32):
        return nc.dram_tensor(name, list(shape), dt, kind="ExternalInput").ap()

    def dscr(name, shape, dt):
        kind = "ExternalOutput" if name in dbg else "Internal"
        return nc.dram_tensor(name, list(shape), dt, kind=kind).ap()

    k.x = din("x", [T, D])
    k.cT = din("cT", [128, KC])
    k.w_ada = din("w_ada", [DEPTH, D, 6 * D])
    k.b_ada = din("b_ada", [DEPTH, 6 * D])
    k.w_in = din("w_in", [DEPTH, D, IN_W])
    k.ident = din("ident", [128, 128])
    k.cos = din("cos", [32, T])
    k.sin = din("sin", [32, T])
    k.pm = din("pm", [128, 128])
    k.cb = din("cb", [128, 128])
    k.wbm = din("wbm", [128, 128])
    k.esel = din("esel", [64, 32 * 128])
    k.ovl = din("ovl", [128, 128])
    k.mb = din("mb", [128, 128])
    k.keep = din("keep", [128, 32 * 64])
    k.addc = din("addc", [128, 32 * 64])
    k.sel = din("sel", [12, 12 * 128])
    k.w_out = din("w_out", [DEPTH, D, D])
    k.ln_mix_g = din("ln_mix_g", [DEPTH, D])
    k.ln_mix_b = din("ln_mix_b", [DEPTH, D])
    k.ln_ffn_g = din("ln_ffn_g", [DEPTH, D])
    k.ln_ffn_b = din("ln_ffn_b", [DEPTH, D])
    k.ffn_w_gate = din("ffn_w_gate", [1, D, D_FF])
    k.ffn_w_up = din("ffn_w_up", [1, D, D_FF])
    k.ffn_w_down = din("ffn_w_down", [1, D_FF, D])
    k.moe_router = din("moe_router", [1, D, NE])
    k.moe_w_gate = din("moe_w_gate", [1, NE, D, D_FFE])
    k.moe_w_up = din("moe_w_up", [1, NE, D, D_FFE])
    k.moe_w_down = din("moe_w_down", [1, NE, D_FFE, D])
    k.slmat = din("slmat", [128, 128])
    k.eoff = din("eoff", [128, NE])
    k.tri2 = din("tri2", [128, 128])
    k.sumat = din("sumat", [128, 128])
    k.gla_w_a2 = din("gla_w_a2", [DEPTH, 16, 512])
    k.gla_b_a = din("gla_b_a", [DEPTH, 512])
    k.gla_nwT = din("gla_nwT", [DEPTH, 128, 2])
    k.cmp_w1 = {"k": din("cmp_w1_k", [DEPTH, 4096, 256]), "v": din("cmp_w1_v", [DEPTH, 4096, 256])}
    k.cmp_w2 = {"k": din("cmp_w2_k", [DEPTH, 256, 128]), "v": din("cmp_w2_v", [DEPTH, 256, 128])}
    k.cmp_peT = {"k": din("cmp_peT_k", [DEPTH, 128, 32]), "v": din("cmp_peT_v", [DEPTH, 128, 32])}
    k.tokidx = din("tokidx", [2048, 1], I32)
    k.out = nc.dram_tensor("out", [2048, D], F32, kind="ExternalOutput").ap()

    k.modv = dscr("modv", [DEPTH, 6 * D], F32)
    k.qn = dscr("qn", [1024, T], BF16)
    k.qr = dscr("qr", [1024, T], BF16)
    k.kcT = dscr("kcT", [256, T], BF16)
    k.vcT = dscr("vcT", [256, T], BF16)
    k.ksT = dscr("ksT", [256, T], BF16)
    k.kwT = dscr("kwT", [256, T], BF16)
    k.ngT = dscr("ngT", [24, T], BF16)
    k.gqT = dscr("gqT", [512, T], BF16)
    k.gkT = dscr("gkT", [512, T], BF16)
    k.gaT = dscr("gaT", [16, T], BF16)
    k.ggT = dscr("ggT", [1024, T], BF16)
    k.vs = dscr("vs", [T, 256], BF16)
    k.vw = dscr("vw", [T, 256], BF16)
    k.gk = dscr("gk", [T, 512], BF16)
    k.gv = dscr("gv", [T, 1024], BF16)
    k.kcmpT = dscr("kcmpT", [2, 128, 256], BF16)
    k.vcmp = dscr("vcmp", [2, 256, 128], BF16)
    k.mixT = dscr("mixT", [D, T], BF16)
    k.x1 = dscr("x1", [T, D], F32)
    k.xa = dscr("xa", [T, D], F32)
    k.yffn = dscr("yffn", [T, D], F32)
    k.AT = dscr("AT", [D_FFE, T], BF16)
    k.Xs = dscr("Xs", [NE * 768, D], BF16)
    k.Ys = dscr("Ys", [NE * 768, D], F32)
    k.midx = dscr("midx", [2048, 2], I32)
    k.mwts = dscr("mwts", [2048, 2], F32)

    P = Prog(nc)
    k.P = P
    ph = phases

    if ph is None or "mod" in ph:
        phase_mod(k)
    for l in layers:
        xsrc = k.x if (l == 0 or l1_from_x) else k.xa
        if ph is None or "proj" in ph:
            phase_proj(k, l, xsrc)
        if ph is None or "cmp" in ph:
            phase_cmp(k, l)
        if ph is None or "nsa" in ph:
            phase_nsa(k, l)
        if ph is None or "gla" in ph:
            phase_gla(k, l)
        if ph is None or "wout" in ph:
            phase_wout(k, l, xsrc, k.x1)
        xdst = k.out if l == DEPTH - 1 else k.xa
        if l % 2 == 0:
            if ph is None or "ffn" in ph:
                ffn_gateup(k, k.x1, T, k.ffn_w_gate[l // 2], k.ffn_w_up[l // 2], D_FF, k.AT[0:D_FF, :], mod=l)
                ffn_down(k, k.AT[0:D_FF, :], T, k.ffn_w_down[l // 2], D_FF, k.yffn)
            if ph is None or "ln2" in ph:
                phase_ln2(k, l, k.x1, k.yffn, xdst)
        else:
            if ph is None or "route" in ph:
                phase_moe_route(k, l)
            if ph is None or "experts" in ph:
                phase_moe_experts(k, l)
            if ph is None or "ln2" in ph:
                phase_moe_ln2(k, l, k.x1, xdst)

    P.flush(final=True)
    P.close()
    return nc


def phase_mod(k):
    P = k.P
    m0 = P.mark()
    cc = P.sb("cc", [128, KC], F32)
    cs = P.sb("cs", [128, KC], F32)
    condB = P.sb("condB", [128, KC, 128], F32)
    wb = [P.sb("wada%d" % i, [128, KC, 512], F32) for i in range(2)]
    bb = [P.sb("bada%d" % i, [128, 512], F32) for i in range(2)]
    mo = [P.sb("mo%d" % i, [128, 512], F32) for i in range(2)]
    pm = [P.ps("pmod%d" % i, [128, 512]) for i in range(2)]
    P.dma("sync", cc[:], k.cT, writes=[cc])
    P.op("scalar", lambda e: e.activation(out=cs[:], in_=cc[:], func=AF.Silu), reads=[cc], writes=[cs])
    P.op("vector", lambda e: e.tensor_copy(out=condB[:], in_=cs[:].unsqueeze(2).to_broadcast([128, KC, 128])),
         reads=[cs], writes=[condB])
    it = 0
    import os
    NBL = int(os.environ.get("NBL", "24"))
    VAR = os.environ.get("VAR", "")
    for l in range(DEPTH):
        wv = k.w_ada[l].rearrange("(c p) n -> p c n", p=128)
        for nb in range(NBL):
            i = it % 2
            it += 1
            n0 = nb * 512
            P.dma("sync", wb[i][:], wv[:, :, n0:n0 + 512], writes=[wb[i]])
            if VAR == "nobb":
                P.op("vector", lambda e, i=i: e.memset(bb[i][:], 0.0), writes=[bb[i]])
            else:
                P.dma("gpsimd", bb[i][:], k.b_ada[l, n0:n0 + 512].partition_broadcast(128), writes=[bb[i]])
            for c in range(KC):
                P.op("tensor", lambda e, i=i, c=c: e.matmul(pm[i][:], lhsT=condB[:, c, :], rhs=wb[i][:, c, :],
                                                             start=(c == 0), stop=(c == KC - 1)),
                     reads=[condB, wb[i]], writes=[pm[i]], signal=(c == KC - 1))
            seg = nb // 4
            add1 = 1.0 if seg in (1, 2, 4, 5) else 0.0
            P.op("vector", lambda e, i=i, add1=add1: e.scalar_tensor_tensor(
                out=mo[i][:], in0=pm[i][:], scalar=add1, in1=bb[i][:], op0=ALU.add, op1=ALU.add),
                reads=[pm[i], bb[i]], writes=[mo[i]])
            P.dma("gpsimd", k.modv[l:l + 1, n0:n0 + 512], mo[i][0:1, :], reads=[mo[i]])
    P.flush()
    P.release(m0)


def build_hT(k, xsrc, t0, ntiles, screp, shrep, identf, hT, hviews, xt, ptr):
    P = k.P
    for j in range(ntiles):
        xb = xt[j % 2]
        P.dma("sync", xb[:], xsrc[t0 + j * 128:t0 + (j + 1) * 128, :], writes=[xb])
        P.op("vector", lambda e, xb=xb: e.tensor_tensor(out=xb[:], in0=xb[:], in1=screp[:], op=ALU.mult),
             reads=[xb, screp], writes=[xb])
        P.op("gpsimd", lambda e, xb=xb: e.tensor_tensor(out=xb[:], in0=xb[:], in1=shrep[:], op=ALU.add),
             reads=[xb, shrep], writes=[xb])
        for g in range(KC // 4):
            pt = ptr[(j * 4 + g) % 2]
            for q in range(4):
                c = g * 4 + q
                P.op("tensor", lambda e, xb=xb, pt=pt, q=q, c=c: e.transpose(
                    out=pt[:, q, :], in_=xb[:, c * 128:(c + 1) * 128], identity=identf[:]),
                    reads=[xb, identf], writes=[pt], signal=(q == 3))
            eng = "scalar" if (g % 2 == 0) else "vector"
            if eng == "scalar":
                P.op("scalar", lambda e, pt=pt, g=g, j=j: e.copy(out=hT[:, g * 4:(g + 1) * 4, j * 128:(j + 1) * 128], in_=pt[:]),
                     reads=[pt], writes=[hviews[j][0]])
            else:
                P.op("vector", lambda e, pt=pt, g=g, j=j: e.tensor_copy(out=hT[:, g * 4:(g + 1) * 4, j * 128:(j + 1) * 128], in_=pt[:]),
                     reads=[pt], writes=[hviews[j][1]])


def phase_proj(k, l, xsrc):
    P = k.P
    TH = 2048
    for th in range(T // TH):
        t0 = th * TH
        m0 = P.mark()
        screp = P.sb("screp", [128, D], F32)
        shrep = P.sb("shrep", [128, D], F32)
        identf = P.sb("identf", [128, 128], F32)
        hT = P.sb("hT", [128, KC, TH], BF16)
        hviews = [(P.view(hT), P.view(hT)) for _ in range(TH // 128)]
        xt = [P.sb("xt%d" % i, [128, D], F32) for i in range(2)]
        ptr = [P.ps("ptr%d" % i, [128, 4, 128]) for i in range(2)]
        cosb = P.sb("cosb", [32, TH], F32)
        sinb = P.sb("sinb", [32, TH], F32)
        pmb = P.sb("pmb", [128, 128], BF16)
        wbk = [P.sb("wblk%d" % i, [128, KC, 512], BF16) for i in range(2)]
        pacc = [P.ps("pacc%d" % i, [128, 512]) for i in range(3)]
        prot = [P.ps("prot%d" % i, [128, 512]) for i in range(2)]
        ost = [P.sb("ost%d" % i, [128, 512], BF16) for i in range(4)]
        ost2 = [P.sb("ost2%d" % i, [128, 512], BF16) for i in range(2)]
        t1 = [P.sb("t1%d" % i, [32, 512], F32) for i in range(2)]
        t2 = [P.sb("t2%d" % i, [32, 512], F32) for i in range(2)]

        P.dma("sync", screp[:], k.modv[l, D:2 * D].partition_broadcast(128), writes=[screp])
        P.dma("sync", shrep[:], k.modv[l, 0:D].partition_broadcast(128), writes=[shrep])
        P.dma("sync", identf[:], k.ident, writes=[identf])
        P.dma("sync", cosb[:], k.cos[:, t0:t0 + TH], writes=[cosb])
        P.dma("sync", sinb[:], k.sin[:, t0:t0 + TH], writes=[sinb])
        P.dma("gpsimd", pmb[:], k.pm, writes=[pmb])
        build_hT(k, xsrc, t0, TH // 128, screp, shrep, identf, hT, hviews, xt, ptr)

        wv = k.w_in[l].rearrange("(c p) n -> p c n", p=128)
        cnt = {"w": 0, "acc": 0, "ost": 0, "ost2": 0, "rot": 0}

        def hreads(tb):
            r = []
            for j in range(tb * 4, tb * 4 + 4):
                r += [hviews[j][0], hviews[j][1]]
            return r

        def load_w(c0, ncols):
            wb = wbk[cnt["w"] % 2]
            cnt["w"] += 1
            P.dma("gpsimd", wb[:, :, 0:ncols], wv[:, :, c0:c0 + ncols], writes=[wb])
            return wb

        def ftype(seg, dst, mode):
            c0, n = OFF[seg]
            for b0 in range(0, n, 512):
                nb = min(512, n - b0)
                wb = load_w(c0 + b0, nb)
                for ct in range(0, nb, 128):
                    m = min(128, nb - ct)
                    row0 = b0 + ct
                    for tb in range(TH // 512):
                        pa = pacc[cnt["acc"] % 3]
                        cnt["acc"] += 1
                        for c in range(KC):
                            P.op("tensor", lambda e, pa=pa, wb=wb, ct=ct, m=m, c=c, tb=tb: e.matmul(
                                pa[0:m, :], lhsT=wb[:, c, ct:ct + m], rhs=hT[:, c, tb * 512:(tb + 1) * 512],
                                start=(c == 0), stop=(c == KC - 1)),
                                reads=[wb] + hreads(tb), writes=[pa], signal=(c == KC - 1))
                        ob = ost[cnt["ost"] % 4]
                        cnt["ost"] += 1
                        tok = slice(t0 + tb * 512, t0 + (tb + 1) * 512)
                        if mode == "plain":
                            P.op("scalar", lambda e, ob=ob, pa=pa, m=m: e.copy(out=ob[0:m, :], in_=pa[0:m, :]),
                                 reads=[pa], writes=[ob])
                            P.dma("sync", dst[row0:row0 + m, tok], ob[0:m, :], reads=[ob])
                        elif mode == "sigmoid":
                            P.op("scalar", lambda e, ob=ob, pa=pa, m=m: e.activation(out=ob[0:m, :], in_=pa[0:m, :], func=AF.Sigmoid),
                                 reads=[pa], writes=[ob])
                            P.dma("sync", dst[row0:row0 + m, tok], ob[0:m, :], reads=[ob])
                        elif mode == "silu":
                            P.op("scalar", lambda e, ob=ob, pa=pa, m=m: e.activation(out=ob[0:m, :], in_=pa[0:m, :], func=AF.Silu),
                                 reads=[pa], writes=[ob])
                            P.dma("sync", dst[row0:row0 + m, tok], ob[0:m, :], reads=[ob])
                        else:
                            dn, dr = mode[1], mode[2]
                            P.op("scalar", lambda e, ob=ob, pa=pa: e.copy(out=ob[:], in_=pa[:]), reads=[pa], writes=[ob])
                            pr = prot[cnt["rot"] % 2]
                            a1 = t1[cnt["rot"] % 2]
                            a2 = t2[cnt["rot"] % 2]
                            cnt["rot"] += 1
                            P.op("tensor", lambda e, pr=pr, ob=ob: e.matmul(pr[:], lhsT=pmb[:], rhs=ob[:], start=True, stop=True),
                                 reads=[pmb, ob], writes=[pr])
                            ltok = slice(tb * 512, (tb + 1) * 512)
                            P.op("vector", lambda e, a1=a1, pa=pa, ltok=ltok: e.tensor_tensor(out=a1[:], in0=pa[0:32, :], in1=cosb[:, ltok], op=ALU.mult),
                                 reads=[pa, cosb], writes=[a1])
                            P.op("vector", lambda e, a2=a2, pr=pr, ltok=ltok: e.tensor_tensor(out=a2[:], in0=pr[0:32, :], in1=sinb[:, ltok], op=ALU.mult),
                                 reads=[pr, sinb], writes=[a2])
                            if dn is not None:
                                P.dma("sync", dn[row0:row0 + 128, tok], ob[:], reads=[ob])
                                o2 = ost2[cnt["ost2"] % 2]
                                cnt["ost2"] += 1
                                P.op("scalar", lambda e, o2=o2, pa=pa: e.copy(out=o2[:], in_=pa[:]), reads=[pa], writes=[o2])
                                P.op("vector", lambda e, o2=o2, a1=a1, a2=a2: e.tensor_tensor(out=o2[0:32, :], in0=a1[:], in1=a2[:], op=ALU.add),
                                     reads=[a1, a2], writes=[o2])
                                P.dma("sync", dr[row0:row0 + 128, tok], o2[:], reads=[o2])
                            else:
                                P.op("vector", lambda e, ob=ob, a1=a1, a2=a2: e.tensor_tensor(out=ob[0:32, :], in0=a1[:], in1=a2[:], op=ALU.add),
                                     reads=[a1, a2, ob], writes=[ob])
                                P.dma("sync", dr[row0:row0 + 128, tok], ob[:], reads=[ob])

        def ttype(seg, dst):
            c0, n = OFF[seg]
            for b0 in range(0, n, 512):
                nb = min(512, n - b0)
                wb = load_w(c0 + b0, nb)
                for j in range(TH // 128):
                    pa = pacc[cnt["acc"] % 3]
                    cnt["acc"] += 1
                    for c in range(KC):
                        P.op("tensor", lambda e, pa=pa, wb=wb, nb=nb, c=c, j=j: e.matmul(
                            pa[:, 0:nb], lhsT=hT[:, c, j * 128:(j + 1) * 128], rhs=wb[:, c, 0:nb],
                            start=(c == 0), stop=(c == KC - 1)),
                            reads=[wb, hviews[j][0], hviews[j][1]], writes=[pa], signal=(c == KC - 1))
                    ob = ost[cnt["ost"] % 4]
                    cnt["ost"] += 1
                    P.op("scalar", lambda e, ob=ob, pa=pa, nb=nb: e.copy(out=ob[:, 0:nb], in_=pa[:, 0:nb]), reads=[pa], writes=[ob])
                    P.dma("sync", dst[t0 + j * 128:t0 + (j + 1) * 128, b0:b0 + nb], ob[:, 0:nb], reads=[ob])

        import os
        SEGS = os.environ.get("SEGS", "")
        plan = [("nq", "f", None, ("rope", k.qn, k.qr)), ("kc", "f", k.kcT, "plain"), ("vc", "f", k.vcT, "plain"),
                ("ks", "f", None, ("rope", None, k.ksT)), ("vs", "t", k.vs, None), ("kw", "f", None, ("rope", None, k.kwT)),
                ("vw", "t", k.vw, None), ("ng", "f", k.ngT, "sigmoid"), ("gq", "f", k.gqT, "plain"), ("gk", "f", k.gkT, "plain"),
                ("gk", "t", k.gk, None), ("gv", "t", k.gv, None), ("ga", "f", k.gaT, "plain"), ("gg", "f", k.ggT, "silu")]
        for (sg, ty, dst, mode) in plan:
            if SEGS and (sg + ty) not in SEGS.split(","):
                continue
            if ty == "f":
                ftype(sg, dst, mode)
            else:
                ttype(sg, dst)
        P.flush()
        P.release(m0)


def phase_cmp(k, l):
    P = k.P
    m0 = P.mark()
    aT = [P.sb("aT%d" % i, [128, T], BF16) for i in range(2)]
    w1b = [P.sb("w1b%d" % i, [128, 32, 256], BF16) for i in range(2)]
    w2b = [P.sb("w2b%d" % i, [128, 2, 128], BF16) for i in range(2)]
    peT = [P.sb("peT%d" % i, [128, 32], BF16) for i in range(2)]
    ph = [P.ps("ph%d" % i, [128, 512]) for i in range(2)]
    pc = P.ps("pc", [128, 512])
    po = P.ps("pcmpo", [128, 512])
    cst = P.sb("cst", [128, 2], F32)
    xh = [P.sb("xh%d" % i, [128, 256], F32) for i in range(2)]
    uu = [P.sb("uu%d" % i, [128, 256], F32) for i in range(2)]
    sg = [P.sb("sgm%d" % i, [128, 256], F32) for i in range(2)]
    gb = [P.sb("gb%d" % i, [128, 256], BF16) for i in range(2)]
    ocp = [P.sb("ocp%d" % i, [128, 256], BF16) for i in range(2)]
    for i in range(2):
        P.op("vector", lambda e, i=i: e.memset(gb[i][:], 0.0), writes=[gb[i]])
        P.op("vector", lambda e, i=i: e.memset(ocp[i][:], 0.0), writes=[ocp[i]])
    it = 0
    for si, src in enumerate(("k", "v")):
        w1 = k.cmp_w1[src][l].rearrange("(l d) m -> d l m", d=128)
        for q4 in range(4):
            P.dma("gpsimd", w1b[si][:, q4 * 8:(q4 + 1) * 8, :], w1[:, q4 * 8:(q4 + 1) * 8, :], writes=[w1b[si]])
        P.dma("gpsimd", w2b[si][:], k.cmp_w2[src][l].rearrange("(c p) n -> p c n", p=128), writes=[w2b[si]])
        P.dma("gpsimd", peT[si][:], k.cmp_peT[src][l], writes=[peT[si]])
        srcT = k.kcT if src == "k" else k.vcT
        for hk in range(2):
            a = aT[it % 2]
            oc = ocp[it % 2]
            it += 1
            P.dma("sync", a[:], srcT[hk * 128:(hk + 1) * 128, :], writes=[a])
            for mc in range(2):
                for ll in range(32):
                    P.op("tensor", lambda e, a=a, mc=mc, ll=ll, si=si: e.matmul(
                        ph[mc][:, 0:255], lhsT=w1b[si][:, ll, mc * 128:(mc + 1) * 128], rhs=a[:, ll:ll + 4065:16],
                        start=(ll == 0), stop=(ll == 31)), reads=[w1b[si], a], writes=[ph[mc]], signal=(ll == 31))
                for ll in range(32):
                    P.op("tensor", lambda e, mc=mc, ll=ll, si=si: e.matmul(
                        pc[:, mc:mc + 1], lhsT=w1b[si][:, ll, mc * 128:(mc + 1) * 128], rhs=peT[si][:, ll:ll + 1],
                        start=(ll == 0), stop=(ll == 31)), reads=[w1b[si], peT[si]], writes=[pc], signal=(ll == 31))
            P.op("vector", lambda e: e.tensor_copy(out=cst[:], in_=pc[:, 0:2]), reads=[pc], writes=[cst])
            for mc in range(2):
                x_, u_, s_, g_ = xh[mc], uu[mc], sg[mc], gb[mc]
                P.op("vector", lambda e, x_=x_, mc=mc: e.tensor_scalar(out=x_[:, 0:255], in0=ph[mc][:, 0:255], scalar1=cst[:, mc:mc + 1],
                                                                      scalar2=None, op0=ALU.add), reads=[ph[mc], cst], writes=[x_])
                P.op("vector", lambda e, x_=x_, u_=u_: e.tensor_tensor(out=u_[:, 0:255], in0=x_[:, 0:255], in1=x_[:, 0:255], op=ALU.mult),
                     reads=[x_], writes=[u_])
                P.op("vector", lambda e, u_=u_: e.tensor_scalar(out=u_[:, 0:255], in0=u_[:, 0:255], scalar1=0.044715, scalar2=1.0,
                                                               op0=ALU.mult, op1=ALU.add), reads=[u_], writes=[u_])
                P.op("vector", lambda e, x_=x_, u_=u_: e.tensor_tensor(out=u_[:, 0:255], in0=u_[:, 0:255], in1=x_[:, 0:255], op=ALU.mult),
                     reads=[x_, u_], writes=[u_])
                P.op("scalar", lambda e, u_=u_, s_=s_: e.activation(out=s_[:, 0:255], in_=u_[:, 0:255], func=AF.Sigmoid, scale=1.5957691216057308),
                     reads=[u_], writes=[s_])
                P.op("vector", lambda e, x_=x_, s_=s_, g_=g_: e.tensor_tensor(out=g_[:, 0:255], in0=x_[:, 0:255], in1=s_[:, 0:255], op=ALU.mult),
                     reads=[x_, s_], writes=[g_])
            if src == "k":
                for mc in range(2):
                    P.op("tensor", lambda e, mc=mc, si=si: e.matmul(po[:, 0:255], lhsT=w2b[si][:, mc, :], rhs=gb[mc][:, 0:255],
                                                                   start=(mc == 0), stop=(mc == 1)),
                         reads=[w2b[si], gb[mc]], writes=[po], signal=(mc == 1))
                P.op("scalar", lambda e, oc=oc: e.copy(out=oc[:, 0:255], in_=po[:, 0:255]), reads=[po], writes=[oc])
                P.dma("sync", k.kcmpT[hk], oc[:], reads=[oc])
            else:
                for ct in range(2):
                    for mc in range(2):
                        P.op("tensor", lambda e, mc=mc, ct=ct, si=si: e.matmul(
                            po[:, ct * 128:(ct + 1) * 128], lhsT=gb[mc][:, ct * 128:(ct + 1) * 128], rhs=w2b[si][:, mc, :],
                            start=(mc == 0), stop=(mc == 1)), reads=[w2b[si], gb[mc]], writes=[po], signal=(mc == 1))
                P.op("scalar", lambda e, oc=oc: e.copy(out=oc[:], in_=po[:, 0:256]), reads=[po], writes=[oc])
                P.dma("sync", k.vcmp[hk].rearrange("(c p) d -> p c d", p=128), oc[:].rearrange("p (c d) -> p c d", c=2), reads=[oc])
    P.flush()
    P.release(m0)


def phase_nsa(k, l, hks=(0, 1)):
    P = k.P
    m0 = P.mark()
    cst_f = {}
    for nm, shp in (("cb", [128, 128]), ("wbm", [128, 128]), ("esel", [64, 32 * 128]), ("ovl", [128, 128]), ("sel", [12, 12 * 128]),
                    ("ident", [128, 128])):
        b = P.sb("c_" + nm, shp, BF16)
        P.dma("gpsimd", b[:], getattr(k, nm), writes=[b])
        cst_f[nm] = b
    cb, wbm, esel, ovl, sel, identb = (cst_f[n] for n in ("cb", "wbm", "esel", "ovl", "sel", "ident"))
    identf = P.sb("identf", [128, 128], F32)
    P.dma("sync", identf[:], k.ident, writes=[identf])
    mb = P.sb("mb", [128, 128], F32)
    P.dma("sync", mb[:], k.mb, writes=[mb])
    keep = P.sb("keep", [128, 32 * 64], F32)
    addc = P.sb("addc", [128, 32 * 64], F32)
    P.dma("sync", keep[:], k.keep, writes=[keep])
    P.dma("sync", addc[:], k.addc, writes=[addc])
    onesb = P.sb("onesb", [128, 128], BF16)
    P.op("vector", lambda e: e.memset(onesb[:], 1.0), writes=[onesb])

    ksT = P.sb("ksT", [128, T], BF16)
    kwT = P.sb("kwT", [128, T], BF16)
    vs = P.sb("vs", [128, 32, 128], BF16)
    vw = P.sb("vw", [128, 32, 128], BF16)
    kcm = P.sb("kcm", [128, 256], BF16)
    vcm = P.sb("vcm", [128, 2, 128], BF16)
    sgt = P.sb("sgt", [12, T], BF16)
    qnb = [P.sb("qnb%d" % i, [128, 4, 512], BF16) for i in range(2)]
    qrb = [P.sb("qrb%d" % i, [128, 4, 512], BF16) for i in range(2)]
    S = [P.ps("S%d" % i, [128, 512]) for i in range(2)]
    BD = [P.ps("BD%d" % i, [128, 512]) for i in range(2)]
    BO = [P.ps("BO%d" % i, [128, 512]) for i in range(2)]
    BG = P.ps("BG", [128, 512])
    BM = P.ps("BM", [128, 512])
    pT = [P.sb("pT%d" % i, [128, 512], BF16) for i in range(3)]
    pTc = [P.sb("pTc%d" % i, [128, 512], BF16) for i in range(2)]
    pn = [P.sb("pn%d" % i, [128, 512], BF16) for i in range(2)]
    bc = [P.sb("bc%d" % i, [128, 128], BF16) for i in range(2)]
    W = [P.sb("W%d" % i, [128, 512], F32) for i in range(2)]
    Wg = [P.sb("Wg%d" % i, [128, 512], F32) for i in range(2)]
    tmp = [P.sb("tmp%d" % i, [128, 512], F32) for i in range(2)]
    acc = [P.sb("acc%d" % i, [128, 512], F32) for i in range(2)]
    accb = [P.sb("accb%d" % i, [128, 512], BF16) for i in range(2)]
    impT = P.sb("impT", [64, 128], F32)
    imp = P.sb("imp", [128, 64], F32)
    imp2 = P.sb("imp2", [128, 64], F32)
    m8a = P.sb("m8a", [128, 8], F32)
    m8b = P.sb("m8b", [128, 8], F32)
    bias = P.sb("bias", [128, 64], BF16)
    biasT = P.sb("biasT", [64, 128], BF16)
    cnt = {"s": 0, "pt": 0, "bc": 0, "set": 0, "w": 0}

    def rhs4(buf, qi):
        return buf[:, :, qi * 128:(qi + 1) * 128]

    def as4(ap):
        return ap.rearrange("p (g t) -> p g t", g=4)

    def bcast4(ap, np_):
        return ap.unsqueeze(1).to_broadcast([np_, 4, 128])

    for hk in hks:
        P.dma("sync", ksT[:], k.ksT[hk * 128:(hk + 1) * 128, :], writes=[ksT])
        P.dma("sync", kwT[:], k.kwT[hk * 128:(hk + 1) * 128, :], writes=[kwT])
        for q4 in range(4):
            P.dma("sync", vs[:, q4 * 8:(q4 + 1) * 8, :],
                  k.vs[q4 * 1024:(q4 + 1) * 1024, hk * 128:(hk + 1) * 128].rearrange("(t p) d -> p t d", p=128), writes=[vs])
            P.dma("sync", vw[:, q4 * 8:(q4 + 1) * 8, :],
                  k.vw[q4 * 1024:(q4 + 1) * 1024, hk * 128:(hk + 1) * 128].rearrange("(t p) d -> p t d", p=128), writes=[vw])
        P.dma("sync", kcm[:], k.kcmpT[hk], writes=[kcm])
        P.dma("sync", vcm[:], k.vcmp[hk].rearrange("(c p) d -> p c d", p=128), writes=[vcm])
        P.dma("sync", sgt[:], k.ngT[hk * 12:(hk + 1) * 12, :], writes=[sgt])
        for qt in range(T // 128):
            qb, qi = qt // 4, qt % 4
            qn_, qr_ = qnb[qb % 2], qrb[qb % 2]
            if qi == 0:
                tok = slice(qb * 512, (qb + 1) * 512)
                P.dma("sync", qn_[:], k.qn[hk * 512:(hk + 1) * 512, tok].rearrange("(g d) t -> d g t", d=128), writes=[qn_])
                P.dma("sync", qr_[:], k.qr[hk * 512:(hk + 1) * 512, tok].rearrange("(g d) t -> d g t", d=128), writes=[qr_])
            tsl = slice(qt * 128, (qt + 1) * 128)

            def gates(j):
                for g in range(4):
                    r = 3 * g + j
                    P.op("tensor", lambda e, g=g, r=r: e.matmul(BG[:, g * 128:(g + 1) * 128], lhsT=sel[:, r * 128:(r + 1) * 128],
                                                                rhs=sgt[:, tsl], start=True, stop=True),
                         reads=[sel, sgt], writes=[BG], signal=(g == 3))

            def combine(st, first, w_):
                wg = Wg[cnt["w"] % 2]
                tp = tmp[cnt["w"] % 2]
                cnt["w"] += 1
                ac = acc[qt % 2]
                P.op("vector", lambda e, wg=wg, w_=w_: e.tensor_tensor(out=wg[:], in0=BG[:], in1=w_[:], op=ALU.mult),
                     reads=[BG, w_], writes=[wg])
                if first:
                    P.op("vector", lambda e, wg=wg, ac=ac, st=st: e.tensor_tensor(out=ac[:], in0=BO[st][:], in1=wg[:], op=ALU.mult),
                         reads=[BO[st], wg], writes=[ac])
                else:
                    P.op("vector", lambda e, wg=wg, tp=tp, st=st: e.tensor_tensor(out=tp[:], in0=BO[st][:], in1=wg[:], op=ALU.mult),
                         reads=[BO[st], wg], writes=[tp])
                    P.op("gpsimd", lambda e, tp=tp, ac=ac: e.tensor_tensor(out=ac[:], in0=ac[:], in1=tp[:], op=ALU.add),
                         reads=[ac, tp], writes=[ac])

            st = cnt["set"] % 2
            cnt["set"] += 1
            nct = 1 if qt <= 15 else 2
            for ct in range(nct):
                s_ = S[cnt["s"] % 2]
                cnt["s"] += 1
                need_mask = not (ct == 0 and qt >= 17)
                P.op("tensor", lambda e, s_=s_, ct=ct, qn_=qn_: e.matmul(as4(s_[:]), lhsT=kcm[:, ct * 128:(ct + 1) * 128], rhs=rhs4(qn_, qi),
                                                                        start=True, stop=not need_mask),
                     reads=[kcm, qn_], writes=[s_], signal=not need_mask)
                if need_mask:
                    b_ = bc[cnt["bc"] % 2]
                    cnt["bc"] += 1
                    thr = float(128 * qt - 2048 * ct - 31)
                    P.op("gpsimd", lambda e, b_=b_, thr=thr: e.tensor_scalar(out=b_[:], in0=mb[:], scalar1=thr, scalar2=NEG, op0=ALU.is_gt, op1=ALU.mult),
                         reads=[mb], writes=[b_])
                    P.op("tensor", lambda e, s_=s_, b_=b_: e.matmul(as4(s_[:]), lhsT=identb[:], rhs=bcast4(b_[:], 128), start=False, stop=True),
                         reads=[identb, b_], writes=[s_])
                p_ = pTc[ct]
                P.op("scalar", lambda e, s_=s_, p_=p_: e.activation(out=p_[:], in_=s_[:], func=AF.Exp, scale=SCALE), reads=[s_], writes=[p_])
                P.op("tensor", lambda e, p_=p_, st=st, ct=ct: e.matmul(BD[st][:], lhsT=onesb[:], rhs=p_[:], start=(ct == 0), stop=(ct == nct - 1)),
                     reads=[onesb, p_], writes=[BD[st]], signal=(ct == nct - 1))
                P.op("tensor", lambda e, p_=p_, st=st, ct=ct: e.matmul(BO[st][:], lhsT=vcm[:, ct, :], rhs=p_[:], start=(ct == 0), stop=(ct == nct - 1)),
                     reads=[vcm, p_], writes=[BO[st]], signal=(ct == nct - 1))
            w_ = W[cnt["w"] % 2]
            P.op("vector", lambda e, w_=w_, st=st: e.tensor_scalar(out=w_[:], in0=BD[st][:], scalar1=1e-30, scalar2=None, op0=ALU.add),
                 reads=[BD[st]], writes=[w_])
            P.op("vector", lambda e, w_=w_: e.reciprocal(out=w_[:], in_=w_[:]), reads=[w_], writes=[w_])
            if qt >= 8:
                for ct in range(nct):
                    P.op("gpsimd", lambda e, ct=ct, w_=w_: e.tensor_tensor(out=pn[ct][:], in0=pTc[ct][:], in1=w_[:], op=ALU.mult),
                         reads=[pTc[ct], w_], writes=[pn[ct]])
                n_mm = nct * 4
                i_mm = 0
                for ct in range(nct):
                    for g in range(4):
                        P.op("tensor", lambda e, ct=ct, g=g, i_mm=i_mm: e.matmul(BM[0:64, 0:128], lhsT=ovl[:, ct * 64:(ct + 1) * 64],
                                                                                rhs=pn[ct][:, g * 128:(g + 1) * 128],
                                                                                start=(i_mm == 0), stop=(i_mm == n_mm - 1)),
                             reads=[ovl, pn[ct]], writes=[BM], signal=(i_mm == n_mm - 1))
                        i_mm += 1
                P.op("vector", lambda e: e.tensor_copy(out=impT[:], in_=BM[0:64, 0:128]), reads=[BM], writes=[impT])
                P.op("tensor", lambda e: e.transpose(out=BM[:, 128:192], in_=impT[:], identity=identf[0:64, 0:64]),
                     reads=[impT, identf], writes=[BM])
                P.op("vector", lambda e: e.tensor_tensor(out=imp[:], in0=BM[:, 128:192], in1=keep[:, qt * 64:(qt + 1) * 64], op=ALU.mult),
                     reads=[BM, keep], writes=[imp])
                P.op("vector", lambda e: e.tensor_tensor(out=imp[:], in0=imp[:], in1=addc[:, qt * 64:(qt + 1) * 64], op=ALU.add),
                     reads=[imp, addc], writes=[imp])
                P.op("vector", lambda e: e.max(out=m8a[:], in_=imp[:]), reads=[imp], writes=[m8a])
                P.op("vector", lambda e: e.match_replace(out=imp2[:], in_to_replace=m8a[:], in_values=imp[:], imm_value=-3.0e38),
                     reads=[imp, m8a], writes=[imp2])
                P.op("vector", lambda e: e.max(out=m8b[:], in_=imp2[:]), reads=[imp2], writes=[m8b])
                P.op("vector", lambda e: e.tensor_scalar(out=bias[:], in0=imp[:], scalar1=m8b[:, 7:8], scalar2=NEG, op0=ALU.is_lt, op1=ALU.mult),
                     reads=[imp, m8b], writes=[bias])
                P.op("tensor", lambda e: e.transpose(out=BM[0:64, 256:320].bitcast(BF16), in_=bias[:], identity=identb[:]),
                     reads=[bias, identb], writes=[BM])
                P.op("vector", lambda e: e.tensor_copy(out=biasT[:], in_=BM[0:64, 256:320].bitcast(BF16)), reads=[BM], writes=[biasT])
            gates(0)
            combine(st, True, w_)

            def branch(kT_, v_, kts, kind):
                st = cnt["set"] % 2
                cnt["set"] += 1
                nk = len(kts)
                for ii, kt in enumerate(kts):
                    s_ = S[cnt["s"] % 2]
                    cnt["s"] += 1
                    extra = []
                    if kind == "slc":
                        if qt >= 8:
                            extra.append(("sel", kt))
                        if kt == qt:
                            extra.append(("cb", None))
                    else:
                        if kt == qt:
                            extra.append(("cb", None))
                        if kt == qt - 4:
                            extra.append(("wb", None))
                    P.op("tensor", lambda e, s_=s_, kt=kt, kT_=kT_: e.matmul(as4(s_[:]), lhsT=kT_[:, kt * 128:(kt + 1) * 128], rhs=rhs4(qr_, qi),
                                                                            start=True, stop=(len(extra) == 0)),
                         reads=[kT_, qr_], writes=[s_], signal=(len(extra) == 0))
                    for xi, (xk, xa) in enumerate(extra):
                        last = (xi == len(extra) - 1)
                        if xk == "sel":
                            P.op("tensor", lambda e, s_=s_, xa=xa, last=last: e.matmul(as4(s_[:]), lhsT=esel[:, xa * 128:(xa + 1) * 128],
                                                                                      rhs=bcast4(biasT[:], 64), start=False, stop=last),
                                 reads=[esel, biasT], writes=[s_], signal=last)
                        else:
                            mk = cb if xk == "cb" else wbm
                            P.op("tensor", lambda e, s_=s_, mk=mk, last=last: e.matmul(as4(s_[:]), lhsT=identb[:], rhs=bcast4(mk[:], 128),
                                                                                      start=False, stop=last),
                                 reads=[identb, mk], writes=[s_], signal=last)
                    p_ = pT[cnt["pt"] % 3]
                    cnt["pt"] += 1
                    P.op("scalar", lambda e, s_=s_, p_=p_: e.activation(out=p_[:], in_=s_[:], func=AF.Exp, scale=SCALE), reads=[s_], writes=[p_])
                    P.op("tensor", lambda e, p_=p_, st=st, ii=ii: e.matmul(BD[st][:], lhsT=onesb[:], rhs=p_[:], start=(ii == 0), stop=(ii == nk - 1)),
                         reads=[onesb, p_], writes=[BD[st]], signal=(ii == nk - 1))
                    P.op("tensor", lambda e, p_=p_, st=st, ii=ii, kt=kt, v_=v_: e.matmul(BO[st][:], lhsT=v_[:, kt, :], rhs=p_[:], start=(ii == 0), stop=(ii == nk - 1)),
                         reads=[v_, p_], writes=[BO[st]], signal=(ii == nk - 1))
                w2 = W[cnt["w"] % 2]
                P.op("vector", lambda e, w2=w2, st=st: e.reciprocal(out=w2[:], in_=BD[st][:]), reads=[BD[st]], writes=[w2])
                return st, w2

            st, w2 = branch(kwT, vw, list(range(max(0, qt - 4), qt + 1)), "win")
            gates(2)
            combine(st, False, w2)
            st, w2 = branch(ksT, vs, list(range(0, qt + 1)), "slc")
            gates(1)
            combine(st, False, w2)
            ac = acc[qt % 2]
            ab = accb[qt % 2]
            P.op("gpsimd", lambda e, ac=ac, ab=ab: e.tensor_copy(out=ab[:], in_=ac[:]), reads=[ac], writes=[ab])
            P.dma("sync", k.mixT[hk * 512:(hk + 1) * 512, tsl].rearrange("(g d) t -> d g t", d=128), as4(ab[:]), reads=[ab])
    P.flush()
    P.release(m0)


def phase_gla(k, l):
    P = k.P
    m0 = P.mark()
    GS = 128 ** -0.5
    wa2 = P.sb("wa2", [16, 512], BF16)
    brow = P.sb("brow", [1, 512], BF16)
    ones1 = P.sb("ones1", [1, 128], BF16)
    tri2f = P.sb("tri2f", [128, 128], F32)
    suf = P.sb("suf", [128, 128], F32)
    onesb = P.sb("onesb", [128, 128], BF16)
    nw = P.sb("nw", [128, 2], F32)
    P.dma("gpsimd", wa2[:], k.gla_w_a2[l], writes=[wa2])
    P.dma("gpsimd", brow[:], k.gla_b_a[l:l + 1, :], writes=[brow])
    P.dma("sync", tri2f[:], k.tri2, writes=[tri2f])
    P.dma("sync", suf[:], k.sumat, writes=[suf])
    P.dma("sync", nw[:], k.gla_nwT[l], writes=[nw])
    P.op("vector", lambda e: e.memset(ones1[:], 1.0), writes=[ones1])
    P.op("vector", lambda e: e.memset(onesb[:], 1.0), writes=[onesb])
    Sf = P.sb("Sf", [128, 4, 256], F32)
    Sb = P.sb("Sb", [128, 4, 256], BF16)
    Sfv = [P.view(Sf) for _ in range(4)]
    Sbv = [P.view(Sb) for _ in range(4)]
    for h in range(4):
        P.op("vector", lambda e, h=h: e.memset(Sf[:, h, :], 0.0), writes=[Sfv[h]])
        P.op("vector", lambda e, h=h: e.memset(Sb[:, h, :], 0.0), writes=[Sbv[h]])
    gqTb = [P.sb("gqTb%d" % i, [128, 4, 512], BF16) for i in range(2)]
    gkTb = [P.sb("gkTb%d" % i, [128, 4, 512], BF16) for i in range(2)]
    ggb = [P.sb("ggb%d" % i, [128, 8, 512], BF16) for i in range(2)]
    gaTb = [P.sb("gaTb%d" % i, [16, 512], BF16) for i in range(2)]
    gkt = [P.sb("gkt%d" % i, [128, 512], BF16) for i in range(2)]
    gvt = [P.sb("gvt%d" % i, [128, 1024], BF16) for i in range(2)]
    Lt = [P.sb("Lt%d" % i, [128, 512], F32) for i in range(2)]
    E1 = [P.sb("E1%d" % i, [128, 512], F32) for i in range(2)]
    kst = [P.sb("kst%d" % i, [128, 512], BF16) for i in range(2)]
    EbT = [P.sb("EbT%d" % i, [128, 128], F32) for i in range(2)]
    EnbT = [P.sb("EnbT%d" % i, [128, 128], F32) for i in range(2)]
    qdT = [P.sb("qdT%d" % i, [128, 128], BF16) for i in range(2)]
    kiT = [P.sb("kiT%d" % i, [128, 128], BF16) for i in range(2)]
    ATm = [P.sb("ATm%d" % i, [128, 128], BF16) for i in range(2)]
    o1 = [P.sb("o1%d" % i, [128, 256], F32) for i in range(2)]
    sq = [P.sb("sq%d" % i, [128, 256], BF16) for i in range(2)]
    lnr = [P.sb("lnr%d" % i, [128, 128], F32) for i in range(2)]
    rstd = [P.sb("rstd%d" % i, [128, 128], F32) for i in range(2)]
    tmpo = [P.sb("tmpo%d" % i, [128, 256], F32) for i in range(2)]
    outb = [P.sb("outb%d" % i, [128, 2, 128], BF16) for i in range(2)]
    pz = P.ps("pz", [128, 512])
    pcs = P.ps("pcs", [128, 512])
    psu = P.ps("psu", [128, 512])
    pcsT = P.ps("pcsT", [128, 512])
    pAT = P.ps("pAT", [128, 512])
    po = P.ps("po", [128, 512])
    pS = P.ps("pS", [128, 512])
    pss = P.ps("pss", [128, 512])
    hc = 0
    for tt in range(T // 128):
        tb, ti = tt // 4, tt % 4
        gq_, gk_, gg_, ga_ = gqTb[tb % 2], gkTb[tb % 2], ggb[tb % 2], gaTb[tb % 2]
        if ti == 0:
            tok = slice(tb * 512, (tb + 1) * 512)
            P.dma("sync", gq_[:], k.gqT[:, tok].rearrange("(h d) t -> d h t", d=128), writes=[gq_])
            P.dma("sync", gk_[:], k.gkT[:, tok].rearrange("(h d) t -> d h t", d=128), writes=[gk_])
            P.dma("sync", gg_[:], k.ggT[:, tok].rearrange("(c d) t -> d c t", d=128), writes=[gg_])
            P.dma("sync", ga_[:], k.gaT[:, tok], writes=[ga_])
        lsl = slice(ti * 128, (ti + 1) * 128)
        tsl = slice(tt * 128, (tt + 1) * 128)
        gkt_, gvt_ = gkt[tt % 2], gvt[tt % 2]
        P.dma("sync", gkt_[:], k.gk[tsl, :], writes=[gkt_])
        P.dma("sync", gvt_[:], k.gv[tsl, :], writes=[gvt_])
        L_, E1_, kst_ = Lt[tt % 2], E1[tt % 2], kst[tt % 2]
        P.op("tensor", lambda e: e.matmul(pz[:], lhsT=ga_[:, lsl], rhs=wa2[:], start=True, stop=False), reads=[ga_, wa2], writes=[pz], signal=False)
        P.op("tensor", lambda e: e.matmul(pz[:], lhsT=ones1[:], rhs=brow[:], start=False, stop=True), reads=[ones1, brow], writes=[pz])
        P.op("scalar", lambda e: e.activation(out=L_[:], in_=pz[:], func=AF.Exp, scale=-1.0), reads=[pz], writes=[L_])
        P.op("scalar", lambda e: e.activation(out=L_[:], in_=L_[:], func=AF.Ln, bias=1.0), reads=[L_], writes=[L_])
        P.op("tensor", lambda e: e.matmul(pcs[:], lhsT=tri2f[:], rhs=L_[:], start=True, stop=True), reads=[tri2f, L_], writes=[pcs])
        P.op("tensor", lambda e: e.matmul(psu[:], lhsT=suf[:], rhs=L_[:], start=True, stop=True), reads=[suf, L_], writes=[psu])
        P.op("scalar", lambda e: e.activation(out=E1_[:], in_=psu[:], func=AF.Exp, scale=-1.0 / 16.0), reads=[psu], writes=[E1_])
        P.op("vector", lambda e: e.tensor_tensor(out=kst_[:], in0=gkt_[:], in1=E1_[:], op=ALU.mult), reads=[gkt_, E1_], writes=[kst_])
        bufsel = {}
        for h in range(4):
            i2 = hc % 2
            hc += 1
            bufsel[h] = i2

        def gla_front(h):
            i2 = bufsel[h]
            Eb_, Enb_, qd_, ki_, AT_ = EbT[i2], EnbT[i2], qdT[i2], kiT[i2], ATm[i2]
            hs = slice(h * 128, (h + 1) * 128)
            P.op("tensor", lambda e: e.matmul(pcsT[:, 0:128], lhsT=L_[:, hs], rhs=tri2f[:], start=True, stop=True),
                 reads=[L_, tri2f], writes=[pcsT])
            P.op("scalar", lambda e: e.activation(out=Eb_[:], in_=pcsT[:, 0:128], func=AF.Exp, scale=-1.0 / 16.0), reads=[pcsT], writes=[Eb_])
            P.op("scalar", lambda e: e.activation(out=Enb_[:], in_=pcsT[:, 0:128], func=AF.Exp, scale=1.0 / 16.0), reads=[pcsT], writes=[Enb_])
            P.op("vector", lambda e: e.scalar_tensor_tensor(out=qd_[:], in0=gq_[:, h, lsl], scalar=GS, in1=Eb_[:], op0=ALU.mult, op1=ALU.mult),
                 reads=[gq_, Eb_], writes=[qd_])
            P.op("gpsimd", lambda e: e.tensor_tensor(out=ki_[:], in0=gk_[:, h, lsl], in1=Enb_[:], op=ALU.mult), reads=[gk_, Enb_], writes=[ki_])
            P.op("tensor", lambda e: e.matmul(pAT[:, 0:128], lhsT=ki_[:], rhs=qd_[:], start=True, stop=True), reads=[ki_, qd_], writes=[pAT])
            P.op("vector", lambda e: e.tensor_tensor(out=AT_[:], in0=pAT[:, 0:128], in1=tri2f[:], op=ALU.mult), reads=[pAT, tri2f], writes=[AT_])

        def gla_back(h):
            i2 = bufsel[h]
            Eb_, Enb_, qd_, ki_, AT_ = EbT[i2], EnbT[i2], qdT[i2], kiT[i2], ATm[i2]
            o1_, sq_, lnr_, rs_, tp_, ob_ = o1[i2], sq[i2], lnr[i2], rstd[i2], tmpo[i2], outb[i2]
            hs = slice(h * 128, (h + 1) * 128)
            for hf in range(2):
                cs_ = slice(hf * 64, (hf + 1) * 64)
                for dvc in range(2):
                    oc = slice(dvc * 128 + hf * 64, dvc * 128 + hf * 64 + 64)
                    P.op("tensor", lambda e: e.matmul(po[:, oc], lhsT=gvt_[:, h * 256 + dvc * 128:h * 256 + dvc * 128 + 128], rhs=AT_[:, cs_],
                                                      start=True, stop=False), reads=[gvt_, AT_], writes=[po], signal=False)
                    P.op("tensor", lambda e: e.matmul(po[:, oc], lhsT=Sb[:, h, dvc * 128:(dvc + 1) * 128], rhs=qd_[:, cs_],
                                                      start=False, stop=True), reads=[Sbv[h], qd_], writes=[po],
                         signal=(hf == 1 and dvc == 1))
                P.op("tensor", lambda e: e.matmul(pS[:, 0:256], lhsT=kst_[cs_, hs], rhs=gvt_[cs_, h * 256:(h + 1) * 256], start=True, stop=True),
                     reads=[kst_, gvt_], writes=[pS])
                col = hf * 64 + 63
                P.op("vector", lambda e: e.scalar_tensor_tensor(out=Sf[:, h, :], in0=Sf[:, h, :], scalar=Eb_[:, col:col + 1], in1=pS[:, 0:256],
                                                                op0=ALU.mult, op1=ALU.add), reads=[Sfv[h], Eb_, pS], writes=[Sfv[h]])
                P.op("gpsimd", lambda e: e.tensor_copy(out=Sb[:, h, :], in_=Sf[:, h, :]), reads=[Sfv[h]], writes=[Sbv[h]])
            P.op("scalar", lambda e: e.copy(out=o1_[:], in_=po[:, 0:256]), reads=[po], writes=[o1_])
            P.op("gpsimd", lambda e: e.tensor_tensor(out=sq_[:], in0=o1_[:], in1=o1_[:], op=ALU.mult), reads=[o1_], writes=[sq_])
            P.op("tensor", lambda e: e.matmul(pss[:, 0:128], lhsT=onesb[:], rhs=sq_[:, 0:128], start=True, stop=False), reads=[onesb, sq_], writes=[pss], signal=False)
            P.op("tensor", lambda e: e.matmul(pss[:, 0:128], lhsT=onesb[:], rhs=sq_[:, 128:256], start=False, stop=True), reads=[onesb, sq_], writes=[pss])
            P.op("scalar", lambda e: e.activation(out=lnr_[:], in_=pss[:, 0:128], func=AF.Ln, scale=1.0 / 256.0, bias=NORM_EPS), reads=[pss], writes=[lnr_])
            P.op("scalar", lambda e: e.activation(out=rs_[:], in_=lnr_[:], func=AF.Exp, scale=-0.5), reads=[lnr_], writes=[rs_])
            for dvc in range(2):
                ds_ = slice(dvc * 128, (dvc + 1) * 128)
                P.op("vector", lambda e: e.tensor_tensor(out=tp_[:, ds_], in0=o1_[:, ds_], in1=rs_[:], op=ALU.mult), reads=[o1_, rs_], writes=[tp_])
                P.op("vector", lambda e: e.scalar_tensor_tensor(out=ob_[:, dvc, :], in0=gg_[:, h * 2 + dvc, lsl], scalar=nw[:, dvc:dvc + 1], in1=tp_[:, ds_],
                                                                op0=ALU.mult, op1=ALU.mult), reads=[gg_, nw, tp_], writes=[ob_])
            P.dma("sync", k.mixT[1024 + h * 256:1024 + (h + 1) * 256, tsl].rearrange("(c d) t -> d c t", d=128), ob_[:], reads=[ob_])

        gla_front(0)
        for h in range(4):
            if h < 3:
                gla_front(h + 1)
            gla_back(h)
    P.flush()
    P.release(m0)


def ln_tile(P, r, lng, lnb, stats, mv, rstd):
    for c in range(4):
        P.op("vector", lambda e, c=c: e.bn_stats(out=stats[:, c, :], in_=r[:, c * 512:(c + 1) * 512]), reads=[r], writes=[stats])
    P.op("vector", lambda e: e.bn_aggr(out=mv[:], in_=stats[:]), reads=[stats], writes=[mv])
    P.op("scalar", lambda e: e.activation(out=rstd[:], in_=mv[:, 1:2], func=AF.Sqrt, bias=LN_EPS), reads=[mv], writes=[rstd])
    P.op("vector", lambda e: e.reciprocal(out=rstd[:], in_=rstd[:]), reads=[rstd], writes=[rstd])
    P.op("vector", lambda e: e.tensor_scalar(out=r[:], in0=r[:], scalar1=mv[:, 0:1], scalar2=rstd[:, 0:1], op0=ALU.subtract, op1=ALU.mult),
         reads=[r, mv, rstd], writes=[r])
    P.op("gpsimd", lambda e: e.tensor_tensor(out=r[:], in0=r[:], in1=lng[:], op=ALU.mult), reads=[r, lng], writes=[r])
    P.op("gpsimd", lambda e: e.tensor_tensor(out=r[:], in0=r[:], in1=lnb[:], op=ALU.add), reads=[r, lnb], writes=[r])


def phase_wout(k, l, xsrc, xdst):
    P = k.P
    m0 = P.mark()
    wo = P.sb("wo", [128, KC, D], BF16)
    wv = k.w_out[l].rearrange("(c p) n -> p c n", p=128)
    for q in range(4):
        P.dma("gpsimd", wo[:, :, q * 512:(q + 1) * 512], wv[:, :, q * 512:(q + 1) * 512], writes=[wo])
    garep = P.sb("garep", [128, D], F32)
    lng = P.sb("lng", [128, D], F32)
    lnb = P.sb("lnb", [128, D], F32)
    P.dma("sync", garep[:], k.modv[l, 2 * D:3 * D].partition_broadcast(128), writes=[garep])
    P.dma("sync", lng[:], k.ln_mix_g[l].partition_broadcast(128), writes=[lng])
    P.dma("sync", lnb[:], k.ln_mix_b[l].partition_broadcast(128), writes=[lnb])
    mixb = [P.sb("mixb%d" % i, [128, KC, 512], BF16) for i in range(2)]
    xt = [P.sb("xt%d" % i, [128, D], F32) for i in range(2)]
    rt = [P.sb("rt%d" % i, [128, D], F32) for i in range(2)]
    stats = P.sb("stats", [128, 4, 6], F32)
    mv = P.sb("mv", [128, 2], F32)
    rstd = P.sb("rstd", [128, 1], F32)
    py = [P.ps("py%d" % i, [128, 512]) for i in range(8)]
    mixv = k.mixT.rearrange("(c p) t -> p c t", p=128)
    for tt in range(T // 128):
        tb, ti = tt // 4, tt % 4
        mb_ = mixb[tb % 2]
        if ti == 0:
            for q in range(4):
                P.dma("sync", mb_[:, q * 4:(q + 1) * 4, :], mixv[:, q * 4:(q + 1) * 4, tb * 512:(tb + 1) * 512], writes=[mb_])
        x_ = xt[tt % 2]
        r_ = rt[tt % 2]
        tsl = slice(tt * 128, (tt + 1) * 128)
        P.dma("sync", x_[:], xsrc[tsl, :], writes=[x_])
        for db in range(4):
            p_ = py[(tt % 2) * 4 + db]
            for c in range(KC):
                P.op("tensor", lambda e: e.matmul(p_[:], lhsT=mb_[:, c, ti * 128:(ti + 1) * 128], rhs=wo[:, c, db * 512:(db + 1) * 512],
                                                  start=(c == 0), stop=(c == KC - 1)), reads=[mb_, wo], writes=[p_], signal=(c == KC - 1))
            P.op("vector", lambda e: e.tensor_tensor(out=r_[:, db * 512:(db + 1) * 512], in0=p_[:], in1=garep[:, db * 512:(db + 1) * 512], op=ALU.mult),
                 reads=[p_, garep], writes=[r_])
        P.op("vector", lambda e: e.scalar_tensor_tensor(out=r_[:], in0=x_[:], scalar=ALPHA, in1=r_[:], op0=ALU.mult, op1=ALU.add),
             reads=[x_, r_], writes=[r_])
        ln_tile(P, r_, lng, lnb, stats, mv, rstd)
        P.dma("sync", xdst[tsl, :], r_[:], reads=[r_])
    P.flush()
    P.release(m0)


def ffn_gateup(k, src, nrows, wg, wu, dff, AT, mod=None, src_bf16=False):
    P = k.P
    RH = min(nrows, 2048)
    for r0 in range(0, nrows, RH):
        nr = min(RH, nrows - r0)
        m0 = P.mark()
        identf = P.sb("identf", [128, 128], F32)
        P.dma("sync", identf[:], k.ident, writes=[identf])
        hT = P.sb("hT", [128, KC, RH], BF16)
        hviews = [(P.view(hT), P.view(hT)) for _ in range(nr // 128)]
        ptr = [P.ps("ptr%d" % i, [128, 4, 128]) for i in range(2)]
        if mod is not None:
            l = mod
            screp = P.sb("screp", [128, D], F32)
            shrep = P.sb("shrep", [128, D], F32)
            xt = [P.sb("xt%d" % i, [128, D], F32) for i in range(2)]
            P.dma("sync", screp[:], k.modv[l, 4 * D:5 * D].partition_broadcast(128), writes=[screp])
            P.dma("sync", shrep[:], k.modv[l, 3 * D:4 * D].partition_broadcast(128), writes=[shrep])
            build_hT(k, src, r0, nr // 128, screp, shrep, identf, hT, hviews, xt, ptr)
        else:
            identb = P.sb("identb", [128, 128], BF16)
            P.dma("gpsimd", identb[:], k.ident, writes=[identb])
            xt = [P.sb("xtb%d" % i, [128, D], BF16) for i in range(2)]
            for j in range(nr // 128):
                xb = xt[j % 2]
                P.dma("sync", xb[:], src[r0 + j * 128:r0 + (j + 1) * 128, :], writes=[xb])
                for g in range(KC // 4):
                    pt = ptr[(j * 4 + g) % 2]
                    ptb = pt[:].rearrange("p q t -> p (q t)")[:, 0:256].bitcast(BF16).rearrange("p (q t) -> p q t", q=4)
                    for q in range(4):
                        c = g * 4 + q
                        P.op("tensor", lambda e: e.transpose(out=ptb[:, q, :], in_=xb[:, c * 128:(c + 1) * 128], identity=identb[:]),
                             reads=[xb, identb], writes=[pt], signal=(q == 3))
                    if g % 2 == 0:
                        P.op("scalar", lambda e: e.copy(out=hT[:, g * 4:(g + 1) * 4, j * 128:(j + 1) * 128], in_=ptb), reads=[pt], writes=[hviews[j][0]])
                    else:
                        P.op("vector", lambda e: e.tensor_copy(out=hT[:, g * 4:(g + 1) * 4, j * 128:(j + 1) * 128], in_=ptb), reads=[pt], writes=[hviews[j][1]])
        wgb = [P.sb("wgb%d" % i, [128, KC, 256], BF16) for i in range(2)]
        wub = [P.sb("wub%d" % i, [128, KC, 256], BF16) for i in range(2)]
        pg = [P.ps("pg%d" % i, [128, 512]) for i in range(2)]
        pu = [P.ps("pu%d" % i, [128, 512]) for i in range(2)]
        sgb = [P.sb("sgb%d" % i, [128, 512], BF16) for i in range(2)]
        ab = [P.sb("ab%d" % i, [128, 512], BF16) for i in range(3)]
        wgv = wg.rearrange("(c p) n -> p c n", p=128)
        wuv = wu.rearrange("(c p) n -> p c n", p=128)
        blocks = [(b0, min(512, nr - b0)) for b0 in range(0, nr, 512)]
        it = 0
        for fb in range(dff // 256):
            wg_, wu_ = wgb[fb % 2], wub[fb % 2]
            P.dma("gpsimd", wg_[:], wgv[:, :, fb * 256:(fb + 1) * 256], writes=[wg_])
            P.dma("gpsimd", wu_[:], wuv[:, :, fb * 256:(fb + 1) * 256], writes=[wu_])
            for ft in range(2):
                f0 = fb * 256 + ft * 128
                for (b0, bn) in blocks:
                    hr = []
                    for j in range(b0 // 128, (b0 + bn) // 128):
                        hr += [hviews[j][0], hviews[j][1]]
                    pg_, pu_ = pg[it % 2], pu[it % 2]
                    sg_, a_ = sgb[it % 2], ab[it % 3]
                    it += 1
                    for c in range(KC):
                        P.op("tensor", lambda e: e.matmul(pg_[:, 0:bn], lhsT=wg_[:, c, ft * 128:(ft + 1) * 128], rhs=hT[:, c, b0:b0 + bn],
                                                          start=(c == 0), stop=(c == KC - 1)), reads=[wg_] + hr, writes=[pg_], signal=(c == KC - 1))
                    for c in range(KC):
                        P.op("tensor", lambda e: e.matmul(pu_[:, 0:bn], lhsT=wu_[:, c, ft * 128:(ft + 1) * 128], rhs=hT[:, c, b0:b0 + bn],
                                                          start=(c == 0), stop=(c == KC - 1)), reads=[wu_] + hr, writes=[pu_], signal=(c == KC - 1))
                    P.op("scalar", lambda e: e.activation(out=sg_[:, 0:bn], in_=pg_[:, 0:bn], func=AF.Silu), reads=[pg_], writes=[sg_])
                    P.op("vector", lambda e: e.tensor_tensor(out=a_[:, 0:bn], in0=pu_[:, 0:bn], in1=sg_[:, 0:bn], op=ALU.mult), reads=[pu_, sg_], writes=[a_])
                    P.dma("sync", AT[f0:f0 + 128, r0 + b0:r0 + b0 + bn], a_[:, 0:bn], reads=[a_])
        P.flush()
        P.release(m0)


class _PV:
    def __init__(self, b):
        self.b = b

    def __getitem__(self, idx):
        return self.b.t[:].rearrange("p (q t) -> p q t", q=4)[idx]


def ffn_down(k, AT, nrows, wd, dff, Y, row_off=0):
    P = k.P
    m0 = P.mark()
    FC = dff // 128
    G = 4
    assert FC % G == 0
    wdb = [P.sb("wdb%d" % i, [128, FC, 512], BF16) for i in range(2)]
    atb = [P.sb("atb%d" % i, [128, FC, 256], BF16) for i in range(2)]
    yb = [P.sb("yb%d" % i, [128, 512], F32) for i in range(3)]
    py = [P.ps("pyd%d" % i, [128, 512]) for i in range(3)]
    wdv = wd.rearrange("(c p) n -> p c n", p=128)
    atv = AT.rearrange("(c p) t -> p c t", p=128)
    it = 0
    ib = 0
    for db in range(4):
        w_ = wdb[db % 2]
        for q in range(G):
            cs = slice(q * (FC // G), (q + 1) * (FC // G))
            P.dma("gpsimd", w_[:, cs, :], wdv[:, cs, db * 512:(db + 1) * 512], writes=[w_])
        for b0 in range(0, nrows, 256):
            bn = min(256, nrows - b0)
            a_ = atb[ib % 2]
            ib += 1
            for q in range(G):
                cs = slice(q * (FC // G), (q + 1) * (FC // G))
                P.dma("sync", a_[:, cs, 0:bn], atv[:, cs, b0:b0 + bn], writes=[a_])
            for j in range(bn // 128):
                p_ = py[it % 3]
                y_ = yb[it % 3]
                it += 1
                for c in range(FC):
                    P.op("tensor", lambda e: e.matmul(p_[:], lhsT=a_[:, c, j * 128:(j + 1) * 128], rhs=w_[:, c, :], start=(c == 0), stop=(c == FC - 1)),
                         reads=[a_, w_], writes=[p_], signal=(c == FC - 1))
                P.op("scalar", lambda e: e.copy(out=y_[:], in_=p_[:]), reads=[p_], writes=[y_])
                rs = slice(row_off + b0 + j * 128, row_off + b0 + (j + 1) * 128)
                P.dma("sync", Y[rs, db * 512:(db + 1) * 512], y_[:], reads=[y_])
    P.flush()
    P.release(m0)


def phase_ln2(k, l, xsrc, ysrc, xdst):
    P = k.P
    m0 = P.mark()
    gfrep = P.sb("gfrep", [128, D], F32)
    lng = P.sb("lng", [128, D], F32)
    lnb = P.sb("lnb", [128, D], F32)
    P.dma("sync", gfrep[:], k.modv[l, 5 * D:6 * D].partition_broadcast(128), writes=[gfrep])
    P.dma("sync", lng[:], k.ln_ffn_g[l].partition_broadcast(128), writes=[lng])
    P.dma("sync", lnb[:], k.ln_ffn_b[l].partition_broadcast(128), writes=[lnb])
    xt = [P.sb("xt%d" % i, [128, D], F32) for i in range(2)]
    rt = [P.sb("rt%d" % i, [128, D], F32) for i in range(2)]
    stats = P.sb("stats", [128, 4, 6], F32)
    mv = P.sb("mv", [128, 2], F32)
    rstd = P.sb("rstd", [128, 1], F32)
    for tt in range(T // 128):
        x_, r_ = xt[tt % 2], rt[tt % 2]
        tsl = slice(tt * 128, (tt + 1) * 128)
        P.dma("sync", x_[:], xsrc[tsl, :], writes=[x_])
        P.dma("gpsimd", r_[:], ysrc[tsl, :], writes=[r_])
        P.op("vector", lambda e: e.tensor_tensor(out=r_[:], in0=r_[:], in1=gfrep[:], op=ALU.mult), reads=[r_, gfrep], writes=[r_])
        P.op("vector", lambda e: e.scalar_tensor_tensor(out=r_[:], in0=x_[:], scalar=ALPHA, in1=r_[:], op0=ALU.mult, op1=ALU.add),
             reads=[x_, r_], writes=[r_])
        ln_tile(P, r_, lng, lnb, stats, mv, rstd)
        P.dma("sync", xdst[tsl, :], r_[:], reads=[r_])
    P.flush()
    P.release(m0)


CAP = 768
TM = 2048
NSLOT = NE * CAP
BIGIDX = 1.0e6


def phase_moe_route(k, l):
    P = k.P
    m0 = P.mark()
    zt = P.sb("zt", [128, D], BF16)
    P.op("vector", lambda e: e.memset(zt[:], 0.0), writes=[zt])
    for s0 in range(0, NSLOT, 128):
        P.dma("sync" if (s0 // 128) % 2 == 0 else "gpsimd", k.Xs[s0:s0 + 128, :], zt[:], reads=[zt])
    P.flush()
    P.release(m0)

    m0 = P.mark()
    screp = P.sb("screp", [128, D], F32)
    shrep = P.sb("shrep", [128, D], F32)
    identf = P.sb("identf", [128, 128], F32)
    wr = P.sb("wr", [128, KC, NE], F32)
    SLb = P.sb("SLb", [128, 128], BF16)
    onesb = P.sb("onesb", [128, 128], BF16)
    eoff = P.sb("eoff", [128, NE], F32)
    base = P.sb("base", [128, NE], F32)
    P.dma("sync", screp[:], k.modv[l, 4 * D:5 * D].partition_broadcast(128), writes=[screp])
    P.dma("sync", shrep[:], k.modv[l, 3 * D:4 * D].partition_broadcast(128), writes=[shrep])
    P.dma("sync", identf[:], k.ident, writes=[identf])
    P.dma("sync", wr[:], k.moe_router[l // 2].rearrange("(c p) e -> p c e", p=128), writes=[wr])
    P.dma("gpsimd", SLb[:], k.slmat, writes=[SLb])
    P.dma("sync", eoff[:], k.eoff, writes=[eoff])
    P.op("vector", lambda e: e.memset(onesb[:], 1.0), writes=[onesb])
    P.op("vector", lambda e: e.memset(base[:], 0.0), writes=[base])
    xt = [P.sb("xt%d" % i, [128, D], F32) for i in range(2)]
    hb = [P.sb("hb%d" % i, [128, D], BF16) for i in range(2)]
    hTf = [P.sb("hTf%d" % i, [128, KC, 128], F32) for i in range(2)]
    ptr = [P.ps("ptr%d" % i, [128, 4, 128]) for i in range(2)]
    plog = P.ps("plog", [128, 512])
    pcum = P.ps("pcum", [128, 512])
    sm = {}
    for nm in ("lg", "m8", "sel", "sel1", "sel2", "ex", "exs", "comb", "tmp8", "pos", "dest", "valid", "selb"):
        sm[nm] = [P.sb(nm + "%d" % i, [128, NE], BF16 if nm == "selb" else F32) for i in range(2)]
    c1 = {}
    for nm in ("nm1", "den", "rden"):
        c1[nm] = [P.sb(nm + "%d" % i, [128, 1], F32) for i in range(2)]
    wts = [P.sb("wts%d" % i, [128, 2], F32) for i in range(2)]
    dd = [P.sb("dd%d" % i, [128, 2], F32) for i in range(2)]
    idx = [P.sb("idx%d" % i, [128, 2], I32) for i in range(2)]
    tix = [P.sb("tix%d" % i, [128, 1], I32) for i in range(2)]
    for tt in range(TM // 128):
        i2 = tt % 2
        x_, hb_, hT_ = xt[i2], hb[i2], hTf[i2]
        tsl = slice(tt * 128, (tt + 1) * 128)
        P.dma("sync", tix[i2][:], k.tokidx[tsl, :], writes=[tix[i2]])
        P.gather(x_[:], k.x1, tix[i2][:, 0:1], reads=[tix[i2]], writes=[x_], bounds_check=T - 1, oob_is_err=False)
        P.op("vector", lambda e: e.tensor_tensor(out=x_[:], in0=x_[:], in1=screp[:], op=ALU.mult), reads=[x_, screp], writes=[x_])
        P.op("gpsimd", lambda e: e.tensor_tensor(out=x_[:], in0=x_[:], in1=shrep[:], op=ALU.add), reads=[x_, shrep], writes=[x_])
        P.op("scalar", lambda e: e.copy(out=hb_[:], in_=x_[:]), reads=[x_], writes=[hb_])
        for g in range(KC // 4):
            pt = ptr[g % 2]
            for q in range(4):
                c = g * 4 + q
                P.op("tensor", lambda e: e.transpose(out=pt[:, q, :], in_=x_[:, c * 128:(c + 1) * 128], identity=identf[:]),
                     reads=[x_, identf], writes=[pt], signal=(q == 3))
            if g % 2 == 0:
                P.op("scalar", lambda e: e.copy(out=hT_[:, g * 4:(g + 1) * 4, :], in_=pt[:]), reads=[pt], writes=[hT_])
            else:
                P.op("vector", lambda e: e.tensor_copy(out=hT_[:, g * 4:(g + 1) * 4, :], in_=pt[:]), reads=[pt], writes=[hT_])
        for c in range(KC):
            P.op("tensor", lambda e: e.matmul(plog[:, 0:NE], lhsT=hT_[:, c, :], rhs=wr[:, c, :], start=(c == 0), stop=(c == KC - 1)),
                 reads=[hT_, wr], writes=[plog], signal=(c == KC - 1))
        S = {n: v[i2] for n, v in sm.items()}
        C1 = {n: v[i2] for n, v in c1.items()}
        w_, d_, ix_ = wts[i2], dd[i2], idx[i2]
        V = "vector"
        P.op(V, lambda e: e.tensor_copy(out=S["lg"][:], in_=plog[:, 0:NE]), reads=[plog], writes=[S["lg"]])
        P.op(V, lambda e: e.max(out=S["m8"][:], in_=S["lg"][:]), reads=[S["lg"]], writes=[S["m8"]])
        P.op(V, lambda e: e.tensor_scalar(out=S["sel"][:], in0=S["lg"][:], scalar1=S["m8"][:, 1:2], scalar2=None, op0=ALU.is_ge),
             reads=[S["lg"], S["m8"]], writes=[S["sel"]])
        P.op(V, lambda e: e.tensor_scalar(out=S["sel1"][:], in0=S["lg"][:], scalar1=S["m8"][:, 0:1], scalar2=None, op0=ALU.is_ge),
             reads=[S["lg"], S["m8"]], writes=[S["sel1"]])
        P.op(V, lambda e: e.tensor_tensor(out=S["sel2"][:], in0=S["sel"][:], in1=S["sel1"][:], op=ALU.subtract),
             reads=[S["sel"], S["sel1"]], writes=[S["sel2"]])
        P.op(V, lambda e: e.tensor_scalar(out=C1["nm1"][:], in0=S["m8"][:, 0:1], scalar1=-1.0, scalar2=None, op0=ALU.mult),
             reads=[S["m8"]], writes=[C1["nm1"]])
        P.op("scalar", lambda e: e.activation(out=S["ex"][:], in_=S["lg"][:], func=AF.Exp, bias=C1["nm1"][:, 0:1]),
             reads=[S["lg"], C1["nm1"]], writes=[S["ex"]])
        P.op(V, lambda e: e.tensor_tensor(out=S["exs"][:], in0=S["ex"][:], in1=S["sel"][:], op=ALU.mult), reads=[S["ex"], S["sel"]], writes=[S["exs"]])
        P.op(V, lambda e: e.reduce_sum(out=C1["den"][:], in_=S["exs"][:], axis=AX.X), reads=[S["exs"]], writes=[C1["den"]])
        P.op(V, lambda e: e.reciprocal(out=C1["rden"][:], in_=C1["den"][:]), reads=[C1["den"]], writes=[C1["rden"]])
        P.op(V, lambda e: e.tensor_scalar(out=S["comb"][:], in0=S["exs"][:], scalar1=C1["rden"][:, 0:1], scalar2=None, op0=ALU.mult),
             reads=[S["exs"], C1["rden"]], writes=[S["comb"]])
        for j, sn in enumerate(("sel1", "sel2")):
            P.op(V, lambda e: e.tensor_tensor(out=S["tmp8"][:], in0=S["comb"][:], in1=S[sn][:], op=ALU.mult), reads=[S["comb"], S[sn]], writes=[S["tmp8"]])
            P.op(V, lambda e: e.reduce_sum(out=w_[:, j:j + 1], in_=S["tmp8"][:], axis=AX.X), reads=[S["tmp8"]], writes=[w_])
        P.op(V, lambda e: e.tensor_copy(out=S["selb"][:], in_=S["sel"][:]), reads=[S["sel"]], writes=[S["selb"]])
        P.op("tensor", lambda e: e.matmul(pcum[:, 0:NE], lhsT=SLb[:], rhs=S["selb"][:], start=True, stop=True), reads=[SLb, S["selb"]], writes=[pcum], signal=False)
        P.op("tensor", lambda e: e.matmul(pcum[:, NE:2 * NE], lhsT=onesb[:], rhs=S["selb"][:], start=True, stop=True), reads=[onesb, S["selb"]], writes=[pcum])
        P.op(V, lambda e: e.tensor_tensor(out=S["pos"][:], in0=pcum[:, 0:NE], in1=base[:], op=ALU.add), reads=[pcum, base], writes=[S["pos"]])
        P.op(V, lambda e: e.tensor_tensor(out=base[:], in0=pcum[:, NE:2 * NE], in1=base[:], op=ALU.add), reads=[pcum, base], writes=[base])
        P.op(V, lambda e: e.tensor_scalar(out=S["valid"][:], in0=S["pos"][:], scalar1=float(CAP), scalar2=None, op0=ALU.is_lt),
             reads=[S["pos"]], writes=[S["valid"]])
        P.op(V, lambda e: e.tensor_tensor(out=S["dest"][:], in0=S["pos"][:], in1=eoff[:], op=ALU.add), reads=[S["pos"], eoff], writes=[S["dest"]])
        P.op(V, lambda e: e.scalar_tensor_tensor(out=S["dest"][:], in0=S["dest"][:], scalar=-BIGIDX, in1=S["valid"][:], op0=ALU.add, op1=ALU.mult),
             reads=[S["dest"], S["valid"]], writes=[S["dest"]])
        P.op(V, lambda e: e.tensor_scalar(out=S["dest"][:], in0=S["dest"][:], scalar1=BIGIDX, scalar2=None, op0=ALU.add),
             reads=[S["dest"]], writes=[S["dest"]])
        for j, sn in enumerate(("sel1", "sel2")):
            P.op(V, lambda e: e.tensor_tensor(out=S["tmp8"][:], in0=S["dest"][:], in1=S[sn][:], op=ALU.mult), reads=[S["dest"], S[sn]], writes=[S["tmp8"]])
            P.op(V, lambda e: e.reduce_sum(out=d_[:, j:j + 1], in_=S["tmp8"][:], axis=AX.X), reads=[S["tmp8"]], writes=[d_])
        P.op(V, lambda e: e.tensor_copy(out=ix_[:], in_=d_[:]), reads=[d_], writes=[ix_])
        P.dma("sync", k.midx[tsl, :], ix_[:], reads=[ix_])
        P.dma("sync", k.mwts[tsl, :], w_[:], reads=[w_])
        for j in range(2):
            P.gather(k.Xs, hb_[:], ix_[:, j:j + 1], reads=[hb_, ix_], scatter=True, bounds_check=NSLOT - 1, oob_is_err=False)
    P.flush()
    P.release(m0)


def phase_moe_experts(k, l):
    i = l // 2
    for e_ in range(NE):
        ffn_gateup(k, k.Xs[e_ * CAP:(e_ + 1) * CAP, :], CAP, k.moe_w_gate[i, e_], k.moe_w_up[i, e_], D_FFE, k.AT[:, 0:CAP], mod=None)
        ffn_down(k, k.AT[:, 0:CAP], CAP, k.moe_w_down[i, e_], D_FFE, k.Ys, row_off=e_ * CAP)


def phase_moe_ln2(k, l, xsrc, xdst):
    P = k.P
    m0 = P.mark()
    gfrep = P.sb("gfrep", [128, D], F32)
    lng = P.sb("lng", [128, D], F32)
    lnb = P.sb("lnb", [128, D], F32)
    P.dma("sync", gfrep[:], k.modv[l, 5 * D:6 * D].partition_broadcast(128), writes=[gfrep])
    P.dma("sync", lng[:], k.ln_ffn_g[l].partition_broadcast(128), writes=[lng])
    P.dma("sync", lnb[:], k.ln_ffn_b[l].partition_broadcast(128), writes=[lnb])
    xt = [P.sb("xt%d" % i, [128, D], F32) for i in range(2)]
    y1 = [P.sb("y1%d" % i, [128, D], F32) for i in range(2)]
    y2 = [P.sb("y2%d" % i, [128, D], F32) for i in range(2)]
    rt = [P.sb("rt%d" % i, [128, D], F32) for i in range(2)]
    idx = [P.sb("idx%d" % i, [128, 2], I32) for i in range(2)]
    wts = [P.sb("wts%d" % i, [128, 2], F32) for i in range(2)]
    stats = P.sb("stats", [128, 4, 6], F32)
    mv = P.sb("mv", [128, 2], F32)
    rstd = P.sb("rstd", [128, 1], F32)
    tix = [P.sb("tix%d" % i, [128, 1], I32) for i in range(2)]
    for tt in range(TM // 128):
        i2 = tt % 2
        x_, r_, a_, b_, ix_, w_ = xt[i2], rt[i2], y1[i2], y2[i2], idx[i2], wts[i2]
        tsl = slice(tt * 128, (tt + 1) * 128)
        P.dma("sync", tix[i2][:], k.tokidx[tsl, :], writes=[tix[i2]])
        P.gather(x_[:], xsrc, tix[i2][:, 0:1], reads=[tix[i2]], writes=[x_], bounds_check=T - 1, oob_is_err=False)
        P.dma("sync", ix_[:], k.midx[tsl, :], writes=[ix_])
        P.dma("sync", w_[:], k.mwts[tsl, :], writes=[w_])
        P.op("gpsimd", lambda e: e.memset(a_[:], 0.0), writes=[a_])
        P.op("gpsimd", lambda e: e.memset(b_[:], 0.0), writes=[b_])
        P.gather(a_[:], k.Ys, ix_[:, 0:1], reads=[ix_], writes=[a_], bounds_check=NSLOT - 1, oob_is_err=False)
        P.gather(b_[:], k.Ys, ix_[:, 1:2], reads=[ix_], writes=[b_], bounds_check=NSLOT - 1, oob_is_err=False)
        P.op("vector", lambda e: e.tensor_scalar(out=r_[:], in0=a_[:], scalar1=w_[:, 0:1], scalar2=None, op0=ALU.mult), reads=[a_, w_], writes=[r_])
        P.op("vector", lambda e: e.scalar_tensor_tensor(out=r_[:], in0=b_[:], scalar=w_[:, 1:2], in1=r_[:], op0=ALU.mult, op1=ALU.add),
             reads=[b_, w_, r_], writes=[r_])
        P.op("gpsimd", lambda e: e.tensor_tensor(out=r_[:], in0=r_[:], in1=gfrep[:], op=ALU.mult), reads=[r_, gfrep], writes=[r_])
        P.op("vector", lambda e: e.scalar_tensor_tensor(out=r_[:], in0=x_[:], scalar=ALPHA, in1=r_[:], op0=ALU.mult, op1=ALU.add),
             reads=[x_, r_], writes=[r_])
        ln_tile(P, r_, lng, lnb, stats, mv, rstd)
        P.dma("sync", xdst[tsl, :], r_[:], reads=[r_])
    P.flush()
    P.release(m0)


_CACHE = {}


def make_in_maps(inputs, n_cores=8):
    hc = host_consts()
    maps = []
    for core in range(n_cores):
        b = (core // 2) % 4
        r = core % 2
        m = {
            "x": np.ascontiguousarray(inputs["x"][b]),
            "cT": np.ascontiguousarray(inputs["c"][b].reshape(KC, 128).T),
            "tokidx": (r * TM + np.arange(TM, dtype=np.int32)).reshape(TM, 1),
            "w_ada": inputs["w_ada"],
            "b_ada": inputs["b_ada"],
            "w_in": inputs["w_in"],
            "w_out": inputs["w_out"], "ln_mix_g": inputs["ln_mix_g"], "ln_mix_b": inputs["ln_mix_b"],
            "ln_ffn_g": inputs["ln_ffn_g"], "ln_ffn_b": inputs["ln_ffn_b"],
            "moe_router": inputs["moe_router"], "moe_w_gate": inputs["moe_w_gate"], "moe_w_up": inputs["moe_w_up"],
            "moe_w_down": inputs["moe_w_down"],
            "ffn_w_gate": inputs["ffn_w_gate"], "ffn_w_up": inputs["ffn_w_up"], "ffn_w_down": inputs["ffn_w_down"],
            "gla_w_a2": inputs["gla_w_a2"], "gla_b_a": inputs["gla_b_a"],
            "gla_nwT": np.ascontiguousarray(inputs["gla_norm_w"].reshape(DEPTH, 2, 128).transpose(0, 2, 1)),
            "cmp_w1_k": inputs["cmp_w1_k"], "cmp_w1_v": inputs["cmp_w1_v"],
            "cmp_w2_k": inputs["cmp_w2_k"], "cmp_w2_v": inputs["cmp_w2_v"],
            "cmp_peT_k": np.ascontiguousarray(inputs["cmp_pos_k"].transpose(0, 2, 1)),
            "cmp_peT_v": np.ascontiguousarray(inputs["cmp_pos_v"].transpose(0, 2, 1)),
        }
        m.update(hc)
        maps.append(m)
    return maps


N_CORES = 8


def kernel(**inputs):
    inputs = {k_: np.asarray(v) for k_, v in inputs.items()}
    nc = build()
    maps = make_in_maps(inputs, n_cores=N_CORES)
    res = run_bass_kernel_spmd(nc, maps, core_ids=list(range(N_CORES)))
    out = np.empty((4, T, D), np.float32)
    for core in range(N_CORES):
        b, r = core // 2, core % 2
        out[b, r * TM:(r + 1) * TM] = np.asarray(res.results[core]["out"])
    return out
```

```python
import numpy as np
import concourse.bass as bass
import concourse.mybir as mybir
from concourse.bass_utils import run_bass_kernel_spmd

F32 = mybir.dt.float32
BF16 = mybir.dt.bfloat16
I32 = mybir.dt.int32
U32 = mybir.dt.uint32
AF = mybir.ActivationFunctionType
ALU = mybir.AluOpType
AX = mybir.AxisListType

ENGS = ("tensor", "vector", "scalar", "gpsimd", "sync")

D = 2048
T = 4096
DEPTH = 2
KC = D // 128
HD = 128
SPLIT = (1024, 256, 256, 256, 256, 256, 256, 24, 512, 512, 1024, 16, 1024)
SEG = ("nq", "kc", "vc", "ks", "vs", "kw", "vw", "ng", "gq", "gk", "gv", "ga", "gg")
OFF = {}
_o = 0
for _n, _s in zip(SEG, SPLIT):
    OFF[_n] = (_o, _s)
    _o += _s
IN_W = _o
D_FF = 5632
NE = 8
D_FFE = 7168
ALPHA = (2 * DEPTH) ** 0.25
LN_EPS = 1e-5
NORM_EPS = 1e-6
NEG = -30000.0
SCALE = HD ** -0.5


class Sem:
    def __init__(self, h, kind, uid):
        self.h = h
        self.kind = kind
        self.count = 0
        self.key = "s%d" % uid


class Buf:
    def __init__(self, t, name):
        self.t = t
        self.name = name
        self.last_w = None
        self.readers = {}
        self.sem = {"sw": None, "hw": None}
        self.dlast = {"sw": 0, "hw": 0}
        self.dma_w = {"sw": 0, "hw": 0}
        self.psum = False
        self.fdeps = []

    def __getitem__(self, idx):
        return self.t[idx]

    def reset(self):
        self.last_w = None
        self.readers = {}
        self.fdeps = []
        self.dlast["hw"] = 0
        self.dma_w["hw"] = 0


class _Rec:
    def __init__(self):
        self.calls = []

    def __getattr__(self, name):
        def f(*a, **kw):
            self.calls.append((name, a, kw))
            return self
        return f


class Prog:
    def __init__(self, nc, same_engine_raw=True):
        self.nc = nc
        self.same_engine_raw = same_engine_raw
        self.psem = {}
        self.cnt = {e: 0 for e in ENGS}
        self.lists = {e: [] for e in ENGS}
        self.seen = {e: {} for e in ENGS}
        self.bufs = []
        self._cms = []
        self._semcms = []
        self.pool = {"sw": [], "hw": []}
        self.allsems = []
        for e in ENGS:
            cm = nc.semaphore("p_" + e)
            self.psem[e] = cm.__enter__()
            self._semcms.append(cm)
        self.n_inst = 0
        self.uid = 0
        self._bcreg = {}
        self._bcset = set()

    def mark(self):
        return len(self._cms)

    def release(self, mark):
        while len(self._cms) > mark:
            cm, b = self._cms.pop()
            cm.__exit__(None, None, None)
            if b is not None:
                self._drop(b)

    def _drop(self, b):
        for kind in ("sw", "hw"):
            if b.sem[kind] is not None:
                self.pool[kind].append(b.sem[kind])
                b.sem[kind] = None
        if b in self.bufs:
            self.bufs.remove(b)
        for v in getattr(b, "views", []):
            self._drop(v)

    def sb(self, name, shape, dt):
        self.uid += 1
        cm = self.nc.sbuf_tensor("%s_%d" % (name, self.uid), list(shape), dt)
        t = cm.__enter__()
        b = Buf(t, "%s_%d" % (name, self.uid))
        b.views = []
        self._cms.append((cm, b))
        self.bufs.append(b)
        return b

    def ps(self, name, shape, dt=F32):
        self.uid += 1
        cm = self.nc.psum_tensor("%s_%d" % (name, self.uid), list(shape), dt)
        t = cm.__enter__()
        b = Buf(t, "%s_%d" % (name, self.uid))
        b.psum = True
        b.views = []
        self._cms.append((cm, b))
        self.bufs.append(b)
        return b

    def view(self, buf, name=None):
        self.uid += 1
        b = Buf(buf.t, "%s_v%d" % (buf.name, self.uid))
        buf.views.append(b)
        self.bufs.append(b)
        return b

    def _getsem(self, b, kind):
        if b.sem[kind] is None:
            if self.pool[kind]:
                b.sem[kind] = self.pool[kind].pop()
            else:
                self.uid += 1
                cm = self.nc.semaphore("d%s_%d" % (kind, self.uid))
                h = cm.__enter__()
                self._semcms.append(cm)
                sm = Sem(h, kind, self.uid)
                self.allsems.append(sm)
                b.sem[kind] = sm
        return b.sem[kind]

    def _waits(self, e, reads, writes):
        w = {}

        def need(sem, val, key):
            if val <= 0:
                return
            if self.seen[e].get(key, 0) >= val:
                return
            if key not in w or w[key][1] < val:
                w[key] = (sem, val)

        for r in reads:
            if r.last_w is not None:
                we, n = r.last_w
                if we != e or self.same_engine_raw:
                    need(self.psem[we], n, "p_" + we)
            for kind in ("sw", "hw"):
                if r.dma_w[kind] > 0:
                    need(r.sem[kind].h, 16 * r.dma_w[kind], r.sem[kind].key)
            if r.psum:
                for re_, n in r.readers.items():
                    if re_ != e:
                        need(self.psem[re_], n, "p_" + re_)
        for b in writes:
            if b.last_w is not None:
                we, n = b.last_w
                if we != e:
                    need(self.psem[we], n, "p_" + we)
            for re_, n in b.readers.items():
                if re_ != e:
                    need(self.psem[re_], n, "p_" + re_)
            for kind in ("sw", "hw"):
                if b.dlast[kind] > 0:
                    need(b.sem[kind].h, 16 * b.dlast[kind], b.sem[kind].key)
            for (fs, fv, fk) in b.fdeps:
                need(fs, fv, fk)
        for key, (sem, val) in w.items():
            self.seen[e][key] = val
        return list(w.values())

    def op(self, e, fn, reads=(), writes=(), signal=True):
        waits = self._waits(e, reads, writes)
        n = self.cnt[e] + 1
        if signal:
            self.cnt[e] = n
        psem = self.psem[e]
        rec = _Rec()
        fn(rec)
        assert len(rec.calls) == 1, rec.calls
        name, a, kw = rec.calls[0]

        def thunk(engine, waits=waits, name=name, a=a, kw=kw, signal=signal, psem=psem):
            for sem, val in waits:
                engine.wait_ge(sem, val)
            ins = getattr(engine, name)(*a, **kw)
            if signal:
                ins.then_inc(psem, 1)

        self.lists[e].append(thunk)
        self.n_inst += 1
        for r in reads:
            r.readers[e] = n
        for b in writes:
            b.last_w = (e, n)
            b.readers = {}
            b.dma_w = {"sw": 0, "hw": 0}
            b.fdeps = []

    def _dma_book(self, q, reads, writes):
        kind = "sw" if q == "gpsimd" else "hw"
        waits = self._waits(q, reads, writes)
        allb = list(writes) + list(reads)
        prim = allb[0]
        sm = self._getsem(prim, kind)
        sm.count += 1
        prim.dlast[kind] = sm.count
        for b in allb[1:]:
            assert b not in writes
            b.fdeps.append((sm.h, 16 * sm.count, sm.key))
        for b in writes:
            b.dma_w[kind] = sm.count
            b.last_w = None
            b.readers = {}
            b.fdeps = []
        return waits, sm.h

    def dma(self, q, out_ap, in_ap, reads=(), writes=(), **kw):
        waits, semh = self._dma_book(q, reads, writes)

        def thunk(engine, waits=waits, semh=semh, out_ap=out_ap, in_ap=in_ap, kw=kw):
            for sem, val in waits:
                engine.wait_ge(sem, val)
            engine.dma_start(out=out_ap, in_=in_ap, **kw).then_inc(semh, 16)

        self.lists[q].append(thunk)
        self.n_inst += 1

    def gather(self, out_ap, in_ap, idx_ap, reads=(), writes=(), scatter=False, **kw):
        q = "gpsimd"
        waits, semh = self._dma_book(q, reads, writes)
        bc = kw.pop("bounds_check", None)

        def thunk(engine, waits=waits, semh=semh, kw=kw, bc=bc):
            for sem, val in waits:
                engine.wait_ge(sem, val)
            if bc is not None:
                if bc not in self._bcreg:
                    self._bcreg[bc] = engine.alloc_register("bcreg_%d" % int(bc))
                if bc not in self._bcset:
                    engine.reg_mov(self._bcreg[bc], bc)
                    self._bcset.add(bc)
                kw = dict(kw)
                kw["bounds_check"] = self._bcreg[bc]
            if scatter:
                ins = engine.indirect_dma_start(out=out_ap, out_offset=bass.IndirectOffsetOnAxis(ap=idx_ap, axis=0),
                                                in_=in_ap, in_offset=None, **kw)
            else:
                ins = engine.indirect_dma_start(out=out_ap, out_offset=None, in_=in_ap,
                                                in_offset=bass.IndirectOffsetOnAxis(ap=idx_ap, axis=0), **kw)
            ins.then_inc(semh, 16)

        self.lists[q].append(thunk)
        self.n_inst += 1

    def flush(self, final=False):
        nc = self.nc
        drain = []
        for sm in self.allsems:
            if sm.count > 0:
                drain.append((sm.h, 16 * sm.count))
        for e in ENGS:
            if e != "sync" and self.cnt[e] > 0:
                drain.append((self.psem[e], self.cnt[e]))

        def dthunk(engine, drain=drain):
            for sem, val in drain:
                engine.wait_ge(sem, val)

        self.lists["sync"].append(dthunk)
        lists = self.lists
        with nc.Block() as block:
            @block.tensor
            def _(eng):
                for th in lists["tensor"]:
                    th(eng)

            @block.vector
            def _(eng):
                for th in lists["vector"]:
                    th(eng)

            @block.scalar
            def _(eng):
                for th in lists["scalar"]:
                    th(eng)

            @block.gpsimd
            def _(eng):
                for th in lists["gpsimd"]:
                    th(eng)

            @block.sync
            def _(eng):
                for th in lists["sync"]:
                    th(eng)
        if not final:
            nc.all_engine_barrier()
            sems = [self.psem[e] for e in ENGS] + [sm.h for sm in self.allsems if sm.kind == "hw" and sm.count > 0]
            with nc.Block() as block:
                @block.sync
                def _(eng):
                    for s_ in sems:
                        eng.sem_clear(s_)
            nc.all_engine_barrier()
        for sm in self.allsems:
            if sm.kind == "hw":
                sm.count = 0
        self.lists = {e: [] for e in ENGS}
        self.cnt = {e: 0 for e in ENGS}
        self.seen = {e: {} for e in ENGS}
        self._bcset = set()
        for b in self.bufs:
            b.reset()

    def close(self):
        self.release(0)
        for cm in reversed(self._semcms):
            cm.__exit__(None, None, None)
        self._semcms = []


def host_consts():
    c = {}
    c["ident"] = np.eye(128, dtype=np.float32)
    half = 16
    inv = (500000.0 ** (-np.arange(half, dtype=np.float32) * 2.0 / 32)).astype(np.float32)
    ang = np.arange(T, dtype=np.float32)[None, :] * inv[:, None]
    cos = np.cos(ang).astype(np.float32)
    sin = np.sin(ang).astype(np.float32)
    c["cos"] = np.concatenate([cos, cos], 0)
    c["sin"] = np.concatenate([sin, sin], 0)
    pm = np.zeros((128, 128), np.float32)
    for i in range(16):
        pm[i + 16, i] = -1.0
        pm[i, i + 16] = 1.0
    c["pm"] = pm
    kk = np.arange(128)[:, None]
    tt = np.arange(128)[None, :]
    c["cb"] = np.where(kk > tt, NEG, 0.0).astype(np.float32)
    c["wbm"] = np.where(kk <= tt, NEG, 0.0).astype(np.float32)
    es = np.zeros((64, 32, 128), np.float32)
    for kt in range(32):
        for m in range(128):
            es[2 * kt + m // 64, kt, m] = 1.0
    c["esel"] = es.reshape(64, 32 * 128)
    cst = np.arange(256) * 16
    bst = np.arange(64) * 64
    ov = ((cst[:, None] < bst[None, :] + 64) & (cst[:, None] + 32 > bst[None, :])).astype(np.float32)
    ov[255] = 0.0
    c["ovl"] = np.ascontiguousarray(ov.reshape(2, 128, 64).transpose(1, 0, 2)).reshape(128, 128)
    c["mb"] = (16.0 * kk - tt).astype(np.float32)
    keep = np.zeros((128, 32, 64), np.float32)
    add = np.zeros((128, 32, 64), np.float32)
    n = np.arange(64)[None, :]
    for qt in range(32):
        t = qt * 128 + np.arange(128)[:, None]
        cur = t // 64
        forced = (n == 0) | (n == cur) | (n == cur - 1)
        future = (n * 64) > t
        keep[:, qt, :] = np.where(forced | future, 0.0, 1.0)
        add[:, qt, :] = np.where(future, -1e30, np.where(forced, 1e9, 0.0))
    c["keep"] = keep.reshape(128, 32 * 64)
    c["addc"] = add.reshape(128, 32 * 64)
    sel = np.zeros((12, 12, 128), np.float32)
    for r in range(12):
        sel[r, r, :] = 1.0
    c["sel"] = sel.reshape(12, 12 * 128)
    same = (kk // 64) == (tt // 64)
    c["slmat"] = (kk < tt).astype(np.float32)
    c["eoff"] = np.tile((np.arange(NE) * 768.0)[None, :], (128, 1)).astype(np.float32)
    c["tri2"] = (same & (kk <= tt)).astype(np.float32)
    c["sumat"] = (same & (kk > tt)).astype(np.float32)
    return c


class K:
    pass


def build(debug=(), phases=None, layers=(0, 1), l1_from_x=False):
    nc = bass.Bass("TRN2", target_bir_lowering=False)
    k = K()
    k.nc = nc
    dbg = set(debug)

    def din(name, shape, dt=F32):
        return nc.dram_tensor(name, list(shape), dt, kind="ExternalInput").ap()

    def dscr(name, shape, dt):
        kind = "ExternalOutput" if name in dbg else "Internal"
        return nc.dram_tensor(name, list(shape), dt, kind=kind).ap()

    k.x = din("x", [T, D])
    k.cT = din("cT", [128, KC])
    k.w_ada = din("w_ada", [DEPTH, D, 6 * D])
    k.b_ada = din("b_ada", [DEPTH, 6 * D])
    k.w_in = din("w_in", [DEPTH, D, IN_W])
    k.ident = din("ident", [128, 128])
    k.cos = din("cos", [32, T])
    k.sin = din("sin", [32, T])
    k.pm = din("pm", [128, 128])
    k.cb = din("cb", [128, 128])
    k.wbm = din("wbm", [128, 128])
    k.esel = din("esel", [64, 32 * 128])
    k.ovl = din("ovl", [128, 128])
    k.mb = din("mb", [128, 128])
    k.keep = din("keep", [128, 32 * 64])
    k.addc = din("addc", [128, 32 * 64])
    k.sel = din("sel", [12, 12 * 128])
    k.w_out = din("w_out", [DEPTH, D, D])
    k.ln_mix_g = din("ln_mix_g", [DEPTH, D])
    k.ln_mix_b = din("ln_mix_b", [DEPTH, D])
    k.ln_ffn_g = din("ln_ffn_g", [DEPTH, D])
    k.ln_ffn_b = din("ln_ffn_b", [DEPTH, D])
    k.ffn_w_gate = din("ffn_w_gate", [1, D, D_FF])
    k.ffn_w_up = din("ffn_w_up", [1, D, D_FF])
    k.ffn_w_down = din("ffn_w_down", [1, D_FF, D])
    k.moe_router = din("moe_router", [1, D, NE])
    k.moe_w_gate = din("moe_w_gate", [1, NE, D, D_FFE])
    k.moe_w_up = din("moe_w_up", [1, NE, D, D_FFE])
    k.moe_w_down = din("moe_w_down", [1, NE, D_FFE, D])
    k.slmat = din("slmat", [128, 128])
    k.eoff = din("eoff", [128, NE])
    k.tri2 = din("tri2", [128, 128])
    k.sumat = din("sumat", [128, 128])
    k.gla_w_a2 = din("gla_w_a2", [DEPTH, 16, 512])
    k.gla_b_a = din("gla_b_a", [DEPTH, 512])
    k.gla_nwT = din("gla_nwT", [DEPTH, 128, 2])
    k.cmp_w1 = {"k": din("cmp_w1_k", [DEPTH, 4096, 256]), "v": din("cmp_w1_v", [DEPTH, 4096, 256])}
    k.cmp_w2 = {"k": din("cmp_w2_k", [DEPTH, 256, 128]), "v": din("cmp_w2_v", [DEPTH, 256, 128])}
    k.cmp_peT = {"k": din("cmp_peT_k", [DEPTH, 128, 32]), "v": din("cmp_peT_v", [DEPTH, 128, 32])}
    k.tokidx = din("tokidx", [2048, 1], I32)
    k.out = nc.dram_tensor("out", [2048, D], F32, kind="ExternalOutput").ap()

    k.modv = dscr("modv", [DEPTH, 6 * D], F32)
    k.qn = dscr("qn", [1024, T], BF16)
    k.qr = dscr("qr", [1024, T], BF16)
    k.kcT = dscr("kcT", [256, T], BF16)
    k.vcT = dscr("vcT", [256, T], BF16)
    k.ksT = dscr("ksT", [256, T], BF16)
    k.kwT = dscr("kwT", [256, T], BF16)
    k.ngT = dscr("ngT", [24, T], BF16)
    k.gqT = dscr("gqT", [512, T], BF16)
    k.gkT = dscr("gkT", [512, T], BF16)
    k.gaT = dscr("gaT", [16, T], BF16)
    k.ggT = dscr("ggT", [1024, T], BF16)
    k.vs = dscr("vs", [T, 256], BF16)
    k.vw = dscr("vw", [T, 256], BF16)
    k.gk = dscr("gk", [T, 512], BF16)
    k.gv = dscr("gv", [T, 1024], BF16)
    k.kcmpT = dscr("kcmpT", [2, 128, 256], BF16)
    k.vcmp = dscr("vcmp", [2, 256, 128], BF16)
    k.mixT = dscr("mixT", [D, T], BF16)
    k.x1 = dscr("x1", [T, D], F32)
    k.xa = dscr("xa", [T, D], F32)
    k.yffn = dscr("yffn", [T, D], F32)
    k.AT = dscr("AT", [D_FFE, T], BF16)
    k.Xs = dscr("Xs", [NE * 768, D], BF16)
    k.Ys = dscr("Ys", [NE * 768, D], F32)
    k.midx = dscr("midx", [2048, 2], I32)
    k.mwts = dscr("mwts", [2048, 2], F32)

    P = Prog(nc)
    k.P = P
    ph = phases

    if ph is None or "mod" in ph:
        phase_mod(k)
    for l in layers:
        xsrc = k.x if (l == 0 or l1_from_x) else k.xa
        if ph is None or "proj" in ph:
            phase_proj(k, l, xsrc)
        if ph is None or "cmp" in ph:
            phase_cmp(k, l)
        if ph is None or "nsa" in ph:
            phase_nsa(k, l)
        if ph is None or "gla" in ph:
            phase_gla(k, l)
        if ph is None or "wout" in ph:
            phase_wout(k, l, xsrc, k.x1)
        xdst = k.out if l == DEPTH - 1 else k.xa
        if l % 2 == 0:
            if ph is None or "ffn" in ph:
                ffn_gateup(k, k.x1, T, k.ffn_w_gate[l // 2], k.ffn_w_up[l // 2], D_FF, k.AT[0:D_FF, :], mod=l)
                ffn_down(k, k.AT[0:D_FF, :], T, k.ffn_w_down[l // 2], D_FF, k.yffn)
            if ph is None or "ln2" in ph:
                phase_ln2(k, l, k.x1, k.yffn, xdst)
        else:
            if ph is None or "route" in ph:
                phase_moe_route(k, l)
            if ph is None or "experts" in ph:
                phase_moe_experts(k, l)
            if ph is None or "ln2" in ph:
                phase_moe_ln2(k, l, k.x1, xdst)

    P.flush(final=True)
    P.close()
    return nc


def phase_mod(k):
    P = k.P
    m0 = P.mark()
    cc = P.sb("cc", [128, KC], F32)
    cs = P.sb("cs", [128, KC], F32)
    condB = P.sb("condB", [128, KC, 128], F32)
    wb = [P.sb("wada%d" % i, [128, KC, 512], F32) for i in range(2)]
    bb = [P.sb("bada%d" % i, [128, 512], F32) for i in range(2)]
    mo = [P.sb("mo%d" % i, [128, 512], F32) for i in range(2)]
    pm = [P.ps("pmod%d" % i, [128, 512]) for i in range(2)]
    P.dma("sync", cc[:], k.cT, writes=[cc])
    P.op("scalar", lambda e: e.activation(out=cs[:], in_=cc[:], func=AF.Silu), reads=[cc], writes=[cs])
    P.op("vector", lambda e: e.tensor_copy(out=condB[:], in_=cs[:].unsqueeze(2).to_broadcast([128, KC, 128])),
         reads=[cs], writes=[condB])
    it = 0
    import os
    NBL = int(os.environ.get("NBL", "24"))
    VAR = os.environ.get("VAR", "")
    for l in range(DEPTH):
        wv = k.w_ada[l].rearrange("(c p) n -> p c n", p=128)
        for nb in range(NBL):
            i = it % 2
            it += 1
            n0 = nb * 512
            P.dma("sync", wb[i][:], wv[:, :, n0:n0 + 512], writes=[wb[i]])
            if VAR == "nobb":
                P.op("vector", lambda e, i=i: e.memset(bb[i][:], 0.0), writes=[bb[i]])
            else:
                P.dma("gpsimd", bb[i][:], k.b_ada[l, n0:n0 + 512].partition_broadcast(128), writes=[bb[i]])
            for c in range(KC):
                P.op("tensor", lambda e, i=i, c=c: e.matmul(pm[i][:], lhsT=condB[:, c, :], rhs=wb[i][:, c, :],
                                                             start=(c == 0), stop=(c == KC - 1)),
                     reads=[condB, wb[i]], writes=[pm[i]], signal=(c == KC - 1))
            seg = nb // 4
            add1 = 1.0 if seg in (1, 2, 4, 5) else 0.0
            P.op("vector", lambda e, i=i, add1=add1: e.scalar_tensor_tensor(
                out=mo[i][:], in0=pm[i][:], scalar=add1, in1=bb[i][:], op0=ALU.add, op1=ALU.add),
                reads=[pm[i], bb[i]], writes=[mo[i]])
            P.dma("gpsimd", k.modv[l:l + 1, n0:n0 + 512], mo[i][0:1, :], reads=[mo[i]])
    P.flush()
    P.release(m0)


def build_hT(k, xsrc, t0, ntiles, screp, shrep, identf, hT, hviews, xt, ptr):
    P = k.P
    for j in range(ntiles):
        xb = xt[j % 2]
        P.dma("sync", xb[:], xsrc[t0 + j * 128:t0 + (j + 1) * 128, :], writes=[xb])
        P.op("vector", lambda e, xb=xb: e.tensor_tensor(out=xb[:], in0=xb[:], in1=screp[:], op=ALU.mult),
             reads=[xb, screp], writes=[xb])
        P.op("gpsimd", lambda e, xb=xb: e.tensor_tensor(out=xb[:], in0=xb[:], in1=shrep[:], op=ALU.add),
             reads=[xb, shrep], writes=[xb])
        for g in range(KC // 4):
            pt = ptr[(j * 4 + g) % 2]
            for q in range(4):
                c = g * 4 + q
                P.op("tensor", lambda e, xb=xb, pt=pt, q=q, c=c: e.transpose(
                    out=pt[:, q, :], in_=xb[:, c * 128:(c + 1) * 128], identity=identf[:]),
                    reads=[xb, identf], writes=[pt], signal=(q == 3))
            eng = "scalar" if (g % 2 == 0) else "vector"
            if eng == "scalar":
                P.op("scalar", lambda e, pt=pt, g=g, j=j: e.copy(out=hT[:, g * 4:(g + 1) * 4, j * 128:(j + 1) * 128], in_=pt[:]),
                     reads=[pt], writes=[hviews[j][0]])
            else:
                P.op("vector", lambda e, pt=pt, g=g, j=j: e.tensor_copy(out=hT[:, g * 4:(g + 1) * 4, j * 128:(j + 1) * 128], in_=pt[:]),
                     reads=[pt], writes=[hviews[j][1]])


def phase_proj(k, l, xsrc):
    P = k.P
    TH = 2048
    for th in range(T // TH):
        t0 = th * TH
        m0 = P.mark()
        screp = P.sb("screp", [128, D], F32)
        shrep = P.sb("shrep", [128, D], F32)
        identf = P.sb("identf", [128, 128], F32)
        hT = P.sb("hT", [128, KC, TH], BF16)
        hviews = [(P.view(hT), P.view(hT)) for _ in range(TH // 128)]
        xt = [P.sb("xt%d" % i, [128, D], F32) for i in range(2)]
        ptr = [P.ps("ptr%d" % i, [128, 4, 128]) for i in range(2)]
        cosb = P.sb("cosb", [32, TH], F32)
        sinb = P.sb("sinb", [32, TH], F32)
        pmb = P.sb("pmb", [128, 128], BF16)
        wbk = [P.sb("wblk%d" % i, [128, KC, 512], BF16) for i in range(2)]
        pacc = [P.ps("pacc%d" % i, [128, 512]) for i in range(3)]
        prot = [P.ps("prot%d" % i, [128, 512]) for i in range(2)]
        ost = [P.sb("ost%d" % i, [128, 512], BF16) for i in range(4)]
        ost2 = [P.sb("ost2%d" % i, [128, 512], BF16) for i in range(2)]
        t1 = [P.sb("t1%d" % i, [32, 512], F32) for i in range(2)]
        t2 = [P.sb("t2%d" % i, [32, 512], F32) for i in range(2)]

        P.dma("sync", screp[:], k.modv[l, D:2 * D].partition_broadcast(128), writes=[screp])
        P.dma("sync", shrep[:], k.modv[l, 0:D].partition_broadcast(128), writes=[shrep])
        P.dma("sync", identf[:], k.ident, writes=[identf])
        P.dma("sync", cosb[:], k.cos[:, t0:t0 + TH], writes=[cosb])
        P.dma("sync", sinb[:], k.sin[:, t0:t0 + TH], writes=[sinb])
        P.dma("gpsimd", pmb[:], k.pm, writes=[pmb])
        build_hT(k, xsrc, t0, TH // 128, screp, shrep, identf, hT, hviews, xt, ptr)

        wv = k.w_in[l].rearrange("(c p) n -> p c n", p=128)
        cnt = {"w": 0, "acc": 0, "ost": 0, "ost2": 0, "rot": 0}

        def hreads(tb):
            r = []
            for j in range(tb * 4, tb * 4 + 4):
                r += [hviews[j][0], hviews[j][1]]
            return r

        def load_w(c0, ncols):
            wb = wbk[cnt["w"] % 2]
            cnt["w"] += 1
            P.dma("gpsimd", wb[:, :, 0:ncols], wv[:, :, c0:c0 + ncols], writes=[wb])
            return wb

        def ftype(seg, dst, mode):
            c0, n = OFF[seg]
            for b0 in range(0, n, 512):
                nb = min(512, n - b0)
                wb = load_w(c0 + b0, nb)
                for ct in range(0, nb, 128):
                    m = min(128, nb - ct)
                    row0 = b0 + ct
                    for tb in range(TH // 512):
                        pa = pacc[cnt["acc"] % 3]
                        cnt["acc"] += 1
                        for c in range(KC):
                            P.op("tensor", lambda e, pa=pa, wb=wb, ct=ct, m=m, c=c, tb=tb: e.matmul(
                                pa[0:m, :], lhsT=wb[:, c, ct:ct + m], rhs=hT[:, c, tb * 512:(tb + 1) * 512],
                                start=(c == 0), stop=(c == KC - 1)),
                                reads=[wb] + hreads(tb), writes=[pa], signal=(c == KC - 1))
                        ob = ost[cnt["ost"] % 4]
                        cnt["ost"] += 1
                        tok = slice(t0 + tb * 512, t0 + (tb + 1) * 512)
                        if mode == "plain":
                            P.op("scalar", lambda e, ob=ob, pa=pa, m=m: e.copy(out=ob[0:m, :], in_=pa[0:m, :]),
                                 reads=[pa], writes=[ob])
                            P.dma("sync", dst[row0:row0 + m, tok], ob[0:m, :], reads=[ob])
                        elif mode == "sigmoid":
                            P.op("scalar", lambda e, ob=ob, pa=pa, m=m: e.activation(out=ob[0:m, :], in_=pa[0:m, :], func=AF.Sigmoid),
                                 reads=[pa], writes=[ob])
                            P.dma("sync", dst[row0:row0 + m, tok], ob[0:m, :], reads=[ob])
                        elif mode == "silu":
                            P.op("scalar", lambda e, ob=ob, pa=pa, m=m: e.activation(out=ob[0:m, :], in_=pa[0:m, :], func=AF.Silu),
                                 reads=[pa], writes=[ob])
                            P.dma("sync", dst[row0:row0 + m, tok], ob[0:m, :], reads=[ob])
                        else:
                            dn, dr = mode[1], mode[2]
                            P.op("scalar", lambda e, ob=ob, pa=pa: e.copy(out=ob[:], in_=pa[:]), reads=[pa], writes=[ob])
                            pr = prot[cnt["rot"] % 2]
                            a1 = t1[cnt["rot"] % 2]
                            a2 = t2[cnt["rot"] % 2]
                            cnt["rot"] += 1
                            P.op("tensor", lambda e, pr=pr, ob=ob: e.matmul(pr[:], lhsT=pmb[:], rhs=ob[:], start=True, stop=True),
                                 reads=[pmb, ob], writes=[pr])
                            ltok = slice(tb * 512, (tb + 1) * 512)
                            P.op("vector", lambda e, a1=a1, pa=pa, ltok=ltok: e.tensor_tensor(out=a1[:], in0=pa[0:32, :], in1=cosb[:, ltok], op=ALU.mult),
                                 reads=[pa, cosb], writes=[a1])
                            P.op("vector", lambda e, a2=a2, pr=pr, ltok=ltok: e.tensor_tensor(out=a2[:], in0=pr[0:32, :], in1=sinb[:, ltok], op=ALU.mult),
                                 reads=[pr, sinb], writes=[a2])
                            if dn is not None:
                                P.dma("sync", dn[row0:row0 + 128, tok], ob[:], reads=[ob])
                                o2 = ost2[cnt["ost2"] % 2]
                                cnt["ost2"] += 1
                                P.op("scalar", lambda e, o2=o2, pa=pa: e.copy(out=o2[:], in_=pa[:]), reads=[pa], writes=[o2])
                                P.op("vector", lambda e, o2=o2, a1=a1, a2=a2: e.tensor_tensor(out=o2[0:32, :], in0=a1[:], in1=a2[:], op=ALU.add),
                                     reads=[a1, a2], writes=[o2])
                                P.dma("sync", dr[row0:row0 + 128, tok], o2[:], reads=[o2])
                            else:
                                P.op("vector", lambda e, ob=ob, a1=a1, a2=a2: e.tensor_tensor(out=ob[0:32, :], in0=a1[:], in1=a2[:], op=ALU.add),
                                     reads=[a1, a2, ob], writes=[ob])
                                P.dma("sync", dr[row0:row0 + 128, tok], ob[:], reads=[ob])

        def ttype(seg, dst):
            c0, n = OFF[seg]
            for b0 in range(0, n, 512):
                nb = min(512, n - b0)
                wb = load_w(c0 + b0, nb)
                for j in range(TH // 128):
                    pa = pacc[cnt["acc"] % 3]
                    cnt["acc"] += 1
                    for c in range(KC):
                        P.op("tensor", lambda e, pa=pa, wb=wb, nb=nb, c=c, j=j: e.matmul(
                            pa[:, 0:nb], lhsT=hT[:, c, j * 128:(j + 1) * 128], rhs=wb[:, c, 0:nb],
                            start=(c == 0), stop=(c == KC - 1)),
                            reads=[wb, hviews[j][0], hviews[j][1]], writes=[pa], signal=(c == KC - 1))
                    ob = ost[cnt["ost"] % 4]
                    cnt["ost"] += 1
                    P.op("scalar", lambda e, ob=ob, pa=pa, nb=nb: e.copy(out=ob[:, 0:nb], in_=pa[:, 0:nb]), reads=[pa], writes=[ob])
                    P.dma("sync", dst[t0 + j * 128:t0 + (j + 1) * 128, b0:b0 + nb], ob[:, 0:nb], reads=[ob])

        import os
        SEGS = os.environ.get("SEGS", "")
        plan = [("nq", "f", None, ("rope", k.qn, k.qr)), ("kc", "f", k.kcT, "plain"), ("vc", "f", k.vcT, "plain"),
                ("ks", "f", None, ("rope", None, k.ksT)), ("vs", "t", k.vs, None), ("kw", "f", None, ("rope", None, k.kwT)),
                ("vw", "t", k.vw, None), ("ng", "f", k.ngT, "sigmoid"), ("gq", "f", k.gqT, "plain"), ("gk", "f", k.gkT, "plain"),
                ("gk", "t", k.gk, None), ("gv", "t", k.gv, None), ("ga", "f", k.gaT, "plain"), ("gg", "f", k.ggT, "silu")]
        for (sg, ty, dst, mode) in plan:
            if SEGS and (sg + ty) not in SEGS.split(","):
                continue
            if ty == "f":
                ftype(sg, dst, mode)
            else:
                ttype(sg, dst)
        P.flush()
        P.release(m0)


def phase_cmp(k, l):
    P = k.P
    m0 = P.mark()
    aT = [P.sb("aT%d" % i, [128, T], BF16) for i in range(2)]
    w1b = [P.sb("w1b%d" % i, [128, 32, 256], BF16) for i in range(2)]
    w2b = [P.sb("w2b%d" % i, [128, 2, 128], BF16) for i in range(2)]
    peT = [P.sb("peT%d" % i, [128, 32], BF16) for i in range(2)]
    ph = [P.ps("ph%d" % i, [128, 512]) for i in range(2)]
    pc = P.ps("pc", [128, 512])
    po = P.ps("pcmpo", [128, 512])
    cst = P.sb("cst", [128, 2], F32)
    xh = [P.sb("xh%d" % i, [128, 256], F32) for i in range(2)]
    uu = [P.sb("uu%d" % i, [128, 256], F32) for i in range(2)]
    sg = [P.sb("sgm%d" % i, [128, 256], F32) for i in range(2)]
    gb = [P.sb("gb%d" % i, [128, 256], BF16) for i in range(2)]
    ocp = [P.sb("ocp%d" % i, [128, 256], BF16) for i in range(2)]
    for i in range(2):
        P.op("vector", lambda e, i=i: e.memset(gb[i][:], 0.0), writes=[gb[i]])
        P.op("vector", lambda e, i=i: e.memset(ocp[i][:], 0.0), writes=[ocp[i]])
    it = 0
    for si, src in enumerate(("k", "v")):
        w1 = k.cmp_w1[src][l].rearrange("(l d) m -> d l m", d=128)
        for q4 in range(4):
            P.dma("gpsimd", w1b[si][:, q4 * 8:(q4 + 1) * 8, :], w1[:, q4 * 8:(q4 + 1) * 8, :], writes=[w1b[si]])
        P.dma("gpsimd", w2b[si][:], k.cmp_w2[src][l].rearrange("(c p) n -> p c n", p=128), writes=[w2b[si]])
        P.dma("gpsimd", peT[si][:], k.cmp_peT[src][l], writes=[peT[si]])
        srcT = k.kcT if src == "k" else k.vcT
        for hk in range(2):
            a = aT[it % 2]
            oc = ocp[it % 2]
            it += 1
            P.dma("sync", a[:], srcT[hk * 128:(hk + 1) * 128, :], writes=[a])
            for mc in range(2):
                for ll in range(32):
                    P.op("tensor", lambda e, a=a, mc=mc, ll=ll, si=si: e.matmul(
                        ph[mc][:, 0:255], lhsT=w1b[si][:, ll, mc * 128:(mc + 1) * 128], rhs=a[:, ll:ll + 4065:16],
                        start=(ll == 0), stop=(ll == 31)), reads=[w1b[si], a], writes=[ph[mc]], signal=(ll == 31))
                for ll in range(32):
                    P.op("tensor", lambda e, mc=mc, ll=ll, si=si: e.matmul(
                        pc[:, mc:mc + 1], lhsT=w1b[si][:, ll, mc * 128:(mc + 1) * 128], rhs=peT[si][:, ll:ll + 1],
                        start=(ll == 0), stop=(ll == 31)), reads=[w1b[si], peT[si]], writes=[pc], signal=(ll == 31))
            P.op("vector", lambda e: e.tensor_copy(out=cst[:], in_=pc[:, 0:2]), reads=[pc], writes=[cst])
            for mc in range(2):
                x_, u_, s_, g_ = xh[mc], uu[mc], sg[mc], gb[mc]
                P.op("vector", lambda e, x_=x_, mc=mc: e.tensor_scalar(out=x_[:, 0:255], in0=ph[mc][:, 0:255], scalar1=cst[:, mc:mc + 1],
                                                                      scalar2=None, op0=ALU.add), reads=[ph[mc], cst], writes=[x_])
                P.op("vector", lambda e, x_=x_, u_=u_: e.tensor_tensor(out=u_[:, 0:255], in0=x_[:, 0:255], in1=x_[:, 0:255], op=ALU.mult),
                     reads=[x_], writes=[u_])
                P.op("vector", lambda e, u_=u_: e.tensor_scalar(out=u_[:, 0:255], in0=u_[:, 0:255], scalar1=0.044715, scalar2=1.0,
                                                               op0=ALU.mult, op1=ALU.add), reads=[u_], writes=[u_])
                P.op("vector", lambda e, x_=x_, u_=u_: e.tensor_tensor(out=u_[:, 0:255], in0=u_[:, 0:255], in1=x_[:, 0:255], op=ALU.mult),
                     reads=[x_, u_], writes=[u_])
                P.op("scalar", lambda e, u_=u_, s_=s_: e.activation(out=s_[:, 0:255], in_=u_[:, 0:255], func=AF.Sigmoid, scale=1.5957691216057308),
                     reads=[u_], writes=[s_])
                P.op("vector", lambda e, x_=x_, s_=s_, g_=g_: e.tensor_tensor(out=g_[:, 0:255], in0=x_[:, 0:255], in1=s_[:, 0:255], op=ALU.mult),
                     reads=[x_, s_], writes=[g_])
            if src == "k":
                for mc in range(2):
                    P.op("tensor", lambda e, mc=mc, si=si: e.matmul(po[:, 0:255], lhsT=w2b[si][:, mc, :], rhs=gb[mc][:, 0:255],
                                                                   start=(mc == 0), stop=(mc == 1)),
                         reads=[w2b[si], gb[mc]], writes=[po], signal=(mc == 1))
                P.op("scalar", lambda e, oc=oc: e.copy(out=oc[:, 0:255], in_=po[:, 0:255]), reads=[po], writes=[oc])
                P.dma("sync", k.kcmpT[hk], oc[:], reads=[oc])
            else:
                for ct in range(2):
                    for mc in range(2):
                        P.op("tensor", lambda e, mc=mc, ct=ct, si=si: e.matmul(
                            po[:, ct * 128:(ct + 1) * 128], lhsT=gb[mc][:, ct * 128:(ct + 1) * 128], rhs=w2b[si][:, mc, :],
                            start=(mc == 0), stop=(mc == 1)), reads=[w2b[si], gb[mc]], writes=[po], signal=(mc == 1))
                P.op("scalar", lambda e, oc=oc: e.copy(out=oc[:], in_=po[:, 0:256]), reads=[po], writes=[oc])
                P.dma("sync", k.vcmp[hk].rearrange("(c p) d -> p c d", p=128), oc[:].rearrange("p (c d) -> p c d", c=2), reads=[oc])
    P.flush()
    P.release(m0)


def phase_nsa(k, l, hks=(0, 1)):
    P = k.P
    m0 = P.mark()
    cst_f = {}
    for nm, shp in (("cb", [128, 128]), ("wbm", [128, 128]), ("esel", [64, 32 * 128]), ("ovl", [128, 128]), ("sel", [12, 12 * 128]),
                    ("ident", [128, 128])):
        b = P.sb("c_" + nm, shp, BF16)
        P.dma("gpsimd", b[:], getattr(k, nm), writes=[b])
        cst_f[nm] = b
    cb, wbm, esel, ovl, sel, identb = (cst_f[n] for n in ("cb", "wbm", "esel", "ovl", "sel", "ident"))
    identf = P.sb("identf", [128, 128], F32)
    P.dma("sync", identf[:], k.ident, writes=[identf])
    mb = P.sb("mb", [128, 128], F32)
    P.dma("sync", mb[:], k.mb, writes=[mb])
    keep = P.sb("keep", [128, 32 * 64], F32)
    addc = P.sb("addc", [128, 32 * 64], F32)
    P.dma("sync", keep[:], k.keep, writes=[keep])
    P.dma("sync", addc[:], k.addc, writes=[addc])
    onesb = P.sb("onesb", [128, 128], BF16)
    P.op("vector", lambda e: e.memset(onesb[:], 1.0), writes=[onesb])

    ksT = P.sb("ksT", [128, T], BF16)
    kwT = P.sb("kwT", [128, T], BF16)
    vs = P.sb("vs", [128, 32, 128], BF16)
    vw = P.sb("vw", [128, 32, 128], BF16)
    kcm = P.sb("kcm", [128, 256], BF16)
    vcm = P.sb("vcm", [128, 2, 128], BF16)
    sgt = P.sb("sgt", [12, T], BF16)
    qnb = [P.sb("qnb%d" % i, [128, 4, 512], BF16) for i in range(2)]
    qrb = [P.sb("qrb%d" % i, [128, 4, 512], BF16) for i in range(2)]
    S = [P.ps("S%d" % i, [128, 512]) for i in range(2)]
    BD = [P.ps("BD%d" % i, [128, 512]) for i in range(2)]
    BO = [P.ps("BO%d" % i, [128, 512]) for i in range(2)]
    BG = P.ps("BG", [128, 512])
    BM = P.ps("BM", [128, 512])
    pT = [P.sb("pT%d" % i, [128, 512], BF16) for i in range(3)]
    pTc = [P.sb("pTc%d" % i, [128, 512], BF16) for i in range(2)]
    pn = [P.sb("pn%d" % i, [128, 512], BF16) for i in range(2)]
    bc = [P.sb("bc%d" % i, [128, 128], BF16) for i in range(2)]
    W = [P.sb("W%d" % i, [128, 512], F32) for i in range(2)]
    Wg = [P.sb("Wg%d" % i, [128, 512], F32) for i in range(2)]
    tmp = [P.sb("tmp%d" % i, [128, 512], F32) for i in range(2)]
    acc = [P.sb("acc%d" % i, [128, 512], F32) for i in range(2)]
    accb = [P.sb("accb%d" % i, [128, 512], BF16) for i in range(2)]
    impT = P.sb("impT", [64, 128], F32)
    imp = P.sb("imp", [128, 64], F32)
    imp2 = P.sb("imp2", [128, 64], F32)
    m8a = P.sb("m8a", [128, 8], F32)
    m8b = P.sb("m8b", [128, 8], F32)
    bias = P.sb("bias", [128, 64], BF16)
    biasT = P.sb("biasT", [64, 128], BF16)
    cnt = {"s": 0, "pt": 0, "bc": 0, "set": 0, "w": 0}

    def rhs4(buf, qi):
        return buf[:, :, qi * 128:(qi + 1) * 128]

    def as4(ap):
        return ap.rearrange("p (g t) -> p g t", g=4)

    def bcast4(ap, np_):
        return ap.unsqueeze(1).to_broadcast([np_, 4, 128])

    for hk in hks:
        P.dma("sync", ksT[:], k.ksT[hk * 128:(hk + 1) * 128, :], writes=[ksT])
        P.dma("sync", kwT[:], k.kwT[hk * 128:(hk + 1) * 128, :], writes=[kwT])
        for q4 in range(4):
            P.dma("sync", vs[:, q4 * 8:(q4 + 1) * 8, :],
                  k.vs[q4 * 1024:(q4 + 1) * 1024, hk * 128:(hk + 1) * 128].rearrange("(t p) d -> p t d", p=128), writes=[vs])
            P.dma("sync", vw[:, q4 * 8:(q4 + 1) * 8, :],
                  k.vw[q4 * 1024:(q4 + 1) * 1024, hk * 128:(hk + 1) * 128].rearrange("(t p) d -> p t d", p=128), writes=[vw])
        P.dma("sync", kcm[:], k.kcmpT[hk], writes=[kcm])
        P.dma("sync", vcm[:], k.vcmp[hk].rearrange("(c p) d -> p c d", p=128), writes=[vcm])
        P.dma("sync", sgt[:], k.ngT[hk * 12:(hk + 1) * 12, :], writes=[sgt])
        for qt in range(T // 128):
            qb, qi = qt // 4, qt % 4
            qn_, qr_ = qnb[qb % 2], qrb[qb % 2]
            if qi == 0:
                tok = slice(qb * 512, (qb + 1) * 512)
                P.dma("sync", qn_[:], k.qn[hk * 512:(hk + 1) * 512, tok].rearrange("(g d) t -> d g t", d=128), writes=[qn_])
                P.dma("sync", qr_[:], k.qr[hk * 512:(hk + 1) * 512, tok].rearrange("(g d) t -> d g t", d=128), writes=[qr_])
            tsl = slice(qt * 128, (qt + 1) * 128)

            def gates(j):
                for g in range(4):
                    r = 3 * g + j
                    P.op("tensor", lambda e, g=g, r=r: e.matmul(BG[:, g * 128:(g + 1) * 128], lhsT=sel[:, r * 128:(r + 1) * 128],
                                                                rhs=sgt[:, tsl], start=True, stop=True),
                         reads=[sel, sgt], writes=[BG], signal=(g == 3))

            def combine(st, first, w_):
                wg = Wg[cnt["w"] % 2]
                tp = tmp[cnt["w"] % 2]
                cnt["w"] += 1
                ac = acc[qt % 2]
                P.op("vector", lambda e, wg=wg, w_=w_: e.tensor_tensor(out=wg[:], in0=BG[:], in1=w_[:], op=ALU.mult),
                     reads=[BG, w_], writes=[wg])
                if first:
                    P.op("vector", lambda e, wg=wg, ac=ac, st=st: e.tensor_tensor(out=ac[:], in0=BO[st][:], in1=wg[:], op=ALU.mult),
                         reads=[BO[st], wg], writes=[ac])
                else:
                    P.op("vector", lambda e, wg=wg, tp=tp, st=st: e.tensor_tensor(out=tp[:], in0=BO[st][:], in1=wg[:], op=ALU.mult),
                         reads=[BO[st], wg], writes=[tp])
                    P.op("gpsimd", lambda e, tp=tp, ac=ac: e.tensor_tensor(out=ac[:], in0=ac[:], in1=tp[:], op=ALU.add),
                         reads=[ac, tp], writes=[ac])

            st = cnt["set"] % 2
            cnt["set"] += 1
            nct = 1 if qt <= 15 else 2
            for ct in range(nct):
                s_ = S[cnt["s"] % 2]
                cnt["s"] += 1
                need_mask = not (ct == 0 and qt >= 17)
                P.op("tensor", lambda e, s_=s_, ct=ct, qn_=qn_: e.matmul(as4(s_[:]), lhsT=kcm[:, ct * 128:(ct + 1) * 128], rhs=rhs4(qn_, qi),
                                                                        start=True, stop=not need_mask),
                     reads=[kcm, qn_], writes=[s_], signal=not need_mask)
                if need_mask:
                    b_ = bc[cnt["bc"] % 2]
                    cnt["bc"] += 1
                    thr = float(128 * qt - 2048 * ct - 31)
                    P.op("gpsimd", lambda e, b_=b_, thr=thr: e.tensor_scalar(out=b_[:], in0=mb[:], scalar1=thr, scalar2=NEG, op0=ALU.is_gt, op1=ALU.mult),
                         reads=[mb], writes=[b_])
                    P.op("tensor", lambda e, s_=s_, b_=b_: e.matmul(as4(s_[:]), lhsT=identb[:], rhs=bcast4(b_[:], 128), start=False, stop=True),
                         reads=[identb, b_], writes=[s_])
                p_ = pTc[ct]
                P.op("scalar", lambda e, s_=s_, p_=p_: e.activation(out=p_[:], in_=s_[:], func=AF.Exp, scale=SCALE), reads=[s_], writes=[p_])
                P.op("tensor", lambda e, p_=p_, st=st, ct=ct: e.matmul(BD[st][:], lhsT=onesb[:], rhs=p_[:], start=(ct == 0), stop=(ct == nct - 1)),
                     reads=[onesb, p_], writes=[BD[st]], signal=(ct == nct - 1))
                P.op("tensor", lambda e, p_=p_, st=st, ct=ct: e.matmul(BO[st][:], lhsT=vcm[:, ct, :], rhs=p_[:], start=(ct == 0), stop=(ct == nct - 1)),
                     reads=[vcm, p_], writes=[BO[st]], signal=(ct == nct - 1))
            w_ = W[cnt["w"] % 2]
            P.op("vector", lambda e, w_=w_, st=st: e.tensor_scalar(out=w_[:], in0=BD[st][:], scalar1=1e-30, scalar2=None, op0=ALU.add),
                 reads=[BD[st]], writes=[w_])
            P.op("vector", lambda e, w_=w_: e.reciprocal(out=w_[:], in_=w_[:]), reads=[w_], writes=[w_])
            if qt >= 8:
                for ct in range(nct):
                    P.op("gpsimd", lambda e, ct=ct, w_=w_: e.tensor_tensor(out=pn[ct][:], in0=pTc[ct][:], in1=w_[:], op=ALU.mult),
                         reads=[pTc[ct], w_], writes=[pn[ct]])
                n_mm = nct * 4
                i_mm = 0
                for ct in range(nct):
                    for g in range(4):
                        P.op("tensor", lambda e, ct=ct, g=g, i_mm=i_mm: e.matmul(BM[0:64, 0:128], lhsT=ovl[:, ct * 64:(ct + 1) * 64],
                                                                                rhs=pn[ct][:, g * 128:(g + 1) * 128],
                                                                                start=(i_mm == 0), stop=(i_mm == n_mm - 1)),
                             reads=[ovl, pn[ct]], writes=[BM], signal=(i_mm == n_mm - 1))
                        i_mm += 1
                P.op("vector", lambda e: e.tensor_copy(out=impT[:], in_=BM[0:64, 0:128]), reads=[BM], writes=[impT])
                P.op("tensor", lambda e: e.transpose(out=BM[:, 128:192], in_=impT[:], identity=identf[0:64, 0:64]),
                     reads=[impT, identf], writes=[BM])
                P.op("vector", lambda e: e.tensor_tensor(out=imp[:], in0=BM[:, 128:192], in1=keep[:, qt * 64:(qt + 1) * 64], op=ALU.mult),
                     reads=[BM, keep], writes=[imp])
                P.op("vector", lambda e: e.tensor_tensor(out=imp[:], in0=imp[:], in1=addc[:, qt * 64:(qt + 1) * 64], op=ALU.add),
                     reads=[imp, addc], writes=[imp])
                P.op("vector", lambda e: e.max(out=m8a[:], in_=imp[:]), reads=[imp], writes=[m8a])
                P.op("vector", lambda e: e.match_replace(out=imp2[:], in_to_replace=m8a[:], in_values=imp[:], imm_value=-3.0e38),
                     reads=[imp, m8a], writes=[imp2])
                P.op("vector", lambda e: e.max(out=m8b[:], in_=imp2[:]), reads=[imp2], writes=[m8b])
                P.op("vector", lambda e: e.tensor_scalar(out=bias[:], in0=imp[:], scalar1=m8b[:, 7:8], scalar2=NEG, op0=ALU.is_lt, op1=ALU.mult),
                     reads=[imp, m8b], writes=[bias])
                P.op("tensor", lambda e: e.transpose(out=BM[0:64, 256:320].bitcast(BF16), in_=bias[:], identity=identb[:]),
                     reads=[bias, identb], writes=[BM])
                P.op("vector", lambda e: e.tensor_copy(out=biasT[:], in_=BM[0:64, 256:320].bitcast(BF16)), reads=[BM], writes=[biasT])
            gates(0)
            combine(st, True, w_)

            def branch(kT_, v_, kts, kind):
                st = cnt["set"] % 2
                cnt["set"] += 1
                nk = len(kts)

                def emit_score(ii):
                    kt = kts[ii]
                    s_ = S[cnt["s"] % 2]
                    cnt["s"] += 1
                    extra = []
                    if kind == "slc":
                        if qt >= 8:
                            extra.append(("sel", kt))
                        if kt == qt:
                            extra.append(("cb", None))
                    else:
                        if kt == qt:
                            extra.append(("cb", None))
                        if kt == qt - 4:
                            extra.append(("wb", None))
                    P.op("tensor", lambda e: e.matmul(as4(s_[:]), lhsT=kT_[:, kt * 128:(kt + 1) * 128], rhs=rhs4(qr_, qi),
                                                      start=True, stop=(len(extra) == 0)),
                         reads=[kT_, qr_], writes=[s_], signal=(len(extra) == 0))
                    for xi, (xk, xa) in enumerate(extra):
                        last = (xi == len(extra) - 1)
                        if xk == "sel":
                            P.op("tensor", lambda e: e.matmul(as4(s_[:]), lhsT=esel[:, xa * 128:(xa + 1) * 128],
                                                              rhs=bcast4(biasT[:], 64), start=False, stop=last),
                                 reads=[esel, biasT], writes=[s_], signal=last)
                        else:
                            mk = cb if xk == "cb" else wbm
                            P.op("tensor", lambda e: e.matmul(as4(s_[:]), lhsT=identb[:], rhs=bcast4(mk[:], 128),
                                                              start=False, stop=last),
                                 reads=[identb, mk], writes=[s_], signal=last)
                    return s_

                def emit_rest(ii, s_):
                    kt = kts[ii]
                    p_ = pT[cnt["pt"] % 3]
                    cnt["pt"] += 1
                    P.op("scalar", lambda e: e.activation(out=p_[:], in_=s_[:], func=AF.Exp, scale=SCALE), reads=[s_], writes=[p_])
                    P.op("tensor", lambda e: e.matmul(BD[st][:], lhsT=onesb[:], rhs=p_[:], start=(ii == 0), stop=(ii == nk - 1)),
                         reads=[onesb, p_], writes=[BD[st]], signal=(ii == nk - 1))
                    P.op("tensor", lambda e: e.matmul(BO[st][:], lhsT=v_[:, kt, :], rhs=p_[:], start=(ii == 0), stop=(ii == nk - 1)),
                         reads=[v_, p_], writes=[BO[st]], signal=(ii == nk - 1))

                s_cur = emit_score(0)
                for ii in range(nk):
                    s_next = emit_score(ii + 1) if ii + 1 < nk else None
                    emit_rest(ii, s_cur)
                    s_cur = s_next
                w2 = W[cnt["w"] % 2]
                P.op("vector", lambda e, w2=w2, st=st: e.reciprocal(out=w2[:], in_=BD[st][:]), reads=[BD[st]], writes=[w2])
                return st, w2

            st, w2 = branch(kwT, vw, list(range(max(0, qt - 4), qt + 1)), "win")
            gates(2)
            combine(st, False, w2)
            st, w2 = branch(ksT, vs, list(range(0, qt + 1)), "slc")
            gates(1)
            combine(st, False, w2)
            ac = acc[qt % 2]
            ab = accb[qt % 2]
            P.op("gpsimd", lambda e, ac=ac, ab=ab: e.tensor_copy(out=ab[:], in_=ac[:]), reads=[ac], writes=[ab])
            P.dma("sync", k.mixT[hk * 512:(hk + 1) * 512, tsl].rearrange("(g d) t -> d g t", d=128), as4(ab[:]), reads=[ab])
    P.flush()
    P.release(m0)


def phase_gla(k, l):
    P = k.P
    m0 = P.mark()
    GS = 128 ** -0.5
    wa2 = P.sb("wa2", [16, 512], BF16)
    brow = P.sb("brow", [1, 512], BF16)
    ones1 = P.sb("ones1", [1, 128], BF16)
    tri2f = P.sb("tri2f", [128, 128], F32)
    suf = P.sb("suf", [128, 128], F32)
    onesb = P.sb("onesb", [128, 128], BF16)
    nw = P.sb("nw", [128, 2], F32)
    P.dma("gpsimd", wa2[:], k.gla_w_a2[l], writes=[wa2])
    P.dma("gpsimd", brow[:], k.gla_b_a[l:l + 1, :], writes=[brow])
    P.dma("sync", tri2f[:], k.tri2, writes=[tri2f])
    P.dma("sync", suf[:], k.sumat, writes=[suf])
    P.dma("sync", nw[:], k.gla_nwT[l], writes=[nw])
    P.op("vector", lambda e: e.memset(ones1[:], 1.0), writes=[ones1])
    P.op("vector", lambda e: e.memset(onesb[:], 1.0), writes=[onesb])
    Sf = P.sb("Sf", [128, 4, 256], F32)
    Sb = P.sb("Sb", [128, 4, 256], BF16)
    Sfv = [P.view(Sf) for _ in range(4)]
    Sbv = [P.view(Sb) for _ in range(4)]
    for h in range(4):
        P.op("vector", lambda e, h=h: e.memset(Sf[:, h, :], 0.0), writes=[Sfv[h]])
        P.op("vector", lambda e, h=h: e.memset(Sb[:, h, :], 0.0), writes=[Sbv[h]])
    gqTb = [P.sb("gqTb%d" % i, [128, 4, 512], BF16) for i in range(2)]
    gkTb = [P.sb("gkTb%d" % i, [128, 4, 512], BF16) for i in range(2)]
    ggb = [P.sb("ggb%d" % i, [128, 8, 512], BF16) for i in range(2)]
    gaTb = [P.sb("gaTb%d" % i, [16, 512], BF16) for i in range(2)]
    gkt = [P.sb("gkt%d" % i, [128, 512], BF16) for i in range(2)]
    gvt = [P.sb("gvt%d" % i, [128, 1024], BF16) for i in range(2)]
    Lt = [P.sb("Lt%d" % i, [128, 512], F32) for i in range(2)]
    E1 = [P.sb("E1%d" % i, [128, 512], F32) for i in range(2)]
    kst = [P.sb("kst%d" % i, [128, 512], BF16) for i in range(2)]
    EbT = [P.sb("EbT%d" % i, [128, 128], F32) for i in range(2)]
    EnbT = [P.sb("EnbT%d" % i, [128, 128], F32) for i in range(2)]
    qdT = [P.sb("qdT%d" % i, [128, 128], BF16) for i in range(2)]
    kiT = [P.sb("kiT%d" % i, [128, 128], BF16) for i in range(2)]
    ATm = [P.sb("ATm%d" % i, [128, 128], BF16) for i in range(2)]
    o1 = [P.sb("o1%d" % i, [128, 256], F32) for i in range(2)]
    sq = [P.sb("sq%d" % i, [128, 256], BF16) for i in range(2)]
    lnr = [P.sb("lnr%d" % i, [128, 128], F32) for i in range(2)]
    rstd = [P.sb("rstd%d" % i, [128, 128], F32) for i in range(2)]
    tmpo = [P.sb("tmpo%d" % i, [128, 256], F32) for i in range(2)]
    outb = [P.sb("outb%d" % i, [128, 2, 128], BF16) for i in range(2)]
    pz = P.ps("pz", [128, 512])
    pcs = P.ps("pcs", [128, 512])
    psu = P.ps("psu", [128, 512])
    pcsT = P.ps("pcsT", [128, 512])
    pAT = P.ps("pAT", [128, 512])
    po = P.ps("po", [128, 512])
    pS = P.ps("pS", [128, 512])
    pss = P.ps("pss", [128, 512])
    hc = 0
    for tt in range(T // 128):
        tb, ti = tt // 4, tt % 4
        gq_, gk_, gg_, ga_ = gqTb[tb % 2], gkTb[tb % 2], ggb[tb % 2], gaTb[tb % 2]
        if ti == 0:
            tok = slice(tb * 512, (tb + 1) * 512)
            P.dma("sync", gq_[:], k.gqT[:, tok].rearrange("(h d) t -> d h t", d=128), writes=[gq_])
            P.dma("sync", gk_[:], k.gkT[:, tok].rearrange("(h d) t -> d h t", d=128), writes=[gk_])
            P.dma("sync", gg_[:], k.ggT[:, tok].rearrange("(c d) t -> d c t", d=128), writes=[gg_])
            P.dma("sync", ga_[:], k.gaT[:, tok], writes=[ga_])
        lsl = slice(ti * 128, (ti + 1) * 128)
        tsl = slice(tt * 128, (tt + 1) * 128)
        gkt_, gvt_ = gkt[tt % 2], gvt[tt % 2]
        P.dma("sync", gkt_[:], k.gk[tsl, :], writes=[gkt_])
        P.dma("sync", gvt_[:], k.gv[tsl, :], writes=[gvt_])
        L_, E1_, kst_ = Lt[tt % 2], E1[tt % 2], kst[tt % 2]
        P.op("tensor", lambda e: e.matmul(pz[:], lhsT=ga_[:, lsl], rhs=wa2[:], start=True, stop=False), reads=[ga_, wa2], writes=[pz], signal=False)
        P.op("tensor", lambda e: e.matmul(pz[:], lhsT=ones1[:], rhs=brow[:], start=False, stop=True), reads=[ones1, brow], writes=[pz])
        P.op("scalar", lambda e: e.activation(out=L_[:], in_=pz[:], func=AF.Exp, scale=-1.0), reads=[pz], writes=[L_])
        P.op("scalar", lambda e: e.activation(out=L_[:], in_=L_[:], func=AF.Ln, bias=1.0), reads=[L_], writes=[L_])
        P.op("tensor", lambda e: e.matmul(pcs[:], lhsT=tri2f[:], rhs=L_[:], start=True, stop=True), reads=[tri2f, L_], writes=[pcs])
        P.op("tensor", lambda e: e.matmul(psu[:], lhsT=suf[:], rhs=L_[:], start=True, stop=True), reads=[suf, L_], writes=[psu])
        P.op("scalar", lambda e: e.activation(out=E1_[:], in_=psu[:], func=AF.Exp, scale=-1.0 / 16.0), reads=[psu], writes=[E1_])
        P.op("vector", lambda e: e.tensor_tensor(out=kst_[:], in0=gkt_[:], in1=E1_[:], op=ALU.mult), reads=[gkt_, E1_], writes=[kst_])
        bufsel = {}
        for h in range(4):
            i2 = hc % 2
            hc += 1
            bufsel[h] = i2

        def gla_front(h):
            i2 = bufsel[h]
            Eb_, Enb_, qd_, ki_, AT_ = EbT[i2], EnbT[i2], qdT[i2], kiT[i2], ATm[i2]
            hs = slice(h * 128, (h + 1) * 128)
            P.op("tensor", lambda e: e.matmul(pcsT[:, 0:128], lhsT=L_[:, hs], rhs=tri2f[:], start=True, stop=True),
                 reads=[L_, tri2f], writes=[pcsT])
            P.op("scalar", lambda e: e.activation(out=Eb_[:], in_=pcsT[:, 0:128], func=AF.Exp, scale=-1.0 / 16.0), reads=[pcsT], writes=[Eb_])
            P.op("scalar", lambda e: e.activation(out=Enb_[:], in_=pcsT[:, 0:128], func=AF.Exp, scale=1.0 / 16.0), reads=[pcsT], writes=[Enb_])
            P.op("vector", lambda e: e.scalar_tensor_tensor(out=qd_[:], in0=gq_[:, h, lsl], scalar=GS, in1=Eb_[:], op0=ALU.mult, op1=ALU.mult),
                 reads=[gq_, Eb_], writes=[qd_])
            P.op("gpsimd", lambda e: e.tensor_tensor(out=ki_[:], in0=gk_[:, h, lsl], in1=Enb_[:], op=ALU.mult), reads=[gk_, Enb_], writes=[ki_])
            P.op("tensor", lambda e: e.matmul(pAT[:, 0:128], lhsT=ki_[:], rhs=qd_[:], start=True, stop=True), reads=[ki_, qd_], writes=[pAT])
            P.op("vector", lambda e: e.tensor_tensor(out=AT_[:], in0=pAT[:, 0:128], in1=tri2f[:], op=ALU.mult), reads=[pAT, tri2f], writes=[AT_])

        def gla_back(h):
            i2 = bufsel[h]
            Eb_, Enb_, qd_, ki_, AT_ = EbT[i2], EnbT[i2], qdT[i2], kiT[i2], ATm[i2]
            o1_, sq_, lnr_, rs_, tp_, ob_ = o1[i2], sq[i2], lnr[i2], rstd[i2], tmpo[i2], outb[i2]
            hs = slice(h * 128, (h + 1) * 128)
            for hf in range(2):
                cs_ = slice(hf * 64, (hf + 1) * 64)
                for dvc in range(2):
                    oc = slice(dvc * 128 + hf * 64, dvc * 128 + hf * 64 + 64)
                    P.op("tensor", lambda e: e.matmul(po[:, oc], lhsT=gvt_[:, h * 256 + dvc * 128:h * 256 + dvc * 128 + 128], rhs=AT_[:, cs_],
                                                      start=True, stop=False), reads=[gvt_, AT_], writes=[po], signal=False)
                    P.op("tensor", lambda e: e.matmul(po[:, oc], lhsT=Sb[:, h, dvc * 128:(dvc + 1) * 128], rhs=qd_[:, cs_],
                                                      start=False, stop=True), reads=[Sbv[h], qd_], writes=[po],
                         signal=(hf == 1 and dvc == 1))
                P.op("tensor", lambda e: e.matmul(pS[:, 0:256], lhsT=kst_[cs_, hs], rhs=gvt_[cs_, h * 256:(h + 1) * 256], start=True, stop=True),
                     reads=[kst_, gvt_], writes=[pS])
                col = hf * 64 + 63
                P.op("vector", lambda e: e.scalar_tensor_tensor(out=Sf[:, h, :], in0=Sf[:, h, :], scalar=Eb_[:, col:col + 1], in1=pS[:, 0:256],
                                                                op0=ALU.mult, op1=ALU.add), reads=[Sfv[h], Eb_, pS], writes=[Sfv[h]])
                P.op("gpsimd", lambda e: e.tensor_copy(out=Sb[:, h, :], in_=Sf[:, h, :]), reads=[Sfv[h]], writes=[Sbv[h]])
            P.op("scalar", lambda e: e.copy(out=o1_[:], in_=po[:, 0:256]), reads=[po], writes=[o1_])
            P.op("gpsimd", lambda e: e.tensor_tensor(out=sq_[:], in0=o1_[:], in1=o1_[:], op=ALU.mult), reads=[o1_], writes=[sq_])
            P.op("tensor", lambda e: e.matmul(pss[:, 0:128], lhsT=onesb[:], rhs=sq_[:, 0:128], start=True, stop=False), reads=[onesb, sq_], writes=[pss], signal=False)
            P.op("tensor", lambda e: e.matmul(pss[:, 0:128], lhsT=onesb[:], rhs=sq_[:, 128:256], start=False, stop=True), reads=[onesb, sq_], writes=[pss])
            P.op("scalar", lambda e: e.activation(out=lnr_[:], in_=pss[:, 0:128], func=AF.Ln, scale=1.0 / 256.0, bias=NORM_EPS), reads=[pss], writes=[lnr_])
            P.op("scalar", lambda e: e.activation(out=rs_[:], in_=lnr_[:], func=AF.Exp, scale=-0.5), reads=[lnr_], writes=[rs_])
            for dvc in range(2):
                ds_ = slice(dvc * 128, (dvc + 1) * 128)
                P.op("vector", lambda e: e.tensor_tensor(out=tp_[:, ds_], in0=o1_[:, ds_], in1=rs_[:], op=ALU.mult), reads=[o1_, rs_], writes=[tp_])
                P.op("vector", lambda e: e.scalar_tensor_tensor(out=ob_[:, dvc, :], in0=gg_[:, h * 2 + dvc, lsl], scalar=nw[:, dvc:dvc + 1], in1=tp_[:, ds_],
                                                                op0=ALU.mult, op1=ALU.mult), reads=[gg_, nw, tp_], writes=[ob_])
            P.dma("sync", k.mixT[1024 + h * 256:1024 + (h + 1) * 256, tsl].rearrange("(c d) t -> d c t", d=128), ob_[:], reads=[ob_])

        gla_front(0)
        for h in range(4):
            if h < 3:
                gla_front(h + 1)
            gla_back(h)
    P.flush()
    P.release(m0)


def ln_tile(P, r, lng, lnb, stats, mv, rstd):
    for c in range(4):
        P.op("vector", lambda e, c=c: e.bn_stats(out=stats[:, c, :], in_=r[:, c * 512:(c + 1) * 512]), reads=[r], writes=[stats])
    P.op("vector", lambda e: e.bn_aggr(out=mv[:], in_=stats[:]), reads=[stats], writes=[mv])
    P.op("scalar", lambda e: e.activation(out=rstd[:], in_=mv[:, 1:2], func=AF.Sqrt, bias=LN_EPS), reads=[mv], writes=[rstd])
    P.op("vector", lambda e: e.reciprocal(out=rstd[:], in_=rstd[:]), reads=[rstd], writes=[rstd])
    P.op("vector", lambda e: e.tensor_scalar(out=r[:], in0=r[:], scalar1=mv[:, 0:1], scalar2=rstd[:, 0:1], op0=ALU.subtract, op1=ALU.mult),
         reads=[r, mv, rstd], writes=[r])
    P.op("gpsimd", lambda e: e.tensor_tensor(out=r[:], in0=r[:], in1=lng[:], op=ALU.mult), reads=[r, lng], writes=[r])
    P.op("gpsimd", lambda e: e.tensor_tensor(out=r[:], in0=r[:], in1=lnb[:], op=ALU.add), reads=[r, lnb], writes=[r])


def phase_wout(k, l, xsrc, xdst):
    P = k.P
    m0 = P.mark()
    wo = P.sb("wo", [128, KC, D], BF16)
    wv = k.w_out[l].rearrange("(c p) n -> p c n", p=128)
    for q in range(4):
        P.dma("gpsimd", wo[:, :, q * 512:(q + 1) * 512], wv[:, :, q * 512:(q + 1) * 512], writes=[wo])
    garep = P.sb("garep", [128, D], F32)
    lng = P.sb("lng", [128, D], F32)
    lnb = P.sb("lnb", [128, D], F32)
    P.dma("sync", garep[:], k.modv[l, 2 * D:3 * D].partition_broadcast(128), writes=[garep])
    P.dma("sync", lng[:], k.ln_mix_g[l].partition_broadcast(128), writes=[lng])
    P.dma("sync", lnb[:], k.ln_mix_b[l].partition_broadcast(128), writes=[lnb])
    mixb = [P.sb("mixb%d" % i, [128, KC, 512], BF16) for i in range(2)]
    xt = [P.sb("xt%d" % i, [128, D], F32) for i in range(2)]
    rt = [P.sb("rt%d" % i, [128, D], F32) for i in range(2)]
    stats = P.sb("stats", [128, 4, 6], F32)
    mv = P.sb("mv", [128, 2], F32)
    rstd = P.sb("rstd", [128, 1], F32)
    py = [P.ps("py%d" % i, [128, 512]) for i in range(8)]
    mixv = k.mixT.rearrange("(c p) t -> p c t", p=128)
    for tt in range(T // 128):
        tb, ti = tt // 4, tt % 4
        mb_ = mixb[tb % 2]
        if ti == 0:
            for q in range(4):
                P.dma("sync", mb_[:, q * 4:(q + 1) * 4, :], mixv[:, q * 4:(q + 1) * 4, tb * 512:(tb + 1) * 512], writes=[mb_])
        x_ = xt[tt % 2]
        r_ = rt[tt % 2]
        tsl = slice(tt * 128, (tt + 1) * 128)
        P.dma("sync", x_[:], xsrc[tsl, :], writes=[x_])
        for db in range(4):
            p_ = py[(tt % 2) * 4 + db]
            for c in range(KC):
                P.op("tensor", lambda e: e.matmul(p_[:], lhsT=mb_[:, c, ti * 128:(ti + 1) * 128], rhs=wo[:, c, db * 512:(db + 1) * 512],
                                                  start=(c == 0), stop=(c == KC - 1)), reads=[mb_, wo], writes=[p_], signal=(c == KC - 1))
            P.op("vector", lambda e: e.tensor_tensor(out=r_[:, db * 512:(db + 1) * 512], in0=p_[:], in1=garep[:, db * 512:(db + 1) * 512], op=ALU.mult),
                 reads=[p_, garep], writes=[r_])
        P.op("vector", lambda e: e.scalar_tensor_tensor(out=r_[:], in0=x_[:], scalar=ALPHA, in1=r_[:], op0=ALU.mult, op1=ALU.add),
             reads=[x_, r_], writes=[r_])
        ln_tile(P, r_, lng, lnb, stats, mv, rstd)
        P.dma("sync", xdst[tsl, :], r_[:], reads=[r_])
    P.flush()
    P.release(m0)


def ffn_gateup(k, src, nrows, wg, wu, dff, AT, mod=None, src_bf16=False):
    P = k.P
    RH = min(nrows, 2048)
    for r0 in range(0, nrows, RH):
        nr = min(RH, nrows - r0)
        m0 = P.mark()
        identf = P.sb("identf", [128, 128], F32)
        P.dma("sync", identf[:], k.ident, writes=[identf])
        hT = P.sb("hT", [128, KC, RH], BF16)
        hviews = [(P.view(hT), P.view(hT)) for _ in range(nr // 128)]
        ptr = [P.ps("ptr%d" % i, [128, 4, 128]) for i in range(2)]
        if mod is not None:
            l = mod
            screp = P.sb("screp", [128, D], F32)
            shrep = P.sb("shrep", [128, D], F32)
            xt = [P.sb("xt%d" % i, [128, D], F32) for i in range(2)]
            P.dma("sync", screp[:], k.modv[l, 4 * D:5 * D].partition_broadcast(128), writes=[screp])
            P.dma("sync", shrep[:], k.modv[l, 3 * D:4 * D].partition_broadcast(128), writes=[shrep])
            build_hT(k, src, r0, nr // 128, screp, shrep, identf, hT, hviews, xt, ptr)
        else:
            identb = P.sb("identb", [128, 128], BF16)
            P.dma("gpsimd", identb[:], k.ident, writes=[identb])
            xt = [P.sb("xtb%d" % i, [128, D], BF16) for i in range(2)]
            for j in range(nr // 128):
                xb = xt[j % 2]
                P.dma("sync", xb[:], src[r0 + j * 128:r0 + (j + 1) * 128, :], writes=[xb])
                for g in range(KC // 4):
                    pt = ptr[(j * 4 + g) % 2]
                    ptb = pt[:].rearrange("p q t -> p (q t)")[:, 0:256].bitcast(BF16).rearrange("p (q t) -> p q t", q=4)
                    for q in range(4):
                        c = g * 4 + q
                        P.op("tensor", lambda e: e.transpose(out=ptb[:, q, :], in_=xb[:, c * 128:(c + 1) * 128], identity=identb[:]),
                             reads=[xb, identb], writes=[pt], signal=(q == 3))
                    if g % 2 == 0:
                        P.op("scalar", lambda e: e.copy(out=hT[:, g * 4:(g + 1) * 4, j * 128:(j + 1) * 128], in_=ptb), reads=[pt], writes=[hviews[j][0]])
                    else:
                        P.op("vector", lambda e: e.tensor_copy(out=hT[:, g * 4:(g + 1) * 4, j * 128:(j + 1) * 128], in_=ptb), reads=[pt], writes=[hviews[j][1]])
        wgb = [P.sb("wgb%d" % i, [128, KC, 256], BF16) for i in range(2)]
        wub = [P.sb("wub%d" % i, [128, KC, 256], BF16) for i in range(2)]
        pg = [P.ps("pg%d" % i, [128, 512]) for i in range(2)]
        pu = [P.ps("pu%d" % i, [128, 512]) for i in range(2)]
        sgb = [P.sb("sgb%d" % i, [128, 512], BF16) for i in range(2)]
        ab = [P.sb("ab%d" % i, [128, 512], BF16) for i in range(3)]
        wgv = wg.rearrange("(c p) n -> p c n", p=128)
        wuv = wu.rearrange("(c p) n -> p c n", p=128)
        blocks = [(b0, min(512, nr - b0)) for b0 in range(0, nr, 512)]
        it = 0
        for fb in range(dff // 256):
            wg_, wu_ = wgb[fb % 2], wub[fb % 2]
            P.dma("gpsimd", wg_[:], wgv[:, :, fb * 256:(fb + 1) * 256], writes=[wg_])
            P.dma("gpsimd", wu_[:], wuv[:, :, fb * 256:(fb + 1) * 256], writes=[wu_])
            for ft in range(2):
                f0 = fb * 256 + ft * 128
                for (b0, bn) in blocks:
                    hr = []
                    for j in range(b0 // 128, (b0 + bn) // 128):
                        hr += [hviews[j][0], hviews[j][1]]
                    pg_, pu_ = pg[it % 2], pu[it % 2]
                    sg_, a_ = sgb[it % 2], ab[it % 3]
                    it += 1
                    for c in range(KC):
                        P.op("tensor", lambda e: e.matmul(pg_[:, 0:bn], lhsT=wg_[:, c, ft * 128:(ft + 1) * 128], rhs=hT[:, c, b0:b0 + bn],
                                                          start=(c == 0), stop=(c == KC - 1)), reads=[wg_] + hr, writes=[pg_], signal=(c == KC - 1))
                    for c in range(KC):
                        P.op("tensor", lambda e: e.matmul(pu_[:, 0:bn], lhsT=wu_[:, c, ft * 128:(ft + 1) * 128], rhs=hT[:, c, b0:b0 + bn],
                                                          start=(c == 0), stop=(c == KC - 1)), reads=[wu_] + hr, writes=[pu_], signal=(c == KC - 1))
                    P.op("scalar", lambda e: e.activation(out=sg_[:, 0:bn], in_=pg_[:, 0:bn], func=AF.Silu), reads=[pg_], writes=[sg_])
                    P.op("vector", lambda e: e.tensor_tensor(out=a_[:, 0:bn], in0=pu_[:, 0:bn], in1=sg_[:, 0:bn], op=ALU.mult), reads=[pu_, sg_], writes=[a_])
                    P.dma("sync", AT[f0:f0 + 128, r0 + b0:r0 + b0 + bn], a_[:, 0:bn], reads=[a_])
        P.flush()
        P.release(m0)


class _PV:
    def __init__(self, b):
        self.b = b

    def __getitem__(self, idx):
        return self.b.t[:].rearrange("p (q t) -> p q t", q=4)[idx]


def ffn_down(k, AT, nrows, wd, dff, Y, row_off=0):
    P = k.P
    m0 = P.mark()
    FC = dff // 128
    G = 4
    assert FC % G == 0
    wdb = [P.sb("wdb%d" % i, [128, FC, 512], BF16) for i in range(2)]
    BLK = 512 if FC <= 44 else 256
    atb = [P.sb("atb%d" % i, [128, FC, BLK], BF16) for i in range(2)]
    yb = [P.sb("yb%d" % i, [128, 512], F32) for i in range(3)]
    py = [P.ps("pyd%d" % i, [128, 512]) for i in range(3)]
    wdv = wd.rearrange("(c p) n -> p c n", p=128)
    atv = AT.rearrange("(c p) t -> p c t", p=128)
    it = 0
    ib = 0
    for db in range(4):
        w_ = wdb[db % 2]
        for q in range(G):
            cs = slice(q * (FC // G), (q + 1) * (FC // G))
            P.dma("gpsimd", w_[:, cs, :], wdv[:, cs, db * 512:(db + 1) * 512], writes=[w_])
        for b0 in range(0, nrows, BLK):
            bn = min(BLK, nrows - b0)
            a_ = atb[ib % 2]
            ib += 1
            for q in range(G):
                cs = slice(q * (FC // G), (q + 1) * (FC // G))
                P.dma("sync", a_[:, cs, 0:bn], atv[:, cs, b0:b0 + bn], writes=[a_])
            for j in range(bn // 128):
                p_ = py[it % 3]
                y_ = yb[it % 3]
                it += 1
                for c in range(FC):
                    P.op("tensor", lambda e: e.matmul(p_[:], lhsT=a_[:, c, j * 128:(j + 1) * 128], rhs=w_[:, c, :], start=(c == 0), stop=(c == FC - 1)),
                         reads=[a_, w_], writes=[p_], signal=(c == FC - 1))
                P.op("scalar", lambda e: e.copy(out=y_[:], in_=p_[:]), reads=[p_], writes=[y_])
                rs = slice(row_off + b0 + j * 128, row_off + b0 + (j + 1) * 128)
                P.dma("sync", Y[rs, db * 512:(db + 1) * 512], y_[:], reads=[y_])
    P.flush()
    P.release(m0)


def phase_ln2(k, l, xsrc, ysrc, xdst):
    P = k.P
    m0 = P.mark()
    gfrep = P.sb("gfrep", [128, D], F32)
    lng = P.sb("lng", [128, D], F32)
    lnb = P.sb("lnb", [128, D], F32)
    P.dma("sync", gfrep[:], k.modv[l, 5 * D:6 * D].partition_broadcast(128), writes=[gfrep])
    P.dma("sync", lng[:], k.ln_ffn_g[l].partition_broadcast(128), writes=[lng])
    P.dma("sync", lnb[:], k.ln_ffn_b[l].partition_broadcast(128), writes=[lnb])
    xt = [P.sb("xt%d" % i, [128, D], F32) for i in range(2)]
    rt = [P.sb("rt%d" % i, [128, D], F32) for i in range(2)]
    stats = P.sb("stats", [128, 4, 6], F32)
    mv = P.sb("mv", [128, 2], F32)
    rstd = P.sb("rstd", [128, 1], F32)
    for tt in range(T // 128):
        x_, r_ = xt[tt % 2], rt[tt % 2]
        tsl = slice(tt * 128, (tt + 1) * 128)
        P.dma("sync", x_[:], xsrc[tsl, :], writes=[x_])
        P.dma("gpsimd", r_[:], ysrc[tsl, :], writes=[r_])
        P.op("vector", lambda e: e.tensor_tensor(out=r_[:], in0=r_[:], in1=gfrep[:], op=ALU.mult), reads=[r_, gfrep], writes=[r_])
        P.op("vector", lambda e: e.scalar_tensor_tensor(out=r_[:], in0=x_[:], scalar=ALPHA, in1=r_[:], op0=ALU.mult, op1=ALU.add),
             reads=[x_, r_], writes=[r_])
        ln_tile(P, r_, lng, lnb, stats, mv, rstd)
        P.dma("sync", xdst[tsl, :], r_[:], reads=[r_])
    P.flush()
    P.release(m0)


CAP = 768
TM = 2048
NSLOT = NE * CAP
BIGIDX = 1.0e6


def phase_moe_route(k, l):
    P = k.P
    m0 = P.mark()
    zt = P.sb("zt", [128, D], BF16)
    P.op("vector", lambda e: e.memset(zt[:], 0.0), writes=[zt])
    for s0 in range(0, NSLOT, 128):
        P.dma("sync" if (s0 // 128) % 2 == 0 else "gpsimd", k.Xs[s0:s0 + 128, :], zt[:], reads=[zt])
    P.flush()
    P.release(m0)

    m0 = P.mark()
    screp = P.sb("screp", [128, D], F32)
    shrep = P.sb("shrep", [128, D], F32)
    identf = P.sb("identf", [128, 128], F32)
    wr = P.sb("wr", [128, KC, NE], F32)
    SLb = P.sb("SLb", [128, 128], BF16)
    onesb = P.sb("onesb", [128, 128], BF16)
    eoff = P.sb("eoff", [128, NE], F32)
    base = P.sb("base", [128, NE], F32)
    P.dma("sync", screp[:], k.modv[l, 4 * D:5 * D].partition_broadcast(128), writes=[screp])
    P.dma("sync", shrep[:], k.modv[l, 3 * D:4 * D].partition_broadcast(128), writes=[shrep])
    P.dma("sync", identf[:], k.ident, writes=[identf])
    P.dma("sync", wr[:], k.moe_router[l // 2].rearrange("(c p) e -> p c e", p=128), writes=[wr])
    P.dma("gpsimd", SLb[:], k.slmat, writes=[SLb])
    P.dma("sync", eoff[:], k.eoff, writes=[eoff])
    P.op("vector", lambda e: e.memset(onesb[:], 1.0), writes=[onesb])
    P.op("vector", lambda e: e.memset(base[:], 0.0), writes=[base])
    xt = [P.sb("xt%d" % i, [128, D], F32) for i in range(2)]
    hb = [P.sb("hb%d" % i, [128, D], BF16) for i in range(2)]
    hTf = [P.sb("hTf%d" % i, [128, KC, 128], F32) for i in range(2)]
    ptr = [P.ps("ptr%d" % i, [128, 4, 128]) for i in range(2)]
    plog = P.ps("plog", [128, 512])
    pcum = P.ps("pcum", [128, 512])
    sm = {}
    for nm in ("lg", "m8", "sel", "sel1", "sel2", "ex", "exs", "comb", "tmp8", "pos", "dest", "valid", "selb"):
        sm[nm] = [P.sb(nm + "%d" % i, [128, NE], BF16 if nm == "selb" else F32) for i in range(2)]
    c1 = {}
    for nm in ("nm1", "den", "rden"):
        c1[nm] = [P.sb(nm + "%d" % i, [128, 1], F32) for i in range(2)]
    wts = [P.sb("wts%d" % i, [128, 2], F32) for i in range(2)]
    dd = [P.sb("dd%d" % i, [128, 2], F32) for i in range(2)]
    idx = [P.sb("idx%d" % i, [128, 2], I32) for i in range(2)]
    tix = [P.sb("tix%d" % i, [128, 1], I32) for i in range(2)]
    for tt in range(TM // 128):
        i2 = tt % 2
        x_, hb_, hT_ = xt[i2], hb[i2], hTf[i2]
        tsl = slice(tt * 128, (tt + 1) * 128)
        P.dma("sync", tix[i2][:], k.tokidx[tsl, :], writes=[tix[i2]])
        P.gather(x_[:], k.x1, tix[i2][:, 0:1], reads=[tix[i2]], writes=[x_], bounds_check=T - 1, oob_is_err=False)
        P.op("vector", lambda e: e.tensor_tensor(out=x_[:], in0=x_[:], in1=screp[:], op=ALU.mult), reads=[x_, screp], writes=[x_])
        P.op("gpsimd", lambda e: e.tensor_tensor(out=x_[:], in0=x_[:], in1=shrep[:], op=ALU.add), reads=[x_, shrep], writes=[x_])
        P.op("scalar", lambda e: e.copy(out=hb_[:], in_=x_[:]), reads=[x_], writes=[hb_])
        for g in range(KC // 4):
            pt = ptr[g % 2]
            for q in range(4):
                c = g * 4 + q
                P.op("tensor", lambda e: e.transpose(out=pt[:, q, :], in_=x_[:, c * 128:(c + 1) * 128], identity=identf[:]),
                     reads=[x_, identf], writes=[pt], signal=(q == 3))
            if g % 2 == 0:
                P.op("scalar", lambda e: e.copy(out=hT_[:, g * 4:(g + 1) * 4, :], in_=pt[:]), reads=[pt], writes=[hT_])
            else:
                P.op("vector", lambda e: e.tensor_copy(out=hT_[:, g * 4:(g + 1) * 4, :], in_=pt[:]), reads=[pt], writes=[hT_])
        for c in range(KC):
            P.op("tensor", lambda e: e.matmul(plog[:, 0:NE], lhsT=hT_[:, c, :], rhs=wr[:, c, :], start=(c == 0), stop=(c == KC - 1)),
                 reads=[hT_, wr], writes=[plog], signal=(c == KC - 1))
        S = {n: v[i2] for n, v in sm.items()}
        C1 = {n: v[i2] for n, v in c1.items()}
        w_, d_, ix_ = wts[i2], dd[i2], idx[i2]
        V = "vector"
        P.op(V, lambda e: e.tensor_copy(out=S["lg"][:], in_=plog[:, 0:NE]), reads=[plog], writes=[S["lg"]])
        P.op(V, lambda e: e.max(out=S["m8"][:], in_=S["lg"][:]), reads=[S["lg"]], writes=[S["m8"]])
        P.op(V, lambda e: e.tensor_scalar(out=S["sel"][:], in0=S["lg"][:], scalar1=S["m8"][:, 1:2], scalar2=None, op0=ALU.is_ge),
             reads=[S["lg"], S["m8"]], writes=[S["sel"]])
        P.op(V, lambda e: e.tensor_scalar(out=S["sel1"][:], in0=S["lg"][:], scalar1=S["m8"][:, 0:1], scalar2=None, op0=ALU.is_ge),
             reads=[S["lg"], S["m8"]], writes=[S["sel1"]])
        P.op(V, lambda e: e.tensor_tensor(out=S["sel2"][:], in0=S["sel"][:], in1=S["sel1"][:], op=ALU.subtract),
             reads=[S["sel"], S["sel1"]], writes=[S["sel2"]])
        P.op(V, lambda e: e.tensor_scalar(out=C1["nm1"][:], in0=S["m8"][:, 0:1], scalar1=-1.0, scalar2=None, op0=ALU.mult),
             reads=[S["m8"]], writes=[C1["nm1"]])
        P.op("scalar", lambda e: e.activation(out=S["ex"][:], in_=S["lg"][:], func=AF.Exp, bias=C1["nm1"][:, 0:1]),
             reads=[S["lg"], C1["nm1"]], writes=[S["ex"]])
        P.op(V, lambda e: e.tensor_tensor(out=S["exs"][:], in0=S["ex"][:], in1=S["sel"][:], op=ALU.mult), reads=[S["ex"], S["sel"]], writes=[S["exs"]])
        P.op(V, lambda e: e.reduce_sum(out=C1["den"][:], in_=S["exs"][:], axis=AX.X), reads=[S["exs"]], writes=[C1["den"]])
        P.op(V, lambda e: e.reciprocal(out=C1["rden"][:], in_=C1["den"][:]), reads=[C1["den"]], writes=[C1["rden"]])
        P.op(V, lambda e: e.tensor_scalar(out=S["comb"][:], in0=S["exs"][:], scalar1=C1["rden"][:, 0:1], scalar2=None, op0=ALU.mult),
             reads=[S["exs"], C1["rden"]], writes=[S["comb"]])
        for j, sn in enumerate(("sel1", "sel2")):
            P.op(V, lambda e: e.tensor_tensor(out=S["tmp8"][:], in0=S["comb"][:], in1=S[sn][:], op=ALU.mult), reads=[S["comb"], S[sn]], writes=[S["tmp8"]])
            P.op(V, lambda e: e.reduce_sum(out=w_[:, j:j + 1], in_=S["tmp8"][:], axis=AX.X), reads=[S["tmp8"]], writes=[w_])
        P.op(V, lambda e: e.tensor_copy(out=S["selb"][:], in_=S["sel"][:]), reads=[S["sel"]], writes=[S["selb"]])
        P.op("tensor", lambda e: e.matmul(pcum[:, 0:NE], lhsT=SLb[:], rhs=S["selb"][:], start=True, stop=True), reads=[SLb, S["selb"]], writes=[pcum], signal=False)
        P.op("tensor", lambda e: e.matmul(pcum[:, NE:2 * NE], lhsT=onesb[:], rhs=S["selb"][:], start=True, stop=True), reads=[onesb, S["selb"]], writes=[pcum])
        P.op(V, lambda e: e.tensor_tensor(out=S["pos"][:], in0=pcum[:, 0:NE], in1=base[:], op=ALU.add), reads=[pcum, base], writes=[S["pos"]])
        P.op(V, lambda e: e.tensor_tensor(out=base[:], in0=pcum[:, NE:2 * NE], in1=base[:], op=ALU.add), reads=[pcum, base], writes=[base])
        P.op(V, lambda e: e.tensor_scalar(out=S["valid"][:], in0=S["pos"][:], scalar1=float(CAP), scalar2=None, op0=ALU.is_lt),
             reads=[S["pos"]], writes=[S["valid"]])
        P.op(V, lambda e: e.tensor_tensor(out=S["dest"][:], in0=S["pos"][:], in1=eoff[:], op=ALU.add), reads=[S["pos"], eoff], writes=[S["dest"]])
        P.op(V, lambda e: e.scalar_tensor_tensor(out=S["dest"][:], in0=S["dest"][:], scalar=-BIGIDX, in1=S["valid"][:], op0=ALU.add, op1=ALU.mult),
             reads=[S["dest"], S["valid"]], writes=[S["dest"]])
        P.op(V, lambda e: e.tensor_scalar(out=S["dest"][:], in0=S["dest"][:], scalar1=BIGIDX, scalar2=None, op0=ALU.add),
             reads=[S["dest"]], writes=[S["dest"]])
        for j, sn in enumerate(("sel1", "sel2")):
            P.op(V, lambda e: e.tensor_tensor(out=S["tmp8"][:], in0=S["dest"][:], in1=S[sn][:], op=ALU.mult), reads=[S["dest"], S[sn]], writes=[S["tmp8"]])
            P.op(V, lambda e: e.reduce_sum(out=d_[:, j:j + 1], in_=S["tmp8"][:], axis=AX.X), reads=[S["tmp8"]], writes=[d_])
        P.op(V, lambda e: e.tensor_copy(out=ix_[:], in_=d_[:]), reads=[d_], writes=[ix_])
        P.dma("sync", k.midx[tsl, :], ix_[:], reads=[ix_])
        P.dma("sync", k.mwts[tsl, :], w_[:], reads=[w_])
        for j in range(2):
            P.gather(k.Xs, hb_[:], ix_[:, j:j + 1], reads=[hb_, ix_], scatter=True, bounds_check=NSLOT - 1, oob_is_err=False)
    P.flush()
    P.release(m0)


def phase_moe_experts(k, l):
    i = l // 2
    for e_ in range(NE):
        ffn_gateup(k, k.Xs[e_ * CAP:(e_ + 1) * CAP, :], CAP, k.moe_w_gate[i, e_], k.moe_w_up[i, e_], D_FFE, k.AT[:, 0:CAP], mod=None)
        ffn_down(k, k.AT[:, 0:CAP], CAP, k.moe_w_down[i, e_], D_FFE, k.Ys, row_off=e_ * CAP)


def phase_moe_ln2(k, l, xsrc, xdst):
    P = k.P
    m0 = P.mark()
    gfrep = P.sb("gfrep", [128, D], F32)
    lng = P.sb("lng", [128, D], F32)
    lnb = P.sb("lnb", [128, D], F32)
    P.dma("sync", gfrep[:], k.modv[l, 5 * D:6 * D].partition_broadcast(128), writes=[gfrep])
    P.dma("sync", lng[:], k.ln_ffn_g[l].partition_broadcast(128), writes=[lng])
    P.dma("sync", lnb[:], k.ln_ffn_b[l].partition_broadcast(128), writes=[lnb])
    xt = [P.sb("xt%d" % i, [128, D], F32) for i in range(2)]
    y1 = [P.sb("y1%d" % i, [128, D], F32) for i in range(2)]
    y2 = [P.sb("y2%d" % i, [128, D], F32) for i in range(2)]
    rt = [P.sb("rt%d" % i, [128, D], F32) for i in range(2)]
    idx = [P.sb("idx%d" % i, [128, 2], I32) for i in range(2)]
    wts = [P.sb("wts%d" % i, [128, 2], F32) for i in range(2)]
    stats = P.sb("stats", [128, 4, 6], F32)
    mv = P.sb("mv", [128, 2], F32)
    rstd = P.sb("rstd", [128, 1], F32)
    tix = [P.sb("tix%d" % i, [128, 1], I32) for i in range(2)]
    for tt in range(TM // 128):
        i2 = tt % 2
        x_, r_, a_, b_, ix_, w_ = xt[i2], rt[i2], y1[i2], y2[i2], idx[i2], wts[i2]
        tsl = slice(tt * 128, (tt + 1) * 128)
        P.dma("sync", tix[i2][:], k.tokidx[tsl, :], writes=[tix[i2]])
        P.gather(x_[:], xsrc, tix[i2][:, 0:1], reads=[tix[i2]], writes=[x_], bounds_check=T - 1, oob_is_err=False)
        P.dma("sync", ix_[:], k.midx[tsl, :], writes=[ix_])
        P.dma("sync", w_[:], k.mwts[tsl, :], writes=[w_])
        P.op("gpsimd", lambda e: e.memset(a_[:], 0.0), writes=[a_])
        P.op("gpsimd", lambda e: e.memset(b_[:], 0.0), writes=[b_])
        P.gather(a_[:], k.Ys, ix_[:, 0:1], reads=[ix_], writes=[a_], bounds_check=NSLOT - 1, oob_is_err=False)
        P.gather(b_[:], k.Ys, ix_[:, 1:2], reads=[ix_], writes=[b_], bounds_check=NSLOT - 1, oob_is_err=False)
        P.op("vector", lambda e: e.tensor_scalar(out=r_[:], in0=a_[:], scalar1=w_[:, 0:1], scalar2=None, op0=ALU.mult), reads=[a_, w_], writes=[r_])
        P.op("vector", lambda e: e.scalar_tensor_tensor(out=r_[:], in0=b_[:], scalar=w_[:, 1:2], in1=r_[:], op0=ALU.mult, op1=ALU.add),
             reads=[b_, w_, r_], writes=[r_])
        P.op("gpsimd", lambda e: e.tensor_tensor(out=r_[:], in0=r_[:], in1=gfrep[:], op=ALU.mult), reads=[r_, gfrep], writes=[r_])
        P.op("vector", lambda e: e.scalar_tensor_tensor(out=r_[:], in0=x_[:], scalar=ALPHA, in1=r_[:], op0=ALU.mult, op1=ALU.add),
             reads=[x_, r_], writes=[r_])
        ln_tile(P, r_, lng, lnb, stats, mv, rstd)
        P.dma("sync", xdst[tsl, :], r_[:], reads=[r_])
    P.flush()
    P.release(m0)


_CACHE = {}


def make_in_maps(inputs, n_cores=8):
    hc = host_consts()
    maps = []
    for core in range(n_cores):
        b = (core // 2) % 4
        r = core % 2
        m = {
            "x": np.ascontiguousarray(inputs["x"][b]),
            "cT": np.ascontiguousarray(inputs["c"][b].reshape(KC, 128).T),
            "tokidx": (r * TM + np.arange(TM, dtype=np.int32)).reshape(TM, 1),
            "w_ada": inputs["w_ada"],
            "b_ada": inputs["b_ada"],
            "w_in": inputs["w_in"],
            "w_out": inputs["w_out"], "ln_mix_g": inputs["ln_mix_g"], "ln_mix_b": inputs["ln_mix_b"],
            "ln_ffn_g": inputs["ln_ffn_g"], "ln_ffn_b": inputs["ln_ffn_b"],
            "moe_router": inputs["moe_router"], "moe_w_gate": inputs["moe_w_gate"], "moe_w_up": inputs["moe_w_up"],
            "moe_w_down": inputs["moe_w_down"],
            "ffn_w_gate": inputs["ffn_w_gate"], "ffn_w_up": inputs["ffn_w_up"], "ffn_w_down": inputs["ffn_w_down"],
            "gla_w_a2": inputs["gla_w_a2"], "gla_b_a": inputs["gla_b_a"],
            "gla_nwT": np.ascontiguousarray(inputs["gla_norm_w"].reshape(DEPTH, 2, 128).transpose(0, 2, 1)),
            "cmp_w1_k": inputs["cmp_w1_k"], "cmp_w1_v": inputs["cmp_w1_v"],
            "cmp_w2_k": inputs["cmp_w2_k"], "cmp_w2_v": inputs["cmp_w2_v"],
            "cmp_peT_k": np.ascontiguousarray(inputs["cmp_pos_k"].transpose(0, 2, 1)),
            "cmp_peT_v": np.ascontiguousarray(inputs["cmp_pos_v"].transpose(0, 2, 1)),
        }
        m.update(hc)
        maps.append(m)
    return maps


N_CORES = 8


def kernel(**inputs):
    inputs = {k_: np.asarray(v) for k_, v in inputs.items()}
    nc = build()
    maps = make_in_maps(inputs, n_cores=N_CORES)
    res = run_bass_kernel_spmd(nc, maps, core_ids=list(range(N_CORES)))
    out = np.empty((4, T, D), np.float32)
    for core in range(N_CORES):
        b, r = core // 2, core % 2
        out[b, r * TM:(r + 1) * TM] = np.asarray(res.results[core]["out"])
    return out
```

```python
import numpy as np
import concourse.bass as bass
import concourse.mybir as mybir
from concourse.bass_utils import run_bass_kernel_spmd

F32 = mybir.dt.float32
BF16 = mybir.dt.bfloat16
I32 = mybir.dt.int32
U32 = mybir.dt.uint32
AF = mybir.ActivationFunctionType
ALU = mybir.AluOpType
AX = mybir.AxisListType

ENGS = ("tensor", "vector", "scalar", "gpsimd", "sync")

D = 2048
T = 4096
DEPTH = 2
KC = D // 128
HD = 128
SPLIT = (1024, 256, 256, 256, 256, 256, 256, 24, 512, 512, 1024, 16, 1024)
SEG = ("nq", "kc", "vc", "ks", "vs", "kw", "vw", "ng", "gq", "gk", "gv", "ga", "gg")
OFF = {}
_o = 0
for _n, _s in zip(SEG, SPLIT):
    OFF[_n] = (_o, _s)
    _o += _s
IN_W = _o
D_FF = 5632
NE = 8
D_FFE = 7168
ALPHA = (2 * DEPTH) ** 0.25
LN_EPS = 1e-5
NORM_EPS = 1e-6
NEG = -30000.0
SCALE = HD ** -0.5


class Sem:
    def __init__(self, h, kind, uid):
        self.h = h
        self.kind = kind
        self.count = 0
        self.key = "s%d" % uid


class Buf:
    def __init__(self, t, name):
        self.t = t
        self.name = name
        self.last_w = None
        self.readers = {}
        self.sem = {"sw": None, "hw": None}
        self.dlast = {"sw": 0, "hw": 0}
        self.dma_w = {"sw": 0, "hw": 0}
        self.psum = False
        self.fdeps = []

    def __getitem__(self, idx):
        return self.t[idx]

    def reset(self):
        self.last_w = None
        self.readers = {}
        self.fdeps = []
        self.dlast["hw"] = 0
        self.dma_w["hw"] = 0


class _Rec:
    def __init__(self):
        self.calls = []

    def __getattr__(self, name):
        def f(*a, **kw):
            self.calls.append((name, a, kw))
            return self
        return f


class Prog:
    def __init__(self, nc, same_engine_raw=True):
        self.nc = nc
        self.same_engine_raw = same_engine_raw
        self.psem = {}
        self.cnt = {e: 0 for e in ENGS}
        self.lists = {e: [] for e in ENGS}
        self.seen = {e: {} for e in ENGS}
        self.bufs = []
        self._cms = []
        self._semcms = []
        self.pool = {"sw": [], "hw": []}
        self.allsems = []
        for e in ENGS:
            cm = nc.semaphore("p_" + e)
            self.psem[e] = cm.__enter__()
            self._semcms.append(cm)
        self.n_inst = 0
        self.uid = 0
        self._bcreg = {}
        self._bcset = set()

    def mark(self):
        return len(self._cms)

    def release(self, mark):
        while len(self._cms) > mark:
            cm, b = self._cms.pop()
            cm.__exit__(None, None, None)
            if b is not None:
                self._drop(b)

    def _drop(self, b):
        for kind in ("sw", "hw"):
            if b.sem[kind] is not None:
                self.pool[kind].append(b.sem[kind])
                b.sem[kind] = None
        if b in self.bufs:
            self.bufs.remove(b)
        for v in getattr(b, "views", []):
            self._drop(v)

    def sb(self, name, shape, dt):
        self.uid += 1
        cm = self.nc.sbuf_tensor("%s_%d" % (name, self.uid), list(shape), dt)
        t = cm.__enter__()
        b = Buf(t, "%s_%d" % (name, self.uid))
        b.views = []
        self._cms.append((cm, b))
        self.bufs.append(b)
        return b

    def ps(self, name, shape, dt=F32):
        self.uid += 1
        cm = self.nc.psum_tensor("%s_%d" % (name, self.uid), list(shape), dt)
        t = cm.__enter__()
        b = Buf(t, "%s_%d" % (name, self.uid))
        b.psum = True
        b.views = []
        self._cms.append((cm, b))
        self.bufs.append(b)
        return b

    def view(self, buf, name=None):
        self.uid += 1
        b = Buf(buf.t, "%s_v%d" % (buf.name, self.uid))
        buf.views.append(b)
        self.bufs.append(b)
        return b

    def _getsem(self, b, kind):
        if b.sem[kind] is None:
            if self.pool[kind]:
                b.sem[kind] = self.pool[kind].pop()
            else:
                self.uid += 1
                cm = self.nc.semaphore("d%s_%d" % (kind, self.uid))
                h = cm.__enter__()
                self._semcms.append(cm)
                sm = Sem(h, kind, self.uid)
                self.allsems.append(sm)
                b.sem[kind] = sm
        return b.sem[kind]

    def _waits(self, e, reads, writes):
        w = {}

        def need(sem, val, key):
            if val <= 0:
                return
            if self.seen[e].get(key, 0) >= val:
                return
            if key not in w or w[key][1] < val:
                w[key] = (sem, val)

        for r in reads:
            if r.last_w is not None:
                we, n = r.last_w
                if we != e or self.same_engine_raw:
                    need(self.psem[we], n, "p_" + we)
            for kind in ("sw", "hw"):
                if r.dma_w[kind] > 0:
                    need(r.sem[kind].h, 16 * r.dma_w[kind], r.sem[kind].key)
            if r.psum:
                for re_, n in r.readers.items():
                    if re_ != e:
                        need(self.psem[re_], n, "p_" + re_)
        for b in writes:
            if b.last_w is not None:
                we, n = b.last_w
                if we != e:
                    need(self.psem[we], n, "p_" + we)
            for re_, n in b.readers.items():
                if re_ != e:
                    need(self.psem[re_], n, "p_" + re_)
            for kind in ("sw", "hw"):
                if b.dlast[kind] > 0:
                    need(b.sem[kind].h, 16 * b.dlast[kind], b.sem[kind].key)
            for (fs, fv, fk) in b.fdeps:
                need(fs, fv, fk)
        for key, (sem, val) in w.items():
            self.seen[e][key] = val
        return list(w.values())

    def op(self, e, fn, reads=(), writes=(), signal=True):
        waits = self._waits(e, reads, writes)
        n = self.cnt[e] + 1
        if signal:
            self.cnt[e] = n
        psem = self.psem[e]
        rec = _Rec()
        fn(rec)
        assert len(rec.calls) == 1, rec.calls
        name, a, kw = rec.calls[0]

        def thunk(engine, waits=waits, name=name, a=a, kw=kw, signal=signal, psem=psem):
            for sem, val in waits:
                engine.wait_ge(sem, val)
            ins = getattr(engine, name)(*a, **kw)
            if signal:
                ins.then_inc(psem, 1)

        self.lists[e].append(thunk)
        self.n_inst += 1
        for r in reads:
            r.readers[e] = n
        for b in writes:
            b.last_w = (e, n)
            b.readers = {}
            b.dma_w = {"sw": 0, "hw": 0}
            b.fdeps = []

    def _dma_book(self, q, reads, writes):
        kind = "sw" if q == "gpsimd" else "hw"
        waits = self._waits(q, reads, writes)
        allb = list(writes) + list(reads)
        prim = allb[0]
        sm = self._getsem(prim, kind)
        sm.count += 1
        prim.dlast[kind] = sm.count
        for b in allb[1:]:
            assert b not in writes
            b.fdeps.append((sm.h, 16 * sm.count, sm.key))
        for b in writes:
            b.dma_w[kind] = sm.count
            b.last_w = None
            b.readers = {}
            b.fdeps = []
        return waits, sm.h

    def dma(self, q, out_ap, in_ap, reads=(), writes=(), **kw):
        waits, semh = self._dma_book(q, reads, writes)

        def thunk(engine, waits=waits, semh=semh, out_ap=out_ap, in_ap=in_ap, kw=kw):
            for sem, val in waits:
                engine.wait_ge(sem, val)
            engine.dma_start(out=out_ap, in_=in_ap, **kw).then_inc(semh, 16)

        self.lists[q].append(thunk)
        self.n_inst += 1

    def gather(self, out_ap, in_ap, idx_ap, reads=(), writes=(), scatter=False, **kw):
        q = "gpsimd"
        waits, semh = self._dma_book(q, reads, writes)
        bc = kw.pop("bounds_check", None)

        def thunk(engine, waits=waits, semh=semh, kw=kw, bc=bc):
            for sem, val in waits:
                engine.wait_ge(sem, val)
            if bc is not None:
                if bc not in self._bcreg:
                    self._bcreg[bc] = engine.alloc_register("bcreg_%d" % int(bc))
                if bc not in self._bcset:
                    engine.reg_mov(self._bcreg[bc], bc)
                    self._bcset.add(bc)
                kw = dict(kw)
                kw["bounds_check"] = self._bcreg[bc]
            if scatter:
                ins = engine.indirect_dma_start(out=out_ap, out_offset=bass.IndirectOffsetOnAxis(ap=idx_ap, axis=0),
                                                in_=in_ap, in_offset=None, **kw)
            else:
                ins = engine.indirect_dma_start(out=out_ap, out_offset=None, in_=in_ap,
                                                in_offset=bass.IndirectOffsetOnAxis(ap=idx_ap, axis=0), **kw)
            ins.then_inc(semh, 16)

        self.lists[q].append(thunk)
        self.n_inst += 1

    def flush(self, final=False):
        nc = self.nc
        drain = []
        for sm in self.allsems:
            if sm.count > 0:
                drain.append((sm.h, 16 * sm.count))
        for e in ENGS:
            if e != "sync" and self.cnt[e] > 0:
                drain.append((self.psem[e], self.cnt[e]))

        def dthunk(engine, drain=drain):
            for sem, val in drain:
                engine.wait_ge(sem, val)

        self.lists["sync"].append(dthunk)
        lists = self.lists
        with nc.Block() as block:
            @block.tensor
            def _(eng):
                for th in lists["tensor"]:
                    th(eng)

            @block.vector
            def _(eng):
                for th in lists["vector"]:
                    th(eng)

            @block.scalar
            def _(eng):
                for th in lists["scalar"]:
                    th(eng)

            @block.gpsimd
            def _(eng):
                for th in lists["gpsimd"]:
                    th(eng)

            @block.sync
            def _(eng):
                for th in lists["sync"]:
                    th(eng)
        if not final:
            nc.all_engine_barrier()
            sems = [self.psem[e] for e in ENGS] + [sm.h for sm in self.allsems if sm.kind == "hw" and sm.count > 0]
            with nc.Block() as block:
                @block.sync
                def _(eng):
                    for s_ in sems:
                        eng.sem_clear(s_)
            nc.all_engine_barrier()
        for sm in self.allsems:
            if sm.kind == "hw":
                sm.count = 0
        self.lists = {e: [] for e in ENGS}
        self.cnt = {e: 0 for e in ENGS}
        self.seen = {e: {} for e in ENGS}
        self._bcset = set()
        for b in self.bufs:
            b.reset()

    def close(self):
        self.release(0)
        for cm in reversed(self._semcms):
            cm.__exit__(None, None, None)
        self._semcms = []


def host_consts():
    c = {}
    c["ident"] = np.eye(128, dtype=np.float32)
    half = 16
    inv = (500000.0 ** (-np.arange(half, dtype=np.float32) * 2.0 / 32)).astype(np.float32)
    ang = np.arange(T, dtype=np.float32)[None, :] * inv[:, None]
    cos = np.cos(ang).astype(np.float32)
    sin = np.sin(ang).astype(np.float32)
    c["cos"] = np.concatenate([cos, cos], 0)
    c["sin"] = np.concatenate([sin, sin], 0)
    pm = np.zeros((128, 128), np.float32)
    for i in range(16):
        pm[i + 16, i] = -1.0
        pm[i, i + 16] = 1.0
    c["pm"] = pm
    kk = np.arange(128)[:, None]
    tt = np.arange(128)[None, :]
    c["cb"] = np.where(kk > tt, NEG, 0.0).astype(np.float32)
    c["wbm"] = np.where(kk <= tt, NEG, 0.0).astype(np.float32)
    es = np.zeros((64, 32, 128), np.float32)
    for kt in range(32):
        for m in range(128):
            es[2 * kt + m // 64, kt, m] = 1.0
    c["esel"] = es.reshape(64, 32 * 128)
    cst = np.arange(256) * 16
    bst = np.arange(64) * 64
    ov = ((cst[:, None] < bst[None, :] + 64) & (cst[:, None] + 32 > bst[None, :])).astype(np.float32)
    ov[255] = 0.0
    c["ovl"] = np.ascontiguousarray(ov.reshape(2, 128, 64).transpose(1, 0, 2)).reshape(128, 128)
    c["mb"] = (16.0 * kk - tt).astype(np.float32)
    keep = np.zeros((128, 32, 64), np.float32)
    add = np.zeros((128, 32, 64), np.float32)
    n = np.arange(64)[None, :]
    for qt in range(32):
        t = qt * 128 + np.arange(128)[:, None]
        cur = t // 64
        forced = (n == 0) | (n == cur) | (n == cur - 1)
        future = (n * 64) > t
        keep[:, qt, :] = np.where(forced | future, 0.0, 1.0)
        add[:, qt, :] = np.where(future, -1e30, np.where(forced, 1e9, 0.0))
    c["keep"] = keep.reshape(128, 32 * 64)
    c["addc"] = add.reshape(128, 32 * 64)
    sel = np.zeros((12, 12, 128), np.float32)
    for r in range(12):
        sel[r, r, :] = 1.0
    c["sel"] = sel.reshape(12, 12 * 128)
    same = (kk // 64) == (tt // 64)
    c["slmat"] = (kk < tt).astype(np.float32)
    c["eoff"] = np.tile((np.arange(NE) * 768.0)[None, :], (128, 1)).astype(np.float32)
    c["tri2"] = (same & (kk <= tt)).astype(np.float32)
    c["sumat"] = (same & (kk > tt)).astype(np.float32)
    return c


class K:
    pass


def build(debug=(), phases=None, layers=(0, 1), l1_from_x=False):
    nc = bass.Bass("TRN2", target_bir_lowering=False)
    k = K()
    k.nc = nc
    dbg = set(debug)

    def din(name, shape, dt=F32):
        return nc.dram_tensor(name, list(shape), dt, kind="ExternalInput").ap()

    def dscr(name, shape, dt):
        kind = "ExternalOutput" if name in dbg else "Internal"
        return nc.dram_tensor(name, list(shape), dt, kind=kind).ap()

    k.x = din("x", [T, D])
    k.cT = din("cT", [128, KC])
    k.w_ada = din("w_ada", [DEPTH, D, 6 * D])
    k.b_ada = din("b_ada", [DEPTH, 6 * D])
    k.w_in = din("w_in", [DEPTH, D, IN_W])
    k.ident = din("ident", [128, 128])
    k.cos = din("cos", [32, T])
    k.sin = din("sin", [32, T])
    k.pm = din("pm", [128, 128])
    k.cb = din("cb", [128, 128])
    k.wbm = din("wbm", [128, 128])
    k.esel = din("esel", [64, 32 * 128])
    k.ovl = din("ovl", [128, 128])
    k.mb = din("mb", [128, 128])
    k.keep = din("keep", [128, 32 * 64])
    k.addc = din("addc", [128, 32 * 64])
    k.sel = din("sel", [12, 12 * 128])
    k.w_out = din("w_out", [DEPTH, D, D])
    k.ln_mix_g = din("ln_mix_g", [DEPTH, D])
    k.ln_mix_b = din("ln_mix_b", [DEPTH, D])
    k.ln_ffn_g = din("ln_ffn_g", [DEPTH, D])
    k.ln_ffn_b = din("ln_ffn_b", [DEPTH, D])
    k.ffn_w_gate = din("ffn_w_gate", [1, D, D_FF])
    k.ffn_w_up = din("ffn_w_up", [1, D, D_FF])
    k.ffn_w_down = din("ffn_w_down", [1, D_FF, D])
    k.moe_router = din("moe_router", [1, D, NE])
    k.moe_w_gate = din("moe_w_gate", [1, NE, D, D_FFE])
    k.moe_w_up = din("moe_w_up", [1, NE, D, D_FFE])
    k.moe_w_down = din("moe_w_down", [1, NE, D_FFE, D])
    k.slmat = din("slmat", [128, 128])
    k.eoff = din("eoff", [128, NE])
    k.tri2 = din("tri2", [128, 128])
    k.sumat = din("sumat", [128, 128])
    k.gla_w_a2 = din("gla_w_a2", [DEPTH, 16, 512])
    k.gla_b_a = din("gla_b_a", [DEPTH, 512])
    k.gla_nwT = din("gla_nwT", [DEPTH, 128, 2])
    k.cmp_w1 = {"k": din("cmp_w1_k", [DEPTH, 4096, 256]), "v": din("cmp_w1_v", [DEPTH, 4096, 256])}
    k.cmp_w2 = {"k": din("cmp_w2_k", [DEPTH, 256, 128]), "v": din("cmp_w2_v", [DEPTH, 256, 128])}
    k.cmp_peT = {"k": din("cmp_peT_k", [DEPTH, 128, 32]), "v": din("cmp_peT_v", [DEPTH, 128, 32])}
    k.tokidx = din("tokidx", [2048, 1], I32)
    k.out = nc.dram_tensor("out", [2048, D], F32, kind="ExternalOutput").ap()

    k.modv = dscr("modv", [DEPTH, 6 * D], F32)
    k.qn = dscr("qn", [1024, T], BF16)
    k.qr = dscr("qr", [1024, T], BF16)
    k.kcT = dscr("kcT", [256, T], BF16)
    k.vcT = dscr("vcT", [256, T], BF16)
    k.ksT = dscr("ksT", [256, T], BF16)
    k.kwT = dscr("kwT", [256, T], BF16)
    k.ngT = dscr("ngT", [24, T], BF16)
    k.gqT = dscr("gqT", [512, T], BF16)
    k.gkT = dscr("gkT", [512, T], BF16)
    k.gaT = dscr("gaT", [16, T], BF16)
    k.ggT = dscr("ggT", [1024, T], BF16)
    k.vs = dscr("vs", [T, 256], BF16)
    k.vw = dscr("vw", [T, 256], BF16)
    k.gk = dscr("gk", [T, 512], BF16)
    k.gv = dscr("gv", [T, 1024], BF16)
    k.kcmpT = dscr("kcmpT", [2, 128, 256], BF16)
    k.vcmp = dscr("vcmp", [2, 256, 128], BF16)
    k.mixT = dscr("mixT", [D, T], BF16)
    k.x1 = dscr("x1", [T, D], F32)
    k.xa = dscr("xa", [T, D], F32)
    k.yffn = dscr("yffn", [T, D], F32)
    k.AT = dscr("AT", [D_FFE, T], BF16)
    k.Xs = dscr("Xs", [NE * 768, D], BF16)
    k.Ys = dscr("Ys", [NE * 768, D], F32)
    k.midx = dscr("midx", [2048, 2], I32)
    k.mwts = dscr("mwts", [2048, 2], F32)

    P = Prog(nc)
    k.P = P
    ph = phases

    if ph is None or "mod" in ph:
        phase_mod(k)
    for l in layers:
        xsrc = k.x if (l == 0 or l1_from_x) else k.xa
        if ph is None or "proj" in ph:
            phase_proj(k, l, xsrc)
        if ph is None or "cmp" in ph:
            phase_cmp(k, l)
        if ph is None or "nsa" in ph:
            phase_nsa(k, l)
        if ph is None or "gla" in ph:
            phase_gla(k, l)
        if ph is None or "wout" in ph:
            phase_wout(k, l, xsrc, k.x1)
        xdst = k.out if l == DEPTH - 1 else k.xa
        if l % 2 == 0:
            if ph is None or "ffn" in ph:
                ffn_gateup(k, k.x1, T, k.ffn_w_gate[l // 2], k.ffn_w_up[l // 2], D_FF, k.AT[0:D_FF, :], mod=l)
                ffn_down(k, k.AT[0:D_FF, :], T, k.ffn_w_down[l // 2], D_FF, k.yffn)
            if ph is None or "ln2" in ph:
                phase_ln2(k, l, k.x1, k.yffn, xdst)
        else:
            if ph is None or "route" in ph:
                phase_moe_route(k, l)
            if ph is None or "experts" in ph:
                phase_moe_experts(k, l)
            if ph is None or "ln2" in ph:
                phase_moe_ln2(k, l, k.x1, xdst)

    P.flush(final=True)
    P.close()
    return nc


def phase_mod(k):
    P = k.P
    m0 = P.mark()
    cc = P.sb("cc", [128, KC], F32)
    cs = P.sb("cs", [128, KC], F32)
    condB = P.sb("condB", [128, KC, 128], F32)
    wb = [P.sb("wada%d" % i, [128, KC, 512], F32) for i in range(2)]
    bb = [P.sb("bada%d" % i, [128, 512], F32) for i in range(2)]
    mo = [P.sb("mo%d" % i, [128, 512], F32) for i in range(2)]
    pm = [P.ps("pmod%d" % i, [128, 512]) for i in range(2)]
    P.dma("sync", cc[:], k.cT, writes=[cc])
    P.op("scalar", lambda e: e.activation(out=cs[:], in_=cc[:], func=AF.Silu), reads=[cc], writes=[cs])
    P.op("vector", lambda e: e.tensor_copy(out=condB[:], in_=cs[:].unsqueeze(2).to_broadcast([128, KC, 128])),
         reads=[cs], writes=[condB])
    it = 0
    import os
    NBL = int(os.environ.get("NBL", "24"))
    VAR = os.environ.get("VAR", "")
    for l in range(DEPTH):
        wv = k.w_ada[l].rearrange("(c p) n -> p c n", p=128)
        for nb in range(NBL):
            i = it % 2
            it += 1
            n0 = nb * 512
            P.dma("sync", wb[i][:], wv[:, :, n0:n0 + 512], writes=[wb[i]])
            if VAR == "nobb":
                P.op("vector", lambda e, i=i: e.memset(bb[i][:], 0.0), writes=[bb[i]])
            else:
                P.dma("gpsimd", bb[i][:], k.b_ada[l, n0:n0 + 512].partition_broadcast(128), writes=[bb[i]])
            for c in range(KC):
                P.op("tensor", lambda e, i=i, c=c: e.matmul(pm[i][:], lhsT=condB[:, c, :], rhs=wb[i][:, c, :],
                                                             start=(c == 0), stop=(c == KC - 1)),
                     reads=[condB, wb[i]], writes=[pm[i]], signal=(c == KC - 1))
            seg = nb // 4
            add1 = 1.0 if seg in (1, 2, 4, 5) else 0.0
            P.op("vector", lambda e, i=i, add1=add1: e.scalar_tensor_tensor(
                out=mo[i][:], in0=pm[i][:], scalar=add1, in1=bb[i][:], op0=ALU.add, op1=ALU.add),
                reads=[pm[i], bb[i]], writes=[mo[i]])
            P.dma("gpsimd", k.modv[l:l + 1, n0:n0 + 512], mo[i][0:1, :], reads=[mo[i]])
    P.flush()
    P.release(m0)


def build_hT(k, xsrc, t0, ntiles, screp, shrep, identf, hT, hviews, xt, ptr):
    P = k.P
    for j in range(ntiles):
        xb = xt[j % 2]
        P.dma("sync", xb[:], xsrc[t0 + j * 128:t0 + (j + 1) * 128, :], writes=[xb])
        P.op("vector", lambda e, xb=xb: e.tensor_tensor(out=xb[:], in0=xb[:], in1=screp[:], op=ALU.mult),
             reads=[xb, screp], writes=[xb])
        P.op("gpsimd", lambda e, xb=xb: e.tensor_tensor(out=xb[:], in0=xb[:], in1=shrep[:], op=ALU.add),
             reads=[xb, shrep], writes=[xb])
        for g in range(KC // 4):
            pt = ptr[(j * 4 + g) % 2]
            for q in range(4):
                c = g * 4 + q
                P.op("tensor", lambda e, xb=xb, pt=pt, q=q, c=c: e.transpose(
                    out=pt[:, q, :], in_=xb[:, c * 128:(c + 1) * 128], identity=identf[:]),
                    reads=[xb, identf], writes=[pt], signal=(q == 3))
            eng = "scalar" if (g % 2 == 0) else "vector"
            if eng == "scalar":
                P.op("scalar", lambda e, pt=pt, g=g, j=j: e.copy(out=hT[:, g * 4:(g + 1) * 4, j * 128:(j + 1) * 128], in_=pt[:]),
                     reads=[pt], writes=[hviews[j][0]])
            else:
                P.op("vector", lambda e, pt=pt, g=g, j=j: e.tensor_copy(out=hT[:, g * 4:(g + 1) * 4, j * 128:(j + 1) * 128], in_=pt[:]),
                     reads=[pt], writes=[hviews[j][1]])


def phase_proj(k, l, xsrc):
    P = k.P
    TH = 2048
    for th in range(T // TH):
        t0 = th * TH
        m0 = P.mark()
        screp = P.sb("screp", [128, D], F32)
        shrep = P.sb("shrep", [128, D], F32)
        identf = P.sb("identf", [128, 128], F32)
        hT = P.sb("hT", [128, KC, TH], BF16)
        hviews = [(P.view(hT), P.view(hT)) for _ in range(TH // 128)]
        xt = [P.sb("xt%d" % i, [128, D], F32) for i in range(2)]
        ptr = [P.ps("ptr%d" % i, [128, 4, 128]) for i in range(2)]
        cosb = P.sb("cosb", [32, TH], F32)
        sinb = P.sb("sinb", [32, TH], F32)
        pmb = P.sb("pmb", [128, 128], BF16)
        wbk = [P.sb("wblk%d" % i, [128, KC, 512], BF16) for i in range(2)]
        pacc = [P.ps("pacc%d" % i, [128, 512]) for i in range(3)]
        prot = [P.ps("prot%d" % i, [128, 512]) for i in range(2)]
        ost = [P.sb("ost%d" % i, [128, 512], BF16) for i in range(4)]
        ost2 = [P.sb("ost2%d" % i, [128, 512], BF16) for i in range(2)]
        t1 = [P.sb("t1%d" % i, [32, 512], F32) for i in range(2)]
        t2 = [P.sb("t2%d" % i, [32, 512], F32) for i in range(2)]

        P.dma("sync", screp[:], k.modv[l, D:2 * D].partition_broadcast(128), writes=[screp])
        P.dma("sync", shrep[:], k.modv[l, 0:D].partition_broadcast(128), writes=[shrep])
        P.dma("sync", identf[:], k.ident, writes=[identf])
        P.dma("sync", cosb[:], k.cos[:, t0:t0 + TH], writes=[cosb])
        P.dma("sync", sinb[:], k.sin[:, t0:t0 + TH], writes=[sinb])
        P.dma("gpsimd", pmb[:], k.pm, writes=[pmb])
        build_hT(k, xsrc, t0, TH // 128, screp, shrep, identf, hT, hviews, xt, ptr)

        wv = k.w_in[l].rearrange("(c p) n -> p c n", p=128)
        cnt = {"w": 0, "acc": 0, "ost": 0, "ost2": 0, "rot": 0}

        def hreads(tb):
            r = []
            for j in range(tb * 4, tb * 4 + 4):
                r += [hviews[j][0], hviews[j][1]]
            return r

        def load_w(c0, ncols):
            wb = wbk[cnt["w"] % 2]
            cnt["w"] += 1
            P.dma("gpsimd", wb[:, :, 0:ncols], wv[:, :, c0:c0 + ncols], writes=[wb])
            return wb

        def ftype(seg, dst, mode):
            c0, n = OFF[seg]
            for b0 in range(0, n, 512):
                nb = min(512, n - b0)
                wb = load_w(c0 + b0, nb)
                for ct in range(0, nb, 128):
                    m = min(128, nb - ct)
                    row0 = b0 + ct
                    for tb in range(TH // 512):
                        pa = pacc[cnt["acc"] % 3]
                        cnt["acc"] += 1
                        for c in range(KC):
                            P.op("tensor", lambda e, pa=pa, wb=wb, ct=ct, m=m, c=c, tb=tb: e.matmul(
                                pa[0:m, :], lhsT=wb[:, c, ct:ct + m], rhs=hT[:, c, tb * 512:(tb + 1) * 512],
                                start=(c == 0), stop=(c == KC - 1)),
                                reads=[wb] + hreads(tb), writes=[pa], signal=(c == KC - 1))
                        ob = ost[cnt["ost"] % 4]
                        cnt["ost"] += 1
                        tok = slice(t0 + tb * 512, t0 + (tb + 1) * 512)
                        if mode == "plain":
                            P.op("scalar", lambda e, ob=ob, pa=pa, m=m: e.copy(out=ob[0:m, :], in_=pa[0:m, :]),
                                 reads=[pa], writes=[ob])
                            P.dma("sync", dst[row0:row0 + m, tok], ob[0:m, :], reads=[ob])
                        elif mode == "sigmoid":
                            P.op("scalar", lambda e, ob=ob, pa=pa, m=m: e.activation(out=ob[0:m, :], in_=pa[0:m, :], func=AF.Sigmoid),
                                 reads=[pa], writes=[ob])
                            P.dma("sync", dst[row0:row0 + m, tok], ob[0:m, :], reads=[ob])
                        elif mode == "silu":
                            P.op("scalar", lambda e, ob=ob, pa=pa, m=m: e.activation(out=ob[0:m, :], in_=pa[0:m, :], func=AF.Silu),
                                 reads=[pa], writes=[ob])
                            P.dma("sync", dst[row0:row0 + m, tok], ob[0:m, :], reads=[ob])
                        else:
                            dn, dr = mode[1], mode[2]
                            P.op("scalar", lambda e, ob=ob, pa=pa: e.copy(out=ob[:], in_=pa[:]), reads=[pa], writes=[ob])
                            pr = prot[cnt["rot"] % 2]
                            a1 = t1[cnt["rot"] % 2]
                            a2 = t2[cnt["rot"] % 2]
                            cnt["rot"] += 1
                            P.op("tensor", lambda e, pr=pr, ob=ob: e.matmul(pr[:], lhsT=pmb[:], rhs=ob[:], start=True, stop=True),
                                 reads=[pmb, ob], writes=[pr])
                            ltok = slice(tb * 512, (tb + 1) * 512)
                            P.op("vector", lambda e, a1=a1, pa=pa, ltok=ltok: e.tensor_tensor(out=a1[:], in0=pa[0:32, :], in1=cosb[:, ltok], op=ALU.mult),
                                 reads=[pa, cosb], writes=[a1])
                            P.op("vector", lambda e, a2=a2, pr=pr, ltok=ltok: e.tensor_tensor(out=a2[:], in0=pr[0:32, :], in1=sinb[:, ltok], op=ALU.mult),
                                 reads=[pr, sinb], writes=[a2])
                            if dn is not None:
                                P.dma("sync", dn[row0:row0 + 128, tok], ob[:], reads=[ob])
                                o2 = ost2[cnt["ost2"] % 2]
                                cnt["ost2"] += 1
                                P.op("scalar", lambda e, o2=o2, pa=pa: e.copy(out=o2[:], in_=pa[:]), reads=[pa], writes=[o2])
                                P.op("vector", lambda e, o2=o2, a1=a1, a2=a2: e.tensor_tensor(out=o2[0:32, :], in0=a1[:], in1=a2[:], op=ALU.add),
                                     reads=[a1, a2], writes=[o2])
                                P.dma("sync", dr[row0:row0 + 128, tok], o2[:], reads=[o2])
                            else:
                                P.op("vector", lambda e, ob=ob, a1=a1, a2=a2: e.tensor_tensor(out=ob[0:32, :], in0=a1[:], in1=a2[:], op=ALU.add),
                                     reads=[a1, a2, ob], writes=[ob])
                                P.dma("sync", dr[row0:row0 + 128, tok], ob[:], reads=[ob])

        def ttype(seg, dst):
            c0, n = OFF[seg]
            for b0 in range(0, n, 512):
                nb = min(512, n - b0)
                wb = load_w(c0 + b0, nb)
                for j in range(TH // 128):
                    pa = pacc[cnt["acc"] % 3]
                    cnt["acc"] += 1
                    for c in range(KC):
                        P.op("tensor", lambda e, pa=pa, wb=wb, nb=nb, c=c, j=j: e.matmul(
                            pa[:, 0:nb], lhsT=hT[:, c, j * 128:(j + 1) * 128], rhs=wb[:, c, 0:nb],
                            start=(c == 0), stop=(c == KC - 1)),
                            reads=[wb, hviews[j][0], hviews[j][1]], writes=[pa], signal=(c == KC - 1))
                    ob = ost[cnt["ost"] % 4]
                    cnt["ost"] += 1
                    P.op("scalar", lambda e, ob=ob, pa=pa, nb=nb: e.copy(out=ob[:, 0:nb], in_=pa[:, 0:nb]), reads=[pa], writes=[ob])
                    P.dma("sync", dst[t0 + j * 128:t0 + (j + 1) * 128, b0:b0 + nb], ob[:, 0:nb], reads=[ob])

        import os
        SEGS = os.environ.get("SEGS", "")
        plan = [("nq", "f", None, ("rope", k.qn, k.qr)), ("kc", "f", k.kcT, "plain"), ("vc", "f", k.vcT, "plain"),
                ("ks", "f", None, ("rope", None, k.ksT)), ("vs", "t", k.vs, None), ("kw", "f", None, ("rope", None, k.kwT)),
                ("vw", "t", k.vw, None), ("ng", "f", k.ngT, "sigmoid"), ("gq", "f", k.gqT, "plain"), ("gk", "f", k.gkT, "plain"),
                ("gk", "t", k.gk, None), ("gv", "t", k.gv, None), ("ga", "f", k.gaT, "plain"), ("gg", "f", k.ggT, "silu")]
        for (sg, ty, dst, mode) in plan:
            if SEGS and (sg + ty) not in SEGS.split(","):
                continue
            if ty == "f":
                ftype(sg, dst, mode)
            else:
                ttype(sg, dst)
        P.flush()
        P.release(m0)


def phase_cmp(k, l):
    P = k.P
    m0 = P.mark()
    aT = [P.sb("aT%d" % i, [128, T], BF16) for i in range(2)]
    w1b = [P.sb("w1b%d" % i, [128, 32, 256], BF16) for i in range(2)]
    w2b = [P.sb("w2b%d" % i, [128, 2, 128], BF16) for i in range(2)]
    peT = [P.sb("peT%d" % i, [128, 32], BF16) for i in range(2)]
    ph = [P.ps("ph%d" % i, [128, 512]) for i in range(2)]
    pc = P.ps("pc", [128, 512])
    po = P.ps("pcmpo", [128, 512])
    cst = P.sb("cst", [128, 2], F32)
    xh = [P.sb("xh%d" % i, [128, 256], F32) for i in range(2)]
    uu = [P.sb("uu%d" % i, [128, 256], F32) for i in range(2)]
    sg = [P.sb("sgm%d" % i, [128, 256], F32) for i in range(2)]
    gb = [P.sb("gb%d" % i, [128, 256], BF16) for i in range(2)]
    ocp = [P.sb("ocp%d" % i, [128, 256], BF16) for i in range(2)]
    for i in range(2):
        P.op("vector", lambda e, i=i: e.memset(gb[i][:], 0.0), writes=[gb[i]])
        P.op("vector", lambda e, i=i: e.memset(ocp[i][:], 0.0), writes=[ocp[i]])
    it = 0
    for si, src in enumerate(("k", "v")):
        w1 = k.cmp_w1[src][l].rearrange("(l d) m -> d l m", d=128)
        for q4 in range(4):
            P.dma("gpsimd", w1b[si][:, q4 * 8:(q4 + 1) * 8, :], w1[:, q4 * 8:(q4 + 1) * 8, :], writes=[w1b[si]])
        P.dma("gpsimd", w2b[si][:], k.cmp_w2[src][l].rearrange("(c p) n -> p c n", p=128), writes=[w2b[si]])
        P.dma("gpsimd", peT[si][:], k.cmp_peT[src][l], writes=[peT[si]])
        srcT = k.kcT if src == "k" else k.vcT
        for hk in range(2):
            a = aT[it % 2]
            oc = ocp[it % 2]
            it += 1
            P.dma("sync", a[:], srcT[hk * 128:(hk + 1) * 128, :], writes=[a])
            for mc in range(2):
                for ll in range(32):
                    P.op("tensor", lambda e, a=a, mc=mc, ll=ll, si=si: e.matmul(
                        ph[mc][:, 0:255], lhsT=w1b[si][:, ll, mc * 128:(mc + 1) * 128], rhs=a[:, ll:ll + 4065:16],
                        start=(ll == 0), stop=(ll == 31)), reads=[w1b[si], a], writes=[ph[mc]], signal=(ll == 31))
                for ll in range(32):
                    P.op("tensor", lambda e, mc=mc, ll=ll, si=si: e.matmul(
                        pc[:, mc:mc + 1], lhsT=w1b[si][:, ll, mc * 128:(mc + 1) * 128], rhs=peT[si][:, ll:ll + 1],
                        start=(ll == 0), stop=(ll == 31)), reads=[w1b[si], peT[si]], writes=[pc], signal=(ll == 31))
            P.op("vector", lambda e: e.tensor_copy(out=cst[:], in_=pc[:, 0:2]), reads=[pc], writes=[cst])
            for mc in range(2):
                x_, u_, s_, g_ = xh[mc], uu[mc], sg[mc], gb[mc]
                P.op("vector", lambda e, x_=x_, mc=mc: e.tensor_scalar(out=x_[:, 0:255], in0=ph[mc][:, 0:255], scalar1=cst[:, mc:mc + 1],
                                                                      scalar2=None, op0=ALU.add), reads=[ph[mc], cst], writes=[x_])
                P.op("vector", lambda e, x_=x_, u_=u_: e.tensor_tensor(out=u_[:, 0:255], in0=x_[:, 0:255], in1=x_[:, 0:255], op=ALU.mult),
                     reads=[x_], writes=[u_])
                P.op("vector", lambda e, u_=u_: e.tensor_scalar(out=u_[:, 0:255], in0=u_[:, 0:255], scalar1=0.044715, scalar2=1.0,
                                                               op0=ALU.mult, op1=ALU.add), reads=[u_], writes=[u_])
                P.op("vector", lambda e, x_=x_, u_=u_: e.tensor_tensor(out=u_[:, 0:255], in0=u_[:, 0:255], in1=x_[:, 0:255], op=ALU.mult),
                     reads=[x_, u_], writes=[u_])
                P.op("scalar", lambda e, u_=u_, s_=s_: e.activation(out=s_[:, 0:255], in_=u_[:, 0:255], func=AF.Sigmoid, scale=1.5957691216057308),
                     reads=[u_], writes=[s_])
                P.op("vector", lambda e, x_=x_, s_=s_, g_=g_: e.tensor_tensor(out=g_[:, 0:255], in0=x_[:, 0:255], in1=s_[:, 0:255], op=ALU.mult),
                     reads=[x_, s_], writes=[g_])
            if src == "k":
                for mc in range(2):
                    P.op("tensor", lambda e, mc=mc, si=si: e.matmul(po[:, 0:255], lhsT=w2b[si][:, mc, :], rhs=gb[mc][:, 0:255],
                                                                   start=(mc == 0), stop=(mc == 1)),
                         reads=[w2b[si], gb[mc]], writes=[po], signal=(mc == 1))
                P.op("scalar", lambda e, oc=oc: e.copy(out=oc[:, 0:255], in_=po[:, 0:255]), reads=[po], writes=[oc])
                P.dma("sync", k.kcmpT[hk], oc[:], reads=[oc])
            else:
                for ct in range(2):
                    for mc in range(2):
                        P.op("tensor", lambda e, mc=mc, ct=ct, si=si: e.matmul(
                            po[:, ct * 128:(ct + 1) * 128], lhsT=gb[mc][:, ct * 128:(ct + 1) * 128], rhs=w2b[si][:, mc, :],
                            start=(mc == 0), stop=(mc == 1)), reads=[w2b[si], gb[mc]], writes=[po], signal=(mc == 1))
                P.op("scalar", lambda e, oc=oc: e.copy(out=oc[:], in_=po[:, 0:256]), reads=[po], writes=[oc])
                P.dma("sync", k.vcmp[hk].rearrange("(c p) d -> p c d", p=128), oc[:].rearrange("p (c d) -> p c d", c=2), reads=[oc])
    P.flush()
    P.release(m0)


def phase_nsa(k, l, hks=(0, 1)):
    P = k.P
    m0 = P.mark()
    cst_f = {}
    for nm, shp in (("cb", [128, 128]), ("wbm", [128, 128]), ("esel", [64, 32 * 128]), ("ovl", [128, 128]), ("sel", [12, 12 * 128]),
                    ("ident", [128, 128])):
        b = P.sb("c_" + nm, shp, BF16)
        P.dma("gpsimd", b[:], getattr(k, nm), writes=[b])
        cst_f[nm] = b
    cb, wbm, esel, ovl, sel, identb = (cst_f[n] for n in ("cb", "wbm", "esel", "ovl", "sel", "ident"))
    identf = P.sb("identf", [128, 128], F32)
    P.dma("sync", identf[:], k.ident, writes=[identf])
    mb = P.sb("mb", [128, 128], F32)
    P.dma("sync", mb[:], k.mb, writes=[mb])
    keep = P.sb("keep", [128, 32 * 64], F32)
    addc = P.sb("addc", [128, 32 * 64], F32)
    P.dma("sync", keep[:], k.keep, writes=[keep])
    P.dma("sync", addc[:], k.addc, writes=[addc])
    onesb = P.sb("onesb", [128, 128], BF16)
    P.op("vector", lambda e: e.memset(onesb[:], 1.0), writes=[onesb])

    ksT = P.sb("ksT", [128, T], BF16)
    kwT = P.sb("kwT", [128, T], BF16)
    vs = P.sb("vs", [128, 32, 128], BF16)
    vw = P.sb("vw", [128, 32, 128], BF16)
    kcm = P.sb("kcm", [128, 256], BF16)
    vcm = P.sb("vcm", [128, 2, 128], BF16)
    sgt = P.sb("sgt", [12, T], BF16)
    qnb = [P.sb("qnb%d" % i, [128, 4, 512], BF16) for i in range(2)]
    qrb = [P.sb("qrb%d" % i, [128, 4, 512], BF16) for i in range(2)]
    S = [P.ps("S%d" % i, [128, 512]) for i in range(2)]
    BD = [P.ps("BD%d" % i, [128, 512]) for i in range(2)]
    BO = [P.ps("BO%d" % i, [128, 512]) for i in range(2)]
    BG = P.ps("BG", [128, 512])
    BM = P.ps("BM", [128, 512])
    pT = [P.sb("pT%d" % i, [128, 512], BF16) for i in range(3)]
    pTc = [P.sb("pTc%d" % i, [128, 512], BF16) for i in range(2)]
    pn = [P.sb("pn%d" % i, [128, 512], BF16) for i in range(2)]
    bc = [P.sb("bc%d" % i, [128, 128], BF16) for i in range(2)]
    W = [P.sb("W%d" % i, [128, 512], F32) for i in range(2)]
    Wg = [P.sb("Wg%d" % i, [128, 512], F32) for i in range(2)]
    tmp = [P.sb("tmp%d" % i, [128, 512], F32) for i in range(2)]
    acc = [P.sb("acc%d" % i, [128, 512], F32) for i in range(2)]
    accb = [P.sb("accb%d" % i, [128, 512], BF16) for i in range(2)]
    impT = P.sb("impT", [64, 128], F32)
    imp = P.sb("imp", [128, 64], F32)
    imp2 = P.sb("imp2", [128, 64], F32)
    m8a = P.sb("m8a", [128, 8], F32)
    m8b = P.sb("m8b", [128, 8], F32)
    bias = P.sb("bias", [128, 64], BF16)
    biasT = P.sb("biasT", [64, 128], BF16)
    cnt = {"s": 0, "pt": 0, "bc": 0, "set": 0, "w": 0}

    def rhs4(buf, qi):
        return buf[:, :, qi * 128:(qi + 1) * 128]

    def as4(ap):
        return ap.rearrange("p (g t) -> p g t", g=4)

    def bcast4(ap, np_):
        return ap.unsqueeze(1).to_broadcast([np_, 4, 128])

    for hk in hks:
        P.dma("sync", ksT[:], k.ksT[hk * 128:(hk + 1) * 128, :], writes=[ksT])
        P.dma("sync", kwT[:], k.kwT[hk * 128:(hk + 1) * 128, :], writes=[kwT])
        for q4 in range(4):
            P.dma("sync", vs[:, q4 * 8:(q4 + 1) * 8, :],
                  k.vs[q4 * 1024:(q4 + 1) * 1024, hk * 128:(hk + 1) * 128].rearrange("(t p) d -> p t d", p=128), writes=[vs])
            P.dma("sync", vw[:, q4 * 8:(q4 + 1) * 8, :],
                  k.vw[q4 * 1024:(q4 + 1) * 1024, hk * 128:(hk + 1) * 128].rearrange("(t p) d -> p t d", p=128), writes=[vw])
        P.dma("sync", kcm[:], k.kcmpT[hk], writes=[kcm])
        P.dma("sync", vcm[:], k.vcmp[hk].rearrange("(c p) d -> p c d", p=128), writes=[vcm])
        P.dma("sync", sgt[:], k.ngT[hk * 12:(hk + 1) * 12, :], writes=[sgt])
        for qt in range(T // 128):
            qb, qi = qt // 4, qt % 4
            qn_, qr_ = qnb[qb % 2], qrb[qb % 2]
            if qi == 0:
                tok = slice(qb * 512, (qb + 1) * 512)
                P.dma("sync", qn_[:], k.qn[hk * 512:(hk + 1) * 512, tok].rearrange("(g d) t -> d g t", d=128), writes=[qn_])
                P.dma("sync", qr_[:], k.qr[hk * 512:(hk + 1) * 512, tok].rearrange("(g d) t -> d g t", d=128), writes=[qr_])
            tsl = slice(qt * 128, (qt + 1) * 128)

            def gates(j):
                for g in range(4):
                    r = 3 * g + j
                    P.op("tensor", lambda e, g=g, r=r: e.matmul(BG[:, g * 128:(g + 1) * 128], lhsT=sel[:, r * 128:(r + 1) * 128],
                                                                rhs=sgt[:, tsl], start=True, stop=True),
                         reads=[sel, sgt], writes=[BG], signal=(g == 3))

            def combine(st, first, w_):
                wg = Wg[cnt["w"] % 2]
                tp = tmp[cnt["w"] % 2]
                cnt["w"] += 1
                ac = acc[qt % 2]
                P.op("vector", lambda e, wg=wg, w_=w_: e.tensor_tensor(out=wg[:], in0=BG[:], in1=w_[:], op=ALU.mult),
                     reads=[BG, w_], writes=[wg])
                if first:
                    P.op("vector", lambda e, wg=wg, ac=ac, st=st: e.tensor_tensor(out=ac[:], in0=BO[st][:], in1=wg[:], op=ALU.mult),
                         reads=[BO[st], wg], writes=[ac])
                else:
                    P.op("vector", lambda e, wg=wg, tp=tp, st=st: e.tensor_tensor(out=tp[:], in0=BO[st][:], in1=wg[:], op=ALU.mult),
                         reads=[BO[st], wg], writes=[tp])
                    P.op("gpsimd", lambda e, tp=tp, ac=ac: e.tensor_tensor(out=ac[:], in0=ac[:], in1=tp[:], op=ALU.add),
                         reads=[ac, tp], writes=[ac])

            st = cnt["set"] % 2
            cnt["set"] += 1
            nct = 1 if qt <= 15 else 2
            for ct in range(nct):
                s_ = S[cnt["s"] % 2]
                cnt["s"] += 1
                need_mask = not (ct == 0 and qt >= 17)
                P.op("tensor", lambda e, s_=s_, ct=ct, qn_=qn_: e.matmul(as4(s_[:]), lhsT=kcm[:, ct * 128:(ct + 1) * 128], rhs=rhs4(qn_, qi),
                                                                        start=True, stop=not need_mask),
                     reads=[kcm, qn_], writes=[s_], signal=not need_mask)
                if need_mask:
                    b_ = bc[cnt["bc"] % 2]
                    cnt["bc"] += 1
                    thr = float(128 * qt - 2048 * ct - 31)
                    P.op("gpsimd", lambda e, b_=b_, thr=thr: e.tensor_scalar(out=b_[:], in0=mb[:], scalar1=thr, scalar2=NEG, op0=ALU.is_gt, op1=ALU.mult),
                         reads=[mb], writes=[b_])
                    P.op("tensor", lambda e, s_=s_, b_=b_: e.matmul(as4(s_[:]), lhsT=identb[:], rhs=bcast4(b_[:], 128), start=False, stop=True),
                         reads=[identb, b_], writes=[s_])
                p_ = pTc[ct]
                P.op("scalar", lambda e, s_=s_, p_=p_: e.activation(out=p_[:], in_=s_[:], func=AF.Exp, scale=SCALE), reads=[s_], writes=[p_])
                P.op("tensor", lambda e, p_=p_, st=st, ct=ct: e.matmul(BD[st][:], lhsT=onesb[:], rhs=p_[:], start=(ct == 0), stop=(ct == nct - 1)),
                     reads=[onesb, p_], writes=[BD[st]], signal=(ct == nct - 1))
                P.op("tensor", lambda e, p_=p_, st=st, ct=ct: e.matmul(BO[st][:], lhsT=vcm[:, ct, :], rhs=p_[:], start=(ct == 0), stop=(ct == nct - 1)),
                     reads=[vcm, p_], writes=[BO[st]], signal=(ct == nct - 1))
            w_ = W[cnt["w"] % 2]
            P.op("vector", lambda e, w_=w_, st=st: e.tensor_scalar(out=w_[:], in0=BD[st][:], scalar1=1e-30, scalar2=None, op0=ALU.add),
                 reads=[BD[st]], writes=[w_])
            P.op("vector", lambda e, w_=w_: e.reciprocal(out=w_[:], in_=w_[:]), reads=[w_], writes=[w_])
            if qt >= 8:
                for ct in range(nct):
                    P.op("gpsimd", lambda e, ct=ct, w_=w_: e.tensor_tensor(out=pn[ct][:], in0=pTc[ct][:], in1=w_[:], op=ALU.mult),
                         reads=[pTc[ct], w_], writes=[pn[ct]])
                n_mm = nct * 4
                i_mm = 0
                for ct in range(nct):
                    for g in range(4):
                        P.op("tensor", lambda e, ct=ct, g=g, i_mm=i_mm: e.matmul(BM[0:64, 0:128], lhsT=ovl[:, ct * 64:(ct + 1) * 64],
                                                                                rhs=pn[ct][:, g * 128:(g + 1) * 128],
                                                                                start=(i_mm == 0), stop=(i_mm == n_mm - 1)),
                             reads=[ovl, pn[ct]], writes=[BM], signal=(i_mm == n_mm - 1))
                        i_mm += 1
                P.op("vector", lambda e: e.tensor_copy(out=impT[:], in_=BM[0:64, 0:128]), reads=[BM], writes=[impT])
                P.op("tensor", lambda e: e.transpose(out=BM[:, 128:192], in_=impT[:], identity=identf[0:64, 0:64]),
                     reads=[impT, identf], writes=[BM])
                P.op("vector", lambda e: e.tensor_tensor(out=imp[:], in0=BM[:, 128:192], in1=keep[:, qt * 64:(qt + 1) * 64], op=ALU.mult),
                     reads=[BM, keep], writes=[imp])
                P.op("vector", lambda e: e.tensor_tensor(out=imp[:], in0=imp[:], in1=addc[:, qt * 64:(qt + 1) * 64], op=ALU.add),
                     reads=[imp, addc], writes=[imp])
                P.op("vector", lambda e: e.max(out=m8a[:], in_=imp[:]), reads=[imp], writes=[m8a])
                P.op("vector", lambda e: e.match_replace(out=imp2[:], in_to_replace=m8a[:], in_values=imp[:], imm_value=-3.0e38),
                     reads=[imp, m8a], writes=[imp2])
                P.op("vector", lambda e: e.max(out=m8b[:], in_=imp2[:]), reads=[imp2], writes=[m8b])
                P.op("vector", lambda e: e.tensor_scalar(out=bias[:], in0=imp[:], scalar1=m8b[:, 7:8], scalar2=NEG, op0=ALU.is_lt, op1=ALU.mult),
                     reads=[imp, m8b], writes=[bias])
                P.op("tensor", lambda e: e.transpose(out=BM[0:64, 256:320].bitcast(BF16), in_=bias[:], identity=identb[:]),
                     reads=[bias, identb], writes=[BM])
                P.op("vector", lambda e: e.tensor_copy(out=biasT[:], in_=BM[0:64, 256:320].bitcast(BF16)), reads=[BM], writes=[biasT])
            gates(0)
            combine(st, True, w_)

            def branch(kT_, v_, kts, kind):
                st = cnt["set"] % 2
                cnt["set"] += 1
                nk = len(kts)

                def emit_score(ii):
                    kt = kts[ii]
                    s_ = S[cnt["s"] % 2]
                    cnt["s"] += 1
                    extra = []
                    if kind == "slc":
                        if qt >= 8:
                            extra.append(("sel", kt))
                        if kt == qt:
                            extra.append(("cb", None))
                    else:
                        if kt == qt:
                            extra.append(("cb", None))
                        if kt == qt - 4:
                            extra.append(("wb", None))
                    P.op("tensor", lambda e: e.matmul(as4(s_[:]), lhsT=kT_[:, kt * 128:(kt + 1) * 128], rhs=rhs4(qr_, qi),
                                                      start=True, stop=(len(extra) == 0)),
                         reads=[kT_, qr_], writes=[s_], signal=(len(extra) == 0))
                    for xi, (xk, xa) in enumerate(extra):
                        last = (xi == len(extra) - 1)
                        if xk == "sel":
                            P.op("tensor", lambda e: e.matmul(as4(s_[:]), lhsT=esel[:, xa * 128:(xa + 1) * 128],
                                                              rhs=bcast4(biasT[:], 64), start=False, stop=last),
                                 reads=[esel, biasT], writes=[s_], signal=last)
                        else:
                            mk = cb if xk == "cb" else wbm
                            P.op("tensor", lambda e: e.matmul(as4(s_[:]), lhsT=identb[:], rhs=bcast4(mk[:], 128),
                                                              start=False, stop=last),
                                 reads=[identb, mk], writes=[s_], signal=last)
                    return s_

                def emit_rest(ii, s_):
                    kt = kts[ii]
                    p_ = pT[cnt["pt"] % 3]
                    cnt["pt"] += 1
                    P.op("scalar", lambda e: e.activation(out=p_[:], in_=s_[:], func=AF.Exp, scale=SCALE), reads=[s_], writes=[p_])
                    P.op("tensor", lambda e: e.matmul(BD[st][:], lhsT=onesb[:], rhs=p_[:], start=(ii == 0), stop=(ii == nk - 1)),
                         reads=[onesb, p_], writes=[BD[st]], signal=(ii == nk - 1))
                    P.op("tensor", lambda e: e.matmul(BO[st][:], lhsT=v_[:, kt, :], rhs=p_[:], start=(ii == 0), stop=(ii == nk - 1)),
                         reads=[v_, p_], writes=[BO[st]], signal=(ii == nk - 1))

                s_cur = emit_score(0)
                for ii in range(nk):
                    s_next = emit_score(ii + 1) if ii + 1 < nk else None
                    emit_rest(ii, s_cur)
                    s_cur = s_next
                w2 = W[cnt["w"] % 2]
                P.op("vector", lambda e, w2=w2, st=st: e.reciprocal(out=w2[:], in_=BD[st][:]), reads=[BD[st]], writes=[w2])
                return st, w2

            st, w2 = branch(kwT, vw, list(range(max(0, qt - 4), qt + 1)), "win")
            gates(2)
            combine(st, False, w2)
            st, w2 = branch(ksT, vs, list(range(0, qt + 1)), "slc")
            gates(1)
            combine(st, False, w2)
            ac = acc[qt % 2]
            ab = accb[qt % 2]
            P.op("gpsimd", lambda e, ac=ac, ab=ab: e.tensor_copy(out=ab[:], in_=ac[:]), reads=[ac], writes=[ab])
            P.dma("sync", k.mixT[hk * 512:(hk + 1) * 512, tsl].rearrange("(g d) t -> d g t", d=128), as4(ab[:]), reads=[ab])
    P.flush()
    P.release(m0)


def phase_gla(k, l):
    P = k.P
    m0 = P.mark()
    GS = 128 ** -0.5
    wa2 = P.sb("wa2", [16, 512], BF16)
    brow = P.sb("brow", [1, 512], BF16)
    ones1 = P.sb("ones1", [1, 128], BF16)
    tri2f = P.sb("tri2f", [128, 128], F32)
    suf = P.sb("suf", [128, 128], F32)
    onesb = P.sb("onesb", [128, 128], BF16)
    nw = P.sb("nw", [128, 2], F32)
    P.dma("gpsimd", wa2[:], k.gla_w_a2[l], writes=[wa2])
    P.dma("gpsimd", brow[:], k.gla_b_a[l:l + 1, :], writes=[brow])
    P.dma("sync", tri2f[:], k.tri2, writes=[tri2f])
    P.dma("sync", suf[:], k.sumat, writes=[suf])
    P.dma("sync", nw[:], k.gla_nwT[l], writes=[nw])
    P.op("vector", lambda e: e.memset(ones1[:], 1.0), writes=[ones1])
    P.op("vector", lambda e: e.memset(onesb[:], 1.0), writes=[onesb])
    Sf = P.sb("Sf", [128, 4, 256], F32)
    Sb = P.sb("Sb", [128, 4, 256], BF16)
    Sfv = [P.view(Sf) for _ in range(4)]
    Sbv = [P.view(Sb) for _ in range(4)]
    for h in range(4):
        P.op("vector", lambda e, h=h: e.memset(Sf[:, h, :], 0.0), writes=[Sfv[h]])
        P.op("vector", lambda e, h=h: e.memset(Sb[:, h, :], 0.0), writes=[Sbv[h]])
    gqTb = [P.sb("gqTb%d" % i, [128, 4, 512], BF16) for i in range(2)]
    gkTb = [P.sb("gkTb%d" % i, [128, 4, 512], BF16) for i in range(2)]
    ggb = [P.sb("ggb%d" % i, [128, 8, 512], BF16) for i in range(2)]
    gaTb = [P.sb("gaTb%d" % i, [16, 512], BF16) for i in range(2)]
    gkt = [P.sb("gkt%d" % i, [128, 512], BF16) for i in range(2)]
    gvt = [P.sb("gvt%d" % i, [128, 1024], BF16) for i in range(2)]
    Lt = [P.sb("Lt%d" % i, [128, 512], F32) for i in range(2)]
    E1 = [P.sb("E1%d" % i, [128, 512], F32) for i in range(2)]
    kst = [P.sb("kst%d" % i, [128, 512], BF16) for i in range(2)]
    EbT = [P.sb("EbT%d" % i, [128, 128], F32) for i in range(2)]
    EnbT = [P.sb("EnbT%d" % i, [128, 128], F32) for i in range(2)]
    qdT = [P.sb("qdT%d" % i, [128, 128], BF16) for i in range(2)]
    kiT = [P.sb("kiT%d" % i, [128, 128], BF16) for i in range(2)]
    ATm = [P.sb("ATm%d" % i, [128, 128], BF16) for i in range(2)]
    o1 = [P.sb("o1%d" % i, [128, 256], F32) for i in range(2)]
    sq = [P.sb("sq%d" % i, [128, 256], BF16) for i in range(2)]
    lnr = [P.sb("lnr%d" % i, [128, 128], F32) for i in range(2)]
    rstd = [P.sb("rstd%d" % i, [128, 128], F32) for i in range(2)]
    tmpo = [P.sb("tmpo%d" % i, [128, 256], F32) for i in range(2)]
    outb = [P.sb("outb%d" % i, [128, 2, 128], BF16) for i in range(2)]
    pz = P.ps("pz", [128, 512])
    pcs = P.ps("pcs", [128, 512])
    psu = P.ps("psu", [128, 512])
    pcsT = P.ps("pcsT", [128, 512])
    pAT = P.ps("pAT", [128, 512])
    po = P.ps("po", [128, 512])
    pS = P.ps("pS", [128, 512])
    pss = P.ps("pss", [128, 512])
    hc = 0
    for tt in range(T // 128):
        tb, ti = tt // 4, tt % 4
        gq_, gk_, gg_, ga_ = gqTb[tb % 2], gkTb[tb % 2], ggb[tb % 2], gaTb[tb % 2]
        if ti == 0:
            tok = slice(tb * 512, (tb + 1) * 512)
            P.dma("sync", gq_[:], k.gqT[:, tok].rearrange("(h d) t -> d h t", d=128), writes=[gq_])
            P.dma("sync", gk_[:], k.gkT[:, tok].rearrange("(h d) t -> d h t", d=128), writes=[gk_])
            P.dma("sync", gg_[:], k.ggT[:, tok].rearrange("(c d) t -> d c t", d=128), writes=[gg_])
            P.dma("sync", ga_[:], k.gaT[:, tok], writes=[ga_])
        lsl = slice(ti * 128, (ti + 1) * 128)
        tsl = slice(tt * 128, (tt + 1) * 128)
        gkt_, gvt_ = gkt[tt % 2], gvt[tt % 2]
        P.dma("sync", gkt_[:], k.gk[tsl, :], writes=[gkt_])
        P.dma("sync", gvt_[:], k.gv[tsl, :], writes=[gvt_])
        L_, E1_, kst_ = Lt[tt % 2], E1[tt % 2], kst[tt % 2]
        P.op("tensor", lambda e: e.matmul(pz[:], lhsT=ga_[:, lsl], rhs=wa2[:], start=True, stop=False), reads=[ga_, wa2], writes=[pz], signal=False)
        P.op("tensor", lambda e: e.matmul(pz[:], lhsT=ones1[:], rhs=brow[:], start=False, stop=True), reads=[ones1, brow], writes=[pz])
        P.op("scalar", lambda e: e.activation(out=L_[:], in_=pz[:], func=AF.Exp, scale=-1.0), reads=[pz], writes=[L_])
        P.op("scalar", lambda e: e.activation(out=L_[:], in_=L_[:], func=AF.Ln, bias=1.0), reads=[L_], writes=[L_])
        P.op("tensor", lambda e: e.matmul(pcs[:], lhsT=tri2f[:], rhs=L_[:], start=True, stop=True), reads=[tri2f, L_], writes=[pcs])
        P.op("tensor", lambda e: e.matmul(psu[:], lhsT=suf[:], rhs=L_[:], start=True, stop=True), reads=[suf, L_], writes=[psu])
        P.op("scalar", lambda e: e.activation(out=E1_[:], in_=psu[:], func=AF.Exp, scale=-1.0 / 16.0), reads=[psu], writes=[E1_])
        P.op("vector", lambda e: e.tensor_tensor(out=kst_[:], in0=gkt_[:], in1=E1_[:], op=ALU.mult), reads=[gkt_, E1_], writes=[kst_])
        bufsel = {}
        for h in range(4):
            i2 = hc % 2
            hc += 1
            bufsel[h] = i2

        def gla_front(h):
            i2 = bufsel[h]
            Eb_, Enb_, qd_, ki_, AT_ = EbT[i2], EnbT[i2], qdT[i2], kiT[i2], ATm[i2]
            hs = slice(h * 128, (h + 1) * 128)
            P.op("tensor", lambda e: e.matmul(pcsT[:, 0:128], lhsT=L_[:, hs], rhs=tri2f[:], start=True, stop=True),
                 reads=[L_, tri2f], writes=[pcsT])
            P.op("scalar", lambda e: e.activation(out=Eb_[:], in_=pcsT[:, 0:128], func=AF.Exp, scale=-1.0 / 16.0), reads=[pcsT], writes=[Eb_])
            P.op("scalar", lambda e: e.activation(out=Enb_[:], in_=pcsT[:, 0:128], func=AF.Exp, scale=1.0 / 16.0), reads=[pcsT], writes=[Enb_])
            P.op("vector", lambda e: e.scalar_tensor_tensor(out=qd_[:], in0=gq_[:, h, lsl], scalar=GS, in1=Eb_[:], op0=ALU.mult, op1=ALU.mult),
                 reads=[gq_, Eb_], writes=[qd_])
            P.op("gpsimd", lambda e: e.tensor_tensor(out=ki_[:], in0=gk_[:, h, lsl], in1=Enb_[:], op=ALU.mult), reads=[gk_, Enb_], writes=[ki_])
            P.op("tensor", lambda e: e.matmul(pAT[:, 0:128], lhsT=ki_[:], rhs=qd_[:], start=True, stop=True), reads=[ki_, qd_], writes=[pAT])
            P.op("vector", lambda e: e.tensor_tensor(out=AT_[:], in0=pAT[:, 0:128], in1=tri2f[:], op=ALU.mult), reads=[pAT, tri2f], writes=[AT_])

        def gla_back(h):
            i2 = bufsel[h]
            Eb_, Enb_, qd_, ki_, AT_ = EbT[i2], EnbT[i2], qdT[i2], kiT[i2], ATm[i2]
            o1_, sq_, lnr_, rs_, tp_, ob_ = o1[i2], sq[i2], lnr[i2], rstd[i2], tmpo[i2], outb[i2]
            hs = slice(h * 128, (h + 1) * 128)
            for hf in range(2):
                cs_ = slice(hf * 64, (hf + 1) * 64)
                for dvc in range(2):
                    oc = slice(dvc * 128 + hf * 64, dvc * 128 + hf * 64 + 64)
                    P.op("tensor", lambda e: e.matmul(po[:, oc], lhsT=gvt_[:, h * 256 + dvc * 128:h * 256 + dvc * 128 + 128], rhs=AT_[:, cs_],
                                                      start=True, stop=False), reads=[gvt_, AT_], writes=[po], signal=False)
                    P.op("tensor", lambda e: e.matmul(po[:, oc], lhsT=Sb[:, h, dvc * 128:(dvc + 1) * 128], rhs=qd_[:, cs_],
                                                      start=False, stop=True), reads=[Sbv[h], qd_], writes=[po],
                         signal=(hf == 1 and dvc == 1))
                P.op("tensor", lambda e: e.matmul(pS[:, 0:256], lhsT=kst_[cs_, hs], rhs=gvt_[cs_, h * 256:(h + 1) * 256], start=True, stop=True),
                     reads=[kst_, gvt_], writes=[pS])
                col = hf * 64 + 63
                P.op("vector", lambda e: e.scalar_tensor_tensor(out=Sf[:, h, :], in0=Sf[:, h, :], scalar=Eb_[:, col:col + 1], in1=pS[:, 0:256],
                                                                op0=ALU.mult, op1=ALU.add), reads=[Sfv[h], Eb_, pS], writes=[Sfv[h]])
                P.op("gpsimd", lambda e: e.tensor_copy(out=Sb[:, h, :], in_=Sf[:, h, :]), reads=[Sfv[h]], writes=[Sbv[h]])
            P.op("scalar", lambda e: e.copy(out=o1_[:], in_=po[:, 0:256]), reads=[po], writes=[o1_])
            P.op("gpsimd", lambda e: e.tensor_tensor(out=sq_[:], in0=o1_[:], in1=o1_[:], op=ALU.mult), reads=[o1_], writes=[sq_])
            P.op("tensor", lambda e: e.matmul(pss[:, 0:128], lhsT=onesb[:], rhs=sq_[:, 0:128], start=True, stop=False), reads=[onesb, sq_], writes=[pss], signal=False)
            P.op("tensor", lambda e: e.matmul(pss[:, 0:128], lhsT=onesb[:], rhs=sq_[:, 128:256], start=False, stop=True), reads=[onesb, sq_], writes=[pss])
            P.op("scalar", lambda e: e.activation(out=lnr_[:], in_=pss[:, 0:128], func=AF.Ln, scale=1.0 / 256.0, bias=NORM_EPS), reads=[pss], writes=[lnr_])
            P.op("scalar", lambda e: e.activation(out=rs_[:], in_=lnr_[:], func=AF.Exp, scale=-0.5), reads=[lnr_], writes=[rs_])
            for dvc in range(2):
                ds_ = slice(dvc * 128, (dvc + 1) * 128)
                P.op("vector", lambda e: e.tensor_tensor(out=tp_[:, ds_], in0=o1_[:, ds_], in1=rs_[:], op=ALU.mult), reads=[o1_, rs_], writes=[tp_])
                P.op("vector", lambda e: e.scalar_tensor_tensor(out=ob_[:, dvc, :], in0=gg_[:, h * 2 + dvc, lsl], scalar=nw[:, dvc:dvc + 1], in1=tp_[:, ds_],
                                                                op0=ALU.mult, op1=ALU.mult), reads=[gg_, nw, tp_], writes=[ob_])
            P.dma("sync", k.mixT[1024 + h * 256:1024 + (h + 1) * 256, tsl].rearrange("(c d) t -> d c t", d=128), ob_[:], reads=[ob_])

        gla_front(0)
        for h in range(4):
            if h < 3:
                gla_front(h + 1)
            gla_back(h)
    P.flush()
    P.release(m0)


def ln_tile(P, r, lng, lnb, stats, mv, rstd):
    for c in range(4):
        P.op("vector", lambda e, c=c: e.bn_stats(out=stats[:, c, :], in_=r[:, c * 512:(c + 1) * 512]), reads=[r], writes=[stats])
    P.op("vector", lambda e: e.bn_aggr(out=mv[:], in_=stats[:]), reads=[stats], writes=[mv])
    P.op("scalar", lambda e: e.activation(out=rstd[:], in_=mv[:, 1:2], func=AF.Sqrt, bias=LN_EPS), reads=[mv], writes=[rstd])
    P.op("vector", lambda e: e.reciprocal(out=rstd[:], in_=rstd[:]), reads=[rstd], writes=[rstd])
    P.op("vector", lambda e: e.tensor_scalar(out=r[:], in0=r[:], scalar1=mv[:, 0:1], scalar2=rstd[:, 0:1], op0=ALU.subtract, op1=ALU.mult),
         reads=[r, mv, rstd], writes=[r])
    P.op("gpsimd", lambda e: e.tensor_tensor(out=r[:], in0=r[:], in1=lng[:], op=ALU.mult), reads=[r, lng], writes=[r])
    P.op("gpsimd", lambda e: e.tensor_tensor(out=r[:], in0=r[:], in1=lnb[:], op=ALU.add), reads=[r, lnb], writes=[r])


def phase_wout(k, l, xsrc, xdst):
    P = k.P
    m0 = P.mark()
    wo = P.sb("wo", [128, KC, D], BF16)
    wv = k.w_out[l].rearrange("(c p) n -> p c n", p=128)
    for q in range(4):
        P.dma("gpsimd", wo[:, :, q * 512:(q + 1) * 512], wv[:, :, q * 512:(q + 1) * 512], writes=[wo])
    garep = P.sb("garep", [128, D], F32)
    lng = P.sb("lng", [128, D], F32)
    lnb = P.sb("lnb", [128, D], F32)
    P.dma("sync", garep[:], k.modv[l, 2 * D:3 * D].partition_broadcast(128), writes=[garep])
    P.dma("sync", lng[:], k.ln_mix_g[l].partition_broadcast(128), writes=[lng])
    P.dma("sync", lnb[:], k.ln_mix_b[l].partition_broadcast(128), writes=[lnb])
    mixb = [P.sb("mixb%d" % i, [128, KC, 512], BF16) for i in range(2)]
    xt = [P.sb("xt%d" % i, [128, D], F32) for i in range(2)]
    rt = [P.sb("rt%d" % i, [128, D], F32) for i in range(2)]
    stats = P.sb("stats", [128, 4, 6], F32)
    mv = P.sb("mv", [128, 2], F32)
    rstd = P.sb("rstd", [128, 1], F32)
    py = [P.ps("py%d" % i, [128, 512]) for i in range(8)]
    mixv = k.mixT.rearrange("(c p) t -> p c t", p=128)
    for tt in range(T // 128):
        tb, ti = tt // 4, tt % 4
        mb_ = mixb[tb % 2]
        if ti == 0:
            for q in range(4):
                P.dma("sync", mb_[:, q * 4:(q + 1) * 4, :], mixv[:, q * 4:(q + 1) * 4, tb * 512:(tb + 1) * 512], writes=[mb_])
        x_ = xt[tt % 2]
        r_ = rt[tt % 2]
        tsl = slice(tt * 128, (tt + 1) * 128)
        P.dma("sync", x_[:], xsrc[tsl, :], writes=[x_])
        for db in range(4):
            p_ = py[(tt % 2) * 4 + db]
            for c in range(KC):
                P.op("tensor", lambda e: e.matmul(p_[:], lhsT=mb_[:, c, ti * 128:(ti + 1) * 128], rhs=wo[:, c, db * 512:(db + 1) * 512],
                                                  start=(c == 0), stop=(c == KC - 1)), reads=[mb_, wo], writes=[p_], signal=(c == KC - 1))
            P.op("vector", lambda e: e.tensor_tensor(out=r_[:, db * 512:(db + 1) * 512], in0=p_[:], in1=garep[:, db * 512:(db + 1) * 512], op=ALU.mult),
                 reads=[p_, garep], writes=[r_])
        P.op("vector", lambda e: e.scalar_tensor_tensor(out=r_[:], in0=x_[:], scalar=ALPHA, in1=r_[:], op0=ALU.mult, op1=ALU.add),
             reads=[x_, r_], writes=[r_])
        ln_tile(P, r_, lng, lnb, stats, mv, rstd)
        P.dma("sync", xdst[tsl, :], r_[:], reads=[r_])
    P.flush()
    P.release(m0)


def ffn_gateup(k, src, nrows, wg, wu, dff, AT, mod=None, src_bf16=False):
    P = k.P
    RH = min(nrows, 2048)
    for r0 in range(0, nrows, RH):
        nr = min(RH, nrows - r0)
        m0 = P.mark()
        identf = P.sb("identf", [128, 128], F32)
        P.dma("sync", identf[:], k.ident, writes=[identf])
        hT = P.sb("hT", [128, KC, RH], BF16)
        hviews = [(P.view(hT), P.view(hT)) for _ in range(nr // 128)]
        ptr = [P.ps("ptr%d" % i, [128, 4, 128]) for i in range(2)]
        if mod is not None:
            l = mod
            screp = P.sb("screp", [128, D], F32)
            shrep = P.sb("shrep", [128, D], F32)
            xt = [P.sb("xt%d" % i, [128, D], F32) for i in range(2)]
            P.dma("sync", screp[:], k.modv[l, 4 * D:5 * D].partition_broadcast(128), writes=[screp])
            P.dma("sync", shrep[:], k.modv[l, 3 * D:4 * D].partition_broadcast(128), writes=[shrep])
            build_hT(k, src, r0, nr // 128, screp, shrep, identf, hT, hviews, xt, ptr)
        else:
            identb = P.sb("identb", [128, 128], BF16)
            P.dma("gpsimd", identb[:], k.ident, writes=[identb])
            xt = [P.sb("xtb%d" % i, [128, D], BF16) for i in range(2)]
            for j in range(nr // 128):
                xb = xt[j % 2]
                P.dma("sync", xb[:], src[r0 + j * 128:r0 + (j + 1) * 128, :], writes=[xb])
                for g in range(KC // 4):
                    pt = ptr[(j * 4 + g) % 2]
                    ptb = pt[:].rearrange("p q t -> p (q t)")[:, 0:256].bitcast(BF16).rearrange("p (q t) -> p q t", q=4)
                    for q in range(4):
                        c = g * 4 + q
                        P.op("tensor", lambda e: e.transpose(out=ptb[:, q, :], in_=xb[:, c * 128:(c + 1) * 128], identity=identb[:]),
                             reads=[xb, identb], writes=[pt], signal=(q == 3))
                    if g % 2 == 0:
                        P.op("scalar", lambda e: e.copy(out=hT[:, g * 4:(g + 1) * 4, j * 128:(j + 1) * 128], in_=ptb), reads=[pt], writes=[hviews[j][0]])
                    else:
                        P.op("vector", lambda e: e.tensor_copy(out=hT[:, g * 4:(g + 1) * 4, j * 128:(j + 1) * 128], in_=ptb), reads=[pt], writes=[hviews[j][1]])
        wgb = [P.sb("wgb%d" % i, [128, KC, 256], BF16) for i in range(2)]
        wub = [P.sb("wub%d" % i, [128, KC, 256], BF16) for i in range(2)]
        pg = [P.ps("pg%d" % i, [128, 512]) for i in range(2)]
        pu = [P.ps("pu%d" % i, [128, 512]) for i in range(2)]
        sgb = [P.sb("sgb%d" % i, [128, 512], BF16) for i in range(2)]
        ab = [P.sb("ab%d" % i, [128, 512], BF16) for i in range(3)]
        wgv = wg.rearrange("(c p) n -> p c n", p=128)
        wuv = wu.rearrange("(c p) n -> p c n", p=128)
        blocks = [(b0, min(512, nr - b0)) for b0 in range(0, nr, 512)]
        it = 0
        for fb in range(dff // 256):
            wg_, wu_ = wgb[fb % 2], wub[fb % 2]
            P.dma("gpsimd", wg_[:], wgv[:, :, fb * 256:(fb + 1) * 256], writes=[wg_])
            P.dma("gpsimd", wu_[:], wuv[:, :, fb * 256:(fb + 1) * 256], writes=[wu_])
            for ft in range(2):
                f0 = fb * 256 + ft * 128
                for (b0, bn) in blocks:
                    hr = []
                    for j in range(b0 // 128, (b0 + bn) // 128):
                        hr += [hviews[j][0], hviews[j][1]]
                    pg_, pu_ = pg[it % 2], pu[it % 2]
                    sg_, a_ = sgb[it % 2], ab[it % 3]
                    it += 1
                    for c in range(KC):
                        P.op("tensor", lambda e: e.matmul(pg_[:, 0:bn], lhsT=wg_[:, c, ft * 128:(ft + 1) * 128], rhs=hT[:, c, b0:b0 + bn],
                                                          start=(c == 0), stop=(c == KC - 1)), reads=[wg_] + hr, writes=[pg_], signal=(c == KC - 1))
                    for c in range(KC):
                        P.op("tensor", lambda e: e.matmul(pu_[:, 0:bn], lhsT=wu_[:, c, ft * 128:(ft + 1) * 128], rhs=hT[:, c, b0:b0 + bn],
                                                          start=(c == 0), stop=(c == KC - 1)), reads=[wu_] + hr, writes=[pu_], signal=(c == KC - 1))
                    P.op("scalar", lambda e: e.activation(out=sg_[:, 0:bn], in_=pg_[:, 0:bn], func=AF.Silu), reads=[pg_], writes=[sg_])
                    P.op("vector", lambda e: e.tensor_tensor(out=a_[:, 0:bn], in0=pu_[:, 0:bn], in1=sg_[:, 0:bn], op=ALU.mult), reads=[pu_, sg_], writes=[a_])
                    P.dma("sync", AT[f0:f0 + 128, r0 + b0:r0 + b0 + bn], a_[:, 0:bn], reads=[a_])
        P.flush()
        P.release(m0)


class _PV:
    def __init__(self, b):
        self.b = b

    def __getitem__(self, idx):
        return self.b.t[:].rearrange("p (q t) -> p q t", q=4)[idx]


def ffn_down(k, AT, nrows, wd, dff, Y, row_off=0):
    P = k.P
    m0 = P.mark()
    FC = dff // 128
    G = 4
    assert FC % G == 0
    wdb = [P.sb("wdb%d" % i, [128, FC, 512], BF16) for i in range(2 if FC <= 44 else 1)]
    BLK = 512
    atb = [P.sb("atb%d" % i, [128, FC, BLK], BF16) for i in range(2)]
    yb = [P.sb("yb%d" % i, [128, 512], F32) for i in range(3)]
    py = [P.ps("pyd%d" % i, [128, 512]) for i in range(3)]
    wdv = wd.rearrange("(c p) n -> p c n", p=128)
    atv = AT.rearrange("(c p) t -> p c t", p=128)
    it = 0
    ib = 0
    for db in range(4):
        w_ = wdb[db % len(wdb)]
        for q in range(G):
            cs = slice(q * (FC // G), (q + 1) * (FC // G))
            P.dma("gpsimd", w_[:, cs, :], wdv[:, cs, db * 512:(db + 1) * 512], writes=[w_])
        for b0 in range(0, nrows, BLK):
            bn = min(BLK, nrows - b0)
            a_ = atb[ib % 2]
            ib += 1
            for q in range(G):
                cs = slice(q * (FC // G), (q + 1) * (FC // G))
                P.dma("sync", a_[:, cs, 0:bn], atv[:, cs, b0:b0 + bn], writes=[a_])
            for j in range(bn // 128):
                p_ = py[it % 3]
                y_ = yb[it % 3]
                it += 1
                for c in range(FC):
                    P.op("tensor", lambda e: e.matmul(p_[:], lhsT=a_[:, c, j * 128:(j + 1) * 128], rhs=w_[:, c, :], start=(c == 0), stop=(c == FC - 1)),
                         reads=[a_, w_], writes=[p_], signal=(c == FC - 1))
                P.op("scalar", lambda e: e.copy(out=y_[:], in_=p_[:]), reads=[p_], writes=[y_])
                rs = slice(row_off + b0 + j * 128, row_off + b0 + (j + 1) * 128)
                P.dma("sync", Y[rs, db * 512:(db + 1) * 512], y_[:], reads=[y_])
    P.flush()
    P.release(m0)


def phase_ln2(k, l, xsrc, ysrc, xdst):
    P = k.P
    m0 = P.mark()
    gfrep = P.sb("gfrep", [128, D], F32)
    lng = P.sb("lng", [128, D], F32)
    lnb = P.sb("lnb", [128, D], F32)
    P.dma("sync", gfrep[:], k.modv[l, 5 * D:6 * D].partition_broadcast(128), writes=[gfrep])
    P.dma("sync", lng[:], k.ln_ffn_g[l].partition_broadcast(128), writes=[lng])
    P.dma("sync", lnb[:], k.ln_ffn_b[l].partition_broadcast(128), writes=[lnb])
    xt = [P.sb("xt%d" % i, [128, D], F32) for i in range(2)]
    rt = [P.sb("rt%d" % i, [128, D], F32) for i in range(2)]
    stats = P.sb("stats", [128, 4, 6], F32)
    mv = P.sb("mv", [128, 2], F32)
    rstd = P.sb("rstd", [128, 1], F32)
    for tt in range(T // 128):
        x_, r_ = xt[tt % 2], rt[tt % 2]
        tsl = slice(tt * 128, (tt + 1) * 128)
        P.dma("sync", x_[:], xsrc[tsl, :], writes=[x_])
        P.dma("gpsimd", r_[:], ysrc[tsl, :], writes=[r_])
        P.op("vector", lambda e: e.tensor_tensor(out=r_[:], in0=r_[:], in1=gfrep[:], op=ALU.mult), reads=[r_, gfrep], writes=[r_])
        P.op("vector", lambda e: e.scalar_tensor_tensor(out=r_[:], in0=x_[:], scalar=ALPHA, in1=r_[:], op0=ALU.mult, op1=ALU.add),
             reads=[x_, r_], writes=[r_])
        ln_tile(P, r_, lng, lnb, stats, mv, rstd)
        P.dma("sync", xdst[tsl, :], r_[:], reads=[r_])
    P.flush()
    P.release(m0)


CAP = 768
TM = 2048
NSLOT = NE * CAP
BIGIDX = 1.0e6


def phase_moe_route(k, l):
    P = k.P
    m0 = P.mark()
    zt = P.sb("zt", [128, D], BF16)
    P.op("vector", lambda e: e.memset(zt[:], 0.0), writes=[zt])
    for s0 in range(0, NSLOT, 128):
        P.dma("sync" if (s0 // 128) % 2 == 0 else "gpsimd", k.Xs[s0:s0 + 128, :], zt[:], reads=[zt])
    P.flush()
    P.release(m0)

    m0 = P.mark()
    screp = P.sb("screp", [128, D], F32)
    shrep = P.sb("shrep", [128, D], F32)
    identf = P.sb("identf", [128, 128], F32)
    wr = P.sb("wr", [128, KC, NE], F32)
    SLb = P.sb("SLb", [128, 128], BF16)
    onesb = P.sb("onesb", [128, 128], BF16)
    eoff = P.sb("eoff", [128, NE], F32)
    base = P.sb("base", [128, NE], F32)
    P.dma("sync", screp[:], k.modv[l, 4 * D:5 * D].partition_broadcast(128), writes=[screp])
    P.dma("sync", shrep[:], k.modv[l, 3 * D:4 * D].partition_broadcast(128), writes=[shrep])
    P.dma("sync", identf[:], k.ident, writes=[identf])
    P.dma("sync", wr[:], k.moe_router[l // 2].rearrange("(c p) e -> p c e", p=128), writes=[wr])
    P.dma("gpsimd", SLb[:], k.slmat, writes=[SLb])
    P.dma("sync", eoff[:], k.eoff, writes=[eoff])
    P.op("vector", lambda e: e.memset(onesb[:], 1.0), writes=[onesb])
    P.op("vector", lambda e: e.memset(base[:], 0.0), writes=[base])
    xt = [P.sb("xt%d" % i, [128, D], F32) for i in range(2)]
    hb = [P.sb("hb%d" % i, [128, D], BF16) for i in range(2)]
    hTf = [P.sb("hTf%d" % i, [128, KC, 128], F32) for i in range(2)]
    ptr = [P.ps("ptr%d" % i, [128, 4, 128]) for i in range(2)]
    plog = P.ps("plog", [128, 512])
    pcum = P.ps("pcum", [128, 512])
    sm = {}
    for nm in ("lg", "m8", "sel", "sel1", "sel2", "ex", "exs", "comb", "tmp8", "pos", "dest", "valid", "selb"):
        sm[nm] = [P.sb(nm + "%d" % i, [128, NE], BF16 if nm == "selb" else F32) for i in range(2)]
    c1 = {}
    for nm in ("nm1", "den", "rden"):
        c1[nm] = [P.sb(nm + "%d" % i, [128, 1], F32) for i in range(2)]
    wts = [P.sb("wts%d" % i, [128, 2], F32) for i in range(2)]
    dd = [P.sb("dd%d" % i, [128, 2], F32) for i in range(2)]
    idx = [P.sb("idx%d" % i, [128, 2], I32) for i in range(2)]
    tix = [P.sb("tix%d" % i, [128, 1], I32) for i in range(2)]
    for tt in range(TM // 128):
        i2 = tt % 2
        x_, hb_, hT_ = xt[i2], hb[i2], hTf[i2]
        tsl = slice(tt * 128, (tt + 1) * 128)
        P.dma("sync", tix[i2][:], k.tokidx[tsl, :], writes=[tix[i2]])
        P.gather(x_[:], k.x1, tix[i2][:, 0:1], reads=[tix[i2]], writes=[x_], bounds_check=T - 1, oob_is_err=False)
        P.op("vector", lambda e: e.tensor_tensor(out=x_[:], in0=x_[:], in1=screp[:], op=ALU.mult), reads=[x_, screp], writes=[x_])
        P.op("gpsimd", lambda e: e.tensor_tensor(out=x_[:], in0=x_[:], in1=shrep[:], op=ALU.add), reads=[x_, shrep], writes=[x_])
        P.op("scalar", lambda e: e.copy(out=hb_[:], in_=x_[:]), reads=[x_], writes=[hb_])
        for g in range(KC // 4):
            pt = ptr[g % 2]
            for q in range(4):
                c = g * 4 + q
                P.op("tensor", lambda e: e.transpose(out=pt[:, q, :], in_=x_[:, c * 128:(c + 1) * 128], identity=identf[:]),
                     reads=[x_, identf], writes=[pt], signal=(q == 3))
            if g % 2 == 0:
                P.op("scalar", lambda e: e.copy(out=hT_[:, g * 4:(g + 1) * 4, :], in_=pt[:]), reads=[pt], writes=[hT_])
            else:
                P.op("vector", lambda e: e.tensor_copy(out=hT_[:, g * 4:(g + 1) * 4, :], in_=pt[:]), reads=[pt], writes=[hT_])
        for c in range(KC):
            P.op("tensor", lambda e: e.matmul(plog[:, 0:NE], lhsT=hT_[:, c, :], rhs=wr[:, c, :], start=(c == 0), stop=(c == KC - 1)),
                 reads=[hT_, wr], writes=[plog], signal=(c == KC - 1))
        S = {n: v[i2] for n, v in sm.items()}
        C1 = {n: v[i2] for n, v in c1.items()}
        w_, d_, ix_ = wts[i2], dd[i2], idx[i2]
        V = "vector"
        P.op(V, lambda e: e.tensor_copy(out=S["lg"][:], in_=plog[:, 0:NE]), reads=[plog], writes=[S["lg"]])
        P.op(V, lambda e: e.max(out=S["m8"][:], in_=S["lg"][:]), reads=[S["lg"]], writes=[S["m8"]])
        P.op(V, lambda e: e.tensor_scalar(out=S["sel"][:], in0=S["lg"][:], scalar1=S["m8"][:, 1:2], scalar2=None, op0=ALU.is_ge),
             reads=[S["lg"], S["m8"]], writes=[S["sel"]])
        P.op(V, lambda e: e.tensor_scalar(out=S["sel1"][:], in0=S["lg"][:], scalar1=S["m8"][:, 0:1], scalar2=None, op0=ALU.is_ge),
             reads=[S["lg"], S["m8"]], writes=[S["sel1"]])
        P.op(V, lambda e: e.tensor_tensor(out=S["sel2"][:], in0=S["sel"][:], in1=S["sel1"][:], op=ALU.subtract),
             reads=[S["sel"], S["sel1"]], writes=[S["sel2"]])
        P.op(V, lambda e: e.tensor_scalar(out=C1["nm1"][:], in0=S["m8"][:, 0:1], scalar1=-1.0, scalar2=None, op0=ALU.mult),
             reads=[S["m8"]], writes=[C1["nm1"]])
        P.op("scalar", lambda e: e.activation(out=S["ex"][:], in_=S["lg"][:], func=AF.Exp, bias=C1["nm1"][:, 0:1]),
             reads=[S["lg"], C1["nm1"]], writes=[S["ex"]])
        P.op(V, lambda e: e.tensor_tensor(out=S["exs"][:], in0=S["ex"][:], in1=S["sel"][:], op=ALU.mult), reads=[S["ex"], S["sel"]], writes=[S["exs"]])
        P.op(V, lambda e: e.reduce_sum(out=C1["den"][:], in_=S["exs"][:], axis=AX.X), reads=[S["exs"]], writes=[C1["den"]])
        P.op(V, lambda e: e.reciprocal(out=C1["rden"][:], in_=C1["den"][:]), reads=[C1["den"]], writes=[C1["rden"]])
        P.op(V, lambda e: e.tensor_scalar(out=S["comb"][:], in0=S["exs"][:], scalar1=C1["rden"][:, 0:1], scalar2=None, op0=ALU.mult),
             reads=[S["exs"], C1["rden"]], writes=[S["comb"]])
        for j, sn in enumerate(("sel1", "sel2")):
            P.op(V, lambda e: e.tensor_tensor(out=S["tmp8"][:], in0=S["comb"][:], in1=S[sn][:], op=ALU.mult), reads=[S["comb"], S[sn]], writes=[S["tmp8"]])
            P.op(V, lambda e: e.reduce_sum(out=w_[:, j:j + 1], in_=S["tmp8"][:], axis=AX.X), reads=[S["tmp8"]], writes=[w_])
        P.op(V, lambda e: e.tensor_copy(out=S["selb"][:], in_=S["sel"][:]), reads=[S["sel"]], writes=[S["selb"]])
        P.op("tensor", lambda e: e.matmul(pcum[:, 0:NE], lhsT=SLb[:], rhs=S["selb"][:], start=True, stop=True), reads=[SLb, S["selb"]], writes=[pcum], signal=False)
        P.op("tensor", lambda e: e.matmul(pcum[:, NE:2 * NE], lhsT=onesb[:], rhs=S["selb"][:], start=True, stop=True), reads=[onesb, S["selb"]], writes=[pcum])
        P.op(V, lambda e: e.tensor_tensor(out=S["pos"][:], in0=pcum[:, 0:NE], in1=base[:], op=ALU.add), reads=[pcum, base], writes=[S["pos"]])
        P.op(V, lambda e: e.tensor_tensor(out=base[:], in0=pcum[:, NE:2 * NE], in1=base[:], op=ALU.add), reads=[pcum, base], writes=[base])
        P.op(V, lambda e: e.tensor_scalar(out=S["valid"][:], in0=S["pos"][:], scalar1=float(CAP), scalar2=None, op0=ALU.is_lt),
             reads=[S["pos"]], writes=[S["valid"]])
        P.op(V, lambda e: e.tensor_tensor(out=S["dest"][:], in0=S["pos"][:], in1=eoff[:], op=ALU.add), reads=[S["pos"], eoff], writes=[S["dest"]])
        P.op(V, lambda e: e.scalar_tensor_tensor(out=S["dest"][:], in0=S["dest"][:], scalar=-BIGIDX, in1=S["valid"][:], op0=ALU.add, op1=ALU.mult),
             reads=[S["dest"], S["valid"]], writes=[S["dest"]])
        P.op(V, lambda e: e.tensor_scalar(out=S["dest"][:], in0=S["dest"][:], scalar1=BIGIDX, scalar2=None, op0=ALU.add),
             reads=[S["dest"]], writes=[S["dest"]])
        for j, sn in enumerate(("sel1", "sel2")):
            P.op(V, lambda e: e.tensor_tensor(out=S["tmp8"][:], in0=S["dest"][:], in1=S[sn][:], op=ALU.mult), reads=[S["dest"], S[sn]], writes=[S["tmp8"]])
            P.op(V, lambda e: e.reduce_sum(out=d_[:, j:j + 1], in_=S["tmp8"][:], axis=AX.X), reads=[S["tmp8"]], writes=[d_])
        P.op(V, lambda e: e.tensor_copy(out=ix_[:], in_=d_[:]), reads=[d_], writes=[ix_])
        P.dma("sync", k.midx[tsl, :], ix_[:], reads=[ix_])
        P.dma("sync", k.mwts[tsl, :], w_[:], reads=[w_])
        for j in range(2):
            P.gather(k.Xs, hb_[:], ix_[:, j:j + 1], reads=[hb_, ix_], scatter=True, bounds_check=NSLOT - 1, oob_is_err=False)
    P.flush()
    P.release(m0)


def phase_moe_experts(k, l):
    i = l // 2
    for e_ in range(NE):
        ffn_gateup(k, k.Xs[e_ * CAP:(e_ + 1) * CAP, :], CAP, k.moe_w_gate[i, e_], k.moe_w_up[i, e_], D_FFE, k.AT[:, 0:CAP], mod=None)
        ffn_down(k, k.AT[:, 0:CAP], CAP, k.moe_w_down[i, e_], D_FFE, k.Ys, row_off=e_ * CAP)


def phase_moe_ln2(k, l, xsrc, xdst):
    P = k.P
    m0 = P.mark()
    gfrep = P.sb("gfrep", [128, D], F32)
    lng = P.sb("lng", [128, D], F32)
    lnb = P.sb("lnb", [128, D], F32)
    P.dma("sync", gfrep[:], k.modv[l, 5 * D:6 * D].partition_broadcast(128), writes=[gfrep])
    P.dma("sync", lng[:], k.ln_ffn_g[l].partition_broadcast(128), writes=[lng])
    P.dma("sync", lnb[:], k.ln_ffn_b[l].partition_broadcast(128), writes=[lnb])
    xt = [P.sb("xt%d" % i, [128, D], F32) for i in range(2)]
    y1 = [P.sb("y1%d" % i, [128, D], F32) for i in range(2)]
    y2 = [P.sb("y2%d" % i, [128, D], F32) for i in range(2)]
    rt = [P.sb("rt%d" % i, [128, D], F32) for i in range(2)]
    idx = [P.sb("idx%d" % i, [128, 2], I32) for i in range(2)]
    wts = [P.sb("wts%d" % i, [128, 2], F32) for i in range(2)]
    stats = P.sb("stats", [128, 4, 6], F32)
    mv = P.sb("mv", [128, 2], F32)
    rstd = P.sb("rstd", [128, 1], F32)
    tix = [P.sb("tix%d" % i, [128, 1], I32) for i in range(2)]
    for tt in range(TM // 128):
        i2 = tt % 2
        x_, r_, a_, b_, ix_, w_ = xt[i2], rt[i2], y1[i2], y2[i2], idx[i2], wts[i2]
        tsl = slice(tt * 128, (tt + 1) * 128)
        P.dma("sync", tix[i2][:], k.tokidx[tsl, :], writes=[tix[i2]])
        P.gather(x_[:], xsrc, tix[i2][:, 0:1], reads=[tix[i2]], writes=[x_], bounds_check=T - 1, oob_is_err=False)
        P.dma("sync", ix_[:], k.midx[tsl, :], writes=[ix_])
        P.dma("sync", w_[:], k.mwts[tsl, :], writes=[w_])
        P.op("gpsimd", lambda e: e.memset(a_[:], 0.0), writes=[a_])
        P.op("gpsimd", lambda e: e.memset(b_[:], 0.0), writes=[b_])
        P.gather(a_[:], k.Ys, ix_[:, 0:1], reads=[ix_], writes=[a_], bounds_check=NSLOT - 1, oob_is_err=False)
        P.gather(b_[:], k.Ys, ix_[:, 1:2], reads=[ix_], writes=[b_], bounds_check=NSLOT - 1, oob_is_err=False)
        P.op("vector", lambda e: e.tensor_scalar(out=r_[:], in0=a_[:], scalar1=w_[:, 0:1], scalar2=None, op0=ALU.mult), reads=[a_, w_], writes=[r_])
        P.op("vector", lambda e: e.scalar_tensor_tensor(out=r_[:], in0=b_[:], scalar=w_[:, 1:2], in1=r_[:], op0=ALU.mult, op1=ALU.add),
             reads=[b_, w_, r_], writes=[r_])
        P.op("gpsimd", lambda e: e.tensor_tensor(out=r_[:], in0=r_[:], in1=gfrep[:], op=ALU.mult), reads=[r_, gfrep], writes=[r_])
        P.op("vector", lambda e: e.scalar_tensor_tensor(out=r_[:], in0=x_[:], scalar=ALPHA, in1=r_[:], op0=ALU.mult, op1=ALU.add),
             reads=[x_, r_], writes=[r_])
        ln_tile(P, r_, lng, lnb, stats, mv, rstd)
        P.dma("sync", xdst[tsl, :], r_[:], reads=[r_])
    P.flush()
    P.release(m0)


_CACHE = {}


def make_in_maps(inputs, n_cores=8):
    hc = host_consts()
    maps = []
    for core in range(n_cores):
        b = (core // 2) % 4
        r = core % 2
        m = {
            "x": np.ascontiguousarray(inputs["x"][b]),
            "cT": np.ascontiguousarray(inputs["c"][b].reshape(KC, 128).T),
            "tokidx": (r * TM + np.arange(TM, dtype=np.int32)).reshape(TM, 1),
            "w_ada": inputs["w_ada"],
            "b_ada": inputs["b_ada"],
            "w_in": inputs["w_in"],
            "w_out": inputs["w_out"], "ln_mix_g": inputs["ln_mix_g"], "ln_mix_b": inputs["ln_mix_b"],
            "ln_ffn_g": inputs["ln_ffn_g"], "ln_ffn_b": inputs["ln_ffn_b"],
            "moe_router": inputs["moe_router"], "moe_w_gate": inputs["moe_w_gate"], "moe_w_up": inputs["moe_w_up"],
            "moe_w_down": inputs["moe_w_down"],
            "ffn_w_gate": inputs["ffn_w_gate"], "ffn_w_up": inputs["ffn_w_up"], "ffn_w_down": inputs["ffn_w_down"],
            "gla_w_a2": inputs["gla_w_a2"], "gla_b_a": inputs["gla_b_a"],
            "gla_nwT": np.ascontiguousarray(inputs["gla_norm_w"].reshape(DEPTH, 2, 128).transpose(0, 2, 1)),
            "cmp_w1_k": inputs["cmp_w1_k"], "cmp_w1_v": inputs["cmp_w1_v"],
            "cmp_w2_k": inputs["cmp_w2_k"], "cmp_w2_v": inputs["cmp_w2_v"],
            "cmp_peT_k": np.ascontiguousarray(inputs["cmp_pos_k"].transpose(0, 2, 1)),
            "cmp_peT_v": np.ascontiguousarray(inputs["cmp_pos_v"].transpose(0, 2, 1)),
        }
        m.update(hc)
        maps.append(m)
    return maps


N_CORES = 8


def kernel(**inputs):
    inputs = {k_: np.asarray(v) for k_, v in inputs.items()}
    nc = build()
    maps = make_in_maps(inputs, n_cores=N_CORES)
    res = run_bass_kernel_spmd(nc, maps, core_ids=list(range(N_CORES)))
    out = np.empty((4, T, D), np.float32)
    for core in range(N_CORES):
        b, r = core // 2, core % 2
        out[b, r * TM:(r + 1) * TM] = np.asarray(res.results[core]["out"])
    return out
```
